# Optimizing a Trainium2 kernel written in Bass

```python
import jax, jax.numpy as jnp
from jax import lax
import numpy as np

D_MODEL = 1024
BATCH = 8
SEQ = 8192
DEPTH = 4

CTX_LEN = 256
GRID_W = 64
EPS = 1e-6

F_GROUPS = 4
F_GROUP_DIM = D_MODEL // 16
F_WIDTH = F_GROUPS * F_GROUP_DIM
M_HEADS = 4
M_HEAD_DIM = D_MODEL // 16
M_WIDTH = M_HEADS * M_HEAD_DIM
M_CONV_W = 3
M_CHUNK = 128
M_GATES = 2 * 2 * M_HEADS
A_HEADS = 8
A_NOPE = D_MODEL // 16
A_ROPE = D_MODEL // 32
A_V = D_MODEL // 16
A_WIDTH = A_HEADS * A_V
Q_LORA = 3 * D_MODEL // 8
KV_LORA = D_MODEL // 4
ROPE_BASE = 10000.0
Q_BLOCK = 128
D_MIX = F_WIDTH + M_WIDTH + A_WIDTH
IN_SIZES = (F_WIDTH, 2 * M_WIDTH, M_WIDTH, M_WIDTH, M_GATES, Q_LORA, KV_LORA, A_ROPE)
IN_COLS = sum(IN_SIZES)
N_EXPERTS = 16
EC_FACTOR = 2
EXPERT_FF = D_MODEL

kernel_name = "hybrid_fnet_mlstm_mla_ec_diffusion"


def rms_norm(x, g):
    xf = x.astype(jnp.float32)
    y = xf * lax.rsqrt(jnp.mean(xf * xf, -1, keepdims=True) + EPS)
    return (y * g.astype(jnp.float32)).astype(x.dtype)


def modulated_rms(x, g, shift, scale):
    return rms_norm(x, g) * (1 + scale) + shift


def split_in(u):
    outs = []
    start = 0
    for size in IN_SIZES:
        outs.append(u[..., start:start + size])
        start += size
    return outs


def axial_rope(n, dtype):
    rows = n // GRID_W
    row_id = jnp.repeat(jnp.arange(rows, dtype=jnp.float32), GRID_W)
    col_id = jnp.tile(jnp.arange(GRID_W, dtype=jnp.float32), rows)
    n_freq = A_ROPE // 4
    inv = ROPE_BASE ** (-jnp.arange(n_freq, dtype=jnp.float32) / n_freq)
    ang = jnp.concatenate([row_id[:, None] * inv, col_id[:, None] * inv], -1)
    return jnp.cos(ang).astype(dtype), jnp.sin(ang).astype(dtype)


def apply_rope(t, cos, sin):
    half = A_ROPE // 2
    t1, t2 = t[..., :half], t[..., half:]
    cs, sn = cos[:, None, :], sin[:, None, :]
    return jnp.concatenate([t1 * cs - t2 * sn, t1 * sn + t2 * cs], -1)


def fourier_mix(u):
    b, t, _ = u.shape
    ug = u.astype(jnp.float32).reshape(b, t, F_GROUPS, F_GROUP_DIM)
    y = jnp.fft.fft2(ug, axes=(1, 3), norm="ortho").real
    return y.reshape(b, t, F_WIDTH).astype(u.dtype)


def centred_dwconv(u, w, bias):
    pad = (M_CONV_W - 1) // 2
    y = lax.conv_general_dilated(u, w[:, None, :], window_strides=(1,), padding=[(pad, pad)],
                                 dimension_numbers=("NWC", "WIO", "NWC"), feature_group_count=u.shape[-1])
    return y + bias


def mlstm_prepare(qk, v, gates, conv_w, conv_b, ib, fb):
    b, t, _ = v.shape
    qk = jax.nn.silu(centred_dwconv(qk, conv_w, conv_b))
    q, k = qk[..., :M_WIDTH], qk[..., M_WIDTH:]
    heads = lambda a: a.reshape(b, t, M_HEADS, M_HEAD_DIM).transpose(0, 2, 1, 3)
    g = gates.astype(jnp.float32).reshape(b, t, 2, 2, M_HEADS)
    logi = (g[:, :, :, 0] + ib.astype(jnp.float32)).transpose(2, 0, 3, 1)
    logf = jax.nn.log_sigmoid(g[:, :, :, 1] + fb.astype(jnp.float32)).transpose(2, 0, 3, 1)
    return heads(q) * (M_HEAD_DIM ** -0.5), heads(k), heads(v), logi, logf


def mlstm_scan(q, k, v, logi, logf, state):
    b, h, t, d = q.shape
    nc = t // M_CHUNK
    to_chunks = lambda a: jnp.moveaxis(a.astype(jnp.float32).reshape(b, h, nc, M_CHUNK, *a.shape[3:]), 2, 0)
    xs = (to_chunks(q), to_chunks(k), to_chunks(v), to_chunks(logi), to_chunks(logf))
    tri = jnp.tril(jnp.ones((M_CHUNK, M_CHUNK), bool))

    def step(carry, inp):
        C, n, m = carry
        qc, kc, vc, ic, fc = inp
        bcum = jnp.cumsum(fc, -1)
        dmat = bcum[..., :, None] - bcum[..., None, :] + ic[..., None, :]
        dmat = jnp.where(tri, dmat, -jnp.inf)
        inter = bcum + m[..., None]
        m_t = jnp.maximum(inter, dmat.max(-1))
        w = jnp.exp(dmat - m_t[..., None])
        a = jnp.exp(inter - m_t)
        s = jnp.einsum("bhtd,bhsd->bhts", qc, kc) * w
        num = a[..., None] * jnp.einsum("bhtd,bhde->bhte", qc, C) + jnp.einsum("bhts,bhse->bhte", s, vc)
        den = a * jnp.einsum("bhtd,bhd->bht", qc, n) + s.sum(-1)
        h_out = num / jnp.maximum(jnp.abs(den), jnp.exp(-m_t))[..., None]
        btot = bcum[..., -1]
        dec = btot[..., None] - bcum + ic
        m_new = jnp.maximum(btot + m, dec.max(-1))
        ws = jnp.exp(dec - m_new[..., None])
        a_st = jnp.exp(btot + m - m_new)
        C_new = a_st[..., None, None] * C + jnp.einsum("bhs,bhsd,bhse->bhde", ws, kc, vc)
        n_new = a_st[..., None] * n + jnp.einsum("bhs,bhsd->bhd", ws, kc)
        return (C_new, n_new, m_new), h_out

    state, hs = lax.scan(step, state, xs)
    return state, jnp.moveaxis(hs, 0, 2).reshape(b, h, t, d)


def mlstm_bidirectional(ctx_m, lat_m):
    qc, kc, vc, ic, fc = ctx_m
    qx, kx, vx, ix, fx = lat_m
    b = qx.shape[0]
    zero = (jnp.zeros((b, M_HEADS, M_HEAD_DIM, M_HEAD_DIM), jnp.float32),
            jnp.zeros((b, M_HEADS, M_HEAD_DIM), jnp.float32),
            jnp.zeros((b, M_HEADS), jnp.float32))
    rev = lambda a: jnp.flip(a, axis=2)
    st_f, hc_f = mlstm_scan(qc, kc, vc, ic[0], fc[0], zero)
    _, hx_f = mlstm_scan(qx, kx, vx, ix[0], fx[0], st_f)
    st_b, hc_b = mlstm_scan(rev(qc), rev(kc), rev(vc), rev(ic[1]), rev(fc[1]), zero)
    _, hx_b = mlstm_scan(rev(qx), rev(kx), rev(vx), rev(ix[1]), rev(fx[1]), st_b)
    return hx_f + rev(hx_b), hc_f + rev(hc_b)


def mlstm_output(h, o, g):
    b, _, t, _ = h.shape
    hn = h * lax.rsqrt(jnp.mean(h * h, -1, keepdims=True) + EPS)
    hn = hn.transpose(0, 2, 1, 3).reshape(b, t, M_WIDTH) * g.astype(jnp.float32)
    return hn.astype(o.dtype) * jax.nn.sigmoid(o)


def mla_project(cq, ckv, kr, qn_g, wq_up, kvn_g, wkv_up, rope):
    b, t, _ = cq.shape
    q = (rms_norm(cq, qn_g) @ wq_up).reshape(b, t, A_HEADS, A_NOPE + A_ROPE)
    kv = (rms_norm(ckv, kvn_g) @ wkv_up).reshape(b, t, A_HEADS, A_NOPE + A_V)
    q_nope, q_rope = q[..., :A_NOPE], q[..., A_NOPE:]
    k_nope, v = kv[..., :A_NOPE], kv[..., A_NOPE:]
    k_rope = kr[:, :, None, :]
    if rope is not None:
        q_rope = apply_rope(q_rope, *rope)
        k_rope = apply_rope(k_rope, *rope)
    q = jnp.concatenate([q_nope, q_rope], -1)
    k = jnp.concatenate([k_nope, jnp.broadcast_to(k_rope, (b, t, A_HEADS, A_ROPE))], -1)
    return q, k, v


def block_attention(q, k, v):
    b, tq, h, dk = q.shape
    nb = tq // Q_BLOCK
    qb = jnp.moveaxis(q.reshape(b, nb, Q_BLOCK, h, dk), 1, 0)
    scale = dk ** -0.5

    def one(qi):
        s = jnp.einsum("bqhd,bkhd->bhqk", qi, k).astype(jnp.float32) * scale
        p = jax.nn.softmax(s, -1).astype(v.dtype)
        return jnp.einsum("bhqk,bkhd->bqhd", p, v)

    out = lax.map(one, qb)
    return jnp.moveaxis(out, 0, 1).reshape(b, tq, h * v.shape[-1])


def expert_choice_moe(h, router_w, w_gate, w_up, w_down):
    b, n, _ = h.shape
    cap = EC_FACTOR * n // N_EXPERTS
    aff = jax.nn.softmax((h @ router_w).astype(jnp.float32), -1)
    g, idx = lax.top_k(jnp.swapaxes(aff, 1, 2), cap)
    xe = jax.vmap(lambda hb, ib: hb[ib])(h, idx)
    hid = jax.nn.silu(jnp.einsum("becd,edf->becf", xe, w_gate)) * jnp.einsum("becd,edf->becf", xe, w_up)
    ye = jnp.einsum("becf,efd->becd", hid, w_down) * g[..., None].astype(h.dtype)
    bidx = jnp.arange(b)[:, None, None]
    return jnp.zeros_like(h).at[bidx, idx].add(ye)


def setup_inputs(seed: int = 0) -> dict:
    key = jax.random.key(seed)
    ks = jax.random.split(key, 24)
    nrm = lambda k, shape, s: jax.random.normal(k, shape, jnp.float32) * s
    L, D = DEPTH, D_MODEL
    return {
        "x": nrm(ks[0], (BATCH, SEQ, D), 1.0),
        "c": nrm(ks[1], (BATCH, D), 1.0),
        "ctx": nrm(ks[2], (BATCH, CTX_LEN, D), 1.0),
        "c_ctx": nrm(ks[3], (D,), 1.0),
        "ada_w": nrm(ks[4], (L, D, 6 * D), 0.5 * D ** -0.5),
        "ada_b": nrm(ks[5], (L, 6 * D), 0.01),
        "norm1_g": 1.0 + nrm(ks[6], (L, D), 0.05),
        "norm2_g": 1.0 + nrm(ks[7], (L, D), 0.05),
        "w_in": nrm(ks[8], (L, D, IN_COLS), D ** -0.5),
        "m_conv_w": nrm(ks[9], (L, M_CONV_W, 2 * M_WIDTH), M_CONV_W ** -0.5),
        "m_conv_b": nrm(ks[10], (L, 2 * M_WIDTH), 0.01),
        "m_ib": nrm(ks[11], (L, 2, M_HEADS), 0.1),
        "m_fb": 3.0 + nrm(ks[12], (L, 2, M_HEADS), 0.5),
        "m_norm_g": 1.0 + nrm(ks[13], (L, M_WIDTH), 0.05),
        "a_qnorm_g": 1.0 + nrm(ks[14], (L, Q_LORA), 0.05),
        "a_wq_up": nrm(ks[15], (L, Q_LORA, A_HEADS * (A_NOPE + A_ROPE)), Q_LORA ** -0.5),
        "a_kvnorm_g": 1.0 + nrm(ks[16], (L, KV_LORA), 0.05),
        "a_wkv_up": nrm(ks[17], (L, KV_LORA, A_HEADS * (A_NOPE + A_V)), KV_LORA ** -0.5),
        "w_out": nrm(ks[18], (L, D_MIX, D), D_MIX ** -0.5),
        "router_w": nrm(ks[19], (L, D, N_EXPERTS), D ** -0.5),
        "e_w_gate": nrm(ks[20], (L, N_EXPERTS, D, EXPERT_FF), D ** -0.5),
        "e_w_up": nrm(ks[21], (L, N_EXPERTS, D, EXPERT_FF), D ** -0.5),
        "e_w_down": nrm(ks[22], (L, N_EXPERTS, EXPERT_FF, D), EXPERT_FF ** -0.5),
        "final_g": 1.0 + nrm(ks[23], (D,), 0.05),
    }


def reference(x, c, ctx, c_ctx, ada_w, ada_b, norm1_g, norm2_g, w_in, m_conv_w, m_conv_b, m_ib, m_fb,
              m_norm_g, a_qnorm_g, a_wq_up, a_kvnorm_g, a_wkv_up, w_out, router_w, e_w_gate, e_w_up,
              e_w_down, final_g):
    b, n, _ = x.shape
    rope = axial_rope(n, x.dtype)
    for layer in range(DEPTH):
        with_ctx_out = layer < DEPTH - 1
        mod_x = (jax.nn.silu(c) @ ada_w[layer] + ada_b[layer])[:, None, :]
        mod_c = (jax.nn.silu(c_ctx) @ ada_w[layer] + ada_b[layer])[None, None, :]
        sh1x, sc1x, g1x, sh2x, sc2x, g2x = jnp.split(mod_x, 6, -1)
        sh1c, sc1c, g1c, sh2c, sc2c, g2c = jnp.split(mod_c, 6, -1)

        ux = modulated_rms(x, norm1_g[layer], sh1x, sc1x) @ w_in[layer]
        uc = modulated_rms(ctx, norm1_g[layer], sh1c, sc1c) @ w_in[layer]
        fx, qkx, vx, ox, gx, cqx, ckvx, krx = split_in(ux)
        fc, qkc, vc, oc, gc, cqc, ckvc, krc = split_in(uc)

        ya_x = fourier_mix(fx)
        lat_m = mlstm_prepare(qkx, vx, gx, m_conv_w[layer], m_conv_b[layer], m_ib[layer], m_fb[layer])
        ctx_m = mlstm_prepare(qkc, vc, gc, m_conv_w[layer], m_conv_b[layer], m_ib[layer], m_fb[layer])
        hb_x, hb_c = mlstm_bidirectional(ctx_m, lat_m)
        yb_x = mlstm_output(hb_x, ox, m_norm_g[layer])
        q_x, k_x, v_x = mla_project(cqx, ckvx, krx, a_qnorm_g[layer], a_wq_up[layer], a_kvnorm_g[layer], a_wkv_up[layer], rope)
        q_c, k_c, v_c = mla_project(cqc, ckvc, krc, a_qnorm_g[layer], a_wq_up[layer], a_kvnorm_g[layer], a_wkv_up[layer], None)
        yc_x = block_attention(q_x, jnp.concatenate([k_x, k_c], 1), jnp.concatenate([v_x, v_c], 1))

        x = x + g1x * (jnp.concatenate([ya_x, yb_x, yc_x], -1) @ w_out[layer])
        if with_ctx_out:
            ya_c = fourier_mix(fc)
            yb_c = mlstm_output(hb_c, oc, m_norm_g[layer])
            yc_c = block_attention(q_c, k_c, v_c)
            ctx = ctx + g1c * (jnp.concatenate([ya_c, yb_c, yc_c], -1) @ w_out[layer])

        hx = modulated_rms(x, norm2_g[layer], sh2x, sc2x)
        x = x + g2x * expert_choice_moe(hx, router_w[layer], e_w_gate[layer], e_w_up[layer], e_w_down[layer])
        if with_ctx_out:
            hc = modulated_rms(ctx, norm2_g[layer], sh2c, sc2c)
            ctx = ctx + g2c * expert_choice_moe(hc, router_w[layer], e_w_gate[layer], e_w_up[layer], e_w_down[layer])
    return rms_norm(x, final_g)
```

```python
import os
import numpy as np
import ml_dtypes
from contextlib import ExitStack
import concourse.bass as bass
import concourse.mybir as mybir
from concourse.bass_utils import run_bass_kernel_spmd

F32 = mybir.dt.float32
BF16 = mybir.dt.bfloat16
I32 = mybir.dt.int32
U32 = mybir.dt.uint32
AF = mybir.ActivationFunctionType
ALU = mybir.AluOpType
AX = mybir.AxisListType

D = 1024
SEQ = 8192
CTX = 256
DEPTH = 4
NT = (SEQ + CTX) // 128
TOK = SEQ + CTX
INC = 1968
NE = 16
CAPX = 1024
CAPC = 32
EPS = 1e-6


class Res:
    __slots__ = ("name", "w", "r", "excl")

    def __init__(self, name, excl=False):
        self.name = name
        self.w = None
        self.r = []
        self.excl = excl


class V:
    __slots__ = ("ap", "res")

    def __init__(self, ap, res):
        self.ap = ap
        self.res = res

    def map(self, fn):
        return V(fn(self.ap), self.res)


class Tile:
    def __init__(self, t, name):
        self.t = t
        self.res = Res(name)

    def __getitem__(self, idx):
        return V(self.t[idx], self.res)


class Eng:
    def __init__(self, name, h, sem):
        self.name = name
        self.h = h
        self.sem = sem
        self.count = 0
        self.seen = {}


def _ap(x):
    return x.ap if isinstance(x, V) else x


class Prog:
    WRITE_KEYS = ("out", "accum_out", "out_max", "out_indices")

    def __init__(self, nc, es, n_dma_sems=56):
        self.nc = nc
        self.es = es
        mk = lambda n: es.enter_context(nc.semaphore(n))
        self.pe = Eng("pe", nc.tensor, mk("s_pe"))
        self.act = Eng("act", nc.scalar, mk("s_act"))
        self.dve = Eng("dve", nc.vector, mk("s_dve"))
        self.pool = Eng("pool", nc.gpsimd, mk("s_pool"))
        self.sp = Eng("sp", nc.sync, mk("s_sp"))
        self.engs = [self.pe, self.act, self.dve, self.pool, self.sp]
        self.dsems = [[mk("s_d%d" % i), 0] for i in range(n_dma_sems)]
        self.dnext = 0
        self.ninst = 0

    def sb(self, es, name, shape, dt):
        self.nalloc = getattr(self, "nalloc", 0) + 1
        return Tile(es.enter_context(self.nc.sbuf_tensor("t%d_%s" % (self.nalloc, name), list(shape), dt)), name)

    def ps(self, es, name, shape, dt=F32):
        self.nalloc = getattr(self, "nalloc", 0) + 1
        t = Tile(es.enter_context(self.nc.psum_tensor("p%d_%s" % (self.nalloc, name), list(shape), dt)), name)
        t.res.excl = True
        return t

    def ring(self, es, name, shape, dt, n, psum=False):
        f = self.ps if psum else self.sb
        return Ring([f(es, "%s_%d" % (name, i), shape, dt) for i in range(n)])

    def _wait(self, E, tok):
        kind, key, val = tok
        if kind == "eng":
            sem = key.sem
            k = ("e", key.name)
        else:
            sem = self.dsems[key][0]
            k = ("d", key)
        if E.seen.get(k, 0) >= val:
            return
        E.h.wait_ge(sem, val)
        E.seen[k] = val
        self.ninst += 1

    def _deps(self, E, reads, writes):
        toks = []
        for r in reads:
            if r is not None and r.w is not None:
                toks.append(r.w)
            if r is not None and r.excl:
                toks.extend(r.r)
        for w in writes:
            if w is None:
                continue
            if w.w is not None:
                toks.append(w.w)
            toks.extend(w.r)
        for t in toks:
            if t[0] == "eng" and t[1] is E:
                continue
            self._wait(E, t)
        if E is not self.pe:
            for r in reads:
                if r is not None and r.w is not None and r.w[0] == "eng" and r.w[1] is E:
                    self._wait(E, r.w)

    def _commit(self, tok, reads, writes):
        for r in reads:
            if r is not None:
                r.r.append(tok)
        for w in writes:
            if w is not None:
                w.w = tok
                w.r = []

    def I(self, E, meth, *args, **kw):
        reads, writes = [], []
        for k, v in kw.items():
            if isinstance(v, V):
                (writes if k in self.WRITE_KEYS else reads).append(v.res)
        for v in args:
            if isinstance(v, V):
                reads.append(v.res)
        extra_r = kw.pop("_reads", ())
        extra_w = kw.pop("_writes", ())
        reads.extend(extra_r)
        writes.extend(extra_w)
        self._deps(E, reads, writes)
        ins = getattr(E.h, meth)(*[_ap(a) for a in args], **{k: _ap(v) for k, v in kw.items()})
        E.count += 1
        ins.then_inc(E.sem, 1)
        self.ninst += 1
        self._commit(("eng", E, E.count), reads, writes)
        return ins

    def dma(self, Q, out, in_, meth="dma_start", **kw):
        reads = [in_.res] if isinstance(in_, V) else []
        writes = [out.res] if isinstance(out, V) else []
        for k, v in kw.items():
            if isinstance(v, V):
                reads.append(v.res)
        reads.extend(kw.pop("_reads", ()))
        writes.extend(kw.pop("_writes", ()))
        self._deps(Q, reads, writes)
        i = self.dnext
        self.dnext = (self.dnext + 1) % len(self.dsems)
        sem, val = self.dsems[i]
        if val > 0:
            self._wait(Q, ("dma", i, val))
        val += 16
        self.dsems[i][1] = val
        ins = getattr(Q.h, meth)(out=_ap(out), in_=_ap(in_), **{k: _ap(v) for k, v in kw.items()})
        ins.then_inc(sem, 16)
        self.ninst += 1
        self._commit(("dma", i, val), reads, writes)
        return ins

    def barrier(self):
        toks = [("eng", e, e.count) for e in self.engs if e.count > 0]
        toks += [("dma", i, v) for i, (s, v) in enumerate(self.dsems) if v > 0]
        for e in self.engs:
            for t in toks:
                if t[0] == "eng" and t[1] is e:
                    continue
                self._wait(e, t)

    def mm(self, out, lhsT, rhs, start=True, stop=True):
        return self.I(self.pe, "matmul", out=out, lhsT=lhsT, rhs=rhs, start=start, stop=stop)

    def tr(self, out, in_, ident):
        return self.I(self.pe, "transpose", out=out, in_=in_, identity=ident)


class Ring:
    def __init__(self, tiles):
        self.tiles = tiles
        self.i = 0

    def next(self):
        t = self.tiles[self.i]
        self.i = (self.i + 1) % len(self.tiles)
        return t


def _consts():
    c = {}
    c["identb"] = np.eye(128, dtype=np.float32).astype(ml_dtypes.bfloat16)
    c["identf"] = np.eye(128, dtype=np.float32)
    k = np.arange(64)
    ang = 2 * np.pi * np.outer(k, k) / 64.0
    C64 = np.cos(ang) / 8.0
    S64 = -np.sin(ang) / 8.0
    bdc = np.zeros((128, 128)); bds = np.zeros((128, 128))
    for g in range(2):
        bdc[g * 64:(g + 1) * 64, g * 64:(g + 1) * 64] = C64
        bds[g * 64:(g + 1) * 64, g * 64:(g + 1) * 64] = S64
    c["bdc"] = bdc.astype(np.float32).astype(ml_dtypes.bfloat16)
    c["bds"] = bds.astype(np.float32).astype(ml_dtypes.bfloat16)
    t = np.arange(SEQ)
    row = (t // 64).astype(np.float64); col = (t % 64).astype(np.float64)
    inv = 10000.0 ** (-np.arange(8) / 8.0)
    ang = np.concatenate([row[:, None] * inv, col[:, None] * inv], -1)
    bf = lambda a: a.astype(np.float32).astype(ml_dtypes.bfloat16)
    k1 = np.arange(128); a1 = 2 * np.pi * np.outer(k1, k1) / 128.0
    c["f_c1"] = bf(np.cos(a1) / np.sqrt(128.0)); c["f_s1"] = bf(np.sin(a1) / np.sqrt(128.0))
    c["f_ns1"] = bf(-np.sin(a1) / np.sqrt(128.0))
    t2 = np.arange(64); atw = 2 * np.pi * np.outer(k1, t2) / 8192.0
    c["f_twc"] = np.cos(atw).astype(np.float32); c["f_tws"] = np.sin(atw).astype(np.float32)
    a2 = 2 * np.pi * np.outer(t2, t2) / 64.0
    c["f_c2"] = bf(np.cos(a2) / 8.0); c["f_s2"] = bf(np.sin(a2) / 8.0)
    tc = np.arange(256); ac = 2 * np.pi * np.outer(tc, tc) / 256.0
    c["f_cc"] = bf(np.cos(ac) / 16.0); c["f_sc"] = bf(np.sin(ac) / 16.0)
    ii_ = np.arange(128)
    c["utri"] = bf((ii_[:, None] < ii_[None, :]).astype(np.float32))
    c["onesb"] = bf(np.ones((128, 128)))
    capv = np.zeros((128, 32), np.float32); capv[:, 0:16] = CAPC; capv[:, 16:32] = CAPX
    c["capv"] = capv
    c["jrev"] = np.eye(128, dtype=np.float32)[::-1].copy()
    ii = np.arange(128)
    c["trif"] = (ii[:, None] <= ii[None, :]).astype(np.float32)
    c["trib"] = (ii[:, None] >= ii[None, :]).astype(np.float32)
    c["rcos"] = np.cos(ang).astype(np.float32)
    c["rsin"] = np.sin(ang).astype(np.float32)
    return c


WNAMES = [("ada_w", [DEPTH, D, 6 * D]), ("ada_b", [DEPTH, 6 * D]), ("norm1_g", [DEPTH, D]),
          ("norm2_g", [DEPTH, D]), ("w_in", [DEPTH, D, INC]), ("m_conv_w", [DEPTH, 3, 512]),
          ("m_conv_b", [DEPTH, 512]), ("m_ib", [DEPTH, 2, 4]), ("m_fb", [DEPTH, 2, 4]),
          ("m_norm_g", [DEPTH, 256]), ("a_qnorm_g", [DEPTH, 384]), ("a_wq_up", [DEPTH, 384, 768]),
          ("a_kvnorm_g", [DEPTH, 256]), ("a_wkv_up", [DEPTH, 256, 1024]), ("w_out", [DEPTH, D, D]),
          ("router_w", [DEPTH, D, NE]), ("e_w_gate", [DEPTH, NE, D, D]), ("e_w_up", [DEPTH, NE, D, D]),
          ("e_w_down", [DEPTH, NE, D, D]), ("final_g", [D])]


def build(nlayers=DEPTH, dbg=False, tiles=None, stages=None, lite=False, heads=None, qgroups=None):
    nc = bass.Bass("TRN2", target_bir_lowering=False)
    tiles = list(range(NT)) if tiles is None else tiles
    inp = {}
    inp["x"] = nc.dram_tensor("x", [SEQ, D], F32, kind="ExternalInput").ap()
    inp["c"] = nc.dram_tensor("c", [D], F32, kind="ExternalInput").ap()
    inp["ctx"] = nc.dram_tensor("ctx", [CTX, D], F32, kind="ExternalInput").ap()
    inp["c_ctx"] = nc.dram_tensor("c_ctx", [D], F32, kind="ExternalInput").ap()
    for n, shp in WNAMES:
        shp = list(shp)
        if n != "final_g":
            shp[0] = nlayers
        if lite and n.startswith("e_w_"):
            shp[1] = 1
        inp[n] = nc.dram_tensor(n, shp, F32, kind="ExternalInput").ap()
    cn = _consts()
    for n, a in cn.items():
        inp[n] = nc.dram_tensor("k_" + n, list(a.shape), BF16 if a.dtype == ml_dtypes.bfloat16 else F32,
                                kind="ExternalInput").ap()
    out = nc.dram_tensor("out", [SEQ, D], F32, kind="ExternalOutput").ap()
    skind = "ExternalOutput" if dbg else "Internal"
    scr = {}

    def scratch(name, shape, dt):
        scr[name] = nc.dram_tensor(name, shape, dt, kind=skind).ap()
        return scr[name]

    XR = scratch("XR", [TOK, D], F32)
    MOD = scratch("MOD", [DEPTH, 2, 6 * D], F32)
    FAB = scratch("FAB", [TOK, 512], BF16)
    QKT = scratch("QKT", [512, TOK], F32)
    VOG = scratch("VOG", [TOK, 528], F32)
    QT = scratch("QT", [8, 96, TOK], BF16)
    KT = scratch("KT", [8, 96, TOK], BF16)
    VA = scratch("VA", [TOK, 8, 65], BF16)
    MIXT = scratch("MIXT", [D, TOK], BF16)
    HFB = scratch("HFB", [2, TOK, 256], F32)
    FY = scratch("FY", [128, 64, 512], BF16)
    XN2 = scratch("XN2", [TOK, D], BF16)
    XEx = [scratch("XEx%d" % e, [CAPX, D], BF16) for e in range(NE)]
    XEc = [scratch("XEc%d" % e, [CAPC, D], BF16) for e in range(NE)]
    YEx = [scratch("YEx%d" % e, [CAPX, D], BF16) for e in range(NE)]
    YEc = [scratch("YEc%d" % e, [CAPC, D], BF16) for e in range(NE)]

    es = ExitStack()
    with es:
        P = Prog(nc, es)
        pe, act, dve, pool, sp = P.pe, P.act, P.dve, P.pool, P.sp
        bcreg = {}
        for cap_ in (CAPC, CAPX):
            bcreg[cap_] = nc.gpsimd.alloc_register("bc%d" % cap_)
            nc.gpsimd.reg_mov(bcreg[cap_], cap_ - 1)
        identb = P.sb(es, "identb", [128, 128], BF16)
        identf = P.sb(es, "identf", [128, 128], F32)
        P.dma(sp, identb[:], inp["identb"][:, :])
        P.dma(sp, identf[:], inp["identf"][:, :])
        banks = P.ring(es, "bank", [128, 512], F32, 6, psum=True)
        accb = P.ring(es, "accb", [128, 512], F32, 2, psum=True)
        epsc = P.sb(es, "epsc", [128, 1], F32)
        onec = P.sb(es, "onec", [128, 128], F32)
        P.I(dve, "memset", onec[:], 1.0, _writes=[onec.res])
        jrev = P.sb(es, "jrev", [128, 128], F32)
        trif = P.sb(es, "trif", [128, 128], F32)
        trib = P.sb(es, "trib", [128, 128], F32)
        P.dma(sp, jrev[:], inp["jrev"][:, :])
        P.dma(sp, trif[:], inp["trif"][:, :])
        P.dma(sp, trib[:], inp["trib"][:, :])
        P.I(dve, "memset", epsc[:], EPS, _writes=[epsc.res])

        def bank_bf(shape3):
            b = banks.next()
            v = b[:].map(lambda a: a.bitcast(BF16))
            if shape3 is not None:
                v = v.map(lambda a: a[:, 0:shape3[0] * shape3[1]].rearrange("p (a b) -> p a b", b=shape3[1]))
            return v

        P.dma(sp, XR[0:CTX, :], inp["ctx"][:, :])
        for q in range(4):
            P.dma(sp, XR[CTX + q * 2048:CTX + (q + 1) * 2048, :], inp["x"][q * 2048:(q + 1) * 2048, :])

        with ExitStack() as sa:
            cc = P.sb(sa, "cc", [128, 2, 8], F32)
            cs = P.sb(sa, "cs", [128, 2, 8], F32)
            P.dma(sp, cc[:, 0, :], inp["c"].rearrange("(p k) -> p k", k=8))
            P.dma(sp, cc[:, 1, :], inp["c_ctx"].rearrange("(p k) -> p k", k=8))
            P.I(act, "activation", out=cs[:], in_=cc[:], func=AF.Silu)
            adab = P.sb(sa, "adab", [1, 6 * D], F32)
            awr = P.ring(sa, "aw", [128, 8, 512], F32, 3)
            mrow = P.ring(sa, "mrow", [1, 512], F32, 4)
            for l in range(nlayers):
                P.dma(sp, adab[:], inp["ada_b"][l:l + 1, :])
                awv = inp["ada_w"][l].rearrange("(p k) n -> p k n", k=8)
                for j in range(12):
                    aw = awr.next()
                    P.dma(sp if j % 2 == 0 else pool, aw[:], awv[:, :, j * 512:(j + 1) * 512])
                    for v in range(2):
                        b = banks.next()
                        for k in range(8):
                            P.mm(b[0:1, :], cs[:, v, k:k + 1], aw[:, k, :], start=(k == 0), stop=(k == 7))
                        mr = mrow.next()
                        P.I(dve, "tensor_tensor", out=mr[:], in0=b[0:1, :], in1=adab[:, j * 512:(j + 1) * 512],
                            op=ALU.add)
                        P.dma(sp, MOD[l, v:v + 1, j * 512:(j + 1) * 512], mr[:])
            P.barrier()
        if stages == "A":
            P.barrier()
            return nc, cn, scr

        for l in range(nlayers):
            with ExitStack() as sl:
                colv = P.sb(sl, "colv", [128, 12, 8], F32)
                for v in range(2):
                    P.dma(sp, colv[:, v * 6:(v + 1) * 6, :],
                          MOD[l, v, :].rearrange("(s k p) -> p s k", p=128, k=8), allow_slow_non_contiguous=True)
                n1g = P.sb(sl, "n1g", [128, 8], F32)
                n2g = P.sb(sl, "n2g", [128, 8], F32)
                P.dma(sp, n1g[:], inp["norm1_g"][l].rearrange("(k p) -> p k", p=128), allow_slow_non_contiguous=True)
                P.dma(sp, n2g[:], inp["norm2_g"][l].rearrange("(k p) -> p k", p=128), allow_slow_non_contiguous=True)
                Amod = P.sb(sl, "Amod", [128, 4, 8], F32)
                for v in range(2):
                    P.I(dve, "scalar_tensor_tensor", out=Amod[:, v, :], in0=colv[:, v * 6 + 1, :], scalar=1.0,
                        in1=n1g[:], op0=ALU.add, op1=ALU.mult)
                    P.I(dve, "scalar_tensor_tensor", out=Amod[:, 2 + v, :], in0=colv[:, v * 6 + 4, :], scalar=1.0,
                        in1=n2g[:], op0=ALU.add, op1=ALU.mult)
                qg = P.sb(sl, "qg", [128, 3], F32)
                kvg = P.sb(sl, "kvg", [128, 2], F32)
                P.dma(sp, qg[:], inp["a_qnorm_g"][l].rearrange("(k p) -> p k", p=128), allow_slow_non_contiguous=True)
                P.dma(sp, kvg[:], inp["a_kvnorm_g"][l].rearrange("(k p) -> p k", p=128), allow_slow_non_contiguous=True)

                with ExitStack() as sB:
                    Win = P.sb(sB, "Win", [128, 8, INC], BF16)
                    for k in range(8):
                        P.dma(pool, Win[:, k, :], inp["w_in"][l, k * 128:(k + 1) * 128, :])
                    Wq = P.sb(sB, "Wq", [128, 3, 768], BF16)
                    P.dma(pool, Wq[:], inp["a_wq_up"][l].rearrange("(k p) n -> p k n", p=128))
                    Wkv = P.sb(sB, "Wkv", [128, 2, 1024], BF16)
                    P.dma(pool, Wkv[:], inp["a_wkv_up"][l].rearrange("(k p) n -> p k n", p=128))
                    bdc = P.sb(sB, "bdc", [128, 128], BF16)
                    bds = P.sb(sB, "bds", [128, 128], BF16)
                    P.dma(sp, bdc[:], inp["bdc"][:, :])
                    P.dma(sp, bds[:], inp["bds"][:, :])
                    xr_ = P.ring(sB, "xt", [128, D], F32, 2)
                    junk = P.sb(sB, "junk", [128, D], BF16)
                    st_ = P.ring(sB, "st", [128, 8], F32, 3)
                    xn_ = P.ring(sB, "xn", [128, D], BF16, 2)
                    hT_ = P.ring(sB, "hT", [128, 8, 128], BF16, 2)
                    ub_ = P.ring(sB, "ub", [128, 1040], F32, 2)
                    fb_ = P.ring(sB, "fb", [128, 256], BF16, 2)
                    fT_ = P.ring(sB, "fT", [128, 2, 128], BF16, 2)
                    ab_ = P.ring(sB, "ab", [128, 512], BF16, 2)
                    qkT_ = P.ring(sB, "qkT", [128, 4, 128], F32, 2)
                    cqn_ = P.ring(sB, "cqn", [128, 640], BF16, 2)
                    cT_ = P.ring(sB, "cT", [128, 5, 128], BF16, 2)
                    kr_ = P.ring(sB, "kr", [128, 32], F32, 2)
                    qs_ = P.ring(sB, "qs", [128, 8, 96], F32, 2)
                    rt_ = P.ring(sB, "rt", [128, 4, 8, 16], F32, 2)
                    cs_ = P.ring(sB, "cs", [128, 2, 16], F32, 2)
                    qb_ = P.ring(sB, "qb", [128, 8, 96], BF16, 2)
                    kb_ = P.ring(sB, "kb", [128, 8, 96], BF16, 2)
                    va_ = P.ring(sB, "va", [128, 8, 65], BF16, 2)
                    for t_ in va_.tiles:
                        P.I(dve, "memset", t_[:], 1.0, _writes=[t_.res])
                    qT_ = P.ring(sB, "qT", [96, 8, 128], BF16, 2)
                    kT_ = P.ring(sB, "kT", [96, 8, 128], BF16, 2)

                    for i in tiles:
                        isx = i >= 2
                        v = 0 if isx else 1
                        tok0 = i * 128
                        xt = xr_.next()
                        P.dma(sp, xt[:], XR[tok0:tok0 + 128, :])
                        st = st_.next()
                        P.I(act, "activation", out=junk[:], in_=xt[:], func=AF.Square, accum_out=st[:, 0:1])
                        P.I(act, "activation", out=st[:, 1:2], in_=st[:, 0:1], func=AF.Sqrt, scale=1.0 / D, bias=epsc[:])
                        P.I(dve, "reciprocal", out=st[:, 2:3], in_=st[:, 1:2])
                        xn = xn_.next()
                        P.I(dve, "tensor_scalar", out=xn[:], in0=xt[:], scalar1=st[:, 2:3], scalar2=None, op0=ALU.mult)
                        pT = bank_bf((8, 128))
                        for k in range(8):
                            P.tr(pT.map(lambda a: a[:, k, :]), xn[:, k * 128:(k + 1) * 128], identb[:])
                        hT = hT_.next()
                        bc = lambda vv: vv.map(lambda a: a.unsqueeze(2).to_broadcast([128, 8, 128]))
                        P.I(dve, "tensor_tensor", out=hT[:], in0=pT, in1=bc(Amod[:, v, :]), op=ALU.mult)
                        P.I(pool, "tensor_tensor", out=hT[:], in0=hT[:], in1=bc(colv[:, v * 6 + 0, :]), op=ALU.add)
                        ub = [banks.next() for _ in range(4)]
                        for n in range(4):
                            lo, hi = n * 512, min(INC, (n + 1) * 512)
                            for k in range(8):
                                P.mm(ub[n][:, 0:hi - lo], hT[:, k, :], Win[:, k, lo:hi], start=(k == 0), stop=(k == 7))
                        fb = fb_.next()
                        P.I(act, "activation", out=fb[:], in_=ub[0][:, 0:256], func=AF.Copy)
                        ubuf = ub_.next()
                        P.I(act, "activation", out=ubuf[:, 0:256], in_=ub[0][:, 256:512], func=AF.Copy)
                        P.I(dve, "tensor_copy", out=ubuf[:, 256:768], in_=ub[1][:, :])
                        P.I(act, "activation", out=ubuf[:, 768:1040], in_=ub[2][:, 0:272], func=AF.Copy)
                        P.I(act, "activation", out=junk[:, 0:240], in_=ub[2][:, 272:512], func=AF.Square,
                            accum_out=st[:, 3:4])
                        P.I(act, "activation", out=junk[:, 240:384], in_=ub[3][:, 0:144], func=AF.Square,
                            accum_out=st[:, 4:5])
                        P.I(act, "activation", out=junk[:, 384:640], in_=ub[3][:, 144:400], func=AF.Square,
                            accum_out=st[:, 5:6])
                        P.I(dve, "tensor_tensor", out=st[:, 3:4], in0=st[:, 3:4], in1=st[:, 4:5], op=ALU.add)
                        P.I(act, "activation", out=st[:, 4:5], in_=st[:, 3:4], func=AF.Sqrt, scale=1.0 / 384, bias=epsc[:])
                        P.I(dve, "reciprocal", out=st[:, 6:7], in_=st[:, 4:5])
                        P.I(act, "activation", out=st[:, 3:4], in_=st[:, 5:6], func=AF.Sqrt, scale=1.0 / 256, bias=epsc[:])
                        P.I(dve, "reciprocal", out=st[:, 7:8], in_=st[:, 3:4])
                        cqn = cqn_.next()
                        P.I(dve, "tensor_scalar", out=cqn[:, 0:240], in0=ub[2][:, 272:512], scalar1=st[:, 6:7],
                            scalar2=None, op0=ALU.mult)
                        P.I(dve, "tensor_scalar", out=cqn[:, 240:384], in0=ub[3][:, 0:144], scalar1=st[:, 6:7],
                            scalar2=None, op0=ALU.mult)
                        P.I(dve, "tensor_scalar", out=cqn[:, 384:640], in0=ub[3][:, 144:400], scalar1=st[:, 7:8],
                            scalar2=None, op0=ALU.mult)
                        kr = kr_.next()
                        P.I(act, "activation", out=kr[:], in_=ub[3][:, 400:432], func=AF.Copy)
                        P.dma(sp, VOG[tok0:tok0 + 128, :], ubuf[:, 512:1040])
                        pf = bank_bf((2, 128))
                        for cc in range(2):
                            P.tr(pf.map(lambda a: a[:, cc, :]), fb[:, cc * 128:(cc + 1) * 128], identb[:])
                        fT = fT_.next()
                        P.I(act, "activation", out=fT[:], in_=pf, func=AF.Copy)
                        abp = banks.next()
                        for cc in range(2):
                            P.mm(abp[:, cc * 128:(cc + 1) * 128], fT[:, cc, :], bdc[:])
                            P.mm(abp[:, 256 + cc * 128:256 + (cc + 1) * 128], fT[:, cc, :], bds[:])
                        ab = ab_.next()
                        P.I(act, "activation", out=ab[:], in_=abp[:], func=AF.Copy)
                        P.dma(sp, FAB[tok0:tok0 + 128, :], ab[:])
                        pq = banks.next()
                        for cc in range(4):
                            P.tr(pq[:, cc * 128:(cc + 1) * 128], ubuf[:, cc * 128:(cc + 1) * 128], identf[:])
                        qkT = qkT_.next()
                        P.I(dve, "tensor_copy", out=qkT[:], in_=pq[:].map(lambda a: a.rearrange("p (c t) -> p c t", t=128)))
                        P.dma(sp, QKT.rearrange("(c p) t -> p c t", p=128)[:, :, tok0:tok0 + 128], qkT[:])
                        pc = bank_bf((5, 128))
                        for cc in range(5):
                            P.tr(pc.map(lambda a: a[:, cc, :]), cqn[:, cc * 128:(cc + 1) * 128], identb[:])
                        cT = cT_.next()
                        P.I(dve, "tensor_tensor", out=cT[:, 0:3, :], in0=pc.map(lambda a: a[:, 0:3, :]),
                            in1=qg[:].map(lambda a: a.unsqueeze(2).to_broadcast([128, 3, 128])), op=ALU.mult)
                        P.I(dve, "tensor_tensor", out=cT[:, 3:5, :], in0=pc.map(lambda a: a[:, 3:5, :]),
                            in1=kvg[:].map(lambda a: a.unsqueeze(2).to_broadcast([128, 2, 128])), op=ALU.mult)
                        qp = [banks.next(), banks.next()]
                        for n, (lo, hi) in enumerate([(0, 512), (512, 768)]):
                            for kk in range(3):
                                P.mm(qp[n][:, 0:hi - lo], cT[:, kk, :], Wq[:, kk, lo:hi], start=(kk == 0), stop=(kk == 2))
                        kvp = [banks.next(), banks.next()]
                        for n in range(2):
                            for kk in range(2):
                                P.mm(kvp[n][:, :], cT[:, 3 + kk, :], Wkv[:, kk, n * 512:(n + 1) * 512],
                                     start=(kk == 0), stop=(kk == 1))
                        qs = qs_.next()
                        sc = 96.0 ** -0.5
                        qsf = qs[:].map(lambda a: a.rearrange("p h e -> p (h e)"))
                        P.I(act, "activation", out=qsf.map(lambda a: a[:, 0:512]), in_=qp[0][:, :], func=AF.Copy, scale=sc)
                        P.I(act, "activation", out=qsf.map(lambda a: a[:, 512:768]), in_=qp[1][:, 0:256], func=AF.Copy, scale=sc)
                        qb = qb_.next()
                        kb = kb_.next()
                        va = va_.next()
                        P.I(act, "activation", out=qb[:, :, 0:64], in_=qs[:, :, 0:64], func=AF.Copy)
                        if isx:
                            cst = cs_.next()
                            P.dma(sp, cst[:, 0, :], inp["rcos"][tok0 - CTX:tok0 - CTX + 128, :])
                            P.dma(sp, cst[:, 1, :], inp["rsin"][tok0 - CTX:tok0 - CTX + 128, :])
                            rt = rt_.next()
                            cb = cst[:, 0, :].map(lambda a: a.unsqueeze(1).to_broadcast([128, 8, 16]))
                            sbn = cst[:, 1, :].map(lambda a: a.unsqueeze(1).to_broadcast([128, 8, 16]))
                            P.I(dve, "tensor_tensor", out=rt[:, 0], in0=qs[:, :, 64:80], in1=cb, op=ALU.mult)
                            P.I(dve, "tensor_tensor", out=rt[:, 1], in0=qs[:, :, 80:96], in1=sbn, op=ALU.mult)
                            P.I(pool, "tensor_tensor", out=rt[:, 2], in0=qs[:, :, 64:80], in1=sbn, op=ALU.mult)
                            P.I(pool, "tensor_tensor", out=rt[:, 3], in0=qs[:, :, 80:96], in1=cb, op=ALU.mult)
                            P.I(dve, "tensor_tensor", out=qb[:, :, 64:80], in0=rt[:, 0], in1=rt[:, 1], op=ALU.subtract)
                            P.I(dve, "tensor_tensor", out=qb[:, :, 80:96], in0=rt[:, 2], in1=rt[:, 3], op=ALU.add)
                            P.I(dve, "tensor_tensor", out=rt[:, 0, 0, :], in0=kr[:, 0:16], in1=cst[:, 0, :], op=ALU.mult)
                            P.I(dve, "tensor_tensor", out=rt[:, 1, 0, :], in0=kr[:, 16:32], in1=cst[:, 1, :], op=ALU.mult)
                            P.I(dve, "tensor_tensor", out=rt[:, 2, 0, :], in0=kr[:, 0:16], in1=cst[:, 1, :], op=ALU.mult)
                            P.I(dve, "tensor_tensor", out=rt[:, 3, 0, :], in0=kr[:, 16:32], in1=cst[:, 0, :], op=ALU.mult)
                            P.I(dve, "tensor_tensor", out=kr[:, 0:16], in0=rt[:, 0, 0, :], in1=rt[:, 1, 0, :], op=ALU.subtract)
                            P.I(dve, "tensor_tensor", out=kr[:, 16:32], in0=rt[:, 2, 0, :], in1=rt[:, 3, 0, :], op=ALU.add)
                        else:
                            P.I(act, "activation", out=qb[:, :, 64:96], in_=qs[:, :, 64:96], func=AF.Copy)
                        kv3 = lambda n: kvp[n][:, :].map(lambda a: a.rearrange("p (h e) -> p h e", e=128))
                        for n in range(2):
                            P.I(act, "activation", out=kb[:, n * 4:(n + 1) * 4, 0:64], in_=kv3(n).map(lambda a: a[:, :, 0:64]),
                                func=AF.Copy)
                            P.I(dve, "tensor_copy", out=va[:, n * 4:(n + 1) * 4, 0:64], in_=kv3(n).map(lambda a: a[:, :, 64:128]))
                        P.I(pool, "tensor_copy", out=kb[:, :, 64:96],
                            in_=kr[:].map(lambda a: a.unsqueeze(1).to_broadcast([128, 8, 32])))
                        P.dma(sp, VA[tok0:tok0 + 128, :, :], va[:])
                        for (src, ring_, dst) in ((qb, qT_, QT), (kb, kT_, KT)):
                            pt = banks.next()
                            ptv = pt[0:96, :].map(lambda a: a.bitcast(BF16)[:, 0:1024].rearrange("p (h t) -> p h t", t=128))
                            for h in range(8):
                                P.tr(ptv.map(lambda a: a[:, h, :]), src[:, h, :], identb[:])
                            tt = ring_.next()
                            P.I(act, "activation", out=tt[:], in_=ptv, func=AF.Copy)
                            P.dma(sp, dst.rearrange("h r t -> r h t")[:, :, tok0:tok0 + 128], tt[:])
                    P.barrier()
                if stages == "B":
                    return nc, cn, scr

                with_ctx = l < DEPTH - 1
                with ExitStack() as sC:
                    KTh_ = P.ring(sC, "KTh", [96, TOK], BF16, 2)
                    VAh_ = P.ring(sC, "VAh", [128, NT, 65], BF16, 2)
                    QTg_ = P.ring(sC, "QTg", [96, 512], BF16, 3)
                    PT_ = P.ring(sC, "PT", [128, 512], BF16, 4)
                    Usb_ = P.ring(sC, "Usb", [65, 512], F32, 2)
                    rec_ = P.ring(sC, "rec", [64, 512], F32, 2)
                    yc_ = P.ring(sC, "yc", [64, 512], BF16, 2)
                    esel = P.sb(sC, "esel", [65, 64], F32)
                    P.I(dve, "memset", esel[:], 0.0, _writes=[esel.res])
                    P.I(dve, "memset", esel[64:65, :], 1.0, _writes=[esel.res])
                    for h in (heads if heads is not None else range(8)):
                        KTh = KTh_.next()
                        VAh = VAh_.next()
                        P.dma(sp, KTh[:], KT[h, :, :])
                        for j0 in range(0, NT, 11):
                            P.dma(sp, VAh[:, j0:j0 + 11, :],
                                  VA[j0 * 128:(j0 + 11) * 128, h, :].rearrange("(j p) e -> p j e", p=128))
                        groups = [(CTX + g * 512, 512, list(range(NT))) for g in range(16)]
                        if qgroups is not None:
                            groups = [groups[g] for g in qgroups]
                        if with_ctx:
                            groups.append((0, 256, [0, 1]))
                        for (q0, nq, ktiles) in groups:
                            QTg = QTg_.next()
                            P.dma(sp, QTg[:, 0:nq], QT[h, :, q0:q0 + nq])
                            acc = accb.next()
                            for jj, j in enumerate(ktiles):
                                sp_ = banks.next()
                                P.mm(sp_[:, 0:nq], KTh[:, j * 128:(j + 1) * 128], QTg[:, 0:nq])
                                PT = PT_.next()
                                P.I(act, "activation", out=PT[:, 0:nq], in_=sp_[:, 0:nq], func=AF.Exp)
                                P.mm(acc[0:65, 0:nq], VAh[:, j, :], PT[:, 0:nq], start=(jj == 0),
                                     stop=(jj == len(ktiles) - 1))
                            Usb = Usb_.next()
                            P.I(dve, "tensor_copy", out=Usb[:, 0:nq], in_=acc[0:65, 0:nq])
                            rp = banks.next()
                            P.mm(rp[0:64, 0:nq], esel[:], Usb[:, 0:nq])
                            rec = rec_.next()
                            P.I(dve, "reciprocal", out=rec[:, 0:nq], in_=rp[0:64, 0:nq])
                            yc = yc_.next()
                            P.I(dve, "tensor_tensor", out=yc[:, 0:nq], in0=Usb[0:64, 0:nq], in1=rec[:, 0:nq], op=ALU.mult)
                            P.dma(sp, MIXT[512 + h * 64:512 + (h + 1) * 64, q0:q0 + nq], yc[:, 0:nq])
                    P.barrier()
                if stages == "C":
                    return nc, cn, scr

                def rtile(i):
                    return 1 - i if i < 2 else 67 - i

                def qkcol(i):
                    return i * 128 + (2 if i < 2 else 4)

                if os.environ.get("M2CUT") == "-1":
                    P.barrier()
                    return nc, cn, scr

                with ExitStack() as sM:
                    TKS = P.sb(sM, "TKS", [128, NT, 16], F32)
                    ECB = P.sb(sM, "ECB", [128, 8, NT], F32)
                    with ExitStack() as s2:
                        GI = P.sb(s2, "GI", [8, TOK], F32)
                        GF = P.sb(s2, "GF", [8, TOK], F32)
                        MM = P.sb(s2, "MM", [8, TOK], F32)
                        MST = P.sb(s2, "MST", [16, TOK], F32)
                        EF = P.sb(s2, "EF", [40, TOK], F32)
                        gall = P.sb(s2, "gall", [128, NT, 16], F32)
                        gb = P.sb(s2, "gb", [8, 4], F32)
                        ecs = P.sb(s2, "ecs", [8, NT], F32)
                        Dg = P.sb(s2, "Dg", [8, 8, NT], F32)
                        tk_ = P.ring(s2, "tk", [128, 40], F32, 2)
                        P.I(pool, "memset", EF[:], 0.0, _writes=[EF.res])

                        if os.environ.get("M2CUT") == "-0.5":
                            P.barrier()
                            return nc, cn, scr
                        P.dma(sp, gall[:], VOG[:, 512:528].rearrange("(j p) g -> p j g", p=128))

                        if os.environ.get("M2CUT") == "-0.3":
                            P.barrier()
                            return nc, cn, scr
                        grow = P.sb(s2, "grow", [1, 16], F32)
                        P.dma(sp, grow[:, 0:8], inp["m_ib"][l:l + 1].rearrange("o d h -> o (d h)"))
                        P.dma(sp, grow[:, 8:16], inp["m_fb"][l:l + 1].rearrange("o d h -> o (d h)"))
                        pgb = banks.next()
                        P.mm(pgb[0:8, 0:1], grow[:, 0:8], onec[0:1, 0:1])
                        P.mm(pgb[0:8, 1:2], grow[:, 8:16], onec[0:1, 0:1])
                        P.I(dve, "tensor_copy", out=gb[:, 0:2], in_=pgb[0:8, 0:2])
                        P.I(dve, "tensor_scalar", out=gb[:, 2:3], in0=gb[:, 1:2], scalar1=-1.0, scalar2=None, op0=ALU.mult)

                        if os.environ.get("M2CUT") == "0":
                            P.barrier()
                            return nc, cn, scr
                        for i in range(NT):
                            pg = banks.next()
                            sk = os.environ.get("M2SKIP", "")
                            if "a" not in sk:
                                P.mm(pg[0:16, 0:128], gall[:, i, :], identf[:])
                            if "b" not in sk:
                                P.mm(pg[0:16, 128:256], gall[:, i, :], jrev[:])
                            if "c" not in sk:
                                P.I(act, "activation", out=MST[0:16, i * 128:(i + 1) * 128], in_=pg[0:16, 0:128], func=AF.Copy)
                            r_ = rtile(i)
                            if "d" not in sk:
                                P.I(dve, "tensor_copy", out=EF[0:16, r_ * 128:(r_ + 1) * 128], in_=pg[0:16, 128:256])

                        if os.environ.get("M2CUT") == "0b":
                            P.barrier()
                            return nc, cn, scr
                        P.dma(sp, GI[0:4, :], MST[0:4, :])
                        P.dma(sp, GF[0:4, :], MST[4:8, :])
                        P.dma(sp, GI[4:8, :], EF[8:12, :])
                        P.dma(sp, GF[4:8, :], EF[12:16, :])

                        if os.environ.get("M2CUT") == "1":
                            P.barrier()
                            return nc, cn, scr
                        P.I(dve, "tensor_scalar", out=GI[:], in0=GI[:], scalar1=gb[:, 0:1], scalar2=None, op0=ALU.add)
                        P.I(act, "activation", out=GF[:], in_=GF[:], func=AF.Exp, scale=-1.0, bias=gb[:, 2:3])
                        P.I(act, "activation", out=GF[:], in_=GF[:], func=AF.Ln, bias=onec[0:8, 0:1])
                        onesb = onec[0:8, 0:1].map(lambda a: a.to_broadcast([8, TOK]))
                        P.I(dve, "tensor_tensor_scan", out=GF[:], data0=onesb, data1=GF[:], initial=0.0, op0=ALU.mult, op1=ALU.add)
                        P.I(dve, "tensor_tensor", out=GI[:], in0=GI[:], in1=GF[:], op=ALU.add)
                        P.I(dve, "tensor_tensor_scan", out=MM[:], data0=onesb, data1=GI[:], initial=0.0, op0=ALU.mult, op1=ALU.max)

                        if os.environ.get("M2CUT") == "2":
                            P.barrier()
                            return nc, cn, scr
                        v3 = lambda vv: vv.map(lambda a: a.rearrange("p (c t) -> p c t", t=128))
                        P.I(pool, "memset", MST[0:8, 0:128], 0.0, _writes=[MST.res])
                        P.I(dve, "tensor_copy", out=v3(MST[0:8, :]).map(lambda a: a[:, 1:NT, :]),
                            in_=v3(MM[:]).map(lambda a: a[:, 0:NT - 1, 127:128].to_broadcast([8, NT - 1, 128])))
                        P.I(dve, "tensor_tensor", out=ecs[:], in0=v3(MST[0:8, :]).map(lambda a: a[:, :, 0]),
                            in1=v3(MM[:]).map(lambda a: a[:, :, 127]), op=ALU.subtract)
                        P.I(act, "activation", out=ecs[:], in_=ecs[:], func=AF.Exp)
                        P.I(dve, "tensor_tensor", out=GI[:], in0=GI[:], in1=MST[0:8, :], op=ALU.subtract)
                        P.I(dve, "tensor_tensor", out=GF[:], in0=GF[:], in1=MST[0:8, :], op=ALU.subtract)
                        P.I(act, "activation", out=EF[0:8, :], in_=GI[:], func=AF.Exp)
                        P.I(act, "activation", out=EF[32:40, :], in_=GF[:], func=AF.Exp)

                        if os.environ.get("M2CUT") == "3":
                            P.barrier()
                            return nc, cn, scr
                        P.I(dve, "tensor_tensor", out=Dg[:],
                            in0=ecs[:].map(lambda a: a.unsqueeze(1).to_broadcast([8, 8, NT])),
                            in1=identf[0:8, 0:8].map(lambda a: a.unsqueeze(2).to_broadcast([8, 8, NT])), op=ALU.mult)
                        pe_ = [banks.next(), banks.next()]
                        Dgf = Dg[:].map(lambda a: a.rearrange("p a c -> p (a c)"))
                        hN = 4 * NT
                        for n in range(2):
                            P.mm(pe_[n][:, 0:hN], onec[0:8, :], Dgf.map(lambda a: a[:, n * hN:(n + 1) * hN]))
                            P.I(act, "activation", out=ECB[:, n * 4:(n + 1) * 4, :].map(lambda a: a.rearrange("p a c -> p (a c)")),
                                in_=pe_[n][:, 0:hN], func=AF.Copy)
                        for i in range(NT):
                            pt = banks.next()
                            P.tr(pt[:, 0:40], EF[0:40, i * 128:(i + 1) * 128], identf[0:40, 0:40])
                            r_ = rtile(i)
                            P.tr(pt[:, 64:104], EF[0:40, r_ * 128:(r_ + 1) * 128], identf[0:40, 0:40])
                            tk = tk_.next()
                            P.I(act, "activation", out=tk[:], in_=pt[:, 64:104], func=AF.Copy)
                            P.mm(pt[:, 128:168], jrev[:], tk[:])
                            tv = TKS[:, i, :].map(lambda a: a.rearrange("p (q j) -> p q j", j=8))
                            pv = lambda c0: pt[:, c0:c0 + 64].map(lambda a: a.rearrange("p (q j) -> p q j", j=32))
                            P.I(dve, "tensor_copy", out=tv.map(lambda a: a[:, :, 0:4]), in_=pv(0).map(lambda a: a[:, :, 0:4]))
                            P.I(dve, "tensor_copy", out=tv.map(lambda a: a[:, :, 4:8]), in_=pv(128).map(lambda a: a[:, :, 4:8]))
                        P.barrier()
                    PADW = TOK + 6
                    QKb = P.sb(sM, "QKb", [128, 4, PADW], BF16)
                    ktm = P.sb(sM, "ktm", [128, NT, 256], BF16)
                    with ExitStack() as s1:
                        cw = P.sb(s1, "cw", [128, 4, 3], F32)
                        cbi = P.sb(s1, "cbi", [128, 4], F32)
                        for kk in range(3):
                            P.dma(sp, cw[:, :, kk], inp["m_conv_w"][l, kk].rearrange("(c p) -> p c", p=128),
                                  allow_slow_non_contiguous=True)
                        P.dma(sp, cbi[:], inp["m_conv_b"][l].rearrange("(c p) -> p c", p=128), allow_slow_non_contiguous=True)
                        HP = 4230
                        stg_ = P.ring(s1, "stg", [128, HP], F32, 2)
                        yb_ = P.ring(s1, "ybuf", [128, HP], F32, 2)
                        for cc in range(4):
                            rows = QKT[cc * 128:(cc + 1) * 128, :]
                            for piece in range(2):
                                stg = stg_.next()
                                yb = yb_.next()
                                if piece == 0:
                                    n = 4230
                                    P.I(pool, "memset", stg[:, 0:2], 0.0, _writes=[stg.res])
                                    P.I(pool, "memset", stg[:, 258:260], 0.0, _writes=[stg.res])
                                    P.dma(sp, stg[:, 2:258], rows[:, 0:256])
                                    P.dma(sp, stg[:, 260:4230], rows[:, 256:256 + 3970])
                                    oc0 = 1
                                else:
                                    n = 4226
                                    P.dma(sp, stg[:, 0:4224], rows[:, 4224:8448])
                                    P.I(pool, "memset", stg[:, 4224:4226], 0.0, _writes=[stg.res])
                                    oc0 = 4229
                                m = n - 2
                                P.I(dve, "tensor_scalar", out=yb[:, 0:m], in0=stg[:, 1:1 + m], scalar1=cw[:, cc, 1:2],
                                    scalar2=cbi[:, cc:cc + 1], op0=ALU.mult, op1=ALU.add)
                                P.I(dve, "scalar_tensor_tensor", out=yb[:, 0:m], in0=stg[:, 0:m], scalar=cw[:, cc, 0:1],
                                    in1=yb[:, 0:m], op0=ALU.mult, op1=ALU.add)
                                P.I(dve, "scalar_tensor_tensor", out=yb[:, 0:m], in0=stg[:, 2:2 + m], scalar=cw[:, cc, 2:3],
                                    in1=yb[:, 0:m], op0=ALU.mult, op1=ALU.add)
                                if cc < 2:
                                    P.I(act, "activation", out=yb[:, 0:m], in_=yb[:, 0:m], func=AF.Silu)
                                    P.I(pool, "tensor_scalar", out=QKb[:, cc, oc0:oc0 + m], in0=yb[:, 0:m], scalar1=0.125,
                                        scalar2=None, op0=ALU.mult)
                                else:
                                    P.I(act, "activation", out=QKb[:, cc, oc0:oc0 + m], in_=yb[:, 0:m], func=AF.Silu)
                        for i in range(NT):
                            pk = bank_bf((2, 128))
                            c0 = qkcol(i)
                            for kc in range(2):
                                P.tr(pk.map(lambda a: a[:, kc, :]), QKb[:, 2 + kc, c0:c0 + 128], identb[:])
                            P.I(act, "activation", out=ktm[:, i, :].map(lambda a: a.rearrange("p (c t) -> p c t", t=128)),
                                in_=pk, func=AF.Copy)
                        P.barrier()
                    if stages == "M1":
                        return nc, cn, scr
                    with ExitStack() as s3:
                        V1 = P.sb(s3, "V1", [128, NT, 4, 65], BF16)
                        P.I(pool, "memset", V1[:], 1.0, _writes=[V1.res])
                        for j0 in range(0, NT, 11):
                            for h in range(4):
                                P.dma(pool, V1[:, j0:j0 + 11, h, 0:64],
                                      VOG[j0 * 128:(j0 + 11) * 128, h * 64:(h + 1) * 64].rearrange("(j p) e -> p j e", p=128))
                        CN = [P.sb(s3, "CN%d" % k, [128, 65], F32) for k in range(8)]
                        CNb = [P.sb(s3, "CNb%d" % k, [128, 65], BF16) for k in range(8)]
                        for k in range(8):
                            P.I(dve, "memset", CN[k][:], 0.0, _writes=[CN[k].res])
                            P.I(dve, "memset", CNb[k][:], 0.0, _writes=[CNb[k].res])
                        Sm_ = P.ring(s3, "Sm", [128, 128], BF16, 6)
                        vpp_ = P.ring(s3, "vpp", [128, 65], BF16, 6)
                        dn_ = P.ring(s3, "dn", [128, 2], F32, 6)
                        tmp_ = P.ring(s3, "ctmp", [128, 65], F32, 6)
                        Hst_ = [P.ring(s3, "Hst%d" % d_, [128, 256], F32, 3) for d_ in range(2)]
                        for c in range(NT):
                            for d_ in range(2):
                                i = c if d_ == 0 else (1 - c if c < 2 else 67 - c)
                                c0 = qkcol(i)
                                Hst = Hst_[d_].next()
                                tri = trif if d_ == 0 else trib
                                for h in range(4):
                                    k = d_ * 4 + h
                                    pb = (h % 2) * 64
                                    qv = QKb[pb:pb + 64, h // 2, c0:c0 + 128]
                                    kv_ = QKb[pb:pb + 64, 2 + h // 2, c0:c0 + 128]
                                    sps = banks.next()
                                    P.mm(sps[:, 0:128], kv_, qv)
                                    Sm = Sm_.next()
                                    P.I(dve, "tensor_tensor", out=Sm[:], in0=sps[:, 0:128], in1=tri[:], op=ALU.mult)
                                    vpp = vpp_.next()
                                    P.I(pool, "tensor_scalar", out=vpp[:], in0=V1[:, i, h, :], scalar1=TKS[:, i, k:k + 1],
                                        scalar2=None, op0=ALU.mult)
                                    nd = banks.next()
                                    P.mm(nd[:, 0:65], Sm[:], vpp[:], start=True, stop=False)
                                    P.mm(nd[:, 0:65], qv, CNb[k][pb:pb + 64, :], start=False, stop=True)
                                    P.mm(nd[0:64, 128:193], ktm[:, i, h * 64:(h + 1) * 64], vpp[:])
                                    dn = dn_.next()
                                    P.I(dve, "tensor_scalar", out=dn[:, 1:2], in0=nd[:, 64:65], scalar1=-1.0,
                                        scalar2=None, op0=ALU.mult)
                                    P.I(dve, "scalar_tensor_tensor", out=dn[:, 0:1], in0=dn[:, 1:2], scalar=TKS[:, i, 8 + k:9 + k],
                                        in1=nd[:, 64:65], op0=ALU.max, op1=ALU.max)
                                    P.I(dve, "reciprocal", out=dn[:, 1:2], in_=dn[:, 0:1])
                                    P.I(dve, "tensor_scalar", out=Hst[:, h * 64:(h + 1) * 64], in0=nd[:, 0:64],
                                        scalar1=dn[:, 1:2], scalar2=None, op0=ALU.mult)
                                    tmp = tmp_.next()
                                    P.I(dve, "tensor_tensor", out=tmp[0:64, :], in0=CN[k][0:64, :],
                                        in1=nd[0:64, 128:193], op=ALU.add)
                                    P.I(pool, "tensor_scalar", out=CN[k][0:64, :], in0=tmp[0:64, :],
                                        scalar1=ECB[0:64, k, c:c + 1], scalar2=None, op0=ALU.mult)
                                    P.I(act, "activation", out=CNb[k][pb:pb + 64, :], in_=tmp[0:64, :], func=AF.Copy,
                                        scale=ECB[0:64, k, c:c + 1])
                                P.dma(sp, HFB[d_, i * 128:(i + 1) * 128, :], Hst[:])
                        P.barrier()
                    if stages == "M3":
                        return nc, cn, scr
                    with ExitStack() as s4:
                        mng = P.sb(s4, "mng", [128, 256], F32)
                        P.dma(sp, mng[:], inp["m_norm_g"][l:l + 1, :].partition_broadcast(128))
                        hf_ = P.ring(s4, "hf", [128, 2, 256], F32, 2)
                        og_ = P.ring(s4, "og", [128, 256], F32, 2)
                        hs_ = P.ring(s4, "hs", [128, 256], F32, 2)
                        sq_ = P.ring(s4, "sq", [128, 256], F32, 2)
                        ms_ = P.ring(s4, "ms", [128, 8], F32, 2)
                        ybf_ = P.ring(s4, "ybf", [128, 256], BF16, 2)
                        ybT_ = P.ring(s4, "ybT", [128, 2, 128], BF16, 2)
                        for i in range(NT):
                            if i < 2 and not with_ctx:
                                continue
                            hf = hf_.next()
                            P.dma(sp, hf[:], HFB[:, i * 128:(i + 1) * 128, :].rearrange("d p e -> p d e"))
                            og = og_.next()
                            P.dma(sp, og[:], VOG[i * 128:(i + 1) * 128, 256:512])
                            hs = hs_.next()
                            P.I(dve, "tensor_tensor", out=hs[:], in0=hf[:, 0, :], in1=hf[:, 1, :], op=ALU.add)
                            sq = sq_.next()
                            P.I(pool, "tensor_tensor", out=sq[:], in0=hs[:], in1=hs[:], op=ALU.mult)
                            ms = ms_.next()
                            P.I(dve, "tensor_reduce", out=ms[:, 0:4], in_=sq[:].map(lambda a: a.rearrange("p (h e) -> p h e", e=64)),
                                axis=AX.X, op=ALU.add)
                            P.I(act, "activation", out=ms[:, 0:4], in_=ms[:, 0:4], func=AF.Sqrt, scale=1.0 / 64, bias=epsc[:])
                            P.I(dve, "reciprocal", out=ms[:, 4:8], in_=ms[:, 0:4])
                            P.I(act, "activation", out=og[:], in_=og[:], func=AF.Sigmoid)
                            h3 = lambda vv: vv.map(lambda a: a.rearrange("p (h e) -> p h e", e=64))
                            P.I(dve, "tensor_tensor", out=h3(hs[:]), in0=h3(hs[:]),
                                in1=ms[:, 4:8].map(lambda a: a.unsqueeze(2).to_broadcast([128, 4, 64])), op=ALU.mult)
                            P.I(pool, "tensor_tensor", out=hs[:], in0=hs[:], in1=mng[:], op=ALU.mult)
                            ybf = ybf_.next()
                            P.I(dve, "tensor_tensor", out=ybf[:], in0=hs[:], in1=og[:], op=ALU.mult)
                            py = bank_bf((2, 128))
                            for cc in range(2):
                                P.tr(py.map(lambda a: a[:, cc, :]), ybf[:, cc * 128:(cc + 1) * 128], identb[:])
                            ybT = ybT_.next()
                            P.I(act, "activation", out=ybT[:], in_=py, func=AF.Copy)
                            P.dma(sp, MIXT[256:512, i * 128:(i + 1) * 128].rearrange("(c p) t -> p c t", p=128), ybT[:])
                        P.barrier()
                if stages == "M":
                    return nc, cn, scr

                with ExitStack() as sF:
                    ld = lambda nm, shp, dt: P.sb(sF, nm, shp, dt)
                    c1 = ld("f_c1", [128, 128], BF16); s1t = ld("f_s1", [128, 128], BF16); ns1 = ld("f_ns1", [128, 128], BF16)
                    twc = ld("f_twc", [128, 64], F32); tws = ld("f_tws", [128, 64], F32)
                    c2 = ld("f_c2", [64, 64], BF16); s2t = ld("f_s2", [64, 64], BF16)
                    ccs = ld("f_ccs", [128, 2, 2, 256], BF16)
                    for tl, nm in ((c1, "f_c1"), (s1t, "f_s1"), (ns1, "f_ns1"), (twc, "f_twc"), (tws, "f_tws"), (c2, "f_c2"), (s2t, "f_s2")):
                        P.dma(sp, tl[:], inp[nm][:, :])
                    P.dma(sp, ccs[:, 0, :, :], inp["f_cc"].rearrange("(a p) k -> p a k", p=128))
                    P.dma(sp, ccs[:, 1, :, :], inp["f_sc"].rearrange("(a p) k -> p a k", p=128))
                    with ExitStack() as sF1:
                        X1 = P.sb(sF1, "X1", [128, 64, 512], BF16)
                        YT = P.sb(sF1, "YT", [128, 64, 512], BF16)
                        ftmp_ = P.ring(sF1, "ftmp", [128, 256], F32, 4)
                        for q in range(4):
                            P.dma(sp, X1[:, q * 16:(q + 1) * 16, :],
                                  FAB[CTX:, :].rearrange("(a b) c -> a b c", b=64)[:, q * 16:(q + 1) * 16, :])
                        for tp in range(32):
                            zr = X1[:, 2 * tp:2 * tp + 2, 0:256]
                            zi = X1[:, 2 * tp:2 * tp + 2, 256:512]
                            br = banks.next()
                            bi = banks.next()
                            P.mm(br[:, :], c1[:], zr, start=True, stop=False)
                            P.mm(br[:, :], s1t[:], zi, start=False, stop=True)
                            P.mm(bi[:, :], c1[:], zi, start=True, stop=False)
                            P.mm(bi[:, :], ns1[:], zr, start=False, stop=True)
                            for u in range(2):
                                t2_ = 2 * tp + u
                                yr = br[:, u * 256:(u + 1) * 256]
                                yi = bi[:, u * 256:(u + 1) * 256]
                                ta = ftmp_.next()
                                tb = ftmp_.next()
                                P.I(dve, "tensor_scalar", out=ta[:], in0=yi, scalar1=tws[:, t2_:t2_ + 1], scalar2=None, op0=ALU.mult)
                                P.I(dve, "scalar_tensor_tensor", out=YT[:, t2_, 0:256], in0=yr, scalar=twc[:, t2_:t2_ + 1],
                                    in1=ta[:], op0=ALU.mult, op1=ALU.add)
                                P.I(dve, "tensor_scalar", out=tb[:], in0=yr, scalar1=tws[:, t2_:t2_ + 1], scalar2=None, op0=ALU.mult)
                                P.I(dve, "scalar_tensor_tensor", out=YT[:, t2_, 256:512], in0=yi, scalar=twc[:, t2_:t2_ + 1],
                                    in1=tb[:], op0=ALU.mult, op1=ALU.subtract)
                        for q in range(4):
                            P.dma(sp, FY[:, q * 16:(q + 1) * 16, :], YT[:, q * 16:(q + 1) * 16, :])
                        P.barrier()
                    with ExitStack() as sF2:
                        y2_ = P.ring(sF2, "y2", [64, 8, 512], BF16, 3)
                        yaT = P.sb(sF2, "yaT", [128, 2, SEQ], BF16)
                        FYv = FY.rearrange("k t c -> t k c")
                        for kb in range(16):
                            y2 = y2_.next()
                            P.dma(sp, y2[:], FYv[:, kb * 8:(kb + 1) * 8, :])
                            for jc in range(2):
                                bk = banks.next()
                                for kl in range(8):
                                    P.mm(bk[:, kl * 64:(kl + 1) * 64], y2[:, kl, jc * 128:(jc + 1) * 128], c2[:], start=True, stop=False)
                                    P.mm(bk[:, kl * 64:(kl + 1) * 64], y2[:, kl, 256 + jc * 128:256 + (jc + 1) * 128], s2t[:],
                                         start=False, stop=True)
                                ov = yaT[:, jc, :].map(lambda a: a.rearrange("p (k2 k1) -> p k1 k2", k1=128)[:, kb * 8:(kb + 1) * 8, :])
                                iv = bk[:, :].map(lambda a: a.rearrange("p (kl k2) -> p kl k2", k2=64))
                                P.I(act if jc == 0 else dve, "activation" if jc == 0 else "tensor_copy", out=ov, in_=iv,
                                    **({"func": AF.Copy} if jc == 0 else {}))
                        P.dma(sp, MIXT[0:256, CTX:].rearrange("(c p) t -> p c t", p=128), yaT[:])
                        if with_ctx:
                            zc = P.sb(sF2, "zc", [128, 2, 512], BF16)
                            yc2 = P.sb(sF2, "yc2", [128, 2, 256], BF16)
                            P.dma(sp, zc[:], FAB[0:CTX, :].rearrange("(a p) c -> p a c", p=128))
                            for jc in range(2):
                                bk = banks.next()
                                for a_ in range(2):
                                    P.mm(bk[:, 0:256], zc[:, a_, jc * 128:(jc + 1) * 128], ccs[:, 0, a_, :], start=(a_ == 0), stop=False)
                                    P.mm(bk[:, 0:256], zc[:, a_, 256 + jc * 128:256 + (jc + 1) * 128], ccs[:, 1, a_, :],
                                         start=False, stop=(a_ == 1))
                                P.I(act, "activation", out=yc2[:, jc, :], in_=bk[:, 0:256], func=AF.Copy)
                            P.dma(sp, MIXT[0:256, 0:CTX].rearrange("(c p) t -> p c t", p=128), yc2[:])
                        P.barrier()
                if stages == "F":
                    return nc, cn, scr

                last = (l == nlayers - 1)
                tiles_e = [i for i in range(NT) if (i >= 2 or with_ctx)]
                sets = ([(1, [0, 1], CAPC, XEc, YEc, 0)] if with_ctx else []) + [(0, list(range(2, NT)), CAPX, XEx, YEx, 16)]
                with ExitStack() as sE:
                    AFF = P.sb(sE, "AFF", [128, NT, NE], F32)
                    GM = P.sb(sE, "GM", [128, NT, NE], F32)
                    SLOT = P.sb(sE, "SLOT", [128, NT, NE], I32)
                    Gb = P.sb(sE, "Gb", [128, 4, D], F32)
                    for v in range(2):
                        P.dma(sp, Gb[:, v, :], MOD[l, v:v + 1, 2 * D:3 * D].partition_broadcast(128))
                        P.dma(sp, Gb[:, 2 + v, :], MOD[l, v:v + 1, 5 * D:6 * D].partition_broadcast(128))
                    if not with_ctx:
                        P.I(pool, "memset", AFF[:, 0:2, :], 0.0, _writes=[AFF.res])
                    with ExitStack() as sE1:
                        Wout = P.sb(sE1, "Wout", [128, 8, D], BF16)
                        for k in range(8):
                            P.dma(pool, Wout[:, k, :], inp["w_out"][l, k * 128:(k + 1) * 128, :])
                        RW = P.sb(sE1, "RW", [128, 8, NE], BF16)
                        P.dma(pool, RW[:], inp["router_w"][l].rearrange("(k p) e -> p k e", p=128))
                        mT_ = P.ring(sE1, "mT", [128, 8, 128], BF16, 2)
                        xe_ = P.ring(sE1, "xe", [128, D], F32, 2)
                        tm_ = P.ring(sE1, "tm", [128, D], F32, 2)
                        st2_ = P.ring(sE1, "st2", [128, 8], F32, 3)
                        xn2_ = P.ring(sE1, "xn2", [128, D], BF16, 2)
                        h2_ = P.ring(sE1, "h2", [128, 8, 128], BF16, 2)
                        lg_ = P.ring(sE1, "lg", [128, NE], F32, 2)
                        jk2 = P.sb(sE1, "jk2", [128, D], BF16)
                        for i in tiles_e:
                            v = 0 if i >= 2 else 1
                            tok0 = i * 128
                            mT = mT_.next()
                            P.dma(sp, mT[:], MIXT[:, tok0:tok0 + 128].rearrange("(k p) t -> p k t", p=128))
                            xt = xe_.next()
                            P.dma(sp, xt[:], XR[tok0:tok0 + 128, :])
                            ob = [banks.next(), banks.next()]
                            for n in range(2):
                                for k in range(8):
                                    P.mm(ob[n][:, :], mT[:, k, :], Wout[:, k, n * 512:(n + 1) * 512], start=(k == 0), stop=(k == 7))
                            tm = tm_.next()
                            for n in range(2):
                                P.I(dve, "tensor_tensor", out=tm[:, n * 512:(n + 1) * 512], in0=ob[n][:, :],
                                    in1=Gb[:, v, n * 512:(n + 1) * 512], op=ALU.mult)
                            P.I(pool, "tensor_tensor", out=xt[:], in0=xt[:], in1=tm[:], op=ALU.add)
                            P.dma(sp, XR[tok0:tok0 + 128, :], xt[:])
                            st = st2_.next()
                            P.I(act, "activation", out=jk2[:], in_=xt[:], func=AF.Square, accum_out=st[:, 0:1])
                            P.I(act, "activation", out=st[:, 1:2], in_=st[:, 0:1], func=AF.Sqrt, scale=1.0 / D, bias=epsc[:])
                            P.I(dve, "reciprocal", out=st[:, 2:3], in_=st[:, 1:2])
                            xn2 = xn2_.next()
                            P.I(dve, "tensor_scalar", out=xn2[:], in0=xt[:], scalar1=st[:, 2:3], scalar2=None, op0=ALU.mult)
                            P.dma(sp, XN2[tok0:tok0 + 128, :], xn2[:])
                            pT = bank_bf((8, 128))
                            for k in range(8):
                                P.tr(pT.map(lambda a: a[:, k, :]), xn2[:, k * 128:(k + 1) * 128], identb[:])
                            h2 = h2_.next()
                            bc = lambda vv: vv.map(lambda a: a.unsqueeze(2).to_broadcast([128, 8, 128]))
                            P.I(dve, "tensor_tensor", out=h2[:], in0=pT, in1=bc(Amod[:, 2 + v, :]), op=ALU.mult)
                            P.I(pool, "tensor_tensor", out=h2[:], in0=h2[:], in1=bc(colv[:, v * 6 + 3, :]), op=ALU.add)
                            lb = banks.next()
                            for k in range(8):
                                P.mm(lb[:, 0:NE], h2[:, k, :], RW[:, k, :], start=(k == 0), stop=(k == 7))
                            lg = lg_.next()
                            P.I(dve, "tensor_reduce", out=st[:, 3:4], in_=lb[:, 0:NE], axis=AX.X, op=ALU.max)
                            P.I(dve, "tensor_scalar", out=st[:, 4:5], in0=st[:, 3:4], scalar1=-1.0, scalar2=None, op0=ALU.mult)
                            P.I(act, "activation", out=lg[:], in_=lb[:, 0:NE], func=AF.Exp, bias=st[:, 4:5], accum_out=st[:, 5:6])
                            P.I(dve, "reciprocal", out=st[:, 6:7], in_=st[:, 5:6])
                            P.I(dve, "tensor_scalar", out=AFF[:, i, :], in0=lg[:], scalar1=st[:, 6:7], scalar2=None, op0=ALU.mult)
                        P.barrier()
                    if stages == "E":
                        dbg_a = nc.dram_tensor("dbg_AFF", [128, NT, NE], F32, kind="ExternalOutput").ap()
                        P.dma(sp, dbg_a[:, :, :], AFF[:])
                        P.barrier()
                        return nc, cn, scr

                    with ExitStack() as sD2:
                        lo = P.sb(sD2, "lo", [128, 32], F32)
                        hi = P.sb(sD2, "hi", [128, 32], F32)
                        mid = P.sb(sD2, "mid", [128, 32], F32)
                        capv = P.sb(sD2, "capv", [128, 32], F32)
                        onesb = P.sb(sD2, "onesb", [128, 128], BF16)
                        utri = P.sb(sD2, "utri", [128, 128], BF16)
                        P.dma(sp, capv[:], inp["capv"][:, :])
                        P.dma(sp, onesb[:], inp["onesb"][:, :])
                        P.dma(sp, utri[:], inp["utri"][:, :])
                        P.I(dve, "memset", lo[:], 0.0, _writes=[lo.res])
                        P.I(dve, "memset", hi[:], 1.0, _writes=[hi.res])
                        cmp_ = P.sb(sD2, "cmp", [128, NT, NE], BF16)
                        pc = P.sb(sD2, "pc", [128, 32], BF16)
                        pcf = P.sb(sD2, "pcf", [128, 32], F32)
                        P.I(dve, "memset", pcf[:], 0.0, _writes=[pcf.res])
                        mge = P.sb(sD2, "mge", [128, 32], U32)
                        mlt = P.sb(sD2, "mlt", [128, 32], U32)
                        P.I(dve, "memset", pc[:], 0.0, _writes=[pc.res])
                        for it in range(34):
                            P.I(dve, "tensor_tensor", out=mid[:], in0=lo[:], in1=hi[:], op=ALU.add)
                            P.I(dve, "tensor_scalar", out=mid[:], in0=mid[:], scalar1=0.5, scalar2=None, op0=ALU.mult)
                            for (v, tl, cap, XE, YE, co) in sets:
                                nt_ = len(tl)
                                t0_ = tl[0]
                                P.I(dve, "tensor_tensor", out=cmp_[:, t0_:t0_ + nt_, :], in0=AFF[:, t0_:t0_ + nt_, :],
                                    in1=mid[:, co:co + 16].map(lambda a: a.unsqueeze(1).to_broadcast([128, nt_, NE])), op=ALU.is_gt)
                                P.I(dve, "tensor_reduce", out=pcf[:, co:co + 16],
                                    in_=cmp_[:, t0_:t0_ + nt_, :].map(lambda a: a.rearrange("p j e -> p e j")), axis=AX.X, op=ALU.add)
                            P.I(dve, "tensor_copy", out=pc[:], in_=pcf[:])
                            tb = banks.next()
                            P.mm(tb[:, 0:32], onesb[:], pc[:])
                            P.I(dve, "tensor_tensor", out=mge[:], in0=tb[:, 0:32], in1=capv[:], op=ALU.is_ge)
                            P.I(dve, "tensor_tensor", out=mlt[:], in0=tb[:, 0:32], in1=capv[:], op=ALU.is_lt)
                            P.I(dve, "copy_predicated", out=lo[:], mask=mge[:], data=mid[:])
                            P.I(dve, "copy_predicated", out=hi[:], mask=mlt[:], data=mid[:])
                        offb = P.sb(sD2, "offb", [128, NE], F32)
                        mk_ = P.ring(sD2, "mk", [128, NE], F32, 2)
                        mkb_ = P.ring(sD2, "mkb", [128, NE], BF16, 2)
                        sl_ = P.ring(sD2, "sl", [128, NE], F32, 2)
                        xs_ = P.ring(sD2, "xs", [128, D], BF16, 3)
                        BIG = 1.0e6
                        for (v, tl, cap, XE, YE, co) in sets:
                            P.I(dve, "memset", offb[:], 0.0, _writes=[offb.res])
                            for i in tl:
                                mk = mk_.next()
                                P.I(dve, "tensor_tensor", out=mk[:], in0=AFF[:, i, :], in1=lo[:, co:co + 16], op=ALU.is_gt)
                                mkb = mkb_.next()
                                P.I(dve, "tensor_copy", out=mkb[:], in_=mk[:])
                                P.I(dve, "tensor_tensor", out=GM[:, i, :], in0=AFF[:, i, :], in1=mk[:], op=ALU.mult)
                                rb = banks.next()
                                P.mm(rb[:, 0:NE], utri[:], mkb[:])
                                P.mm(rb[:, NE:2 * NE], onesb[:], mkb[:])
                                sl = sl_.next()
                                P.I(dve, "tensor_tensor", out=sl[:], in0=rb[:, 0:NE], in1=offb[:], op=ALU.add)
                                P.I(dve, "tensor_tensor", out=offb[:], in0=rb[:, NE:2 * NE], in1=offb[:], op=ALU.add)
                                P.I(dve, "tensor_scalar", out=sl[:], in0=sl[:], scalar1=-BIG, scalar2=None, op0=ALU.add)
                                P.I(dve, "tensor_tensor", out=sl[:], in0=sl[:], in1=mk[:], op=ALU.mult)
                                P.I(dve, "tensor_scalar", out=SLOT[:, i, :], in0=sl[:], scalar1=BIG, scalar2=None, op0=ALU.add)
                                xs = xs_.next()
                                P.dma(sp, xs[:], XN2[i * 128:(i + 1) * 128, :])
                                for e in range(NE):
                                    P.dma(pool, XE[e][:, :], xs[:], meth="indirect_dma_start",
                                          out_offset=bass.IndirectOffsetOnAxis(ap=SLOT[:, i, e:e + 1].ap, axis=0),
                                          in_offset=None, bounds_check=bcreg[cap], oob_is_err=False, _reads=[SLOT.res])
                        P.barrier()
                    if stages == "D3":
                        dbg_s = nc.dram_tensor("dbg_SLOT", [128, NT, NE], I32, kind="ExternalOutput").ap()
                        dbg_g = nc.dram_tensor("dbg_GM", [128, NT, NE], F32, kind="ExternalOutput").ap()
                        P.dma(sp, dbg_s[:, :, :], SLOT[:])
                        P.dma(sp, dbg_g[:, :, :], GM[:])
                        P.barrier()
                        return nc, cn, scr

                    with ExitStack() as sD4:
                        Wg_ = P.ring(sD4, "Wg", [128, 8, D], BF16, 2)
                        Wu_ = P.ring(sD4, "Wu", [128, 8, D], BF16, 2)
                        Wd_ = P.ring(sD4, "Wd", [128, 8, D], BF16, 2)
                        xer_ = P.ring(sD4, "xer", [128, 4, D], BF16, 2)
                        xeT_ = P.ring(sD4, "xeT", [128, 8, 512], BF16, 2)
                        hid_ = P.ring(sD4, "hid", [128, 8, 512], BF16, 2)
                        sg_ = P.ring(sD4, "sg", [128, 512], F32, 3)
                        ye_ = P.ring(sD4, "ye", [128, D], BF16, 3)
                        ne_w = 1 if lite else NE
                        for e in range(NE):
                            ew = e % ne_w
                            Wg = Wg_.next(); Wu = Wu_.next(); Wd = Wd_.next()
                            for k in range(8):
                                P.dma(pool, Wg[:, k, :], inp["e_w_gate"][l, ew, k * 128:(k + 1) * 128, :])
                                P.dma(pool, Wu[:, k, :], inp["e_w_up"][l, ew, k * 128:(k + 1) * 128, :])
                                P.dma(pool, Wd[:, k, :], inp["e_w_down"][l, ew, k * 128:(k + 1) * 128, :])
                            for (v, tl, cap, XE, YE, co) in sets:
                                for ch0 in range(0, cap, 512):
                                    ns = min(512, cap - ch0)
                                    nsub = (ns + 127) // 128
                                    pr = min(128, ns)
                                    xer = xer_.next()
                                    P.dma(sp, xer[0:pr, 0:nsub, :], XE[e][ch0:ch0 + ns, :].rearrange("(a p) d -> p a d", p=pr))
                                    xeT = xeT_.next()
                                    for a_ in range(nsub):
                                        pT = bank_bf((8, 128))
                                        for k in range(8):
                                            P.tr(pT.map(lambda a: a[:, k, 0:pr]), xer[0:pr, a_, k * 128:(k + 1) * 128], identb[0:pr, 0:pr])
                                        bcx = lambda vv: vv.map(lambda a: a.unsqueeze(2).to_broadcast([128, 8, pr]))
                                        P.I(dve, "tensor_tensor", out=xeT[:, :, a_ * 128:a_ * 128 + pr], in0=pT.map(lambda a: a[:, :, 0:pr]),
                                            in1=bcx(Amod[:, 2 + v, :]), op=ALU.mult)
                                        P.I(pool, "tensor_tensor", out=xeT[:, :, a_ * 128:a_ * 128 + pr], in0=xeT[:, :, a_ * 128:a_ * 128 + pr],
                                            in1=bcx(colv[:, v * 6 + 3, :]), op=ALU.add)
                                    hid = hid_.next()
                                    for f in range(8):
                                        bg = banks.next()
                                        bu = banks.next()
                                        for k in range(8):
                                            P.mm(bg[:, 0:ns], Wg[:, k, f * 128:(f + 1) * 128], xeT[:, k, 0:ns], start=(k == 0), stop=(k == 7))
                                        for k in range(8):
                                            P.mm(bu[:, 0:ns], Wu[:, k, f * 128:(f + 1) * 128], xeT[:, k, 0:ns], start=(k == 0), stop=(k == 7))
                                        sg = sg_.next()
                                        P.I(act, "activation", out=sg[:, 0:ns], in_=bg[:, 0:ns], func=AF.Silu)
                                        P.I(dve, "tensor_tensor", out=hid[:, f, 0:ns], in0=sg[:, 0:ns], in1=bu[:, 0:ns], op=ALU.mult)
                                    for a_ in range(nsub):
                                        ye = ye_.next()
                                        for n in range(2):
                                            bd = banks.next()
                                            for f in range(8):
                                                P.mm(bd[0:pr, :], hid[:, f, a_ * 128:a_ * 128 + pr], Wd[:, f, n * 512:(n + 1) * 512],
                                                     start=(f == 0), stop=(f == 7))
                                            if n == 0:
                                                P.I(act, "activation", out=ye[0:pr, 0:512], in_=bd[0:pr, :], func=AF.Copy)
                                            else:
                                                P.I(dve, "tensor_copy", out=ye[0:pr, 512:1024], in_=bd[0:pr, :])
                                        P.dma(sp, YE[e][ch0 + a_ * 128:ch0 + a_ * 128 + pr, :], ye[0:pr, :])
                        P.barrier()
                    with ExitStack() as sD5:
                        gt_ = P.ring(sD5, "gt", [128, D], BF16, 6)
                        for t_ in gt_.tiles:
                            P.I(pool, "memset", t_[:], 0.0, _writes=[t_.res])
                        acc_ = P.ring(sD5, "acc", [128, D], F32, 2)
                        xf_ = P.ring(sD5, "xf", [128, D], F32, 2)
                        st5_ = P.ring(sD5, "st5", [128, 4], F32, 2)
                        jk5 = P.sb(sD5, "jk5", [128, D], BF16)
                        fgb = P.sb(sD5, "fgb", [128, D], F32)
                        P.dma(sp, fgb[:], inp["final_g"].unsqueeze(0).partition_broadcast(128))
                        for (v, tl, cap, XE, YE, co) in sets:
                            for i in tl:
                                acc = acc_.next()
                                P.I(pool, "memset", acc[:], 0.0, _writes=[acc.res])
                                for e in range(NE):
                                    gt = gt_.next()
                                    P.dma(pool, gt[:], YE[e][:, :], meth="indirect_dma_start", out_offset=None,
                                          in_offset=bass.IndirectOffsetOnAxis(ap=SLOT[:, i, e:e + 1].ap, axis=0),
                                          bounds_check=bcreg[cap], oob_is_err=False, _reads=[SLOT.res])
                                    P.I(dve, "scalar_tensor_tensor", out=acc[:], in0=gt[:], scalar=GM[:, i, e:e + 1], in1=acc[:],
                                        op0=ALU.mult, op1=ALU.add)
                                xf = xf_.next()
                                P.dma(sp, xf[:], XR[i * 128:(i + 1) * 128, :])
                                P.I(pool, "tensor_tensor", out=acc[:], in0=acc[:], in1=Gb[:, 2 + v, :], op=ALU.mult)
                                P.I(pool, "tensor_tensor", out=xf[:], in0=xf[:], in1=acc[:], op=ALU.add)
                                if not last:
                                    P.dma(sp, XR[i * 128:(i + 1) * 128, :], xf[:])
                                elif i >= 2:
                                    st = st5_.next()
                                    P.I(act, "activation", out=jk5[:], in_=xf[:], func=AF.Square, accum_out=st[:, 0:1])
                                    P.I(act, "activation", out=st[:, 1:2], in_=st[:, 0:1], func=AF.Sqrt, scale=1.0 / D, bias=epsc[:])
                                    P.I(dve, "reciprocal", out=st[:, 2:3], in_=st[:, 1:2])
                                    P.I(dve, "scalar_tensor_tensor", out=xf[:], in0=xf[:], scalar=st[:, 2:3], in1=fgb[:],
                                        op0=ALU.mult, op1=ALU.mult)
                                    P.dma(sp, out[(i - 2) * 128:(i - 1) * 128, :], xf[:])
                        P.barrier()
    return nc, cn, scr


_CACHE = {}


def kernel(**inputs):
    nb = inputs["x"].shape[0]
    if "nc" not in _CACHE:
        _CACHE["nc"] = build(nlayers=DEPTH, dbg=False)
    nc, cn, _ = _CACHE["nc"]
    shared = {n: np.ascontiguousarray(inputs[n], dtype=np.float32) for n, _ in WNAMES}
    shared["c_ctx"] = np.ascontiguousarray(inputs["c_ctx"], dtype=np.float32)
    for n, a in cn.items():
        shared["k_" + n] = a
    in_maps = []
    for b in range(nb):
        m = dict(shared)
        m["x"] = np.ascontiguousarray(inputs["x"][b], dtype=np.float32)
        m["c"] = np.ascontiguousarray(inputs["c"][b], dtype=np.float32)
        m["ctx"] = np.ascontiguousarray(inputs["ctx"][b], dtype=np.float32)
        in_maps.append(m)
    res = run_bass_kernel_spmd(nc, in_maps, core_ids=list(range(nb)))
    return np.stack([np.asarray(r["out"], dtype=np.float32) for r in res.results], axis=0)
```

```python
import os
import numpy as np
import ml_dtypes
from contextlib import ExitStack
import concourse.bass as bass
import concourse.mybir as mybir
from concourse.bass_utils import run_bass_kernel_spmd

F32 = mybir.dt.float32
BF16 = mybir.dt.bfloat16
I32 = mybir.dt.int32
U32 = mybir.dt.uint32
AF = mybir.ActivationFunctionType
ALU = mybir.AluOpType
AX = mybir.AxisListType

D = 1024
SEQ = 8192
CTX = 256
DEPTH = 4
NT = (SEQ + CTX) // 128
TOK = SEQ + CTX
INC = 1968
NE = 16
CAPX = 1024
CAPC = 32
EPS = 1e-6


class Res:
    __slots__ = ("name", "w", "r", "excl")

    def __init__(self, name, excl=False):
        self.name = name
        self.w = None
        self.r = []
        self.excl = excl


class V:
    __slots__ = ("ap", "res")

    def __init__(self, ap, res):
        self.ap = ap
        self.res = res

    def map(self, fn):
        return V(fn(self.ap), self.res)


class Tile:
    def __init__(self, t, name):
        self.t = t
        self.res = Res(name)

    def __getitem__(self, idx):
        return V(self.t[idx], self.res)


class Eng:
    def __init__(self, name, h, sem):
        self.name = name
        self.h = h
        self.sem = sem
        self.count = 0
        self.seen = {}


def _ap(x):
    return x.ap if isinstance(x, V) else x


class Prog:
    WRITE_KEYS = ("out", "accum_out", "out_max", "out_indices")

    def __init__(self, nc, es, n_dma_sems=56):
        self.nc = nc
        self.es = es
        mk = lambda n: es.enter_context(nc.semaphore(n))
        self.pe = Eng("pe", nc.tensor, mk("s_pe"))
        self.act = Eng("act", nc.scalar, mk("s_act"))
        self.dve = Eng("dve", nc.vector, mk("s_dve"))
        self.pool = Eng("pool", nc.gpsimd, mk("s_pool"))
        self.sp = Eng("sp", nc.sync, mk("s_sp"))
        self.engs = [self.pe, self.act, self.dve, self.pool, self.sp]
        self.dsems = [[mk("s_d%d" % i), 0] for i in range(n_dma_sems)]
        self.dnext = 0
        self.ninst = 0

    def sb(self, es, name, shape, dt):
        self.nalloc = getattr(self, "nalloc", 0) + 1
        return Tile(es.enter_context(self.nc.sbuf_tensor("t%d_%s" % (self.nalloc, name), list(shape), dt)), name)

    def ps(self, es, name, shape, dt=F32):
        self.nalloc = getattr(self, "nalloc", 0) + 1
        t = Tile(es.enter_context(self.nc.psum_tensor("p%d_%s" % (self.nalloc, name), list(shape), dt)), name)
        t.res.excl = True
        return t

    def ring(self, es, name, shape, dt, n, psum=False):
        f = self.ps if psum else self.sb
        return Ring([f(es, "%s_%d" % (name, i), shape, dt) for i in range(n)])

    def _wait(self, E, tok):
        kind, key, val = tok
        if kind == "eng":
            sem = key.sem
            k = ("e", key.name)
        else:
            sem = self.dsems[key][0]
            k = ("d", key)
        if E.seen.get(k, 0) >= val:
            return
        E.h.wait_ge(sem, val)
        E.seen[k] = val
        self.ninst += 1

    def _deps(self, E, reads, writes):
        toks = []
        for r in reads:
            if r is not None and r.w is not None:
                toks.append(r.w)
            if r is not None and r.excl:
                toks.extend(r.r)
        for w in writes:
            if w is None:
                continue
            if w.w is not None:
                toks.append(w.w)
            toks.extend(w.r)
        for t in toks:
            if t[0] == "eng" and t[1] is E:
                continue
            self._wait(E, t)
        if E is not self.pe:
            for r in reads:
                if r is not None and r.w is not None and r.w[0] == "eng" and r.w[1] is E:
                    self._wait(E, r.w)

    def _commit(self, tok, reads, writes):
        for r in reads:
            if r is not None:
                r.r.append(tok)
        for w in writes:
            if w is not None:
                w.w = tok
                w.r = []

    def I(self, E, meth, *args, **kw):
        reads, writes = [], []
        for k, v in kw.items():
            if isinstance(v, V):
                (writes if k in self.WRITE_KEYS else reads).append(v.res)
        for v in args:
            if isinstance(v, V):
                reads.append(v.res)
        extra_r = kw.pop("_reads", ())
        extra_w = kw.pop("_writes", ())
        reads.extend(extra_r)
        writes.extend(extra_w)
        self._deps(E, reads, writes)
        ins = getattr(E.h, meth)(*[_ap(a) for a in args], **{k: _ap(v) for k, v in kw.items()})
        E.count += 1
        ins.then_inc(E.sem, 1)
        self.ninst += 1
        self._commit(("eng", E, E.count), reads, writes)
        return ins

    def dma(self, Q, out, in_, meth="dma_start", **kw):
        reads = [in_.res] if isinstance(in_, V) else []
        writes = [out.res] if isinstance(out, V) else []
        for k, v in kw.items():
            if isinstance(v, V):
                reads.append(v.res)
        reads.extend(kw.pop("_reads", ()))
        writes.extend(kw.pop("_writes", ()))
        self._deps(Q, reads, writes)
        i = self.dnext
        self.dnext = (self.dnext + 1) % len(self.dsems)
        sem, val = self.dsems[i]
        if val > 0:
            self._wait(Q, ("dma", i, val))
        val += 16
        self.dsems[i][1] = val
        ins = getattr(Q.h, meth)(out=_ap(out), in_=_ap(in_), **{k: _ap(v) for k, v in kw.items()})
        ins.then_inc(sem, 16)
        self.ninst += 1
        self._commit(("dma", i, val), reads, writes)
        return ins

    def barrier(self):
        toks = [("eng", e, e.count) for e in self.engs if e.count > 0]
        toks += [("dma", i, v) for i, (s, v) in enumerate(self.dsems) if v > 0]
        for e in self.engs:
            for t in toks:
                if t[0] == "eng" and t[1] is e:
                    continue
                self._wait(e, t)

    def mm(self, out, lhsT, rhs, start=True, stop=True):
        return self.I(self.pe, "matmul", out=out, lhsT=lhsT, rhs=rhs, start=start, stop=stop)

    def tr(self, out, in_, ident):
        return self.I(self.pe, "transpose", out=out, in_=in_, identity=ident)


class Ring:
    def __init__(self, tiles):
        self.tiles = tiles
        self.i = 0

    def next(self):
        t = self.tiles[self.i]
        self.i = (self.i + 1) % len(self.tiles)
        return t


def _consts():
    c = {}
    c["identb"] = np.eye(128, dtype=np.float32).astype(ml_dtypes.bfloat16)
    c["identf"] = np.eye(128, dtype=np.float32)
    k = np.arange(64)
    ang = 2 * np.pi * np.outer(k, k) / 64.0
    C64 = np.cos(ang) / 8.0
    S64 = -np.sin(ang) / 8.0
    bdc = np.zeros((128, 128)); bds = np.zeros((128, 128))
    for g in range(2):
        bdc[g * 64:(g + 1) * 64, g * 64:(g + 1) * 64] = C64
        bds[g * 64:(g + 1) * 64, g * 64:(g + 1) * 64] = S64
    c["bdc"] = bdc.astype(np.float32).astype(ml_dtypes.bfloat16)
    c["bds"] = bds.astype(np.float32).astype(ml_dtypes.bfloat16)
    t = np.arange(SEQ)
    row = (t // 64).astype(np.float64); col = (t % 64).astype(np.float64)
    inv = 10000.0 ** (-np.arange(8) / 8.0)
    ang = np.concatenate([row[:, None] * inv, col[:, None] * inv], -1)
    bf = lambda a: a.astype(np.float32).astype(ml_dtypes.bfloat16)
    k1 = np.arange(128); a1 = 2 * np.pi * np.outer(k1, k1) / 128.0
    c["f_c1"] = bf(np.cos(a1) / np.sqrt(128.0)); c["f_s1"] = bf(np.sin(a1) / np.sqrt(128.0))
    c["f_ns1"] = bf(-np.sin(a1) / np.sqrt(128.0))
    t2 = np.arange(64); atw = 2 * np.pi * np.outer(k1, t2) / 8192.0
    c["f_twc"] = np.cos(atw).astype(np.float32); c["f_tws"] = np.sin(atw).astype(np.float32)
    a2 = 2 * np.pi * np.outer(t2, t2) / 64.0
    c["f_c2"] = bf(np.cos(a2) / 8.0); c["f_s2"] = bf(np.sin(a2) / 8.0)
    tc = np.arange(256); ac = 2 * np.pi * np.outer(tc, tc) / 256.0
    c["f_cc"] = bf(np.cos(ac) / 16.0); c["f_sc"] = bf(np.sin(ac) / 16.0)
    ii_ = np.arange(128)
    c["utri"] = bf((ii_[:, None] < ii_[None, :]).astype(np.float32))
    c["onesb"] = bf(np.ones((128, 128)))
    capv = np.zeros((128, 32), np.float32); capv[:, 0:16] = CAPC; capv[:, 16:32] = CAPX
    c["capv"] = capv
    c["jrev"] = np.eye(128, dtype=np.float32)[::-1].copy()
    ii = np.arange(128)
    c["trif"] = (ii[:, None] <= ii[None, :]).astype(np.float32)
    c["trib"] = (ii[:, None] >= ii[None, :]).astype(np.float32)
    c["rcos"] = np.cos(ang).astype(np.float32)
    c["rsin"] = np.sin(ang).astype(np.float32)
    return c


WNAMES = [("ada_w", [DEPTH, D, 6 * D]), ("ada_b", [DEPTH, 6 * D]), ("norm1_g", [DEPTH, D]),
          ("norm2_g", [DEPTH, D]), ("w_in", [DEPTH, D, INC]), ("m_conv_w", [DEPTH, 3, 512]),
          ("m_conv_b", [DEPTH, 512]), ("m_ib", [DEPTH, 2, 4]), ("m_fb", [DEPTH, 2, 4]),
          ("m_norm_g", [DEPTH, 256]), ("a_qnorm_g", [DEPTH, 384]), ("a_wq_up", [DEPTH, 384, 768]),
          ("a_kvnorm_g", [DEPTH, 256]), ("a_wkv_up", [DEPTH, 256, 1024]), ("w_out", [DEPTH, D, D]),
          ("router_w", [DEPTH, D, NE]), ("e_w_gate", [DEPTH, NE, D, D]), ("e_w_up", [DEPTH, NE, D, D]),
          ("e_w_down", [DEPTH, NE, D, D]), ("final_g", [D])]


def build(nlayers=DEPTH, dbg=False, tiles=None, stages=None, lite=False, heads=None, qgroups=None):
    nc = bass.Bass("TRN2", target_bir_lowering=False)
    tiles = list(range(NT)) if tiles is None else tiles
    inp = {}
    inp["x"] = nc.dram_tensor("x", [SEQ, D], F32, kind="ExternalInput").ap()
    inp["c"] = nc.dram_tensor("c", [D], F32, kind="ExternalInput").ap()
    inp["ctx"] = nc.dram_tensor("ctx", [CTX, D], F32, kind="ExternalInput").ap()
    inp["c_ctx"] = nc.dram_tensor("c_ctx", [D], F32, kind="ExternalInput").ap()
    for n, shp in WNAMES:
        shp = list(shp)
        if n != "final_g":
            shp[0] = nlayers
        if lite and n.startswith("e_w_"):
            shp[1] = 1
        inp[n] = nc.dram_tensor(n, shp, F32, kind="ExternalInput").ap()
    cn = _consts()
    for n, a in cn.items():
        inp[n] = nc.dram_tensor("k_" + n, list(a.shape), BF16 if a.dtype == ml_dtypes.bfloat16 else F32,
                                kind="ExternalInput").ap()
    out = nc.dram_tensor("out", [SEQ, D], F32, kind="ExternalOutput").ap()
    skind = "ExternalOutput" if dbg else "Internal"
    scr = {}

    def scratch(name, shape, dt):
        scr[name] = nc.dram_tensor(name, shape, dt, kind=skind).ap()
        return scr[name]

    XR = scratch("XR", [TOK, D], F32)
    MOD = scratch("MOD", [DEPTH, 2, 6 * D], F32)
    FAB = scratch("FAB", [TOK, 512], BF16)
    QKT = scratch("QKT", [512, TOK], F32)
    VOG = scratch("VOG", [TOK, 528], F32)
    QT = scratch("QT", [8, 96, TOK], BF16)
    KT = scratch("KT", [8, 96, TOK], BF16)
    VA = scratch("VA", [TOK, 8, 65], BF16)
    MIXT = scratch("MIXT", [D, TOK], BF16)
    HFB = scratch("HFB", [2, TOK, 256], F32)
    FY = scratch("FY", [128, 64, 512], BF16)
    XN2 = scratch("XN2", [TOK, D], BF16)
    XEx = [scratch("XEx%d" % e, [CAPX, D], BF16) for e in range(NE)]
    XEc = [scratch("XEc%d" % e, [CAPC, D], BF16) for e in range(NE)]
    YEx = [scratch("YEx%d" % e, [CAPX, D], BF16) for e in range(NE)]
    YEc = [scratch("YEc%d" % e, [CAPC, D], BF16) for e in range(NE)]

    es = ExitStack()
    with es:
        P = Prog(nc, es)
        pe, act, dve, pool, sp = P.pe, P.act, P.dve, P.pool, P.sp
        bcreg = {}
        for cap_ in (CAPC, CAPX):
            bcreg[cap_] = nc.gpsimd.alloc_register("bc%d" % cap_)
            nc.gpsimd.reg_mov(bcreg[cap_], cap_ - 1)
        identb = P.sb(es, "identb", [128, 128], BF16)
        identf = P.sb(es, "identf", [128, 128], F32)
        P.dma(sp, identb[:], inp["identb"][:, :])
        P.dma(sp, identf[:], inp["identf"][:, :])
        banks = P.ring(es, "bank", [128, 512], F32, 5, psum=True)
        accb = P.ring(es, "accb", [128, 512], F32, 3, psum=True)
        epsc = P.sb(es, "epsc", [128, 1], F32)
        onec = P.sb(es, "onec", [128, 128], F32)
        P.I(dve, "memset", onec[:], 1.0, _writes=[onec.res])
        jrev = P.sb(es, "jrev", [128, 128], F32)
        trif = P.sb(es, "trif", [128, 128], F32)
        trib = P.sb(es, "trib", [128, 128], F32)
        P.dma(sp, jrev[:], inp["jrev"][:, :])
        P.dma(sp, trif[:], inp["trif"][:, :])
        P.dma(sp, trib[:], inp["trib"][:, :])
        P.I(dve, "memset", epsc[:], EPS, _writes=[epsc.res])

        def bank_bf(shape3):
            b = banks.next()
            v = b[:].map(lambda a: a.bitcast(BF16))
            if shape3 is not None:
                v = v.map(lambda a: a[:, 0:shape3[0] * shape3[1]].rearrange("p (a b) -> p a b", b=shape3[1]))
            return v

        P.dma(sp, XR[0:CTX, :], inp["ctx"][:, :])
        for q in range(4):
            P.dma(sp, XR[CTX + q * 2048:CTX + (q + 1) * 2048, :], inp["x"][q * 2048:(q + 1) * 2048, :])

        with ExitStack() as sa:
            cc = P.sb(sa, "cc", [128, 2, 8], F32)
            cs = P.sb(sa, "cs", [128, 2, 8], F32)
            P.dma(sp, cc[:, 0, :], inp["c"].rearrange("(p k) -> p k", k=8))
            P.dma(sp, cc[:, 1, :], inp["c_ctx"].rearrange("(p k) -> p k", k=8))
            P.I(act, "activation", out=cs[:], in_=cc[:], func=AF.Silu)
            adab = P.sb(sa, "adab", [1, 6 * D], F32)
            awr = P.ring(sa, "aw", [128, 8, 512], F32, 3)
            mrow = P.ring(sa, "mrow", [1, 512], F32, 4)
            for l in range(nlayers):
                P.dma(sp, adab[:], inp["ada_b"][l:l + 1, :])
                awv = inp["ada_w"][l].rearrange("(p k) n -> p k n", k=8)
                for j in range(12):
                    aw = awr.next()
                    P.dma(sp if j % 2 == 0 else pool, aw[:], awv[:, :, j * 512:(j + 1) * 512])
                    for v in range(2):
                        b = banks.next()
                        for k in range(8):
                            P.mm(b[0:1, :], cs[:, v, k:k + 1], aw[:, k, :], start=(k == 0), stop=(k == 7))
                        mr = mrow.next()
                        P.I(dve, "tensor_tensor", out=mr[:], in0=b[0:1, :], in1=adab[:, j * 512:(j + 1) * 512],
                            op=ALU.add)
                        P.dma(sp, MOD[l, v:v + 1, j * 512:(j + 1) * 512], mr[:])
            P.barrier()
        if stages == "A":
            P.barrier()
            return nc, cn, scr

        for l in range(nlayers):
            with ExitStack() as sl:
                colv = P.sb(sl, "colv", [128, 12, 8], F32)
                for v in range(2):
                    P.dma(sp, colv[:, v * 6:(v + 1) * 6, :],
                          MOD[l, v, :].rearrange("(s k p) -> p s k", p=128, k=8), allow_slow_non_contiguous=True)
                n1g = P.sb(sl, "n1g", [128, 8], F32)
                n2g = P.sb(sl, "n2g", [128, 8], F32)
                P.dma(sp, n1g[:], inp["norm1_g"][l].rearrange("(k p) -> p k", p=128), allow_slow_non_contiguous=True)
                P.dma(sp, n2g[:], inp["norm2_g"][l].rearrange("(k p) -> p k", p=128), allow_slow_non_contiguous=True)
                Amod = P.sb(sl, "Amod", [128, 4, 8], F32)
                for v in range(2):
                    P.I(dve, "scalar_tensor_tensor", out=Amod[:, v, :], in0=colv[:, v * 6 + 1, :], scalar=1.0,
                        in1=n1g[:], op0=ALU.add, op1=ALU.mult)
                    P.I(dve, "scalar_tensor_tensor", out=Amod[:, 2 + v, :], in0=colv[:, v * 6 + 4, :], scalar=1.0,
                        in1=n2g[:], op0=ALU.add, op1=ALU.mult)
                qg = P.sb(sl, "qg", [128, 3], F32)
                kvg = P.sb(sl, "kvg", [128, 2], F32)
                P.dma(sp, qg[:], inp["a_qnorm_g"][l].rearrange("(k p) -> p k", p=128), allow_slow_non_contiguous=True)
                P.dma(sp, kvg[:], inp["a_kvnorm_g"][l].rearrange("(k p) -> p k", p=128), allow_slow_non_contiguous=True)

                with ExitStack() as sB:
                    Win = P.sb(sB, "Win", [128, 8, INC], BF16)
                    for k in range(8):
                        P.dma(pool, Win[:, k, :], inp["w_in"][l, k * 128:(k + 1) * 128, :])
                    Wq = P.sb(sB, "Wq", [128, 3, 768], BF16)
                    P.dma(pool, Wq[:], inp["a_wq_up"][l].rearrange("(k p) n -> p k n", p=128))
                    Wkv = P.sb(sB, "Wkv", [128, 2, 1024], BF16)
                    P.dma(pool, Wkv[:], inp["a_wkv_up"][l].rearrange("(k p) n -> p k n", p=128))
                    bdc = P.sb(sB, "bdc", [128, 128], BF16)
                    bds = P.sb(sB, "bds", [128, 128], BF16)
                    P.dma(sp, bdc[:], inp["bdc"][:, :])
                    P.dma(sp, bds[:], inp["bds"][:, :])
                    xr_ = P.ring(sB, "xt", [128, D], F32, 2)
                    junk = P.sb(sB, "junk", [128, D], BF16)
                    st_ = P.ring(sB, "st", [128, 8], F32, 3)
                    xn_ = P.ring(sB, "xn", [128, D], BF16, 2)
                    hT_ = P.ring(sB, "hT", [128, 8, 128], BF16, 2)
                    ub_ = P.ring(sB, "ub", [128, 1040], F32, 2)
                    fb_ = P.ring(sB, "fb", [128, 256], BF16, 2)
                    fT_ = P.ring(sB, "fT", [128, 2, 128], BF16, 2)
                    ab_ = P.ring(sB, "ab", [128, 512], BF16, 2)
                    qkT_ = P.ring(sB, "qkT", [128, 4, 128], F32, 2)
                    cqn_ = P.ring(sB, "cqn", [128, 640], BF16, 2)
                    cT_ = P.ring(sB, "cT", [128, 5, 128], BF16, 2)
                    kr_ = P.ring(sB, "kr", [128, 32], F32, 2)
                    qs_ = P.ring(sB, "qs", [128, 8, 96], F32, 2)
                    rt_ = P.ring(sB, "rt", [128, 4, 8, 16], F32, 2)
                    cs_ = P.ring(sB, "cs", [128, 2, 16], F32, 2)
                    qb_ = P.ring(sB, "qb", [128, 8, 96], BF16, 2)
                    kb_ = P.ring(sB, "kb", [128, 8, 96], BF16, 2)
                    va_ = P.ring(sB, "va", [128, 8, 65], BF16, 2)
                    for t_ in va_.tiles:
                        P.I(dve, "memset", t_[:], 1.0, _writes=[t_.res])
                    qT_ = P.ring(sB, "qT", [96, 8, 128], BF16, 2)
                    kT_ = P.ring(sB, "kT", [96, 8, 128], BF16, 2)

                    for i in tiles:
                        isx = i >= 2
                        v = 0 if isx else 1
                        tok0 = i * 128
                        xt = xr_.next()
                        P.dma(sp, xt[:], XR[tok0:tok0 + 128, :])
                        st = st_.next()
                        P.I(act, "activation", out=junk[:], in_=xt[:], func=AF.Square, accum_out=st[:, 0:1])
                        P.I(act, "activation", out=st[:, 1:2], in_=st[:, 0:1], func=AF.Sqrt, scale=1.0 / D, bias=epsc[:])
                        P.I(dve, "reciprocal", out=st[:, 2:3], in_=st[:, 1:2])
                        xn = xn_.next()
                        P.I(dve, "tensor_scalar", out=xn[:], in0=xt[:], scalar1=st[:, 2:3], scalar2=None, op0=ALU.mult)
                        pT = bank_bf((8, 128))
                        for k in range(8):
                            P.tr(pT.map(lambda a: a[:, k, :]), xn[:, k * 128:(k + 1) * 128], identb[:])
                        hT = hT_.next()
                        bc = lambda vv: vv.map(lambda a: a.unsqueeze(2).to_broadcast([128, 8, 128]))
                        P.I(dve, "tensor_tensor", out=hT[:], in0=pT, in1=bc(Amod[:, v, :]), op=ALU.mult)
                        P.I(pool, "tensor_tensor", out=hT[:], in0=hT[:], in1=bc(colv[:, v * 6 + 0, :]), op=ALU.add)
                        ub = [banks.next() for _ in range(4)]
                        for n in range(4):
                            lo, hi = n * 512, min(INC, (n + 1) * 512)
                            for k in range(8):
                                P.mm(ub[n][:, 0:hi - lo], hT[:, k, :], Win[:, k, lo:hi], start=(k == 0), stop=(k == 7))
                        fb = fb_.next()
                        P.I(act, "activation", out=fb[:], in_=ub[0][:, 0:256], func=AF.Copy)
                        ubuf = ub_.next()
                        P.I(act, "activation", out=ubuf[:, 0:256], in_=ub[0][:, 256:512], func=AF.Copy)
                        P.I(dve, "tensor_copy", out=ubuf[:, 256:768], in_=ub[1][:, :])
                        P.I(act, "activation", out=ubuf[:, 768:1040], in_=ub[2][:, 0:272], func=AF.Copy)
                        P.I(act, "activation", out=junk[:, 0:240], in_=ub[2][:, 272:512], func=AF.Square,
                            accum_out=st[:, 3:4])
                        P.I(act, "activation", out=junk[:, 240:384], in_=ub[3][:, 0:144], func=AF.Square,
                            accum_out=st[:, 4:5])
                        P.I(act, "activation", out=junk[:, 384:640], in_=ub[3][:, 144:400], func=AF.Square,
                            accum_out=st[:, 5:6])
                        P.I(dve, "tensor_tensor", out=st[:, 3:4], in0=st[:, 3:4], in1=st[:, 4:5], op=ALU.add)
                        P.I(act, "activation", out=st[:, 4:5], in_=st[:, 3:4], func=AF.Sqrt, scale=1.0 / 384, bias=epsc[:])
                        P.I(dve, "reciprocal", out=st[:, 6:7], in_=st[:, 4:5])
                        P.I(act, "activation", out=st[:, 3:4], in_=st[:, 5:6], func=AF.Sqrt, scale=1.0 / 256, bias=epsc[:])
                        P.I(dve, "reciprocal", out=st[:, 7:8], in_=st[:, 3:4])
                        cqn = cqn_.next()
                        P.I(dve, "tensor_scalar", out=cqn[:, 0:240], in0=ub[2][:, 272:512], scalar1=st[:, 6:7],
                            scalar2=None, op0=ALU.mult)
                        P.I(dve, "tensor_scalar", out=cqn[:, 240:384], in0=ub[3][:, 0:144], scalar1=st[:, 6:7],
                            scalar2=None, op0=ALU.mult)
                        P.I(dve, "tensor_scalar", out=cqn[:, 384:640], in0=ub[3][:, 144:400], scalar1=st[:, 7:8],
                            scalar2=None, op0=ALU.mult)
                        kr = kr_.next()
                        P.I(act, "activation", out=kr[:], in_=ub[3][:, 400:432], func=AF.Copy)
                        P.dma(sp, VOG[tok0:tok0 + 128, :], ubuf[:, 512:1040])
                        pf = bank_bf((2, 128))
                        for cc in range(2):
                            P.tr(pf.map(lambda a: a[:, cc, :]), fb[:, cc * 128:(cc + 1) * 128], identb[:])
                        fT = fT_.next()
                        P.I(act, "activation", out=fT[:], in_=pf, func=AF.Copy)
                        abp = banks.next()
                        for cc in range(2):
                            P.mm(abp[:, cc * 128:(cc + 1) * 128], fT[:, cc, :], bdc[:])
                            P.mm(abp[:, 256 + cc * 128:256 + (cc + 1) * 128], fT[:, cc, :], bds[:])
                        ab = ab_.next()
                        P.I(act, "activation", out=ab[:], in_=abp[:], func=AF.Copy)
                        P.dma(sp, FAB[tok0:tok0 + 128, :], ab[:])
                        pq = banks.next()
                        for cc in range(4):
                            P.tr(pq[:, cc * 128:(cc + 1) * 128], ubuf[:, cc * 128:(cc + 1) * 128], identf[:])
                        qkT = qkT_.next()
                        P.I(dve, "tensor_copy", out=qkT[:], in_=pq[:].map(lambda a: a.rearrange("p (c t) -> p c t", t=128)))
                        P.dma(sp, QKT.rearrange("(c p) t -> p c t", p=128)[:, :, tok0:tok0 + 128], qkT[:])
                        pc = bank_bf((5, 128))
                        for cc in range(5):
                            P.tr(pc.map(lambda a: a[:, cc, :]), cqn[:, cc * 128:(cc + 1) * 128], identb[:])
                        cT = cT_.next()
                        P.I(dve, "tensor_tensor", out=cT[:, 0:3, :], in0=pc.map(lambda a: a[:, 0:3, :]),
                            in1=qg[:].map(lambda a: a.unsqueeze(2).to_broadcast([128, 3, 128])), op=ALU.mult)
                        P.I(dve, "tensor_tensor", out=cT[:, 3:5, :], in0=pc.map(lambda a: a[:, 3:5, :]),
                            in1=kvg[:].map(lambda a: a.unsqueeze(2).to_broadcast([128, 2, 128])), op=ALU.mult)
                        qp = [banks.next(), banks.next()]
                        for n, (lo, hi) in enumerate([(0, 512), (512, 768)]):
                            for kk in range(3):
                                P.mm(qp[n][:, 0:hi - lo], cT[:, kk, :], Wq[:, kk, lo:hi], start=(kk == 0), stop=(kk == 2))
                        kvp = [banks.next(), banks.next()]
                        for n in range(2):
                            for kk in range(2):
                                P.mm(kvp[n][:, :], cT[:, 3 + kk, :], Wkv[:, kk, n * 512:(n + 1) * 512],
                                     start=(kk == 0), stop=(kk == 1))
                        qs = qs_.next()
                        sc = 96.0 ** -0.5
                        qsf = qs[:].map(lambda a: a.rearrange("p h e -> p (h e)"))
                        P.I(act, "activation", out=qsf.map(lambda a: a[:, 0:512]), in_=qp[0][:, :], func=AF.Copy, scale=sc)
                        P.I(act, "activation", out=qsf.map(lambda a: a[:, 512:768]), in_=qp[1][:, 0:256], func=AF.Copy, scale=sc)
                        qb = qb_.next()
                        kb = kb_.next()
                        va = va_.next()
                        P.I(act, "activation", out=qb[:, :, 0:64], in_=qs[:, :, 0:64], func=AF.Copy)
                        if isx:
                            cst = cs_.next()
                            P.dma(sp, cst[:, 0, :], inp["rcos"][tok0 - CTX:tok0 - CTX + 128, :])
                            P.dma(sp, cst[:, 1, :], inp["rsin"][tok0 - CTX:tok0 - CTX + 128, :])
                            rt = rt_.next()
                            cb = cst[:, 0, :].map(lambda a: a.unsqueeze(1).to_broadcast([128, 8, 16]))
                            sbn = cst[:, 1, :].map(lambda a: a.unsqueeze(1).to_broadcast([128, 8, 16]))
                            P.I(dve, "tensor_tensor", out=rt[:, 0], in0=qs[:, :, 64:80], in1=cb, op=ALU.mult)
                            P.I(dve, "tensor_tensor", out=rt[:, 1], in0=qs[:, :, 80:96], in1=sbn, op=ALU.mult)
                            P.I(pool, "tensor_tensor", out=rt[:, 2], in0=qs[:, :, 64:80], in1=sbn, op=ALU.mult)
                            P.I(pool, "tensor_tensor", out=rt[:, 3], in0=qs[:, :, 80:96], in1=cb, op=ALU.mult)
                            P.I(dve, "tensor_tensor", out=qb[:, :, 64:80], in0=rt[:, 0], in1=rt[:, 1], op=ALU.subtract)
                            P.I(dve, "tensor_tensor", out=qb[:, :, 80:96], in0=rt[:, 2], in1=rt[:, 3], op=ALU.add)
                            P.I(dve, "tensor_tensor", out=rt[:, 0, 0, :], in0=kr[:, 0:16], in1=cst[:, 0, :], op=ALU.mult)
                            P.I(dve, "tensor_tensor", out=rt[:, 1, 0, :], in0=kr[:, 16:32], in1=cst[:, 1, :], op=ALU.mult)
                            P.I(dve, "tensor_tensor", out=rt[:, 2, 0, :], in0=kr[:, 0:16], in1=cst[:, 1, :], op=ALU.mult)
                            P.I(dve, "tensor_tensor", out=rt[:, 3, 0, :], in0=kr[:, 16:32], in1=cst[:, 0, :], op=ALU.mult)
                            P.I(dve, "tensor_tensor", out=kr[:, 0:16], in0=rt[:, 0, 0, :], in1=rt[:, 1, 0, :], op=ALU.subtract)
                            P.I(dve, "tensor_tensor", out=kr[:, 16:32], in0=rt[:, 2, 0, :], in1=rt[:, 3, 0, :], op=ALU.add)
                        else:
                            P.I(act, "activation", out=qb[:, :, 64:96], in_=qs[:, :, 64:96], func=AF.Copy)
                        kv3 = lambda n: kvp[n][:, :].map(lambda a: a.rearrange("p (h e) -> p h e", e=128))
                        for n in range(2):
                            P.I(act, "activation", out=kb[:, n * 4:(n + 1) * 4, 0:64], in_=kv3(n).map(lambda a: a[:, :, 0:64]),
                                func=AF.Copy)
                            P.I(dve, "tensor_copy", out=va[:, n * 4:(n + 1) * 4, 0:64], in_=kv3(n).map(lambda a: a[:, :, 64:128]))
                        P.I(pool, "tensor_copy", out=kb[:, :, 64:96],
                            in_=kr[:].map(lambda a: a.unsqueeze(1).to_broadcast([128, 8, 32])))
                        P.dma(sp, VA[tok0:tok0 + 128, :, :], va[:])
                        for (src, ring_, dst) in ((qb, qT_, QT), (kb, kT_, KT)):
                            pt = banks.next()
                            ptv = pt[0:96, :].map(lambda a: a.bitcast(BF16)[:, 0:1024].rearrange("p (h t) -> p h t", t=128))
                            for h in range(8):
                                P.tr(ptv.map(lambda a: a[:, h, :]), src[:, h, :], identb[:])
                            tt = ring_.next()
                            P.I(act, "activation", out=tt[:], in_=ptv, func=AF.Copy)
                            P.dma(sp, dst.rearrange("h r t -> r h t")[:, :, tok0:tok0 + 128], tt[:])
                    P.barrier()
                if stages == "B":
                    return nc, cn, scr

                with_ctx = l < DEPTH - 1
                with ExitStack() as sC:
                    KTh_ = P.ring(sC, "KTh", [96, TOK], BF16, 2)
                    VAh_ = P.ring(sC, "VAh", [128, NT, 65], BF16, 2)
                    QTg_ = P.ring(sC, "QTg", [96, 512], BF16, 4)
                    PT_ = P.ring(sC, "PT", [128, 512], BF16, 6)
                    Usb_ = P.ring(sC, "Usb", [65, 512], F32, 2)
                    rec_ = P.ring(sC, "rec", [64, 512], F32, 2)
                    yc_ = P.ring(sC, "yc", [64, 512], BF16, 2)
                    esel = P.sb(sC, "esel", [65, 64], F32)
                    P.I(dve, "memset", esel[:], 0.0, _writes=[esel.res])
                    P.I(dve, "memset", esel[64:65, :], 1.0, _writes=[esel.res])
                    hlist = list(heads if heads is not None else range(8))
                    groups = [(CTX + g * 512, 512, list(range(NT))) for g in range(16)]
                    if qgroups is not None:
                        groups = [groups[g] for g in qgroups]
                    if with_ctx:
                        groups.append((0, 256, [0, 1]))
                    units = []
                    for h in hlist:
                        for gi, (q0, nq, ktiles) in enumerate(groups):
                            for jj, j in enumerate(ktiles):
                                units.append((h, gi, q0, nq, jj, j, len(ktiles)))
                    hd = {}
                    qt = {}
                    accs = {}
                    sbk = {}

                    def ensure_head(h):
                        if h in hd or h not in hlist:
                            return
                        KTh = KTh_.next()
                        VAh = VAh_.next()
                        P.dma(sp, KTh[:], KT[h, :, :])
                        for j0 in range(0, NT, 11):
                            P.dma(sp, VAh[:, j0:j0 + 11, :],
                                  VA[j0 * 128:(j0 + 11) * 128, h, :].rearrange("(j p) e -> p j e", p=128))
                        hd[h] = (KTh, VAh)

                    def ensure_q(h, gi):
                        if (h, gi) in qt or h not in hlist or gi >= len(groups):
                            return
                        q0, nq, _ = groups[gi]
                        QTg = QTg_.next()
                        P.dma(sp, QTg[:, 0:nq], QT[h, :, q0:q0 + nq])
                        qt[(h, gi)] = QTg

                    def finalize(h, gi):
                        q0, nq, _ = groups[gi]
                        acc = accs.pop((h, gi))
                        Usb = Usb_.next()
                        P.I(dve, "tensor_copy", out=Usb[:, 0:nq], in_=acc[0:65, 0:nq])
                        rp = banks.next()
                        P.mm(rp[0:64, 0:nq], esel[:], Usb[:, 0:nq])
                        rec = rec_.next()
                        P.I(dve, "reciprocal", out=rec[:, 0:nq], in_=rp[0:64, 0:nq])
                        yc = yc_.next()
                        P.I(dve, "tensor_tensor", out=yc[:, 0:nq], in0=Usb[0:64, 0:nq], in1=rec[:, 0:nq], op=ALU.mult)
                        P.dma(sp, MIXT[512 + h * 64:512 + (h + 1) * 64, q0:q0 + nq], yc[:, 0:nq])

                    LOOK = 2
                    pending = []
                    for idx in range(len(units) + LOOK + 4):
                        if idx < len(units):
                            h, gi, q0, nq, jj, j, nk = units[idx]
                            if jj == 0:
                                ensure_head(h)
                                ensure_q(h, gi)
                                if gi == 0:
                                    nh = hlist.index(h) + 1
                                    if nh < len(hlist):
                                        ensure_head(hlist[nh])
                                if gi + 1 < len(groups):
                                    ensure_q(h, gi + 1)
                                else:
                                    nh = hlist.index(h) + 1
                                    if nh < len(hlist):
                                        ensure_q(hlist[nh], 0)
                            sp_ = banks.next()
                            P.mm(sp_[:, 0:nq], hd[h][0][:, j * 128:(j + 1) * 128], qt[(h, gi)][:, 0:nq])
                            sbk[idx] = sp_
                        while pending and pending[0][0] <= idx:
                            _, h_, gi_ = pending.pop(0)
                            finalize(h_, gi_)
                        k = idx - LOOK
                        if 0 <= k < len(units):
                            h, gi, q0, nq, jj, j, nk = units[k]
                            sp_ = sbk.pop(k)
                            if jj == 0:
                                accs[(h, gi)] = accb.next()
                            acc = accs[(h, gi)]
                            PT = PT_.next()
                            P.I(act, "activation", out=PT[:, 0:nq], in_=sp_[:, 0:nq], func=AF.Exp)
                            P.mm(acc[0:65, 0:nq], hd[h][1][:, j, :], PT[:, 0:nq], start=(jj == 0), stop=(jj == nk - 1))
                            if jj == nk - 1:
                                pending.append((idx + 2, h, gi))
                                qt.pop((h, gi), None)
                        while pending and pending[0][0] <= idx:
                            _, h_, gi_ = pending.pop(0)
                            finalize(h_, gi_)
                    assert not pending and not accs
                    P.barrier()
                if stages == "C":
                    return nc, cn, scr

                def rtile(i):
                    return 1 - i if i < 2 else 67 - i

                def qkcol(i):
                    return i * 128 + (2 if i < 2 else 4)

                if os.environ.get("M2CUT") == "-1":
                    P.barrier()
                    return nc, cn, scr

                with ExitStack() as sM:
                    TKS = P.sb(sM, "TKS", [128, NT, 16], F32)
                    ECB = P.sb(sM, "ECB", [128, 8, NT], F32)
                    with ExitStack() as s2:
                        GI = P.sb(s2, "GI", [8, TOK], F32)
                        GF = P.sb(s2, "GF", [8, TOK], F32)
                        MM = P.sb(s2, "MM", [8, TOK], F32)
                        MST = P.sb(s2, "MST", [16, TOK], F32)
                        EF = P.sb(s2, "EF", [40, TOK], F32)
                        gall = P.sb(s2, "gall", [128, NT, 16], F32)
                        gb = P.sb(s2, "gb", [8, 4], F32)
                        ecs = P.sb(s2, "ecs", [8, NT], F32)
                        Dg = P.sb(s2, "Dg", [8, 8, NT], F32)
                        tk_ = P.ring(s2, "tk", [128, 40], F32, 2)
                        P.I(pool, "memset", EF[:], 0.0, _writes=[EF.res])

                        if os.environ.get("M2CUT") == "-0.5":
                            P.barrier()
                            return nc, cn, scr
                        P.dma(sp, gall[:], VOG[:, 512:528].rearrange("(j p) g -> p j g", p=128))

                        if os.environ.get("M2CUT") == "-0.3":
                            P.barrier()
                            return nc, cn, scr
                        grow = P.sb(s2, "grow", [1, 16], F32)
                        P.dma(sp, grow[:, 0:8], inp["m_ib"][l:l + 1].rearrange("o d h -> o (d h)"))
                        P.dma(sp, grow[:, 8:16], inp["m_fb"][l:l + 1].rearrange("o d h -> o (d h)"))
                        pgb = banks.next()
                        P.mm(pgb[0:8, 0:1], grow[:, 0:8], onec[0:1, 0:1])
                        P.mm(pgb[0:8, 1:2], grow[:, 8:16], onec[0:1, 0:1])
                        P.I(dve, "tensor_copy", out=gb[:, 0:2], in_=pgb[0:8, 0:2])
                        P.I(dve, "tensor_scalar", out=gb[:, 2:3], in0=gb[:, 1:2], scalar1=-1.0, scalar2=None, op0=ALU.mult)

                        if os.environ.get("M2CUT") == "0":
                            P.barrier()
                            return nc, cn, scr
                        for i in range(NT):
                            pg = banks.next()
                            sk = os.environ.get("M2SKIP", "")
                            if "a" not in sk:
                                P.mm(pg[0:16, 0:128], gall[:, i, :], identf[:])
                            if "b" not in sk:
                                P.mm(pg[0:16, 128:256], gall[:, i, :], jrev[:])
                            if "c" not in sk:
                                P.I(act, "activation", out=MST[0:16, i * 128:(i + 1) * 128], in_=pg[0:16, 0:128], func=AF.Copy)
                            r_ = rtile(i)
                            if "d" not in sk:
                                P.I(dve, "tensor_copy", out=EF[0:16, r_ * 128:(r_ + 1) * 128], in_=pg[0:16, 128:256])

                        if os.environ.get("M2CUT") == "0b":
                            P.barrier()
                            return nc, cn, scr
                        P.dma(sp, GI[0:4, :], MST[0:4, :])
                        P.dma(sp, GF[0:4, :], MST[4:8, :])
                        P.dma(sp, GI[4:8, :], EF[8:12, :])
                        P.dma(sp, GF[4:8, :], EF[12:16, :])

                        if os.environ.get("M2CUT") == "1":
                            P.barrier()
                            return nc, cn, scr
                        P.I(dve, "tensor_scalar", out=GI[:], in0=GI[:], scalar1=gb[:, 0:1], scalar2=None, op0=ALU.add)
                        P.I(act, "activation", out=GF[:], in_=GF[:], func=AF.Exp, scale=-1.0, bias=gb[:, 2:3])
                        P.I(act, "activation", out=GF[:], in_=GF[:], func=AF.Ln, bias=onec[0:8, 0:1])
                        onesb = onec[0:8, 0:1].map(lambda a: a.to_broadcast([8, TOK]))
                        P.I(dve, "tensor_tensor_scan", out=GF[:], data0=onesb, data1=GF[:], initial=0.0, op0=ALU.mult, op1=ALU.add)
                        P.I(dve, "tensor_tensor", out=GI[:], in0=GI[:], in1=GF[:], op=ALU.add)
                        P.I(dve, "tensor_tensor_scan", out=MM[:], data0=onesb, data1=GI[:], initial=0.0, op0=ALU.mult, op1=ALU.max)

                        if os.environ.get("M2CUT") == "2":
                            P.barrier()
                            return nc, cn, scr
                        v3 = lambda vv: vv.map(lambda a: a.rearrange("p (c t) -> p c t", t=128))
                        P.I(pool, "memset", MST[0:8, 0:128], 0.0, _writes=[MST.res])
                        P.I(dve, "tensor_copy", out=v3(MST[0:8, :]).map(lambda a: a[:, 1:NT, :]),
                            in_=v3(MM[:]).map(lambda a: a[:, 0:NT - 1, 127:128].to_broadcast([8, NT - 1, 128])))
                        P.I(dve, "tensor_tensor", out=ecs[:], in0=v3(MST[0:8, :]).map(lambda a: a[:, :, 0]),
                            in1=v3(MM[:]).map(lambda a: a[:, :, 127]), op=ALU.subtract)
                        P.I(act, "activation", out=ecs[:], in_=ecs[:], func=AF.Exp)
                        P.I(dve, "tensor_tensor", out=GI[:], in0=GI[:], in1=MST[0:8, :], op=ALU.subtract)
                        P.I(dve, "tensor_tensor", out=GF[:], in0=GF[:], in1=MST[0:8, :], op=ALU.subtract)
                        P.I(act, "activation", out=EF[0:8, :], in_=GI[:], func=AF.Exp)
                        P.I(act, "activation", out=EF[32:40, :], in_=GF[:], func=AF.Exp)

                        if os.environ.get("M2CUT") == "3":
                            P.barrier()
                            return nc, cn, scr
                        P.I(dve, "tensor_tensor", out=Dg[:],
                            in0=ecs[:].map(lambda a: a.unsqueeze(1).to_broadcast([8, 8, NT])),
                            in1=identf[0:8, 0:8].map(lambda a: a.unsqueeze(2).to_broadcast([8, 8, NT])), op=ALU.mult)
                        pe_ = [banks.next(), banks.next()]
                        Dgf = Dg[:].map(lambda a: a.rearrange("p a c -> p (a c)"))
                        hN = 4 * NT
                        for n in range(2):
                            P.mm(pe_[n][:, 0:hN], onec[0:8, :], Dgf.map(lambda a: a[:, n * hN:(n + 1) * hN]))
                            P.I(act, "activation", out=ECB[:, n * 4:(n + 1) * 4, :].map(lambda a: a.rearrange("p a c -> p (a c)")),
                                in_=pe_[n][:, 0:hN], func=AF.Copy)
                        for i in range(NT):
                            pt = banks.next()
                            P.tr(pt[:, 0:40], EF[0:40, i * 128:(i + 1) * 128], identf[0:40, 0:40])
                            r_ = rtile(i)
                            P.tr(pt[:, 64:104], EF[0:40, r_ * 128:(r_ + 1) * 128], identf[0:40, 0:40])
                            tk = tk_.next()
                            P.I(act, "activation", out=tk[:], in_=pt[:, 64:104], func=AF.Copy)
                            P.mm(pt[:, 128:168], jrev[:], tk[:])
                            tv = TKS[:, i, :].map(lambda a: a.rearrange("p (q j) -> p q j", j=8))
                            pv = lambda c0: pt[:, c0:c0 + 64].map(lambda a: a.rearrange("p (q j) -> p q j", j=32))
                            P.I(dve, "tensor_copy", out=tv.map(lambda a: a[:, :, 0:4]), in_=pv(0).map(lambda a: a[:, :, 0:4]))
                            P.I(dve, "tensor_copy", out=tv.map(lambda a: a[:, :, 4:8]), in_=pv(128).map(lambda a: a[:, :, 4:8]))
                        P.barrier()
                    PADW = TOK + 6
                    QKb = P.sb(sM, "QKb", [128, 4, PADW], BF16)
                    ktm = P.sb(sM, "ktm", [128, NT, 256], BF16)
                    with ExitStack() as s1:
                        cw = P.sb(s1, "cw", [128, 4, 3], F32)
                        cbi = P.sb(s1, "cbi", [128, 4], F32)
                        for kk in range(3):
                            P.dma(sp, cw[:, :, kk], inp["m_conv_w"][l, kk].rearrange("(c p) -> p c", p=128),
                                  allow_slow_non_contiguous=True)
                        P.dma(sp, cbi[:], inp["m_conv_b"][l].rearrange("(c p) -> p c", p=128), allow_slow_non_contiguous=True)
                        HP = 4230
                        stg_ = P.ring(s1, "stg", [128, HP], F32, 2)
                        yb_ = P.ring(s1, "ybuf", [128, HP], F32, 2)
                        for cc in range(4):
                            rows = QKT[cc * 128:(cc + 1) * 128, :]
                            for piece in range(2):
                                stg = stg_.next()
                                yb = yb_.next()
                                if piece == 0:
                                    n = 4230
                                    P.I(pool, "memset", stg[:, 0:2], 0.0, _writes=[stg.res])
                                    P.I(pool, "memset", stg[:, 258:260], 0.0, _writes=[stg.res])
                                    P.dma(sp, stg[:, 2:258], rows[:, 0:256])
                                    P.dma(sp, stg[:, 260:4230], rows[:, 256:256 + 3970])
                                    oc0 = 1
                                else:
                                    n = 4226
                                    P.dma(sp, stg[:, 0:4224], rows[:, 4224:8448])
                                    P.I(pool, "memset", stg[:, 4224:4226], 0.0, _writes=[stg.res])
                                    oc0 = 4229
                                m = n - 2
                                P.I(dve, "tensor_scalar", out=yb[:, 0:m], in0=stg[:, 1:1 + m], scalar1=cw[:, cc, 1:2],
                                    scalar2=cbi[:, cc:cc + 1], op0=ALU.mult, op1=ALU.add)
                                P.I(dve, "scalar_tensor_tensor", out=yb[:, 0:m], in0=stg[:, 0:m], scalar=cw[:, cc, 0:1],
                                    in1=yb[:, 0:m], op0=ALU.mult, op1=ALU.add)
                                P.I(dve, "scalar_tensor_tensor", out=yb[:, 0:m], in0=stg[:, 2:2 + m], scalar=cw[:, cc, 2:3],
                                    in1=yb[:, 0:m], op0=ALU.mult, op1=ALU.add)
                                if cc < 2:
                                    P.I(act, "activation", out=yb[:, 0:m], in_=yb[:, 0:m], func=AF.Silu)
                                    P.I(pool, "tensor_scalar", out=QKb[:, cc, oc0:oc0 + m], in0=yb[:, 0:m], scalar1=0.125,
                                        scalar2=None, op0=ALU.mult)
                                else:
                                    P.I(act, "activation", out=QKb[:, cc, oc0:oc0 + m], in_=yb[:, 0:m], func=AF.Silu)
                        for i in range(NT):
                            pk = bank_bf((2, 128))
                            c0 = qkcol(i)
                            for kc in range(2):
                                P.tr(pk.map(lambda a: a[:, kc, :]), QKb[:, 2 + kc, c0:c0 + 128], identb[:])
                            P.I(act, "activation", out=ktm[:, i, :].map(lambda a: a.rearrange("p (c t) -> p c t", t=128)),
                                in_=pk, func=AF.Copy)
                        P.barrier()
                    if stages == "M1":
                        return nc, cn, scr
                    with ExitStack() as s3:
                        V1 = P.sb(s3, "V1", [128, NT, 4, 65], BF16)
                        P.I(pool, "memset", V1[:], 1.0, _writes=[V1.res])
                        for j0 in range(0, NT, 11):
                            for h in range(4):
                                P.dma(pool, V1[:, j0:j0 + 11, h, 0:64],
                                      VOG[j0 * 128:(j0 + 11) * 128, h * 64:(h + 1) * 64].rearrange("(j p) e -> p j e", p=128))
                        CN = [P.sb(s3, "CN%d" % k, [128, 65], F32) for k in range(8)]
                        CNb = [P.sb(s3, "CNb%d" % k, [128, 65], BF16) for k in range(8)]
                        for k in range(8):
                            P.I(dve, "memset", CN[k][:], 0.0, _writes=[CN[k].res])
                            P.I(dve, "memset", CNb[k][:], 0.0, _writes=[CNb[k].res])
                        Sm_ = P.ring(s3, "Sm", [128, 128], BF16, 6)
                        Sr_ = P.ring(s3, "Sr", [128, 128], BF16, 6)
                        trifb = P.sb(s3, "trifb", [128, 128], BF16)
                        tribb = P.sb(s3, "tribb", [128, 128], BF16)
                        P.I(dve, "tensor_copy", out=trifb[:], in_=trif[:])
                        P.I(dve, "tensor_copy", out=tribb[:], in_=trib[:])
                        vpp_ = P.ring(s3, "vpp", [128, 65], BF16, 6)
                        dn_ = P.ring(s3, "dn", [128, 2], F32, 6)
                        tmp_ = P.ring(s3, "ctmp", [128, 65], F32, 6)
                        Hst_ = [P.ring(s3, "Hst%d" % d_, [128, 256], F32, 3) for d_ in range(2)]
                        for c in range(NT):
                            for d_ in range(2):
                                i = c if d_ == 0 else (1 - c if c < 2 else 67 - c)
                                c0 = qkcol(i)
                                Hst = Hst_[d_].next()
                                tri = trif if d_ == 0 else trib
                                for h in range(4):
                                    k = d_ * 4 + h
                                    pb = (h % 2) * 64
                                    qv = QKb[pb:pb + 64, h // 2, c0:c0 + 128]
                                    kv_ = QKb[pb:pb + 64, 2 + h // 2, c0:c0 + 128]
                                    sps = banks.next()
                                    P.mm(sps[:, 0:128], kv_, qv)
                                    Sr = Sr_.next()
                                    P.I(act, "activation", out=Sr[:], in_=sps[:, 0:128], func=AF.Copy)
                                    Sm = Sm_.next()
                                    P.I(pool, "tensor_tensor", out=Sm[:], in0=Sr[:], in1=(trifb if d_ == 0 else tribb)[:], op=ALU.mult)
                                    vpp = vpp_.next()
                                    P.I(pool, "tensor_scalar", out=vpp[:], in0=V1[:, i, h, :], scalar1=TKS[:, i, k:k + 1],
                                        scalar2=None, op0=ALU.mult)
                                    nd = banks.next()
                                    P.mm(nd[:, 0:65], Sm[:], vpp[:], start=True, stop=False)
                                    P.mm(nd[:, 0:65], qv, CNb[k][pb:pb + 64, :], start=False, stop=True)
                                    P.mm(nd[0:64, 128:193], ktm[:, i, h * 64:(h + 1) * 64], vpp[:])
                                    tmp = tmp_.next()
                                    P.I(dve, "tensor_tensor", out=tmp[0:64, :], in0=CN[k][0:64, :],
                                        in1=nd[0:64, 128:193], op=ALU.add)
                                    P.I(pool, "tensor_scalar", out=CN[k][0:64, :], in0=tmp[0:64, :],
                                        scalar1=ECB[0:64, k, c:c + 1], scalar2=None, op0=ALU.mult)
                                    P.I(act, "activation", out=CNb[k][pb:pb + 64, :], in_=tmp[0:64, :], func=AF.Copy,
                                        scale=ECB[0:64, k, c:c + 1])
                                    dn = dn_.next()
                                    P.I(dve, "tensor_scalar", out=dn[:, 1:2], in0=nd[:, 64:65], scalar1=-1.0,
                                        scalar2=None, op0=ALU.mult)
                                    P.I(dve, "scalar_tensor_tensor", out=dn[:, 0:1], in0=dn[:, 1:2], scalar=TKS[:, i, 8 + k:9 + k],
                                        in1=nd[:, 64:65], op0=ALU.max, op1=ALU.max)
                                    P.I(dve, "reciprocal", out=dn[:, 1:2], in_=dn[:, 0:1])
                                    P.I(act, "activation", out=Hst[:, h * 64:(h + 1) * 64], in_=nd[:, 0:64], func=AF.Copy,
                                        scale=dn[:, 1:2])
                                P.dma(sp, HFB[d_, i * 128:(i + 1) * 128, :], Hst[:])
                        P.barrier()
                    if stages == "M3":
                        return nc, cn, scr
                    with ExitStack() as s4:
                        mng = P.sb(s4, "mng", [128, 256], F32)
                        P.dma(sp, mng[:], inp["m_norm_g"][l:l + 1, :].partition_broadcast(128))
                        hf_ = P.ring(s4, "hf", [128, 2, 256], F32, 2)
                        og_ = P.ring(s4, "og", [128, 256], F32, 2)
                        hs_ = P.ring(s4, "hs", [128, 256], F32, 2)
                        sq_ = P.ring(s4, "sq", [128, 256], F32, 2)
                        ms_ = P.ring(s4, "ms", [128, 8], F32, 2)
                        ybf_ = P.ring(s4, "ybf", [128, 256], BF16, 2)
                        ybT_ = P.ring(s4, "ybT", [128, 2, 128], BF16, 2)
                        for i in range(NT):
                            if i < 2 and not with_ctx:
                                continue
                            hf = hf_.next()
                            P.dma(sp, hf[:], HFB[:, i * 128:(i + 1) * 128, :].rearrange("d p e -> p d e"))
                            og = og_.next()
                            P.dma(sp, og[:], VOG[i * 128:(i + 1) * 128, 256:512])
                            hs = hs_.next()
                            P.I(dve, "tensor_tensor", out=hs[:], in0=hf[:, 0, :], in1=hf[:, 1, :], op=ALU.add)
                            sq = sq_.next()
                            P.I(pool, "tensor_tensor", out=sq[:], in0=hs[:], in1=hs[:], op=ALU.mult)
                            ms = ms_.next()
                            P.I(dve, "tensor_reduce", out=ms[:, 0:4], in_=sq[:].map(lambda a: a.rearrange("p (h e) -> p h e", e=64)),
                                axis=AX.X, op=ALU.add)
                            P.I(act, "activation", out=ms[:, 0:4], in_=ms[:, 0:4], func=AF.Sqrt, scale=1.0 / 64, bias=epsc[:])
                            P.I(dve, "reciprocal", out=ms[:, 4:8], in_=ms[:, 0:4])
                            P.I(act, "activation", out=og[:], in_=og[:], func=AF.Sigmoid)
                            h3 = lambda vv: vv.map(lambda a: a.rearrange("p (h e) -> p h e", e=64))
                            P.I(dve, "tensor_tensor", out=h3(hs[:]), in0=h3(hs[:]),
                                in1=ms[:, 4:8].map(lambda a: a.unsqueeze(2).to_broadcast([128, 4, 64])), op=ALU.mult)
                            P.I(pool, "tensor_tensor", out=hs[:], in0=hs[:], in1=mng[:], op=ALU.mult)
                            ybf = ybf_.next()
                            P.I(dve, "tensor_tensor", out=ybf[:], in0=hs[:], in1=og[:], op=ALU.mult)
                            py = bank_bf((2, 128))
                            for cc in range(2):
                                P.tr(py.map(lambda a: a[:, cc, :]), ybf[:, cc * 128:(cc + 1) * 128], identb[:])
                            ybT = ybT_.next()
                            P.I(act, "activation", out=ybT[:], in_=py, func=AF.Copy)
                            P.dma(sp, MIXT[256:512, i * 128:(i + 1) * 128].rearrange("(c p) t -> p c t", p=128), ybT[:])
                        P.barrier()
                if stages == "M":
                    return nc, cn, scr

                with ExitStack() as sF:
                    ld = lambda nm, shp, dt: P.sb(sF, nm, shp, dt)
                    c1 = ld("f_c1", [128, 128], BF16); s1t = ld("f_s1", [128, 128], BF16); ns1 = ld("f_ns1", [128, 128], BF16)
                    twc = ld("f_twc", [128, 64], F32); tws = ld("f_tws", [128, 64], F32)
                    c2 = ld("f_c2", [64, 64], BF16); s2t = ld("f_s2", [64, 64], BF16)
                    ccs = ld("f_ccs", [128, 2, 2, 256], BF16)
                    for tl, nm in ((c1, "f_c1"), (s1t, "f_s1"), (ns1, "f_ns1"), (twc, "f_twc"), (tws, "f_tws"), (c2, "f_c2"), (s2t, "f_s2")):
                        P.dma(sp, tl[:], inp[nm][:, :])
                    P.dma(sp, ccs[:, 0, :, :], inp["f_cc"].rearrange("(a p) k -> p a k", p=128))
                    P.dma(sp, ccs[:, 1, :, :], inp["f_sc"].rearrange("(a p) k -> p a k", p=128))
                    with ExitStack() as sF1:
                        X1 = P.sb(sF1, "X1", [128, 64, 512], BF16)
                        YT = P.sb(sF1, "YT", [128, 64, 512], BF16)
                        ftmp_ = P.ring(sF1, "ftmp", [128, 256], F32, 4)
                        for q in range(4):
                            P.dma(sp, X1[:, q * 16:(q + 1) * 16, :],
                                  FAB[CTX:, :].rearrange("(a b) c -> a b c", b=64)[:, q * 16:(q + 1) * 16, :])
                        for tp in range(32):
                            zr = X1[:, 2 * tp:2 * tp + 2, 0:256]
                            zi = X1[:, 2 * tp:2 * tp + 2, 256:512]
                            br = banks.next()
                            bi = banks.next()
                            P.mm(br[:, :], c1[:], zr, start=True, stop=False)
                            P.mm(br[:, :], s1t[:], zi, start=False, stop=True)
                            P.mm(bi[:, :], c1[:], zi, start=True, stop=False)
                            P.mm(bi[:, :], ns1[:], zr, start=False, stop=True)
                            for u in range(2):
                                t2_ = 2 * tp + u
                                yr = br[:, u * 256:(u + 1) * 256]
                                yi = bi[:, u * 256:(u + 1) * 256]
                                ta = ftmp_.next()
                                tb = ftmp_.next()
                                P.I(dve, "tensor_scalar", out=ta[:], in0=yi, scalar1=tws[:, t2_:t2_ + 1], scalar2=None, op0=ALU.mult)
                                P.I(dve, "scalar_tensor_tensor", out=YT[:, t2_, 0:256], in0=yr, scalar=twc[:, t2_:t2_ + 1],
                                    in1=ta[:], op0=ALU.mult, op1=ALU.add)
                                P.I(dve, "tensor_scalar", out=tb[:], in0=yr, scalar1=tws[:, t2_:t2_ + 1], scalar2=None, op0=ALU.mult)
                                P.I(dve, "scalar_tensor_tensor", out=YT[:, t2_, 256:512], in0=yi, scalar=twc[:, t2_:t2_ + 1],
                                    in1=tb[:], op0=ALU.mult, op1=ALU.subtract)
                        for q in range(4):
                            P.dma(sp, FY[:, q * 16:(q + 1) * 16, :], YT[:, q * 16:(q + 1) * 16, :])
                        P.barrier()
                    with ExitStack() as sF2:
                        y2_ = P.ring(sF2, "y2", [64, 8, 512], BF16, 3)
                        yaT = P.sb(sF2, "yaT", [128, 2, SEQ], BF16)
                        FYv = FY.rearrange("k t c -> t k c")
                        for kb in range(16):
                            y2 = y2_.next()
                            P.dma(sp, y2[:], FYv[:, kb * 8:(kb + 1) * 8, :])
                            for jc in range(2):
                                bk = banks.next()
                                for kl in range(8):
                                    P.mm(bk[:, kl * 64:(kl + 1) * 64], y2[:, kl, jc * 128:(jc + 1) * 128], c2[:], start=True, stop=False)
                                    P.mm(bk[:, kl * 64:(kl + 1) * 64], y2[:, kl, 256 + jc * 128:256 + (jc + 1) * 128], s2t[:],
                                         start=False, stop=True)
                                ov = yaT[:, jc, :].map(lambda a: a.rearrange("p (k2 k1) -> p k1 k2", k1=128)[:, kb * 8:(kb + 1) * 8, :])
                                iv = bk[:, :].map(lambda a: a.rearrange("p (kl k2) -> p kl k2", k2=64))
                                P.I(act if jc == 0 else dve, "activation" if jc == 0 else "tensor_copy", out=ov, in_=iv,
                                    **({"func": AF.Copy} if jc == 0 else {}))
                        P.dma(sp, MIXT[0:256, CTX:].rearrange("(c p) t -> p c t", p=128), yaT[:])
                        if with_ctx:
                            zc = P.sb(sF2, "zc", [128, 2, 512], BF16)
                            yc2 = P.sb(sF2, "yc2", [128, 2, 256], BF16)
                            P.dma(sp, zc[:], FAB[0:CTX, :].rearrange("(a p) c -> p a c", p=128))
                            for jc in range(2):
                                bk = banks.next()
                                for a_ in range(2):
                                    P.mm(bk[:, 0:256], zc[:, a_, jc * 128:(jc + 1) * 128], ccs[:, 0, a_, :], start=(a_ == 0), stop=False)
                                    P.mm(bk[:, 0:256], zc[:, a_, 256 + jc * 128:256 + (jc + 1) * 128], ccs[:, 1, a_, :],
                                         start=False, stop=(a_ == 1))
                                P.I(act, "activation", out=yc2[:, jc, :], in_=bk[:, 0:256], func=AF.Copy)
                            P.dma(sp, MIXT[0:256, 0:CTX].rearrange("(c p) t -> p c t", p=128), yc2[:])
                        P.barrier()
                if stages == "F":
                    return nc, cn, scr

                last = (l == nlayers - 1)
                tiles_e = [i for i in range(NT) if (i >= 2 or with_ctx)]
                sets = ([(1, [0, 1], CAPC, XEc, YEc, 0)] if with_ctx else []) + [(0, list(range(2, NT)), CAPX, XEx, YEx, 16)]
                with ExitStack() as sE:
                    AFF = P.sb(sE, "AFF", [128, NT, NE], F32)
                    GM = P.sb(sE, "GM", [128, NT, NE], F32)
                    SLOT = P.sb(sE, "SLOT", [128, NT, NE], I32)
                    Gb = P.sb(sE, "Gb", [128, 4, D], F32)
                    for v in range(2):
                        P.dma(sp, Gb[:, v, :], MOD[l, v:v + 1, 2 * D:3 * D].partition_broadcast(128))
                        P.dma(sp, Gb[:, 2 + v, :], MOD[l, v:v + 1, 5 * D:6 * D].partition_broadcast(128))
                    if not with_ctx:
                        P.I(pool, "memset", AFF[:, 0:2, :], 0.0, _writes=[AFF.res])
                    with ExitStack() as sE1:
                        Wout = P.sb(sE1, "Wout", [128, 8, D], BF16)
                        for k in range(8):
                            P.dma(pool, Wout[:, k, :], inp["w_out"][l, k * 128:(k + 1) * 128, :])
                        RW = P.sb(sE1, "RW", [128, 8, NE], BF16)
                        P.dma(pool, RW[:], inp["router_w"][l].rearrange("(k p) e -> p k e", p=128))
                        mT_ = P.ring(sE1, "mT", [128, 8, 128], BF16, 2)
                        xe_ = P.ring(sE1, "xe", [128, D], F32, 2)
                        tm_ = P.ring(sE1, "tm", [128, D], F32, 2)
                        st2_ = P.ring(sE1, "st2", [128, 8], F32, 3)
                        xn2_ = P.ring(sE1, "xn2", [128, D], BF16, 2)
                        h2_ = P.ring(sE1, "h2", [128, 8, 128], BF16, 2)
                        lg_ = P.ring(sE1, "lg", [128, NE], F32, 2)
                        jk2 = P.sb(sE1, "jk2", [128, D], BF16)
                        for i in tiles_e:
                            v = 0 if i >= 2 else 1
                            tok0 = i * 128
                            mT = mT_.next()
                            P.dma(sp, mT[:], MIXT[:, tok0:tok0 + 128].rearrange("(k p) t -> p k t", p=128))
                            xt = xe_.next()
                            P.dma(sp, xt[:], XR[tok0:tok0 + 128, :])
                            ob = [banks.next(), banks.next()]
                            for n in range(2):
                                for k in range(8):
                                    P.mm(ob[n][:, :], mT[:, k, :], Wout[:, k, n * 512:(n + 1) * 512], start=(k == 0), stop=(k == 7))
                            tm = tm_.next()
                            for n in range(2):
                                P.I(dve, "tensor_tensor", out=tm[:, n * 512:(n + 1) * 512], in0=ob[n][:, :],
                                    in1=Gb[:, v, n * 512:(n + 1) * 512], op=ALU.mult)
                            P.I(pool, "tensor_tensor", out=xt[:], in0=xt[:], in1=tm[:], op=ALU.add)
                            P.dma(sp, XR[tok0:tok0 + 128, :], xt[:])
                            st = st2_.next()
                            P.I(act, "activation", out=jk2[:], in_=xt[:], func=AF.Square, accum_out=st[:, 0:1])
                            P.I(act, "activation", out=st[:, 1:2], in_=st[:, 0:1], func=AF.Sqrt, scale=1.0 / D, bias=epsc[:])
                            P.I(dve, "reciprocal", out=st[:, 2:3], in_=st[:, 1:2])
                            xn2 = xn2_.next()
                            P.I(dve, "tensor_scalar", out=xn2[:], in0=xt[:], scalar1=st[:, 2:3], scalar2=None, op0=ALU.mult)
                            P.dma(sp, XN2[tok0:tok0 + 128, :], xn2[:])
                            pT = bank_bf((8, 128))
                            for k in range(8):
                                P.tr(pT.map(lambda a: a[:, k, :]), xn2[:, k * 128:(k + 1) * 128], identb[:])
                            h2 = h2_.next()
                            bc = lambda vv: vv.map(lambda a: a.unsqueeze(2).to_broadcast([128, 8, 128]))
                            P.I(dve, "tensor_tensor", out=h2[:], in0=pT, in1=bc(Amod[:, 2 + v, :]), op=ALU.mult)
                            P.I(pool, "tensor_tensor", out=h2[:], in0=h2[:], in1=bc(colv[:, v * 6 + 3, :]), op=ALU.add)
                            lb = banks.next()
                            for k in range(8):
                                P.mm(lb[:, 0:NE], h2[:, k, :], RW[:, k, :], start=(k == 0), stop=(k == 7))
                            lg = lg_.next()
                            P.I(dve, "tensor_reduce", out=st[:, 3:4], in_=lb[:, 0:NE], axis=AX.X, op=ALU.max)
                            P.I(dve, "tensor_scalar", out=st[:, 4:5], in0=st[:, 3:4], scalar1=-1.0, scalar2=None, op0=ALU.mult)
                            P.I(act, "activation", out=lg[:], in_=lb[:, 0:NE], func=AF.Exp, bias=st[:, 4:5], accum_out=st[:, 5:6])
                            P.I(dve, "reciprocal", out=st[:, 6:7], in_=st[:, 5:6])
                            P.I(dve, "tensor_scalar", out=AFF[:, i, :], in0=lg[:], scalar1=st[:, 6:7], scalar2=None, op0=ALU.mult)
                        P.barrier()
                    if stages == "E":
                        dbg_a = nc.dram_tensor("dbg_AFF", [128, NT, NE], F32, kind="ExternalOutput").ap()
                        P.dma(sp, dbg_a[:, :, :], AFF[:])
                        P.barrier()
                        return nc, cn, scr

                    with ExitStack() as sD2:
                        lo = P.sb(sD2, "lo", [128, 32], F32)
                        hi = P.sb(sD2, "hi", [128, 32], F32)
                        mid = P.sb(sD2, "mid", [128, 32], F32)
                        capv = P.sb(sD2, "capv", [128, 32], F32)
                        onesb = P.sb(sD2, "onesb", [128, 128], BF16)
                        utri = P.sb(sD2, "utri", [128, 128], BF16)
                        P.dma(sp, capv[:], inp["capv"][:, :])
                        P.dma(sp, onesb[:], inp["onesb"][:, :])
                        P.dma(sp, utri[:], inp["utri"][:, :])
                        P.I(dve, "memset", lo[:], 0.0, _writes=[lo.res])
                        P.I(dve, "memset", hi[:], 1.0, _writes=[hi.res])
                        cmp_ = P.sb(sD2, "cmp", [128, NT, NE], BF16)
                        pc = P.sb(sD2, "pc", [128, 32], BF16)
                        pcf = P.sb(sD2, "pcf", [128, 32], F32)
                        P.I(dve, "memset", pcf[:], 0.0, _writes=[pcf.res])
                        mge = P.sb(sD2, "mge", [128, 32], U32)
                        mlt = P.sb(sD2, "mlt", [128, 32], U32)
                        P.I(dve, "memset", pc[:], 0.0, _writes=[pc.res])
                        for it in range(34):
                            P.I(dve, "tensor_tensor", out=mid[:], in0=lo[:], in1=hi[:], op=ALU.add)
                            P.I(dve, "tensor_scalar", out=mid[:], in0=mid[:], scalar1=0.5, scalar2=None, op0=ALU.mult)
                            for (v, tl, cap, XE, YE, co) in sets:
                                nt_ = len(tl)
                                t0_ = tl[0]
                                P.I(dve, "tensor_tensor", out=cmp_[:, t0_:t0_ + nt_, :], in0=AFF[:, t0_:t0_ + nt_, :],
                                    in1=mid[:, co:co + 16].map(lambda a: a.unsqueeze(1).to_broadcast([128, nt_, NE])), op=ALU.is_gt)
                                P.I(dve, "tensor_reduce", out=pcf[:, co:co + 16],
                                    in_=cmp_[:, t0_:t0_ + nt_, :].map(lambda a: a.rearrange("p j e -> p e j")), axis=AX.X, op=ALU.add)
                            P.I(dve, "tensor_copy", out=pc[:], in_=pcf[:])
                            tb = banks.next()
                            P.mm(tb[:, 0:32], onesb[:], pc[:])
                            P.I(dve, "tensor_tensor", out=mge[:], in0=tb[:, 0:32], in1=capv[:], op=ALU.is_ge)
                            P.I(dve, "tensor_tensor", out=mlt[:], in0=tb[:, 0:32], in1=capv[:], op=ALU.is_lt)
                            P.I(dve, "copy_predicated", out=lo[:], mask=mge[:], data=mid[:])
                            P.I(dve, "copy_predicated", out=hi[:], mask=mlt[:], data=mid[:])
                        offb = P.sb(sD2, "offb", [128, NE], F32)
                        mk_ = P.ring(sD2, "mk", [128, NE], F32, 2)
                        mkb_ = P.ring(sD2, "mkb", [128, NE], BF16, 2)
                        sl_ = P.ring(sD2, "sl", [128, NE], F32, 2)
                        xs_ = P.ring(sD2, "xs", [128, D], BF16, 3)
                        BIG = 1.0e6
                        for (v, tl, cap, XE, YE, co) in sets:
                            P.I(dve, "memset", offb[:], 0.0, _writes=[offb.res])
                            for i in tl:
                                mk = mk_.next()
                                P.I(dve, "tensor_tensor", out=mk[:], in0=AFF[:, i, :], in1=lo[:, co:co + 16], op=ALU.is_gt)
                                mkb = mkb_.next()
                                P.I(dve, "tensor_copy", out=mkb[:], in_=mk[:])
                                P.I(dve, "tensor_tensor", out=GM[:, i, :], in0=AFF[:, i, :], in1=mk[:], op=ALU.mult)
                                rb = banks.next()
                                P.mm(rb[:, 0:NE], utri[:], mkb[:])
                                P.mm(rb[:, NE:2 * NE], onesb[:], mkb[:])
                                sl = sl_.next()
                                P.I(dve, "tensor_tensor", out=sl[:], in0=rb[:, 0:NE], in1=offb[:], op=ALU.add)
                                P.I(dve, "tensor_tensor", out=offb[:], in0=rb[:, NE:2 * NE], in1=offb[:], op=ALU.add)
                                P.I(dve, "tensor_scalar", out=sl[:], in0=sl[:], scalar1=-BIG, scalar2=None, op0=ALU.add)
                                P.I(dve, "tensor_tensor", out=sl[:], in0=sl[:], in1=mk[:], op=ALU.mult)
                                P.I(dve, "tensor_scalar", out=SLOT[:, i, :], in0=sl[:], scalar1=BIG, scalar2=None, op0=ALU.add)
                                xs = xs_.next()
                                P.dma(sp, xs[:], XN2[i * 128:(i + 1) * 128, :])
                                for e in range(NE):
                                    P.dma(pool, XE[e][:, :], xs[:], meth="indirect_dma_start",
                                          out_offset=bass.IndirectOffsetOnAxis(ap=SLOT[:, i, e:e + 1].ap, axis=0),
                                          in_offset=None, bounds_check=bcreg[cap], oob_is_err=False, _reads=[SLOT.res])
                        P.barrier()
                    if stages == "D3":
                        dbg_s = nc.dram_tensor("dbg_SLOT", [128, NT, NE], I32, kind="ExternalOutput").ap()
                        dbg_g = nc.dram_tensor("dbg_GM", [128, NT, NE], F32, kind="ExternalOutput").ap()
                        P.dma(sp, dbg_s[:, :, :], SLOT[:])
                        P.dma(sp, dbg_g[:, :, :], GM[:])
                        P.barrier()
                        return nc, cn, scr

                    with ExitStack() as sD4:
                        Wg_ = P.ring(sD4, "Wg", [128, 8, D], BF16, 2)
                        Wu_ = P.ring(sD4, "Wu", [128, 8, D], BF16, 2)
                        Wd_ = P.ring(sD4, "Wd", [128, 8, D], BF16, 2)
                        xer_ = P.ring(sD4, "xer", [128, 4, D], BF16, 2)
                        xeT_ = P.ring(sD4, "xeT", [128, 8, 512], BF16, 2)
                        hid_ = P.ring(sD4, "hid", [128, 8, 512], BF16, 2)
                        sg_ = P.ring(sD4, "sg", [128, 512], F32, 3)
                        ye_ = P.ring(sD4, "ye", [128, D], BF16, 3)
                        ne_w = 1 if lite else NE
                        for e in range(NE):
                            ew = e % ne_w
                            Wg = Wg_.next(); Wu = Wu_.next(); Wd = Wd_.next()
                            for k in range(8):
                                P.dma(pool, Wg[:, k, :], inp["e_w_gate"][l, ew, k * 128:(k + 1) * 128, :])
                                P.dma(pool, Wu[:, k, :], inp["e_w_up"][l, ew, k * 128:(k + 1) * 128, :])
                                P.dma(pool, Wd[:, k, :], inp["e_w_down"][l, ew, k * 128:(k + 1) * 128, :])
                            for (v, tl, cap, XE, YE, co) in sets:
                                for ch0 in range(0, cap, 512):
                                    ns = min(512, cap - ch0)
                                    nsub = (ns + 127) // 128
                                    pr = min(128, ns)
                                    xer = xer_.next()
                                    P.dma(sp, xer[0:pr, 0:nsub, :], XE[e][ch0:ch0 + ns, :].rearrange("(a p) d -> p a d", p=pr))
                                    xeT = xeT_.next()
                                    for a_ in range(nsub):
                                        pT = bank_bf((8, 128))
                                        for k in range(8):
                                            P.tr(pT.map(lambda a: a[:, k, 0:pr]), xer[0:pr, a_, k * 128:(k + 1) * 128], identb[0:pr, 0:pr])
                                        bcx = lambda vv: vv.map(lambda a: a.unsqueeze(2).to_broadcast([128, 8, pr]))
                                        P.I(dve, "tensor_tensor", out=xeT[:, :, a_ * 128:a_ * 128 + pr], in0=pT.map(lambda a: a[:, :, 0:pr]),
                                            in1=bcx(Amod[:, 2 + v, :]), op=ALU.mult)
                                        P.I(pool, "tensor_tensor", out=xeT[:, :, a_ * 128:a_ * 128 + pr], in0=xeT[:, :, a_ * 128:a_ * 128 + pr],
                                            in1=bcx(colv[:, v * 6 + 3, :]), op=ALU.add)
                                    hid = hid_.next()
                                    for f in range(8):
                                        bg = banks.next()
                                        bu = banks.next()
                                        for k in range(8):
                                            P.mm(bg[:, 0:ns], Wg[:, k, f * 128:(f + 1) * 128], xeT[:, k, 0:ns], start=(k == 0), stop=(k == 7))
                                        for k in range(8):
                                            P.mm(bu[:, 0:ns], Wu[:, k, f * 128:(f + 1) * 128], xeT[:, k, 0:ns], start=(k == 0), stop=(k == 7))
                                        sg = sg_.next()
                                        P.I(act, "activation", out=sg[:, 0:ns], in_=bg[:, 0:ns], func=AF.Silu)
                                        P.I(dve, "tensor_tensor", out=hid[:, f, 0:ns], in0=sg[:, 0:ns], in1=bu[:, 0:ns], op=ALU.mult)
                                    for a_ in range(nsub):
                                        ye = ye_.next()
                                        for n in range(2):
                                            bd = banks.next()
                                            for f in range(8):
                                                P.mm(bd[0:pr, :], hid[:, f, a_ * 128:a_ * 128 + pr], Wd[:, f, n * 512:(n + 1) * 512],
                                                     start=(f == 0), stop=(f == 7))
                                            if n == 0:
                                                P.I(act, "activation", out=ye[0:pr, 0:512], in_=bd[0:pr, :], func=AF.Copy)
                                            else:
                                                P.I(dve, "tensor_copy", out=ye[0:pr, 512:1024], in_=bd[0:pr, :])
                                        P.dma(sp, YE[e][ch0 + a_ * 128:ch0 + a_ * 128 + pr, :], ye[0:pr, :])
                        P.barrier()
                    with ExitStack() as sD5:
                        gt_ = P.ring(sD5, "gt", [128, D], BF16, 8)
                        for t_ in gt_.tiles:
                            P.I(pool, "memset", t_[:], 0.0, _writes=[t_.res])
                        xf_ = P.ring(sD5, "xf", [128, D], F32, 2)
                        tm5_ = P.ring(sD5, "tm5", [128, D], F32, 2)
                        st5_ = P.ring(sD5, "st5", [128, 4], F32, 2)
                        gh_ = P.ring(sD5, "gh", [128, 3, NE], F32, 2)
                        ghb_ = P.ring(sD5, "ghb", [128, NE], BF16, 2)
                        Dg_ = P.ring(sD5, "Dgd", [128, 2, NE, 128], BF16, 2)
                        jk5 = P.sb(sD5, "jk5", [128, D], BF16)
                        fgb = P.sb(sD5, "fgb", [128, D], F32)
                        P.dma(sp, fgb[:], inp["final_g"].unsqueeze(0).partition_broadcast(128))
                        idb = identf[:].map(lambda a: a.unsqueeze(1).to_broadcast([128, NE, 128]))
                        for (v, tl, cap, XE, YE, co) in sets:
                            for i in tl:
                                gh = gh_.next()
                                ghb = ghb_.next()
                                P.I(dve, "tensor_copy", out=ghb[:], in_=GM[:, i, :])
                                P.I(dve, "tensor_copy", out=gh[:, 0, :], in_=ghb[:])
                                P.I(dve, "tensor_tensor", out=gh[:, 1, :], in0=GM[:, i, :], in1=gh[:, 0, :], op=ALU.subtract)
                                Dgd = Dg_.next()
                                for q_ in range(2):
                                    P.I(dve if q_ == 0 else pool, "tensor_tensor", out=Dgd[:, q_, :, :], in0=idb,
                                        in1=gh[:, q_, :].map(lambda a: a.unsqueeze(2).to_broadcast([128, NE, 128])), op=ALU.mult)
                                ab = [banks.next(), banks.next()]
                                for e in range(NE):
                                    gt = gt_.next()
                                    P.dma(pool, gt[:], YE[e][:, :], meth="indirect_dma_start", out_offset=None,
                                          in_offset=bass.IndirectOffsetOnAxis(ap=SLOT[:, i, e:e + 1].ap, axis=0),
                                          bounds_check=bcreg[cap], oob_is_err=False, _reads=[SLOT.res])
                                    for q_ in range(2):
                                        for n in range(2):
                                            P.mm(ab[n][:, :], Dgd[:, q_, e, :], gt[:, n * 512:(n + 1) * 512],
                                                 start=(e == 0 and q_ == 0), stop=(e == NE - 1 and q_ == 1))
                                xf = xf_.next()
                                P.dma(sp, xf[:], XR[i * 128:(i + 1) * 128, :])
                                tm = tm5_.next()
                                for n in range(2):
                                    P.I(dve, "tensor_tensor", out=tm[:, n * 512:(n + 1) * 512], in0=ab[n][:, :],
                                        in1=Gb[:, 2 + v, n * 512:(n + 1) * 512], op=ALU.mult)
                                P.I(dve, "tensor_tensor", out=xf[:], in0=xf[:], in1=tm[:], op=ALU.add)
                                if not last:
                                    P.dma(sp, XR[i * 128:(i + 1) * 128, :], xf[:])
                                elif i >= 2:
                                    st = st5_.next()
                                    P.I(act, "activation", out=jk5[:], in_=xf[:], func=AF.Square, accum_out=st[:, 0:1])
                                    P.I(act, "activation", out=st[:, 1:2], in_=st[:, 0:1], func=AF.Sqrt, scale=1.0 / D, bias=epsc[:])
                                    P.I(dve, "reciprocal", out=st[:, 2:3], in_=st[:, 1:2])
                                    P.I(dve, "scalar_tensor_tensor", out=xf[:], in0=xf[:], scalar=st[:, 2:3], in1=fgb[:],
                                        op0=ALU.mult, op1=ALU.mult)
                                    P.dma(sp, out[(i - 2) * 128:(i - 1) * 128, :], xf[:])
                        P.barrier()
    return nc, cn, scr


_CACHE = {}


def kernel(**inputs):
    nb = inputs["x"].shape[0]
    if "nc" not in _CACHE:
        _CACHE["nc"] = build(nlayers=DEPTH, dbg=False)
    nc, cn, _ = _CACHE["nc"]
    shared = {n: np.ascontiguousarray(inputs[n], dtype=np.float32) for n, _ in WNAMES}
    shared["c_ctx"] = np.ascontiguousarray(inputs["c_ctx"], dtype=np.float32)
    for n, a in cn.items():
        shared["k_" + n] = a
    in_maps = []
    for b in range(nb):
        m = dict(shared)
        m["x"] = np.ascontiguousarray(inputs["x"][b], dtype=np.float32)
        m["c"] = np.ascontiguousarray(inputs["c"][b], dtype=np.float32)
        m["ctx"] = np.ascontiguousarray(inputs["ctx"][b], dtype=np.float32)
        in_maps.append(m)
    res = run_bass_kernel_spmd(nc, in_maps, core_ids=list(range(nb)))
    return np.stack([np.asarray(r["out"], dtype=np.float32) for r in res.results], axis=0)
```

```python
import os
import numpy as np
import ml_dtypes
from contextlib import ExitStack
import concourse.bass as bass
import concourse.mybir as mybir
from concourse.bass_utils import run_bass_kernel_spmd

F32 = mybir.dt.float32
BF16 = mybir.dt.bfloat16
I32 = mybir.dt.int32
U32 = mybir.dt.uint32
AF = mybir.ActivationFunctionType
ALU = mybir.AluOpType
AX = mybir.AxisListType

D = 1024
SEQ = 8192
CTX = 256
DEPTH = 4
NT = (SEQ + CTX) // 128
TOK = SEQ + CTX
INC = 1968
NE = 16
CAPX = 1024
CAPC = 32
EPS = 1e-6


class Res:
    __slots__ = ("name", "w", "r", "excl", "multi", "wl")

    def __init__(self, name, excl=False, multi=False):
        self.name = name
        self.w = None
        self.r = []
        self.excl = excl
        self.multi = multi
        self.wl = []


class V:
    __slots__ = ("ap", "res")

    def __init__(self, ap, res):
        self.ap = ap
        self.res = res

    def map(self, fn):
        return V(fn(self.ap), self.res)


class Tile:
    def __init__(self, t, name):
        self.t = t
        self.res = Res(name)

    def __getitem__(self, idx):
        return V(self.t[idx], self.res)


class Eng:
    def __init__(self, name, h, sem):
        self.name = name
        self.h = h
        self.sem = sem
        self.count = 0
        self.seen = {}


def _ap(x):
    return x.ap if isinstance(x, V) else x


class Prog:
    WRITE_KEYS = ("out", "accum_out", "out_max", "out_indices")

    def __init__(self, nc, es, n_dma_sems=56):
        self.nc = nc
        self.es = es
        mk = lambda n: es.enter_context(nc.semaphore(n))
        self.pe = Eng("pe", nc.tensor, mk("s_pe"))
        self.act = Eng("act", nc.scalar, mk("s_act"))
        self.dve = Eng("dve", nc.vector, mk("s_dve"))
        self.pool = Eng("pool", nc.gpsimd, mk("s_pool"))
        self.sp = Eng("sp", nc.sync, mk("s_sp"))
        self.engs = [self.pe, self.act, self.dve, self.pool, self.sp]
        self.dsems = [[mk("s_d%d" % i), 0] for i in range(n_dma_sems)]
        self.dnext = 0
        self.ninst = 0

    def sb(self, es, name, shape, dt):
        self.nalloc = getattr(self, "nalloc", 0) + 1
        return Tile(es.enter_context(self.nc.sbuf_tensor("t%d_%s" % (self.nalloc, name), list(shape), dt)), name)

    def ps(self, es, name, shape, dt=F32):
        self.nalloc = getattr(self, "nalloc", 0) + 1
        t = Tile(es.enter_context(self.nc.psum_tensor("p%d_%s" % (self.nalloc, name), list(shape), dt)), name)
        t.res.excl = True
        return t

    def ring(self, es, name, shape, dt, n, psum=False):
        f = self.ps if psum else self.sb
        return Ring([f(es, "%s_%d" % (name, i), shape, dt) for i in range(n)])

    def _wait(self, E, tok):
        kind, key, val = tok
        if kind == "eng":
            sem = key.sem
            k = ("e", key.name)
        else:
            sem = self.dsems[key][0]
            k = ("d", key)
        if E.seen.get(k, 0) >= val:
            return
        E.h.wait_ge(sem, val)
        E.seen[k] = val
        self.ninst += 1

    def _deps(self, E, reads, writes):
        toks = []
        for r in reads:
            if r is not None and r.w is not None:
                toks.append(r.w)
            if r is not None and r.excl:
                toks.extend(r.r)
            if r is not None and r.multi:
                toks.extend(r.wl)
        for w in writes:
            if w is None:
                continue
            if w.w is not None and not w.multi:
                toks.append(w.w)
            toks.extend(w.r)
        for t in toks:
            if t[0] == "eng" and t[1] is E:
                continue
            self._wait(E, t)
        if E is not self.pe:
            for r in reads:
                if r is not None and r.w is not None and r.w[0] == "eng" and r.w[1] is E:
                    self._wait(E, r.w)

    def _commit(self, tok, reads, writes):
        for r in reads:
            if r is not None:
                r.r.append(tok)
        for w in writes:
            if w is not None:
                if w.multi:
                    w.wl.append(tok)
                    continue
                w.w = tok
                w.r = []

    def I(self, E, meth, *args, **kw):
        reads, writes = [], []
        for k, v in kw.items():
            if isinstance(v, V):
                (writes if k in self.WRITE_KEYS else reads).append(v.res)
        for v in args:
            if isinstance(v, V):
                reads.append(v.res)
        extra_r = kw.pop("_reads", ())
        extra_w = kw.pop("_writes", ())
        reads.extend(extra_r)
        writes.extend(extra_w)
        self._deps(E, reads, writes)
        ins = getattr(E.h, meth)(*[_ap(a) for a in args], **{k: _ap(v) for k, v in kw.items()})
        E.count += 1
        ins.then_inc(E.sem, 1)
        self.ninst += 1
        self._commit(("eng", E, E.count), reads, writes)
        return ins

    def dma(self, Q, out, in_, meth="dma_start", **kw):
        reads = [in_.res] if isinstance(in_, V) else []
        writes = [out.res] if isinstance(out, V) else []
        for k, v in kw.items():
            if isinstance(v, V):
                reads.append(v.res)
        reads.extend(kw.pop("_reads", ()))
        writes.extend(kw.pop("_writes", ()))
        self._deps(Q, reads, writes)
        i = self.dnext
        self.dnext = (self.dnext + 1) % len(self.dsems)
        sem, val = self.dsems[i]
        if val > 0:
            self._wait(Q, ("dma", i, val))
        val += 16
        self.dsems[i][1] = val
        ins = getattr(Q.h, meth)(out=_ap(out), in_=_ap(in_), **{k: _ap(v) for k, v in kw.items()})
        ins.then_inc(sem, 16)
        self.ninst += 1
        self._commit(("dma", i, val), reads, writes)
        return ins

    def barrier(self):
        toks = [("eng", e, e.count) for e in self.engs if e.count > 0]
        toks += [("dma", i, v) for i, (s, v) in enumerate(self.dsems) if v > 0]
        for e in self.engs:
            for t in toks:
                if t[0] == "eng" and t[1] is e:
                    continue
                self._wait(e, t)

    def mm(self, out, lhsT, rhs, start=True, stop=True):
        return self.I(self.pe, "matmul", out=out, lhsT=lhsT, rhs=rhs, start=start, stop=stop)

    def tr(self, out, in_, ident):
        return self.I(self.pe, "transpose", out=out, in_=in_, identity=ident)


class Ring:
    def __init__(self, tiles):
        self.tiles = tiles
        self.i = 0

    def next(self):
        t = self.tiles[self.i]
        self.i = (self.i + 1) % len(self.tiles)
        return t


def _consts():
    c = {}
    c["identb"] = np.eye(128, dtype=np.float32).astype(ml_dtypes.bfloat16)
    c["identf"] = np.eye(128, dtype=np.float32)
    k = np.arange(64)
    ang = 2 * np.pi * np.outer(k, k) / 64.0
    C64 = np.cos(ang) / 8.0
    S64 = -np.sin(ang) / 8.0
    bdc = np.zeros((128, 128)); bds = np.zeros((128, 128))
    for g in range(2):
        bdc[g * 64:(g + 1) * 64, g * 64:(g + 1) * 64] = C64
        bds[g * 64:(g + 1) * 64, g * 64:(g + 1) * 64] = S64
    c["bdc"] = bdc.astype(np.float32).astype(ml_dtypes.bfloat16)
    c["bds"] = bds.astype(np.float32).astype(ml_dtypes.bfloat16)
    t = np.arange(SEQ)
    row = (t // 64).astype(np.float64); col = (t % 64).astype(np.float64)
    inv = 10000.0 ** (-np.arange(8) / 8.0)
    ang = np.concatenate([row[:, None] * inv, col[:, None] * inv], -1)
    bf = lambda a: a.astype(np.float32).astype(ml_dtypes.bfloat16)
    k1 = np.arange(128); a1 = 2 * np.pi * np.outer(k1, k1) / 128.0
    c["f_c1"] = bf(np.cos(a1) / np.sqrt(128.0)); c["f_s1"] = bf(np.sin(a1) / np.sqrt(128.0))
    c["f_ns1"] = bf(-np.sin(a1) / np.sqrt(128.0))
    t2 = np.arange(64); atw = 2 * np.pi * np.outer(k1, t2) / 8192.0
    c["f_twc"] = np.cos(atw).astype(np.float32); c["f_tws"] = np.sin(atw).astype(np.float32)
    a2 = 2 * np.pi * np.outer(t2, t2) / 64.0
    c["f_c2"] = bf(np.cos(a2) / 8.0); c["f_s2"] = bf(np.sin(a2) / 8.0)
    tc = np.arange(256); ac = 2 * np.pi * np.outer(tc, tc) / 256.0
    c["f_cc"] = bf(np.cos(ac) / 16.0); c["f_sc"] = bf(np.sin(ac) / 16.0)
    ii_ = np.arange(128)
    c["utri"] = bf((ii_[:, None] < ii_[None, :]).astype(np.float32))
    c["onesb"] = bf(np.ones((128, 128)))
    capv = np.zeros((128, 32), np.float32); capv[:, 0:16] = CAPC; capv[:, 16:32] = CAPX
    c["capv"] = capv
    c["jrev"] = np.eye(128, dtype=np.float32)[::-1].copy()
    ii = np.arange(128)
    c["trif"] = (ii[:, None] <= ii[None, :]).astype(np.float32)
    c["trib"] = (ii[:, None] >= ii[None, :]).astype(np.float32)
    c["rcos"] = np.cos(ang).astype(np.float32)
    c["rsin"] = np.sin(ang).astype(np.float32)
    return c


WNAMES = [("ada_w", [DEPTH, D, 6 * D]), ("ada_b", [DEPTH, 6 * D]), ("norm1_g", [DEPTH, D]),
          ("norm2_g", [DEPTH, D]), ("w_in", [DEPTH, D, INC]), ("m_conv_w", [DEPTH, 3, 512]),
          ("m_conv_b", [DEPTH, 512]), ("m_ib", [DEPTH, 2, 4]), ("m_fb", [DEPTH, 2, 4]),
          ("m_norm_g", [DEPTH, 256]), ("a_qnorm_g", [DEPTH, 384]), ("a_wq_up", [DEPTH, 384, 768]),
          ("a_kvnorm_g", [DEPTH, 256]), ("a_wkv_up", [DEPTH, 256, 1024]), ("w_out", [DEPTH, D, D]),
          ("router_w", [DEPTH, D, NE]), ("e_w_gate", [DEPTH, NE, D, D]), ("e_w_up", [DEPTH, NE, D, D]),
          ("e_w_down", [DEPTH, NE, D, D]), ("final_g", [D])]


def build(nlayers=DEPTH, dbg=False, tiles=None, stages=None, lite=False, heads=None, qgroups=None):
    nc = bass.Bass("TRN2", target_bir_lowering=False)
    tiles = list(range(NT)) if tiles is None else tiles
    inp = {}
    inp["x"] = nc.dram_tensor("x", [SEQ, D], F32, kind="ExternalInput").ap()
    inp["c"] = nc.dram_tensor("c", [D], F32, kind="ExternalInput").ap()
    inp["ctx"] = nc.dram_tensor("ctx", [CTX, D], F32, kind="ExternalInput").ap()
    inp["c_ctx"] = nc.dram_tensor("c_ctx", [D], F32, kind="ExternalInput").ap()
    for n, shp in WNAMES:
        shp = list(shp)
        if n != "final_g":
            shp[0] = nlayers
        if lite and n.startswith("e_w_"):
            shp[1] = 1
        inp[n] = nc.dram_tensor(n, shp, F32, kind="ExternalInput").ap()
    cn = _consts()
    for n, a in cn.items():
        inp[n] = nc.dram_tensor("k_" + n, list(a.shape), BF16 if a.dtype == ml_dtypes.bfloat16 else F32,
                                kind="ExternalInput").ap()
    out = nc.dram_tensor("out", [SEQ, D], F32, kind="ExternalOutput").ap()
    skind = "ExternalOutput" if dbg else "Internal"
    scr = {}

    def scratch(name, shape, dt):
        scr[name] = nc.dram_tensor(name, shape, dt, kind=skind).ap()
        return scr[name]

    XR = scratch("XR", [TOK, D], F32)
    MOD = scratch("MOD", [DEPTH, 2, 6 * D], F32)
    FAB = scratch("FAB", [TOK, 512], BF16)
    QKT = scratch("QKT", [512, TOK], F32)
    VOG = scratch("VOG", [TOK, 528], F32)
    QT = scratch("QT", [8, 96, TOK], BF16)
    KT = scratch("KT", [8, 96, TOK], BF16)
    VA = scratch("VA", [TOK, 8, 65], BF16)
    MIXT = scratch("MIXT", [D, TOK], BF16)
    HFB = scratch("HFB", [2, TOK, 256], F32)
    FY = scratch("FY", [128, 64, 512], BF16)
    XN2 = scratch("XN2", [TOK, D], BF16)
    XEx = [scratch("XEx%d" % e, [CAPX, D], BF16) for e in range(NE)]
    XEc = [scratch("XEc%d" % e, [CAPC, D], BF16) for e in range(NE)]
    YEx = [scratch("YEx%d" % e, [CAPX, D], BF16) for e in range(NE)]
    YEc = [scratch("YEc%d" % e, [CAPC, D], BF16) for e in range(NE)]

    es = ExitStack()
    with es:
        P = Prog(nc, es)
        pe, act, dve, pool, sp = P.pe, P.act, P.dve, P.pool, P.sp
        bcreg = {}
        for cap_ in (CAPC, CAPX):
            bcreg[cap_] = nc.gpsimd.alloc_register("bc%d" % cap_)
            nc.gpsimd.reg_mov(bcreg[cap_], cap_ - 1)
        identb = P.sb(es, "identb", [128, 128], BF16)
        identf = P.sb(es, "identf", [128, 128], F32)
        P.dma(sp, identb[:], inp["identb"][:, :])
        P.dma(sp, identf[:], inp["identf"][:, :])
        banks = P.ring(es, "bank", [128, 512], F32, 5, psum=True)
        accb = P.ring(es, "accb", [128, 512], F32, 3, psum=True)
        epsc = P.sb(es, "epsc", [128, 1], F32)
        onec = P.sb(es, "onec", [128, 128], F32)
        P.I(dve, "memset", onec[:], 1.0, _writes=[onec.res])
        jrev = P.sb(es, "jrev", [128, 128], F32)
        trif = P.sb(es, "trif", [128, 128], F32)
        trib = P.sb(es, "trib", [128, 128], F32)
        P.dma(sp, jrev[:], inp["jrev"][:, :])
        P.dma(sp, trif[:], inp["trif"][:, :])
        P.dma(sp, trib[:], inp["trib"][:, :])
        P.I(dve, "memset", epsc[:], EPS, _writes=[epsc.res])

        def bank_bf(shape3):
            b = banks.next()
            v = b[:].map(lambda a: a.bitcast(BF16))
            if shape3 is not None:
                v = v.map(lambda a: a[:, 0:shape3[0] * shape3[1]].rearrange("p (a b) -> p a b", b=shape3[1]))
            return v

        P.dma(sp, XR[0:CTX, :], inp["ctx"][:, :])
        for q in range(4):
            P.dma(sp, XR[CTX + q * 2048:CTX + (q + 1) * 2048, :], inp["x"][q * 2048:(q + 1) * 2048, :])

        with ExitStack() as sa:
            cc = P.sb(sa, "cc", [128, 2, 8], F32)
            cs = P.sb(sa, "cs", [128, 2, 8], F32)
            P.dma(sp, cc[:, 0, :], inp["c"].rearrange("(p k) -> p k", k=8))
            P.dma(sp, cc[:, 1, :], inp["c_ctx"].rearrange("(p k) -> p k", k=8))
            P.I(act, "activation", out=cs[:], in_=cc[:], func=AF.Silu)
            adab = P.sb(sa, "adab", [1, 6 * D], F32)
            awr = P.ring(sa, "aw", [128, 8, 512], F32, 3)
            mrow = P.ring(sa, "mrow", [1, 512], F32, 4)
            for l in range(nlayers):
                P.dma(sp, adab[:], inp["ada_b"][l:l + 1, :])
                awv = inp["ada_w"][l].rearrange("(p k) n -> p k n", k=8)
                for j in range(12):
                    aw = awr.next()
                    P.dma(sp if j % 2 == 0 else pool, aw[:], awv[:, :, j * 512:(j + 1) * 512])
                    for v in range(2):
                        b = banks.next()
                        for k in range(8):
                            P.mm(b[0:1, :], cs[:, v, k:k + 1], aw[:, k, :], start=(k == 0), stop=(k == 7))
                        mr = mrow.next()
                        P.I(dve, "tensor_tensor", out=mr[:], in0=b[0:1, :], in1=adab[:, j * 512:(j + 1) * 512],
                            op=ALU.add)
                        P.dma(sp, MOD[l, v:v + 1, j * 512:(j + 1) * 512], mr[:])
            P.barrier()
        if stages == "A":
            P.barrier()
            return nc, cn, scr

        for l in range(nlayers):
            with ExitStack() as sl:
                colv = P.sb(sl, "colv", [128, 12, 8], F32)
                for v in range(2):
                    P.dma(sp, colv[:, v * 6:(v + 1) * 6, :],
                          MOD[l, v, :].rearrange("(s k p) -> p s k", p=128, k=8), allow_slow_non_contiguous=True)
                n1g = P.sb(sl, "n1g", [128, 8], F32)
                n2g = P.sb(sl, "n2g", [128, 8], F32)
                P.dma(sp, n1g[:], inp["norm1_g"][l].rearrange("(k p) -> p k", p=128), allow_slow_non_contiguous=True)
                P.dma(sp, n2g[:], inp["norm2_g"][l].rearrange("(k p) -> p k", p=128), allow_slow_non_contiguous=True)
                Amod = P.sb(sl, "Amod", [128, 4, 8], F32)
                for v in range(2):
                    P.I(dve, "scalar_tensor_tensor", out=Amod[:, v, :], in0=colv[:, v * 6 + 1, :], scalar=1.0,
                        in1=n1g[:], op0=ALU.add, op1=ALU.mult)
                    P.I(dve, "scalar_tensor_tensor", out=Amod[:, 2 + v, :], in0=colv[:, v * 6 + 4, :], scalar=1.0,
                        in1=n2g[:], op0=ALU.add, op1=ALU.mult)
                qg = P.sb(sl, "qg", [128, 3], F32)
                kvg = P.sb(sl, "kvg", [128, 2], F32)
                P.dma(sp, qg[:], inp["a_qnorm_g"][l].rearrange("(k p) -> p k", p=128), allow_slow_non_contiguous=True)
                P.dma(sp, kvg[:], inp["a_kvnorm_g"][l].rearrange("(k p) -> p k", p=128), allow_slow_non_contiguous=True)

                with ExitStack() as sB:
                    Win = P.sb(sB, "Win", [128, 8, INC], BF16)
                    for k in range(8):
                        P.dma(pool, Win[:, k, :], inp["w_in"][l, k * 128:(k + 1) * 128, :])
                    Wq = P.sb(sB, "Wq", [128, 3, 768], BF16)
                    P.dma(pool, Wq[:], inp["a_wq_up"][l].rearrange("(k p) n -> p k n", p=128))
                    Wkv = P.sb(sB, "Wkv", [128, 2, 1024], BF16)
                    P.dma(pool, Wkv[:], inp["a_wkv_up"][l].rearrange("(k p) n -> p k n", p=128))
                    bdc = P.sb(sB, "bdc", [128, 128], BF16)
                    bds = P.sb(sB, "bds", [128, 128], BF16)
                    P.dma(sp, bdc[:], inp["bdc"][:, :])
                    P.dma(sp, bds[:], inp["bds"][:, :])
                    xr_ = P.ring(sB, "xt", [128, D], F32, 2)
                    junk = P.sb(sB, "junk", [128, D], BF16)
                    st_ = P.ring(sB, "st", [128, 8], F32, 3)
                    xn_ = P.ring(sB, "xn", [128, D], BF16, 2)
                    hT_ = P.ring(sB, "hT", [128, 8, 128], BF16, 2)
                    ub_ = P.ring(sB, "ub", [128, 1040], F32, 2)
                    fb_ = P.ring(sB, "fb", [128, 256], BF16, 2)
                    fT_ = P.ring(sB, "fT", [128, 2, 128], BF16, 2)
                    ab_ = P.ring(sB, "ab", [128, 512], BF16, 2)
                    qkT_ = P.ring(sB, "qkT", [128, 4, 128], F32, 2)
                    cqn_ = P.ring(sB, "cqn", [128, 640], BF16, 2)
                    cT_ = P.ring(sB, "cT", [128, 5, 128], BF16, 2)
                    kr_ = P.ring(sB, "kr", [128, 32], F32, 2)
                    qs_ = P.ring(sB, "qs", [128, 8, 96], F32, 2)
                    rt_ = P.ring(sB, "rt", [128, 4, 8, 16], F32, 2)
                    cs_ = P.ring(sB, "cs", [128, 2, 16], F32, 2)
                    qb_ = P.ring(sB, "qb", [128, 8, 96], BF16, 2)
                    kb_ = P.ring(sB, "kb", [128, 8, 96], BF16, 2)
                    va_ = P.ring(sB, "va", [128, 8, 65], BF16, 2)
                    for t_ in va_.tiles:
                        P.I(dve, "memset", t_[:], 1.0, _writes=[t_.res])
                    qT_ = P.ring(sB, "qT", [96, 8, 128], BF16, 2)
                    kT_ = P.ring(sB, "kT", [96, 8, 128], BF16, 2)

                    for i in tiles:
                        isx = i >= 2
                        v = 0 if isx else 1
                        tok0 = i * 128
                        xt = xr_.next()
                        P.dma(sp, xt[:], XR[tok0:tok0 + 128, :])
                        st = st_.next()
                        P.I(act, "activation", out=junk[:], in_=xt[:], func=AF.Square, accum_out=st[:, 0:1])
                        P.I(act, "activation", out=st[:, 1:2], in_=st[:, 0:1], func=AF.Sqrt, scale=1.0 / D, bias=epsc[:])
                        P.I(dve, "reciprocal", out=st[:, 2:3], in_=st[:, 1:2])
                        xn = xn_.next()
                        P.I(dve, "tensor_scalar", out=xn[:], in0=xt[:], scalar1=st[:, 2:3], scalar2=None, op0=ALU.mult)
                        pT = bank_bf((8, 128))
                        for k in range(8):
                            P.tr(pT.map(lambda a: a[:, k, :]), xn[:, k * 128:(k + 1) * 128], identb[:])
                        hT = hT_.next()
                        bc = lambda vv: vv.map(lambda a: a.unsqueeze(2).to_broadcast([128, 8, 128]))
                        P.I(dve, "tensor_tensor", out=hT[:], in0=pT, in1=bc(Amod[:, v, :]), op=ALU.mult)
                        P.I(pool, "tensor_tensor", out=hT[:], in0=hT[:], in1=bc(colv[:, v * 6 + 0, :]), op=ALU.add)
                        ub = [banks.next() for _ in range(4)]
                        for n in range(4):
                            lo, hi = n * 512, min(INC, (n + 1) * 512)
                            for k in range(8):
                                P.mm(ub[n][:, 0:hi - lo], hT[:, k, :], Win[:, k, lo:hi], start=(k == 0), stop=(k == 7))
                        fb = fb_.next()
                        P.I(act, "activation", out=fb[:], in_=ub[0][:, 0:256], func=AF.Copy)
                        ubuf = ub_.next()
                        P.I(act, "activation", out=ubuf[:, 0:256], in_=ub[0][:, 256:512], func=AF.Copy)
                        P.I(dve, "tensor_copy", out=ubuf[:, 256:768], in_=ub[1][:, :])
                        P.I(act, "activation", out=ubuf[:, 768:1040], in_=ub[2][:, 0:272], func=AF.Copy)
                        P.I(act, "activation", out=junk[:, 0:240], in_=ub[2][:, 272:512], func=AF.Square,
                            accum_out=st[:, 3:4])
                        P.I(act, "activation", out=junk[:, 240:384], in_=ub[3][:, 0:144], func=AF.Square,
                            accum_out=st[:, 4:5])
                        P.I(act, "activation", out=junk[:, 384:640], in_=ub[3][:, 144:400], func=AF.Square,
                            accum_out=st[:, 5:6])
                        P.I(dve, "tensor_tensor", out=st[:, 3:4], in0=st[:, 3:4], in1=st[:, 4:5], op=ALU.add)
                        P.I(act, "activation", out=st[:, 4:5], in_=st[:, 3:4], func=AF.Sqrt, scale=1.0 / 384, bias=epsc[:])
                        P.I(dve, "reciprocal", out=st[:, 6:7], in_=st[:, 4:5])
                        P.I(act, "activation", out=st[:, 3:4], in_=st[:, 5:6], func=AF.Sqrt, scale=1.0 / 256, bias=epsc[:])
                        P.I(dve, "reciprocal", out=st[:, 7:8], in_=st[:, 3:4])
                        cqn = cqn_.next()
                        P.I(dve, "tensor_scalar", out=cqn[:, 0:240], in0=ub[2][:, 272:512], scalar1=st[:, 6:7],
                            scalar2=None, op0=ALU.mult)
                        P.I(dve, "tensor_scalar", out=cqn[:, 240:384], in0=ub[3][:, 0:144], scalar1=st[:, 6:7],
                            scalar2=None, op0=ALU.mult)
                        P.I(dve, "tensor_scalar", out=cqn[:, 384:640], in0=ub[3][:, 144:400], scalar1=st[:, 7:8],
                            scalar2=None, op0=ALU.mult)
                        kr = kr_.next()
                        P.I(act, "activation", out=kr[:], in_=ub[3][:, 400:432], func=AF.Copy)
                        P.dma(sp, VOG[tok0:tok0 + 128, :], ubuf[:, 512:1040])
                        pf = bank_bf((2, 128))
                        for cc in range(2):
                            P.tr(pf.map(lambda a: a[:, cc, :]), fb[:, cc * 128:(cc + 1) * 128], identb[:])
                        fT = fT_.next()
                        P.I(act, "activation", out=fT[:], in_=pf, func=AF.Copy)
                        abp = banks.next()
                        for cc in range(2):
                            P.mm(abp[:, cc * 128:(cc + 1) * 128], fT[:, cc, :], bdc[:])
                            P.mm(abp[:, 256 + cc * 128:256 + (cc + 1) * 128], fT[:, cc, :], bds[:])
                        ab = ab_.next()
                        P.I(act, "activation", out=ab[:], in_=abp[:], func=AF.Copy)
                        P.dma(sp, FAB[tok0:tok0 + 128, :], ab[:])
                        pq = banks.next()
                        for cc in range(4):
                            P.tr(pq[:, cc * 128:(cc + 1) * 128], ubuf[:, cc * 128:(cc + 1) * 128], identf[:])
                        qkT = qkT_.next()
                        P.I(dve, "tensor_copy", out=qkT[:], in_=pq[:].map(lambda a: a.rearrange("p (c t) -> p c t", t=128)))
                        P.dma(sp, QKT.rearrange("(c p) t -> p c t", p=128)[:, :, tok0:tok0 + 128], qkT[:])
                        pc = bank_bf((5, 128))
                        for cc in range(5):
                            P.tr(pc.map(lambda a: a[:, cc, :]), cqn[:, cc * 128:(cc + 1) * 128], identb[:])
                        cT = cT_.next()
                        P.I(dve, "tensor_tensor", out=cT[:, 0:3, :], in0=pc.map(lambda a: a[:, 0:3, :]),
                            in1=qg[:].map(lambda a: a.unsqueeze(2).to_broadcast([128, 3, 128])), op=ALU.mult)
                        P.I(dve, "tensor_tensor", out=cT[:, 3:5, :], in0=pc.map(lambda a: a[:, 3:5, :]),
                            in1=kvg[:].map(lambda a: a.unsqueeze(2).to_broadcast([128, 2, 128])), op=ALU.mult)
                        qp = [banks.next(), banks.next()]
                        for n, (lo, hi) in enumerate([(0, 512), (512, 768)]):
                            for kk in range(3):
                                P.mm(qp[n][:, 0:hi - lo], cT[:, kk, :], Wq[:, kk, lo:hi], start=(kk == 0), stop=(kk == 2))
                        kvp = [banks.next(), banks.next()]
                        for n in range(2):
                            for kk in range(2):
                                P.mm(kvp[n][:, :], cT[:, 3 + kk, :], Wkv[:, kk, n * 512:(n + 1) * 512],
                                     start=(kk == 0), stop=(kk == 1))
                        qs = qs_.next()
                        sc = 96.0 ** -0.5
                        qsf = qs[:].map(lambda a: a.rearrange("p h e -> p (h e)"))
                        P.I(act, "activation", out=qsf.map(lambda a: a[:, 0:512]), in_=qp[0][:, :], func=AF.Copy, scale=sc)
                        P.I(act, "activation", out=qsf.map(lambda a: a[:, 512:768]), in_=qp[1][:, 0:256], func=AF.Copy, scale=sc)
                        qb = qb_.next()
                        kb = kb_.next()
                        va = va_.next()
                        P.I(act, "activation", out=qb[:, :, 0:64], in_=qs[:, :, 0:64], func=AF.Copy)
                        if isx:
                            cst = cs_.next()
                            P.dma(sp, cst[:, 0, :], inp["rcos"][tok0 - CTX:tok0 - CTX + 128, :])
                            P.dma(sp, cst[:, 1, :], inp["rsin"][tok0 - CTX:tok0 - CTX + 128, :])
                            rt = rt_.next()
                            cb = cst[:, 0, :].map(lambda a: a.unsqueeze(1).to_broadcast([128, 8, 16]))
                            sbn = cst[:, 1, :].map(lambda a: a.unsqueeze(1).to_broadcast([128, 8, 16]))
                            P.I(dve, "tensor_tensor", out=rt[:, 0], in0=qs[:, :, 64:80], in1=cb, op=ALU.mult)
                            P.I(dve, "tensor_tensor", out=rt[:, 1], in0=qs[:, :, 80:96], in1=sbn, op=ALU.mult)
                            P.I(pool, "tensor_tensor", out=rt[:, 2], in0=qs[:, :, 64:80], in1=sbn, op=ALU.mult)
                            P.I(pool, "tensor_tensor", out=rt[:, 3], in0=qs[:, :, 80:96], in1=cb, op=ALU.mult)
                            P.I(dve, "tensor_tensor", out=qb[:, :, 64:80], in0=rt[:, 0], in1=rt[:, 1], op=ALU.subtract)
                            P.I(dve, "tensor_tensor", out=qb[:, :, 80:96], in0=rt[:, 2], in1=rt[:, 3], op=ALU.add)
                            P.I(dve, "tensor_tensor", out=rt[:, 0, 0, :], in0=kr[:, 0:16], in1=cst[:, 0, :], op=ALU.mult)
                            P.I(dve, "tensor_tensor", out=rt[:, 1, 0, :], in0=kr[:, 16:32], in1=cst[:, 1, :], op=ALU.mult)
                            P.I(dve, "tensor_tensor", out=rt[:, 2, 0, :], in0=kr[:, 0:16], in1=cst[:, 1, :], op=ALU.mult)
                            P.I(dve, "tensor_tensor", out=rt[:, 3, 0, :], in0=kr[:, 16:32], in1=cst[:, 0, :], op=ALU.mult)
                            P.I(dve, "tensor_tensor", out=kr[:, 0:16], in0=rt[:, 0, 0, :], in1=rt[:, 1, 0, :], op=ALU.subtract)
                            P.I(dve, "tensor_tensor", out=kr[:, 16:32], in0=rt[:, 2, 0, :], in1=rt[:, 3, 0, :], op=ALU.add)
                        else:
                            P.I(act, "activation", out=qb[:, :, 64:96], in_=qs[:, :, 64:96], func=AF.Copy)
                        kv3 = lambda n: kvp[n][:, :].map(lambda a: a.rearrange("p (h e) -> p h e", e=128))
                        for n in range(2):
                            P.I(act, "activation", out=kb[:, n * 4:(n + 1) * 4, 0:64], in_=kv3(n).map(lambda a: a[:, :, 0:64]),
                                func=AF.Copy)
                            P.I(dve, "tensor_copy", out=va[:, n * 4:(n + 1) * 4, 0:64], in_=kv3(n).map(lambda a: a[:, :, 64:128]))
                        P.I(pool, "tensor_copy", out=kb[:, :, 64:96],
                            in_=kr[:].map(lambda a: a.unsqueeze(1).to_broadcast([128, 8, 32])))
                        P.dma(sp, VA[tok0:tok0 + 128, :, :], va[:])
                        for (src, ring_, dst) in ((qb, qT_, QT), (kb, kT_, KT)):
                            pt = banks.next()
                            ptv = pt[0:96, :].map(lambda a: a.bitcast(BF16)[:, 0:1024].rearrange("p (h t) -> p h t", t=128))
                            for h in range(8):
                                P.tr(ptv.map(lambda a: a[:, h, :]), src[:, h, :], identb[:])
                            tt = ring_.next()
                            P.I(act, "activation", out=tt[:], in_=ptv, func=AF.Copy)
                            P.dma(sp, dst.rearrange("h r t -> r h t")[:, :, tok0:tok0 + 128], tt[:])
                    P.barrier()
                if stages == "B":
                    return nc, cn, scr

                with_ctx = l < DEPTH - 1
                with ExitStack() as sC:
                    KTh_ = P.ring(sC, "KTh", [96, TOK], BF16, 2)
                    VAh_ = P.ring(sC, "VAh", [128, NT, 65], BF16, 2)
                    QTg_ = P.ring(sC, "QTg", [96, 512], BF16, 4)
                    PT_ = P.ring(sC, "PT", [128, 512], BF16, 6)
                    Usb_ = P.ring(sC, "Usb", [65, 512], F32, 2)
                    rec_ = P.ring(sC, "rec", [64, 512], F32, 2)
                    yc_ = P.ring(sC, "yc", [64, 512], BF16, 2)
                    esel = P.sb(sC, "esel", [65, 64], F32)
                    P.I(dve, "memset", esel[:], 0.0, _writes=[esel.res])
                    P.I(dve, "memset", esel[64:65, :], 1.0, _writes=[esel.res])
                    hlist = list(heads if heads is not None else range(8))
                    groups = [(CTX + g * 512, 512, list(range(NT))) for g in range(16)]
                    if qgroups is not None:
                        groups = [groups[g] for g in qgroups]
                    if with_ctx:
                        groups.append((0, 256, [0, 1]))
                    units = []
                    for h in hlist:
                        for gi, (q0, nq, ktiles) in enumerate(groups):
                            for jj, j in enumerate(ktiles):
                                units.append((h, gi, q0, nq, jj, j, len(ktiles)))
                    hd = {}
                    qt = {}
                    accs = {}
                    sbk = {}

                    def ensure_head(h):
                        if h in hd or h not in hlist:
                            return
                        KTh = KTh_.next()
                        VAh = VAh_.next()
                        P.dma(sp, KTh[:], KT[h, :, :])
                        for j0 in range(0, NT, 11):
                            P.dma(sp, VAh[:, j0:j0 + 11, :],
                                  VA[j0 * 128:(j0 + 11) * 128, h, :].rearrange("(j p) e -> p j e", p=128))
                        hd[h] = (KTh, VAh)

                    def ensure_q(h, gi):
                        if (h, gi) in qt or h not in hlist or gi >= len(groups):
                            return
                        q0, nq, _ = groups[gi]
                        QTg = QTg_.next()
                        P.dma(sp, QTg[:, 0:nq], QT[h, :, q0:q0 + nq])
                        qt[(h, gi)] = QTg

                    def finalize(h, gi):
                        q0, nq, _ = groups[gi]
                        acc = accs.pop((h, gi))
                        Usb = Usb_.next()
                        P.I(dve, "tensor_copy", out=Usb[:, 0:nq], in_=acc[0:65, 0:nq])
                        rp = banks.next()
                        P.mm(rp[0:64, 0:nq], esel[:], Usb[:, 0:nq])
                        rec = rec_.next()
                        P.I(dve, "reciprocal", out=rec[:, 0:nq], in_=rp[0:64, 0:nq])
                        yc = yc_.next()
                        P.I(dve, "tensor_tensor", out=yc[:, 0:nq], in0=Usb[0:64, 0:nq], in1=rec[:, 0:nq], op=ALU.mult)
                        P.dma(sp, MIXT[512 + h * 64:512 + (h + 1) * 64, q0:q0 + nq], yc[:, 0:nq])

                    LOOK = 2
                    pending = []
                    for idx in range(len(units) + LOOK + 4):
                        if idx < len(units):
                            h, gi, q0, nq, jj, j, nk = units[idx]
                            if jj == 0:
                                ensure_head(h)
                                ensure_q(h, gi)
                                if gi == 0:
                                    nh = hlist.index(h) + 1
                                    if nh < len(hlist):
                                        ensure_head(hlist[nh])
                                if gi + 1 < len(groups):
                                    ensure_q(h, gi + 1)
                                else:
                                    nh = hlist.index(h) + 1
                                    if nh < len(hlist):
                                        ensure_q(hlist[nh], 0)
                            sp_ = banks.next()
                            P.mm(sp_[:, 0:nq], hd[h][0][:, j * 128:(j + 1) * 128], qt[(h, gi)][:, 0:nq])
                            sbk[idx] = sp_
                        while pending and pending[0][0] <= idx:
                            _, h_, gi_ = pending.pop(0)
                            finalize(h_, gi_)
                        k = idx - LOOK
                        if 0 <= k < len(units):
                            h, gi, q0, nq, jj, j, nk = units[k]
                            sp_ = sbk.pop(k)
                            if jj == 0:
                                accs[(h, gi)] = accb.next()
                            acc = accs[(h, gi)]
                            PT = PT_.next()
                            P.I(act, "activation", out=PT[:, 0:nq], in_=sp_[:, 0:nq], func=AF.Exp)
                            P.mm(acc[0:65, 0:nq], hd[h][1][:, j, :], PT[:, 0:nq], start=(jj == 0), stop=(jj == nk - 1))
                            if jj == nk - 1:
                                pending.append((idx + 2, h, gi))
                                qt.pop((h, gi), None)
                        while pending and pending[0][0] <= idx:
                            _, h_, gi_ = pending.pop(0)
                            finalize(h_, gi_)
                    assert not pending and not accs
                    P.barrier()
                if stages == "C":
                    return nc, cn, scr

                def rtile(i):
                    return 1 - i if i < 2 else 67 - i

                def qkcol(i):
                    return i * 128 + (2 if i < 2 else 4)

                if os.environ.get("M2CUT") == "-1":
                    P.barrier()
                    return nc, cn, scr

                with ExitStack() as sM:
                    TKS = P.sb(sM, "TKS", [128, NT, 16], F32)
                    ECB = P.sb(sM, "ECB", [128, 8, NT], F32)
                    with ExitStack() as s2:
                        GI = P.sb(s2, "GI", [8, TOK], F32)
                        GF = P.sb(s2, "GF", [8, TOK], F32)
                        MM = P.sb(s2, "MM", [8, TOK], F32)
                        MST = P.sb(s2, "MST", [16, TOK], F32)
                        EF = P.sb(s2, "EF", [40, TOK], F32)
                        gall = P.sb(s2, "gall", [128, NT, 16], F32)
                        gb = P.sb(s2, "gb", [8, 4], F32)
                        ecs = P.sb(s2, "ecs", [8, NT], F32)
                        Dg = P.sb(s2, "Dg", [8, 8, NT], F32)
                        tk_ = P.ring(s2, "tk", [128, 40], F32, 2)
                        P.I(pool, "memset", EF[:], 0.0, _writes=[EF.res])

                        if os.environ.get("M2CUT") == "-0.5":
                            P.barrier()
                            return nc, cn, scr
                        P.dma(sp, gall[:], VOG[:, 512:528].rearrange("(j p) g -> p j g", p=128))

                        if os.environ.get("M2CUT") == "-0.3":
                            P.barrier()
                            return nc, cn, scr
                        grow = P.sb(s2, "grow", [1, 16], F32)
                        P.dma(sp, grow[:, 0:8], inp["m_ib"][l:l + 1].rearrange("o d h -> o (d h)"))
                        P.dma(sp, grow[:, 8:16], inp["m_fb"][l:l + 1].rearrange("o d h -> o (d h)"))
                        pgb = banks.next()
                        P.mm(pgb[0:8, 0:1], grow[:, 0:8], onec[0:1, 0:1])
                        P.mm(pgb[0:8, 1:2], grow[:, 8:16], onec[0:1, 0:1])
                        P.I(dve, "tensor_copy", out=gb[:, 0:2], in_=pgb[0:8, 0:2])
                        P.I(dve, "tensor_scalar", out=gb[:, 2:3], in0=gb[:, 1:2], scalar1=-1.0, scalar2=None, op0=ALU.mult)

                        if os.environ.get("M2CUT") == "0":
                            P.barrier()
                            return nc, cn, scr
                        for i in range(NT):
                            pg = banks.next()
                            sk = os.environ.get("M2SKIP", "")
                            if "a" not in sk:
                                P.mm(pg[0:16, 0:128], gall[:, i, :], identf[:])
                            if "b" not in sk:
                                P.mm(pg[0:16, 128:256], gall[:, i, :], jrev[:])
                            if "c" not in sk:
                                P.I(act, "activation", out=MST[0:16, i * 128:(i + 1) * 128], in_=pg[0:16, 0:128], func=AF.Copy)
                            r_ = rtile(i)
                            if "d" not in sk:
                                P.I(dve, "tensor_copy", out=EF[0:16, r_ * 128:(r_ + 1) * 128], in_=pg[0:16, 128:256])

                        if os.environ.get("M2CUT") == "0b":
                            P.barrier()
                            return nc, cn, scr
                        P.dma(sp, GI[0:4, :], MST[0:4, :])
                        P.dma(sp, GF[0:4, :], MST[4:8, :])
                        P.dma(sp, GI[4:8, :], EF[8:12, :])
                        P.dma(sp, GF[4:8, :], EF[12:16, :])

                        if os.environ.get("M2CUT") == "1":
                            P.barrier()
                            return nc, cn, scr
                        P.I(dve, "tensor_scalar", out=GI[:], in0=GI[:], scalar1=gb[:, 0:1], scalar2=None, op0=ALU.add)
                        P.I(act, "activation", out=GF[:], in_=GF[:], func=AF.Exp, scale=-1.0, bias=gb[:, 2:3])
                        P.I(act, "activation", out=GF[:], in_=GF[:], func=AF.Ln, bias=onec[0:8, 0:1])
                        onesb = onec[0:8, 0:1].map(lambda a: a.to_broadcast([8, TOK]))
                        P.I(dve, "tensor_tensor_scan", out=GF[:], data0=onesb, data1=GF[:], initial=0.0, op0=ALU.mult, op1=ALU.add)
                        P.I(dve, "tensor_tensor", out=GI[:], in0=GI[:], in1=GF[:], op=ALU.add)
                        P.I(dve, "tensor_tensor_scan", out=MM[:], data0=onesb, data1=GI[:], initial=0.0, op0=ALU.mult, op1=ALU.max)

                        if os.environ.get("M2CUT") == "2":
                            P.barrier()
                            return nc, cn, scr
                        v3 = lambda vv: vv.map(lambda a: a.rearrange("p (c t) -> p c t", t=128))
                        P.I(pool, "memset", MST[0:8, 0:128], 0.0, _writes=[MST.res])
                        P.I(dve, "tensor_copy", out=v3(MST[0:8, :]).map(lambda a: a[:, 1:NT, :]),
                            in_=v3(MM[:]).map(lambda a: a[:, 0:NT - 1, 127:128].to_broadcast([8, NT - 1, 128])))
                        P.I(dve, "tensor_tensor", out=ecs[:], in0=v3(MST[0:8, :]).map(lambda a: a[:, :, 0]),
                            in1=v3(MM[:]).map(lambda a: a[:, :, 127]), op=ALU.subtract)
                        P.I(act, "activation", out=ecs[:], in_=ecs[:], func=AF.Exp)
                        P.I(dve, "tensor_tensor", out=GI[:], in0=GI[:], in1=MST[0:8, :], op=ALU.subtract)
                        P.I(dve, "tensor_tensor", out=GF[:], in0=GF[:], in1=MST[0:8, :], op=ALU.subtract)
                        P.I(act, "activation", out=EF[0:8, :], in_=GI[:], func=AF.Exp)
                        P.I(act, "activation", out=EF[32:40, :], in_=GF[:], func=AF.Exp)

                        if os.environ.get("M2CUT") == "3":
                            P.barrier()
                            return nc, cn, scr
                        P.I(dve, "tensor_tensor", out=Dg[:],
                            in0=ecs[:].map(lambda a: a.unsqueeze(1).to_broadcast([8, 8, NT])),
                            in1=identf[0:8, 0:8].map(lambda a: a.unsqueeze(2).to_broadcast([8, 8, NT])), op=ALU.mult)
                        pe_ = [banks.next(), banks.next()]
                        Dgf = Dg[:].map(lambda a: a.rearrange("p a c -> p (a c)"))
                        hN = 4 * NT
                        for n in range(2):
                            P.mm(pe_[n][:, 0:hN], onec[0:8, :], Dgf.map(lambda a: a[:, n * hN:(n + 1) * hN]))
                            P.I(act, "activation", out=ECB[:, n * 4:(n + 1) * 4, :].map(lambda a: a.rearrange("p a c -> p (a c)")),
                                in_=pe_[n][:, 0:hN], func=AF.Copy)
                        for i in range(NT):
                            pt = banks.next()
                            P.tr(pt[:, 0:40], EF[0:40, i * 128:(i + 1) * 128], identf[0:40, 0:40])
                            r_ = rtile(i)
                            P.tr(pt[:, 64:104], EF[0:40, r_ * 128:(r_ + 1) * 128], identf[0:40, 0:40])
                            tk = tk_.next()
                            P.I(act, "activation", out=tk[:], in_=pt[:, 64:104], func=AF.Copy)
                            P.mm(pt[:, 128:168], jrev[:], tk[:])
                            tv = TKS[:, i, :].map(lambda a: a.rearrange("p (q j) -> p q j", j=8))
                            pv = lambda c0: pt[:, c0:c0 + 64].map(lambda a: a.rearrange("p (q j) -> p q j", j=32))
                            P.I(dve, "tensor_copy", out=tv.map(lambda a: a[:, :, 0:4]), in_=pv(0).map(lambda a: a[:, :, 0:4]))
                            P.I(dve, "tensor_copy", out=tv.map(lambda a: a[:, :, 4:8]), in_=pv(128).map(lambda a: a[:, :, 4:8]))
                        P.barrier()
                    PADW = TOK + 6
                    QKb = P.sb(sM, "QKb", [128, 4, PADW], BF16)
                    ktm = P.sb(sM, "ktm", [128, NT, 256], BF16)
                    with ExitStack() as s1:
                        cw = P.sb(s1, "cw", [128, 4, 3], F32)
                        cbi = P.sb(s1, "cbi", [128, 4], F32)
                        for kk in range(3):
                            P.dma(sp, cw[:, :, kk], inp["m_conv_w"][l, kk].rearrange("(c p) -> p c", p=128),
                                  allow_slow_non_contiguous=True)
                        P.dma(sp, cbi[:], inp["m_conv_b"][l].rearrange("(c p) -> p c", p=128), allow_slow_non_contiguous=True)
                        HP = 4230
                        stg_ = P.ring(s1, "stg", [128, HP], F32, 2)
                        yb_ = P.ring(s1, "ybuf", [128, HP], F32, 2)
                        for cc in range(4):
                            rows = QKT[cc * 128:(cc + 1) * 128, :]
                            for piece in range(2):
                                stg = stg_.next()
                                yb = yb_.next()
                                if piece == 0:
                                    n = 4230
                                    P.I(pool, "memset", stg[:, 0:2], 0.0, _writes=[stg.res])
                                    P.I(pool, "memset", stg[:, 258:260], 0.0, _writes=[stg.res])
                                    P.dma(sp, stg[:, 2:258], rows[:, 0:256])
                                    P.dma(sp, stg[:, 260:4230], rows[:, 256:256 + 3970])
                                    oc0 = 1
                                else:
                                    n = 4226
                                    P.dma(sp, stg[:, 0:4224], rows[:, 4224:8448])
                                    P.I(pool, "memset", stg[:, 4224:4226], 0.0, _writes=[stg.res])
                                    oc0 = 4229
                                m = n - 2
                                P.I(dve, "tensor_scalar", out=yb[:, 0:m], in0=stg[:, 1:1 + m], scalar1=cw[:, cc, 1:2],
                                    scalar2=cbi[:, cc:cc + 1], op0=ALU.mult, op1=ALU.add)
                                P.I(dve, "scalar_tensor_tensor", out=yb[:, 0:m], in0=stg[:, 0:m], scalar=cw[:, cc, 0:1],
                                    in1=yb[:, 0:m], op0=ALU.mult, op1=ALU.add)
                                P.I(dve, "scalar_tensor_tensor", out=yb[:, 0:m], in0=stg[:, 2:2 + m], scalar=cw[:, cc, 2:3],
                                    in1=yb[:, 0:m], op0=ALU.mult, op1=ALU.add)
                                if cc < 2:
                                    P.I(act, "activation", out=yb[:, 0:m], in_=yb[:, 0:m], func=AF.Silu)
                                    P.I(pool, "tensor_scalar", out=QKb[:, cc, oc0:oc0 + m], in0=yb[:, 0:m], scalar1=0.125,
                                        scalar2=None, op0=ALU.mult)
                                else:
                                    P.I(act, "activation", out=QKb[:, cc, oc0:oc0 + m], in_=yb[:, 0:m], func=AF.Silu)
                        for i in range(NT):
                            pk = bank_bf((2, 128))
                            c0 = qkcol(i)
                            for kc in range(2):
                                P.tr(pk.map(lambda a: a[:, kc, :]), QKb[:, 2 + kc, c0:c0 + 128], identb[:])
                            P.I(act, "activation", out=ktm[:, i, :].map(lambda a: a.rearrange("p (c t) -> p c t", t=128)),
                                in_=pk, func=AF.Copy)
                        P.barrier()
                    if stages == "M1":
                        return nc, cn, scr
                    with ExitStack() as s3:
                        V1 = P.sb(s3, "V1", [128, NT, 4, 65], BF16)
                        P.I(pool, "memset", V1[:], 1.0, _writes=[V1.res])
                        for j0 in range(0, NT, 11):
                            for h in range(4):
                                P.dma(pool, V1[:, j0:j0 + 11, h, 0:64],
                                      VOG[j0 * 128:(j0 + 11) * 128, h * 64:(h + 1) * 64].rearrange("(j p) e -> p j e", p=128))
                        CN = [P.sb(s3, "CN%d" % k, [128, 65], F32) for k in range(8)]
                        CNb = [P.sb(s3, "CNb%d" % k, [128, 65], BF16) for k in range(8)]
                        for k in range(8):
                            P.I(dve, "memset", CN[k][:], 0.0, _writes=[CN[k].res])
                            P.I(dve, "memset", CNb[k][:], 0.0, _writes=[CNb[k].res])
                        Sm_ = P.ring(s3, "Sm", [128, 128], BF16, 6)
                        Sr_ = P.ring(s3, "Sr", [128, 128], BF16, 6)
                        trifb = P.sb(s3, "trifb", [128, 128], BF16)
                        tribb = P.sb(s3, "tribb", [128, 128], BF16)
                        P.I(dve, "tensor_copy", out=trifb[:], in_=trif[:])
                        P.I(dve, "tensor_copy", out=tribb[:], in_=trib[:])
                        vpp_ = P.ring(s3, "vpp", [128, 65], BF16, 6)
                        dn_ = P.ring(s3, "dn", [128, 2], F32, 6)
                        tmp_ = P.ring(s3, "ctmp", [128, 65], F32, 6)
                        Hst_ = [P.ring(s3, "Hst%d" % d_, [128, 256], F32, 3) for d_ in range(2)]
                        for c in range(NT):
                            for d_ in range(2):
                                i = c if d_ == 0 else (1 - c if c < 2 else 67 - c)
                                c0 = qkcol(i)
                                Hst = Hst_[d_].next()
                                tri = trif if d_ == 0 else trib
                                for h in range(4):
                                    k = d_ * 4 + h
                                    pb = (h % 2) * 64
                                    qv = QKb[pb:pb + 64, h // 2, c0:c0 + 128]
                                    kv_ = QKb[pb:pb + 64, 2 + h // 2, c0:c0 + 128]
                                    sps = banks.next()
                                    P.mm(sps[:, 0:128], kv_, qv)
                                    Sr = Sr_.next()
                                    P.I(act, "activation", out=Sr[:], in_=sps[:, 0:128], func=AF.Copy)
                                    Sm = Sm_.next()
                                    P.I(pool, "tensor_tensor", out=Sm[:], in0=Sr[:], in1=(trifb if d_ == 0 else tribb)[:], op=ALU.mult)
                                    vpp = vpp_.next()
                                    P.I(act, "activation", out=vpp[:], in_=V1[:, i, h, :], func=AF.Copy, scale=TKS[:, i, k:k + 1])
                                    nd = banks.next()
                                    P.mm(nd[:, 0:65], Sm[:], vpp[:], start=True, stop=False)
                                    P.mm(nd[:, 0:65], qv, CNb[k][pb:pb + 64, :], start=False, stop=True)
                                    P.mm(nd[0:64, 128:193], ktm[:, i, h * 64:(h + 1) * 64], vpp[:])
                                    tmp = tmp_.next()
                                    P.I(dve, "tensor_tensor", out=tmp[0:64, :], in0=CN[k][0:64, :],
                                        in1=nd[0:64, 128:193], op=ALU.add)
                                    P.I(act, "activation", out=CNb[k][pb:pb + 64, :], in_=tmp[0:64, :], func=AF.Copy,
                                        scale=ECB[0:64, k, c:c + 1])
                                    P.I(dve, "tensor_scalar", out=CN[k][0:64, :], in0=tmp[0:64, :],
                                        scalar1=ECB[0:64, k, c:c + 1], scalar2=None, op0=ALU.mult)
                                    dn = dn_.next()
                                    P.I(dve, "tensor_scalar", out=dn[:, 1:2], in0=nd[:, 64:65], scalar1=-1.0,
                                        scalar2=None, op0=ALU.mult)
                                    P.I(dve, "scalar_tensor_tensor", out=dn[:, 0:1], in0=dn[:, 1:2], scalar=TKS[:, i, 8 + k:9 + k],
                                        in1=nd[:, 64:65], op0=ALU.max, op1=ALU.max)
                                    P.I(dve, "reciprocal", out=dn[:, 1:2], in_=dn[:, 0:1])
                                    P.I(act, "activation", out=Hst[:, h * 64:(h + 1) * 64], in_=nd[:, 0:64], func=AF.Copy,
                                        scale=dn[:, 1:2])
                                P.dma(sp, HFB[d_, i * 128:(i + 1) * 128, :], Hst[:])
                        P.barrier()
                    if stages == "M3":
                        return nc, cn, scr
                    with ExitStack() as s4:
                        mng = P.sb(s4, "mng", [128, 256], F32)
                        P.dma(sp, mng[:], inp["m_norm_g"][l:l + 1, :].partition_broadcast(128))
                        hf_ = P.ring(s4, "hf", [128, 2, 256], F32, 2)
                        og_ = P.ring(s4, "og", [128, 256], F32, 2)
                        hs_ = P.ring(s4, "hs", [128, 256], F32, 2)
                        sq_ = P.ring(s4, "sq", [128, 256], F32, 2)
                        ms_ = P.ring(s4, "ms", [128, 8], F32, 2)
                        ybf_ = P.ring(s4, "ybf", [128, 256], BF16, 2)
                        ybT_ = P.ring(s4, "ybT", [128, 2, 128], BF16, 2)
                        for i in range(NT):
                            if i < 2 and not with_ctx:
                                continue
                            hf = hf_.next()
                            P.dma(sp, hf[:], HFB[:, i * 128:(i + 1) * 128, :].rearrange("d p e -> p d e"))
                            og = og_.next()
                            P.dma(sp, og[:], VOG[i * 128:(i + 1) * 128, 256:512])
                            hs = hs_.next()
                            P.I(dve, "tensor_tensor", out=hs[:], in0=hf[:, 0, :], in1=hf[:, 1, :], op=ALU.add)
                            sq = sq_.next()
                            P.I(pool, "tensor_tensor", out=sq[:], in0=hs[:], in1=hs[:], op=ALU.mult)
                            ms = ms_.next()
                            P.I(dve, "tensor_reduce", out=ms[:, 0:4], in_=sq[:].map(lambda a: a.rearrange("p (h e) -> p h e", e=64)),
                                axis=AX.X, op=ALU.add)
                            P.I(act, "activation", out=ms[:, 0:4], in_=ms[:, 0:4], func=AF.Sqrt, scale=1.0 / 64, bias=epsc[:])
                            P.I(dve, "reciprocal", out=ms[:, 4:8], in_=ms[:, 0:4])
                            P.I(act, "activation", out=og[:], in_=og[:], func=AF.Sigmoid)
                            h3 = lambda vv: vv.map(lambda a: a.rearrange("p (h e) -> p h e", e=64))
                            P.I(dve, "tensor_tensor", out=h3(hs[:]), in0=h3(hs[:]),
                                in1=ms[:, 4:8].map(lambda a: a.unsqueeze(2).to_broadcast([128, 4, 64])), op=ALU.mult)
                            P.I(pool, "tensor_tensor", out=hs[:], in0=hs[:], in1=mng[:], op=ALU.mult)
                            ybf = ybf_.next()
                            P.I(dve, "tensor_tensor", out=ybf[:], in0=hs[:], in1=og[:], op=ALU.mult)
                            py = bank_bf((2, 128))
                            for cc in range(2):
                                P.tr(py.map(lambda a: a[:, cc, :]), ybf[:, cc * 128:(cc + 1) * 128], identb[:])
                            ybT = ybT_.next()
                            P.I(act, "activation", out=ybT[:], in_=py, func=AF.Copy)
                            P.dma(sp, MIXT[256:512, i * 128:(i + 1) * 128].rearrange("(c p) t -> p c t", p=128), ybT[:])
                        P.barrier()
                if stages == "M":
                    return nc, cn, scr

                with ExitStack() as sF:
                    ld = lambda nm, shp, dt: P.sb(sF, nm, shp, dt)
                    c1 = ld("f_c1", [128, 128], BF16); s1t = ld("f_s1", [128, 128], BF16); ns1 = ld("f_ns1", [128, 128], BF16)
                    twc = ld("f_twc", [128, 64], F32); tws = ld("f_tws", [128, 64], F32)
                    c2 = ld("f_c2", [64, 64], BF16); s2t = ld("f_s2", [64, 64], BF16)
                    ccs = ld("f_ccs", [128, 2, 2, 256], BF16)
                    for tl, nm in ((c1, "f_c1"), (s1t, "f_s1"), (ns1, "f_ns1"), (twc, "f_twc"), (tws, "f_tws"), (c2, "f_c2"), (s2t, "f_s2")):
                        P.dma(sp, tl[:], inp[nm][:, :])
                    P.dma(sp, ccs[:, 0, :, :], inp["f_cc"].rearrange("(a p) k -> p a k", p=128))
                    P.dma(sp, ccs[:, 1, :, :], inp["f_sc"].rearrange("(a p) k -> p a k", p=128))
                    with ExitStack() as sF1:
                        X1 = P.sb(sF1, "X1", [128, 64, 512], BF16)
                        YT = P.sb(sF1, "YT", [128, 64, 512], BF16)
                        ftmp_ = P.ring(sF1, "ftmp", [128, 256], F32, 4)
                        for q in range(4):
                            P.dma(sp, X1[:, q * 16:(q + 1) * 16, :],
                                  FAB[CTX:, :].rearrange("(a b) c -> a b c", b=64)[:, q * 16:(q + 1) * 16, :])
                        for tp in range(32):
                            zr = X1[:, 2 * tp:2 * tp + 2, 0:256]
                            zi = X1[:, 2 * tp:2 * tp + 2, 256:512]
                            br = banks.next()
                            bi = banks.next()
                            P.mm(br[:, :], c1[:], zr, start=True, stop=False)
                            P.mm(br[:, :], s1t[:], zi, start=False, stop=True)
                            P.mm(bi[:, :], c1[:], zi, start=True, stop=False)
                            P.mm(bi[:, :], ns1[:], zr, start=False, stop=True)
                            for u in range(2):
                                t2_ = 2 * tp + u
                                yr = br[:, u * 256:(u + 1) * 256]
                                yi = bi[:, u * 256:(u + 1) * 256]
                                ta = ftmp_.next()
                                tb = ftmp_.next()
                                P.I(dve, "tensor_scalar", out=ta[:], in0=yi, scalar1=tws[:, t2_:t2_ + 1], scalar2=None, op0=ALU.mult)
                                P.I(dve, "scalar_tensor_tensor", out=YT[:, t2_, 0:256], in0=yr, scalar=twc[:, t2_:t2_ + 1],
                                    in1=ta[:], op0=ALU.mult, op1=ALU.add)
                                P.I(dve, "tensor_scalar", out=tb[:], in0=yr, scalar1=tws[:, t2_:t2_ + 1], scalar2=None, op0=ALU.mult)
                                P.I(dve, "scalar_tensor_tensor", out=YT[:, t2_, 256:512], in0=yi, scalar=twc[:, t2_:t2_ + 1],
                                    in1=tb[:], op0=ALU.mult, op1=ALU.subtract)
                        for q in range(4):
                            P.dma(sp, FY[:, q * 16:(q + 1) * 16, :], YT[:, q * 16:(q + 1) * 16, :])
                        P.barrier()
                    with ExitStack() as sF2:
                        y2_ = P.ring(sF2, "y2", [64, 8, 512], BF16, 3)
                        yaT = P.sb(sF2, "yaT", [128, 2, SEQ], BF16)
                        FYv = FY.rearrange("k t c -> t k c")
                        for kb in range(16):
                            y2 = y2_.next()
                            P.dma(sp, y2[:], FYv[:, kb * 8:(kb + 1) * 8, :])
                            for jc in range(2):
                                bk = banks.next()
                                for kl in range(8):
                                    P.mm(bk[:, kl * 64:(kl + 1) * 64], y2[:, kl, jc * 128:(jc + 1) * 128], c2[:], start=True, stop=False)
                                    P.mm(bk[:, kl * 64:(kl + 1) * 64], y2[:, kl, 256 + jc * 128:256 + (jc + 1) * 128], s2t[:],
                                         start=False, stop=True)
                                ov = yaT[:, jc, :].map(lambda a: a.rearrange("p (k2 k1) -> p k1 k2", k1=128)[:, kb * 8:(kb + 1) * 8, :])
                                iv = bk[:, :].map(lambda a: a.rearrange("p (kl k2) -> p kl k2", k2=64))
                                P.I(act if jc == 0 else dve, "activation" if jc == 0 else "tensor_copy", out=ov, in_=iv,
                                    **({"func": AF.Copy} if jc == 0 else {}))
                        P.dma(sp, MIXT[0:256, CTX:].rearrange("(c p) t -> p c t", p=128), yaT[:])
                        if with_ctx:
                            zc = P.sb(sF2, "zc", [128, 2, 512], BF16)
                            yc2 = P.sb(sF2, "yc2", [128, 2, 256], BF16)
                            P.dma(sp, zc[:], FAB[0:CTX, :].rearrange("(a p) c -> p a c", p=128))
                            for jc in range(2):
                                bk = banks.next()
                                for a_ in range(2):
                                    P.mm(bk[:, 0:256], zc[:, a_, jc * 128:(jc + 1) * 128], ccs[:, 0, a_, :], start=(a_ == 0), stop=False)
                                    P.mm(bk[:, 0:256], zc[:, a_, 256 + jc * 128:256 + (jc + 1) * 128], ccs[:, 1, a_, :],
                                         start=False, stop=(a_ == 1))
                                P.I(act, "activation", out=yc2[:, jc, :], in_=bk[:, 0:256], func=AF.Copy)
                            P.dma(sp, MIXT[0:256, 0:CTX].rearrange("(c p) t -> p c t", p=128), yc2[:])
                        P.barrier()
                if stages == "F":
                    return nc, cn, scr

                last = (l == nlayers - 1)
                tiles_e = [i for i in range(NT) if (i >= 2 or with_ctx)]
                sets = ([(1, [0, 1], CAPC, XEc, YEc, 0)] if with_ctx else []) + [(0, list(range(2, NT)), CAPX, XEx, YEx, 16)]
                with ExitStack() as sE:
                    AFF = P.sb(sE, "AFF", [128, NT, NE], F32)
                    GM = P.sb(sE, "GM", [128, NT, NE], F32)
                    SLOT = P.sb(sE, "SLOT", [128, NT, NE], I32)
                    Gb = P.sb(sE, "Gb", [128, 4, D], F32)
                    for v in range(2):
                        P.dma(sp, Gb[:, v, :], MOD[l, v:v + 1, 2 * D:3 * D].partition_broadcast(128))
                        P.dma(sp, Gb[:, 2 + v, :], MOD[l, v:v + 1, 5 * D:6 * D].partition_broadcast(128))
                    if not with_ctx:
                        P.I(pool, "memset", AFF[:, 0:2, :], 0.0, _writes=[AFF.res])
                    with ExitStack() as sE1:
                        Wout = P.sb(sE1, "Wout", [128, 8, D], BF16)
                        for k in range(8):
                            P.dma(pool, Wout[:, k, :], inp["w_out"][l, k * 128:(k + 1) * 128, :])
                        RW = P.sb(sE1, "RW", [128, 8, NE], BF16)
                        P.dma(pool, RW[:], inp["router_w"][l].rearrange("(k p) e -> p k e", p=128))
                        mT_ = P.ring(sE1, "mT", [128, 8, 128], BF16, 2)
                        xe_ = P.ring(sE1, "xe", [128, D], F32, 2)
                        tm_ = P.ring(sE1, "tm", [128, D], F32, 2)
                        st2_ = P.ring(sE1, "st2", [128, 8], F32, 3)
                        xn2_ = P.ring(sE1, "xn2", [128, D], BF16, 2)
                        h2_ = P.ring(sE1, "h2", [128, 8, 128], BF16, 2)
                        lg_ = P.ring(sE1, "lg", [128, NE], F32, 2)
                        jk2 = P.sb(sE1, "jk2", [128, D], BF16)
                        for i in tiles_e:
                            v = 0 if i >= 2 else 1
                            tok0 = i * 128
                            mT = mT_.next()
                            P.dma(sp, mT[:], MIXT[:, tok0:tok0 + 128].rearrange("(k p) t -> p k t", p=128))
                            xt = xe_.next()
                            P.dma(sp, xt[:], XR[tok0:tok0 + 128, :])
                            ob = [banks.next(), banks.next()]
                            for n in range(2):
                                for k in range(8):
                                    P.mm(ob[n][:, :], mT[:, k, :], Wout[:, k, n * 512:(n + 1) * 512], start=(k == 0), stop=(k == 7))
                            tm = tm_.next()
                            for n in range(2):
                                P.I(dve, "tensor_tensor", out=tm[:, n * 512:(n + 1) * 512], in0=ob[n][:, :],
                                    in1=Gb[:, v, n * 512:(n + 1) * 512], op=ALU.mult)
                            P.I(pool, "tensor_tensor", out=xt[:], in0=xt[:], in1=tm[:], op=ALU.add)
                            P.dma(sp, XR[tok0:tok0 + 128, :], xt[:])
                            st = st2_.next()
                            P.I(act, "activation", out=jk2[:], in_=xt[:], func=AF.Square, accum_out=st[:, 0:1])
                            P.I(act, "activation", out=st[:, 1:2], in_=st[:, 0:1], func=AF.Sqrt, scale=1.0 / D, bias=epsc[:])
                            P.I(dve, "reciprocal", out=st[:, 2:3], in_=st[:, 1:2])
                            xn2 = xn2_.next()
                            P.I(dve, "tensor_scalar", out=xn2[:], in0=xt[:], scalar1=st[:, 2:3], scalar2=None, op0=ALU.mult)
                            P.dma(sp, XN2[tok0:tok0 + 128, :], xn2[:])
                            pT = bank_bf((8, 128))
                            for k in range(8):
                                P.tr(pT.map(lambda a: a[:, k, :]), xn2[:, k * 128:(k + 1) * 128], identb[:])
                            h2 = h2_.next()
                            bc = lambda vv: vv.map(lambda a: a.unsqueeze(2).to_broadcast([128, 8, 128]))
                            P.I(dve, "tensor_tensor", out=h2[:], in0=pT, in1=bc(Amod[:, 2 + v, :]), op=ALU.mult)
                            P.I(pool, "tensor_tensor", out=h2[:], in0=h2[:], in1=bc(colv[:, v * 6 + 3, :]), op=ALU.add)
                            lb = banks.next()
                            for k in range(8):
                                P.mm(lb[:, 0:NE], h2[:, k, :], RW[:, k, :], start=(k == 0), stop=(k == 7))
                            lg = lg_.next()
                            P.I(dve, "tensor_reduce", out=st[:, 3:4], in_=lb[:, 0:NE], axis=AX.X, op=ALU.max)
                            P.I(dve, "tensor_scalar", out=st[:, 4:5], in0=st[:, 3:4], scalar1=-1.0, scalar2=None, op0=ALU.mult)
                            P.I(act, "activation", out=lg[:], in_=lb[:, 0:NE], func=AF.Exp, bias=st[:, 4:5], accum_out=st[:, 5:6])
                            P.I(dve, "reciprocal", out=st[:, 6:7], in_=st[:, 5:6])
                            P.I(dve, "tensor_scalar", out=AFF[:, i, :], in0=lg[:], scalar1=st[:, 6:7], scalar2=None, op0=ALU.mult)
                        P.barrier()
                    if stages == "E":
                        dbg_a = nc.dram_tensor("dbg_AFF", [128, NT, NE], F32, kind="ExternalOutput").ap()
                        P.dma(sp, dbg_a[:, :, :], AFF[:])
                        P.barrier()
                        return nc, cn, scr

                    with ExitStack() as sD2:
                        lo = P.sb(sD2, "lo", [128, 32], F32)
                        hi = P.sb(sD2, "hi", [128, 32], F32)
                        mid = P.sb(sD2, "mid", [128, 32], F32)
                        capv = P.sb(sD2, "capv", [128, 32], F32)
                        onesb = P.sb(sD2, "onesb", [128, 128], BF16)
                        utri = P.sb(sD2, "utri", [128, 128], BF16)
                        P.dma(sp, capv[:], inp["capv"][:, :])
                        P.dma(sp, onesb[:], inp["onesb"][:, :])
                        P.dma(sp, utri[:], inp["utri"][:, :])
                        P.I(dve, "memset", lo[:], 0.0, _writes=[lo.res])
                        P.I(dve, "memset", hi[:], 1.0, _writes=[hi.res])
                        cmp_ = P.sb(sD2, "cmp", [128, NT, NE], BF16)
                        pc = P.sb(sD2, "pc", [128, 32], BF16)
                        pcf = P.sb(sD2, "pcf", [128, 32], F32)
                        P.I(dve, "memset", pcf[:], 0.0, _writes=[pcf.res])
                        mge = P.sb(sD2, "mge", [128, 32], U32)
                        mlt = P.sb(sD2, "mlt", [128, 32], U32)
                        P.I(dve, "memset", pc[:], 0.0, _writes=[pc.res])
                        for it in range(34):
                            P.I(dve, "tensor_tensor", out=mid[:], in0=lo[:], in1=hi[:], op=ALU.add)
                            P.I(dve, "tensor_scalar", out=mid[:], in0=mid[:], scalar1=0.5, scalar2=None, op0=ALU.mult)
                            for (v, tl, cap, XE, YE, co) in sets:
                                nt_ = len(tl)
                                t0_ = tl[0]
                                P.I(dve, "tensor_tensor", out=cmp_[:, t0_:t0_ + nt_, :], in0=AFF[:, t0_:t0_ + nt_, :],
                                    in1=mid[:, co:co + 16].map(lambda a: a.unsqueeze(1).to_broadcast([128, nt_, NE])), op=ALU.is_gt)
                                P.I(dve, "tensor_reduce", out=pcf[:, co:co + 16],
                                    in_=cmp_[:, t0_:t0_ + nt_, :].map(lambda a: a.rearrange("p j e -> p e j")), axis=AX.X, op=ALU.add)
                            P.I(dve, "tensor_copy", out=pc[:], in_=pcf[:])
                            tb = banks.next()
                            P.mm(tb[:, 0:32], onesb[:], pc[:])
                            P.I(dve, "tensor_tensor", out=mge[:], in0=tb[:, 0:32], in1=capv[:], op=ALU.is_ge)
                            P.I(dve, "tensor_tensor", out=mlt[:], in0=tb[:, 0:32], in1=capv[:], op=ALU.is_lt)
                            P.I(dve, "copy_predicated", out=lo[:], mask=mge[:], data=mid[:])
                            P.I(dve, "copy_predicated", out=hi[:], mask=mlt[:], data=mid[:])
                        offb = P.sb(sD2, "offb", [128, NE], F32)
                        mk_ = P.ring(sD2, "mk", [128, NE], F32, 2)
                        mkb_ = P.ring(sD2, "mkb", [128, NE], BF16, 2)
                        sl_ = P.ring(sD2, "sl", [128, NE], F32, 2)
                        BIG = 1.0e6
                        for (v, tl, cap, XE, YE, co) in sets:
                            P.I(dve, "memset", offb[:], 0.0, _writes=[offb.res])
                            for i in tl:
                                mk = mk_.next()
                                P.I(dve, "tensor_tensor", out=mk[:], in0=AFF[:, i, :], in1=lo[:, co:co + 16], op=ALU.is_gt)
                                mkb = mkb_.next()
                                P.I(dve, "tensor_copy", out=mkb[:], in_=mk[:])
                                P.I(dve, "tensor_tensor", out=GM[:, i, :], in0=AFF[:, i, :], in1=mk[:], op=ALU.mult)
                                rb = banks.next()
                                P.mm(rb[:, 0:NE], utri[:], mkb[:])
                                P.mm(rb[:, NE:2 * NE], onesb[:], mkb[:])
                                sl = sl_.next()
                                P.I(dve, "tensor_tensor", out=sl[:], in0=rb[:, 0:NE], in1=offb[:], op=ALU.add)
                                P.I(dve, "tensor_tensor", out=offb[:], in0=rb[:, NE:2 * NE], in1=offb[:], op=ALU.add)
                                P.I(dve, "tensor_scalar", out=sl[:], in0=sl[:], scalar1=-BIG, scalar2=None, op0=ALU.add)
                                P.I(dve, "tensor_tensor", out=sl[:], in0=sl[:], in1=mk[:], op=ALU.mult)
                                P.I(dve, "tensor_scalar", out=SLOT[:, i, :], in0=sl[:], scalar1=BIG, scalar2=None, op0=ALU.add)
                    if stages == "D3":
                        dbg_s = nc.dram_tensor("dbg_SLOT", [128, NT, NE], I32, kind="ExternalOutput").ap()
                        dbg_g = nc.dram_tensor("dbg_GM", [128, NT, NE], F32, kind="ExternalOutput").ap()
                        P.dma(sp, dbg_s[:, :, :], SLOT[:])
                        P.dma(sp, dbg_g[:, :, :], GM[:])
                        P.barrier()
                        return nc, cn, scr

                    xeres = [Res("xe%d" % e, multi=True) for e in range(NE)]
                    with ExitStack() as sD4:
                        Wg_ = P.ring(sD4, "Wg", [128, 8, D], BF16, 2)
                        Wu_ = P.ring(sD4, "Wu", [128, 8, D], BF16, 2)
                        Wd_ = P.ring(sD4, "Wd", [128, 8, D], BF16, 2)
                        xer_ = P.ring(sD4, "xer", [128, 4, D], BF16, 2)
                        xeT_ = P.ring(sD4, "xeT", [128, 8, 512], BF16, 2)
                        hid_ = P.ring(sD4, "hid", [128, 8, 512], BF16, 2)
                        sg_ = P.ring(sD4, "sg", [128, 512], F32, 3)
                        ye_ = P.ring(sD4, "ye", [128, D], BF16, 3)
                        ne_w = 1 if lite else NE
                        xs_ = P.ring(sD4, "xs", [128, D], BF16, 3)
                        EG = 4
                        for e in range(NE):
                            ew = e % ne_w
                            if e % EG == 0:
                                for (v, tl, cap, XE, YE, co) in sets:
                                    for i in tl:
                                        xs = xs_.next()
                                        P.dma(sp, xs[:], XN2[i * 128:(i + 1) * 128, :])
                                        for e2 in range(e, e + EG):
                                            P.dma(pool, XE[e2][:, :], xs[:], meth="indirect_dma_start",
                                                  out_offset=bass.IndirectOffsetOnAxis(ap=SLOT[:, i, e2:e2 + 1].ap, axis=0),
                                                  in_offset=None, bounds_check=bcreg[cap], oob_is_err=False,
                                                  _reads=[SLOT.res], _writes=[xeres[e2]])
                            Wg = Wg_.next(); Wu = Wu_.next(); Wd = Wd_.next()
                            for k in range(8):
                                P.dma(pool, Wg[:, k, :], inp["e_w_gate"][l, ew, k * 128:(k + 1) * 128, :])
                                P.dma(pool, Wu[:, k, :], inp["e_w_up"][l, ew, k * 128:(k + 1) * 128, :])
                                P.dma(pool, Wd[:, k, :], inp["e_w_down"][l, ew, k * 128:(k + 1) * 128, :])
                            for (v, tl, cap, XE, YE, co) in sets:
                                for ch0 in range(0, cap, 512):
                                    ns = min(512, cap - ch0)
                                    nsub = (ns + 127) // 128
                                    pr = min(128, ns)
                                    xer = xer_.next()
                                    P.dma(sp, xer[0:pr, 0:nsub, :], XE[e][ch0:ch0 + ns, :].rearrange("(a p) d -> p a d", p=pr),
                                          _reads=[xeres[e]])
                                    xeT = xeT_.next()
                                    for a_ in range(nsub):
                                        pT = bank_bf((8, 128))
                                        for k in range(8):
                                            P.tr(pT.map(lambda a: a[:, k, 0:pr]), xer[0:pr, a_, k * 128:(k + 1) * 128], identb[0:pr, 0:pr])
                                        bcx = lambda vv: vv.map(lambda a: a.unsqueeze(2).to_broadcast([128, 8, pr]))
                                        P.I(dve, "tensor_tensor", out=xeT[:, :, a_ * 128:a_ * 128 + pr], in0=pT.map(lambda a: a[:, :, 0:pr]),
                                            in1=bcx(Amod[:, 2 + v, :]), op=ALU.mult)
                                        P.I(pool, "tensor_tensor", out=xeT[:, :, a_ * 128:a_ * 128 + pr], in0=xeT[:, :, a_ * 128:a_ * 128 + pr],
                                            in1=bcx(colv[:, v * 6 + 3, :]), op=ALU.add)
                                    hid = hid_.next()
                                    for f in range(8):
                                        bg = banks.next()
                                        bu = banks.next()
                                        for k in range(8):
                                            P.mm(bg[:, 0:ns], Wg[:, k, f * 128:(f + 1) * 128], xeT[:, k, 0:ns], start=(k == 0), stop=(k == 7))
                                        for k in range(8):
                                            P.mm(bu[:, 0:ns], Wu[:, k, f * 128:(f + 1) * 128], xeT[:, k, 0:ns], start=(k == 0), stop=(k == 7))
                                        sg = sg_.next()
                                        P.I(act, "activation", out=sg[:, 0:ns], in_=bg[:, 0:ns], func=AF.Silu)
                                        P.I(dve, "tensor_tensor", out=hid[:, f, 0:ns], in0=sg[:, 0:ns], in1=bu[:, 0:ns], op=ALU.mult)
                                    for a_ in range(nsub):
                                        ye = ye_.next()
                                        for n in range(2):
                                            bd = banks.next()
                                            for f in range(8):
                                                P.mm(bd[0:pr, :], hid[:, f, a_ * 128:a_ * 128 + pr], Wd[:, f, n * 512:(n + 1) * 512],
                                                     start=(f == 0), stop=(f == 7))
                                            if n == 0:
                                                P.I(act, "activation", out=ye[0:pr, 0:512], in_=bd[0:pr, :], func=AF.Copy)
                                            else:
                                                P.I(dve, "tensor_copy", out=ye[0:pr, 512:1024], in_=bd[0:pr, :])
                                        P.dma(sp, YE[e][ch0 + a_ * 128:ch0 + a_ * 128 + pr, :], ye[0:pr, :])
                        P.barrier()
                    with ExitStack() as sD5:
                        gt_ = P.ring(sD5, "gt", [128, D], BF16, 8)
                        for t_ in gt_.tiles:
                            P.I(pool, "memset", t_[:], 0.0, _writes=[t_.res])
                        xf_ = P.ring(sD5, "xf", [128, D], F32, 2)
                        tm5_ = P.ring(sD5, "tm5", [128, D], F32, 2)
                        st5_ = P.ring(sD5, "st5", [128, 4], F32, 2)
                        gh_ = P.ring(sD5, "gh", [128, 3, NE], F32, 2)
                        ghb_ = P.ring(sD5, "ghb", [128, NE], BF16, 2)
                        Dg_ = P.ring(sD5, "Dgd", [128, 2, NE, 128], BF16, 2)
                        jk5 = P.sb(sD5, "jk5", [128, D], BF16)
                        fgb = P.sb(sD5, "fgb", [128, D], F32)
                        P.dma(sp, fgb[:], inp["final_g"].unsqueeze(0).partition_broadcast(128))
                        idb = identf[:].map(lambda a: a.unsqueeze(1).to_broadcast([128, NE, 128]))
                        for (v, tl, cap, XE, YE, co) in sets:
                            for i in tl:
                                gh = gh_.next()
                                ghb = ghb_.next()
                                P.I(dve, "tensor_copy", out=ghb[:], in_=GM[:, i, :])
                                P.I(dve, "tensor_copy", out=gh[:, 0, :], in_=ghb[:])
                                P.I(dve, "tensor_tensor", out=gh[:, 1, :], in0=GM[:, i, :], in1=gh[:, 0, :], op=ALU.subtract)
                                Dgd = Dg_.next()
                                for q_ in range(2):
                                    P.I(dve if q_ == 0 else pool, "tensor_tensor", out=Dgd[:, q_, :, :], in0=idb,
                                        in1=gh[:, q_, :].map(lambda a: a.unsqueeze(2).to_broadcast([128, NE, 128])), op=ALU.mult)
                                ab = [banks.next(), banks.next()]
                                for e in range(NE):
                                    gt = gt_.next()
                                    P.dma(pool, gt[:], YE[e][:, :], meth="indirect_dma_start", out_offset=None,
                                          in_offset=bass.IndirectOffsetOnAxis(ap=SLOT[:, i, e:e + 1].ap, axis=0),
                                          bounds_check=bcreg[cap], oob_is_err=False, _reads=[SLOT.res])
                                    for q_ in range(2):
                                        for n in range(2):
                                            P.mm(ab[n][:, :], Dgd[:, q_, e, :], gt[:, n * 512:(n + 1) * 512],
                                                 start=(e == 0 and q_ == 0), stop=(e == NE - 1 and q_ == 1))
                                xf = xf_.next()
                                P.dma(sp, xf[:], XR[i * 128:(i + 1) * 128, :])
                                tm = tm5_.next()
                                for n in range(2):
                                    P.I(dve, "tensor_tensor", out=tm[:, n * 512:(n + 1) * 512], in0=ab[n][:, :],
                                        in1=Gb[:, 2 + v, n * 512:(n + 1) * 512], op=ALU.mult)
                                P.I(dve, "tensor_tensor", out=xf[:], in0=xf[:], in1=tm[:], op=ALU.add)
                                if not last:
                                    P.dma(sp, XR[i * 128:(i + 1) * 128, :], xf[:])
                                elif i >= 2:
                                    st = st5_.next()
                                    P.I(act, "activation", out=jk5[:], in_=xf[:], func=AF.Square, accum_out=st[:, 0:1])
                                    P.I(act, "activation", out=st[:, 1:2], in_=st[:, 0:1], func=AF.Sqrt, scale=1.0 / D, bias=epsc[:])
                                    P.I(dve, "reciprocal", out=st[:, 2:3], in_=st[:, 1:2])
                                    P.I(dve, "scalar_tensor_tensor", out=xf[:], in0=xf[:], scalar=st[:, 2:3], in1=fgb[:],
                                        op0=ALU.mult, op1=ALU.mult)
                                    P.dma(sp, out[(i - 2) * 128:(i - 1) * 128, :], xf[:])
                        P.barrier()
    return nc, cn, scr


_CACHE = {}


def kernel(**inputs):
    nb = inputs["x"].shape[0]
    if "nc" not in _CACHE:
        _CACHE["nc"] = build(nlayers=DEPTH, dbg=False)
    nc, cn, _ = _CACHE["nc"]
    shared = {n: np.ascontiguousarray(inputs[n], dtype=np.float32) for n, _ in WNAMES}
    shared["c_ctx"] = np.ascontiguousarray(inputs["c_ctx"], dtype=np.float32)
    for n, a in cn.items():
        shared["k_" + n] = a
    in_maps = []
    for b in range(nb):
        m = dict(shared)
        m["x"] = np.ascontiguousarray(inputs["x"][b], dtype=np.float32)
        m["c"] = np.ascontiguousarray(inputs["c"][b], dtype=np.float32)
        m["ctx"] = np.ascontiguousarray(inputs["ctx"][b], dtype=np.float32)
        in_maps.append(m)
    res = run_bass_kernel_spmd(nc, in_maps, core_ids=list(range(nb)))
    return np.stack([np.asarray(r["out"], dtype=np.float32) for r in res.results], axis=0)
```

```python
import os
import numpy as np
import ml_dtypes
from contextlib import ExitStack
import concourse.bass as bass
import concourse.mybir as mybir
from concourse.bass_utils import run_bass_kernel_spmd

F32 = mybir.dt.float32
BF16 = mybir.dt.bfloat16
I32 = mybir.dt.int32
U32 = mybir.dt.uint32
AF = mybir.ActivationFunctionType
ALU = mybir.AluOpType
AX = mybir.AxisListType

D = 1024
SEQ = 8192
CTX = 256
DEPTH = 4
NT = (SEQ + CTX) // 128
TOK = SEQ + CTX
INC = 1968
NE = 16
CAPX = 1024
CAPC = 32
EPS = 1e-6


class Res:
    __slots__ = ("name", "w", "r", "excl", "multi", "wl")

    def __init__(self, name, excl=False, multi=False):
        self.name = name
        self.w = None
        self.r = []
        self.excl = excl
        self.multi = multi
        self.wl = []


class V:
    __slots__ = ("ap", "res")

    def __init__(self, ap, res):
        self.ap = ap
        self.res = res

    def map(self, fn):
        return V(fn(self.ap), self.res)


class Tile:
    def __init__(self, t, name):
        self.t = t
        self.res = Res(name)

    def __getitem__(self, idx):
        return V(self.t[idx], self.res)


class Eng:
    def __init__(self, name, h, sem):
        self.name = name
        self.h = h
        self.sem = sem
        self.count = 0
        self.seen = {}


def _ap(x):
    return x.ap if isinstance(x, V) else x


class Prog:
    WRITE_KEYS = ("out", "accum_out", "out_max", "out_indices")

    def __init__(self, nc, es, n_dma_sems=56):
        self.nc = nc
        self.es = es
        mk = lambda n: es.enter_context(nc.semaphore(n))
        self.pe = Eng("pe", nc.tensor, mk("s_pe"))
        self.act = Eng("act", nc.scalar, mk("s_act"))
        self.dve = Eng("dve", nc.vector, mk("s_dve"))
        self.pool = Eng("pool", nc.gpsimd, mk("s_pool"))
        self.sp = Eng("sp", nc.sync, mk("s_sp"))
        self.engs = [self.pe, self.act, self.dve, self.pool, self.sp]
        self.dsems = [[mk("s_d%d" % i), 0] for i in range(n_dma_sems)]
        self.dnext = 0
        self.ninst = 0

    def sb(self, es, name, shape, dt):
        self.nalloc = getattr(self, "nalloc", 0) + 1
        return Tile(es.enter_context(self.nc.sbuf_tensor("t%d_%s" % (self.nalloc, name), list(shape), dt)), name)

    def ps(self, es, name, shape, dt=F32):
        self.nalloc = getattr(self, "nalloc", 0) + 1
        t = Tile(es.enter_context(self.nc.psum_tensor("p%d_%s" % (self.nalloc, name), list(shape), dt)), name)
        t.res.excl = True
        return t

    def ring(self, es, name, shape, dt, n, psum=False):
        f = self.ps if psum else self.sb
        return Ring([f(es, "%s_%d" % (name, i), shape, dt) for i in range(n)])

    def _wait(self, E, tok):
        kind, key, val = tok
        if kind == "eng":
            sem = key.sem
            k = ("e", key.name)
        else:
            sem = self.dsems[key][0]
            k = ("d", key)
        if E.seen.get(k, 0) >= val:
            return
        E.h.wait_ge(sem, val)
        E.seen[k] = val
        self.ninst += 1

    def _deps(self, E, reads, writes):
        toks = []
        for r in reads:
            if r is not None and r.w is not None:
                toks.append(r.w)
            if r is not None and r.excl:
                toks.extend(r.r)
            if r is not None and r.multi:
                toks.extend(r.wl)
        for w in writes:
            if w is None:
                continue
            if w.w is not None and not w.multi:
                toks.append(w.w)
            toks.extend(w.r)
        for t in toks:
            if t[0] == "eng" and t[1] is E:
                continue
            self._wait(E, t)
        if E is not self.pe:
            for r in reads:
                if r is not None and r.w is not None and r.w[0] == "eng" and r.w[1] is E:
                    self._wait(E, r.w)

    def _commit(self, tok, reads, writes):
        for r in reads:
            if r is not None:
                r.r.append(tok)
        for w in writes:
            if w is not None:
                if w.multi:
                    w.wl.append(tok)
                    continue
                w.w = tok
                w.r = []

    def I(self, E, meth, *args, **kw):
        reads, writes = [], []
        for k, v in kw.items():
            if isinstance(v, V):
                (writes if k in self.WRITE_KEYS else reads).append(v.res)
        for v in args:
            if isinstance(v, V):
                reads.append(v.res)
        extra_r = kw.pop("_reads", ())
        extra_w = kw.pop("_writes", ())
        reads.extend(extra_r)
        writes.extend(extra_w)
        self._deps(E, reads, writes)
        ins = getattr(E.h, meth)(*[_ap(a) for a in args], **{k: _ap(v) for k, v in kw.items()})
        E.count += 1
        ins.then_inc(E.sem, 1)
        self.ninst += 1
        self._commit(("eng", E, E.count), reads, writes)
        return ins

    def dma(self, Q, out, in_, meth="dma_start", **kw):
        reads = [in_.res] if isinstance(in_, V) else []
        writes = [out.res] if isinstance(out, V) else []
        for k, v in kw.items():
            if isinstance(v, V):
                reads.append(v.res)
        reads.extend(kw.pop("_reads", ()))
        writes.extend(kw.pop("_writes", ()))
        self._deps(Q, reads, writes)
        i = self.dnext
        self.dnext = (self.dnext + 1) % len(self.dsems)
        sem, val = self.dsems[i]
        if val > 0:
            self._wait(Q, ("dma", i, val))
        val += 16
        self.dsems[i][1] = val
        ins = getattr(Q.h, meth)(out=_ap(out), in_=_ap(in_), **{k: _ap(v) for k, v in kw.items()})
        ins.then_inc(sem, 16)
        self.ninst += 1
        self._commit(("dma", i, val), reads, writes)
        return ins

    def barrier(self):
        toks = [("eng", e, e.count) for e in self.engs if e.count > 0]
        toks += [("dma", i, v) for i, (s, v) in enumerate(self.dsems) if v > 0]
        for e in self.engs:
            for t in toks:
                if t[0] == "eng" and t[1] is e:
                    continue
                self._wait(e, t)

    def mm(self, out, lhsT, rhs, start=True, stop=True):
        return self.I(self.pe, "matmul", out=out, lhsT=lhsT, rhs=rhs, start=start, stop=stop)

    def tr(self, out, in_, ident):
        return self.I(self.pe, "transpose", out=out, in_=in_, identity=ident)


class Ring:
    def __init__(self, tiles):
        self.tiles = tiles
        self.i = 0

    def next(self):
        t = self.tiles[self.i]
        self.i = (self.i + 1) % len(self.tiles)
        return t


def _consts():
    c = {}
    c["identb"] = np.eye(128, dtype=np.float32).astype(ml_dtypes.bfloat16)
    c["identf"] = np.eye(128, dtype=np.float32)
    k = np.arange(64)
    ang = 2 * np.pi * np.outer(k, k) / 64.0
    C64 = np.cos(ang) / 8.0
    S64 = -np.sin(ang) / 8.0
    bdc = np.zeros((128, 128)); bds = np.zeros((128, 128))
    for g in range(2):
        bdc[g * 64:(g + 1) * 64, g * 64:(g + 1) * 64] = C64
        bds[g * 64:(g + 1) * 64, g * 64:(g + 1) * 64] = S64
    c["bdc"] = bdc.astype(np.float32).astype(ml_dtypes.bfloat16)
    c["bds"] = bds.astype(np.float32).astype(ml_dtypes.bfloat16)
    t = np.arange(SEQ)
    row = (t // 64).astype(np.float64); col = (t % 64).astype(np.float64)
    inv = 10000.0 ** (-np.arange(8) / 8.0)
    ang = np.concatenate([row[:, None] * inv, col[:, None] * inv], -1)
    bf = lambda a: a.astype(np.float32).astype(ml_dtypes.bfloat16)
    k1 = np.arange(128); a1 = 2 * np.pi * np.outer(k1, k1) / 128.0
    c["f_c1"] = bf(np.cos(a1) / np.sqrt(128.0)); c["f_s1"] = bf(np.sin(a1) / np.sqrt(128.0))
    c["f_ns1"] = bf(-np.sin(a1) / np.sqrt(128.0))
    t2 = np.arange(64); atw = 2 * np.pi * np.outer(k1, t2) / 8192.0
    c["f_twc"] = np.cos(atw).astype(np.float32); c["f_tws"] = np.sin(atw).astype(np.float32)
    a2 = 2 * np.pi * np.outer(t2, t2) / 64.0
    c["f_c2"] = bf(np.cos(a2) / 8.0); c["f_s2"] = bf(np.sin(a2) / 8.0)
    tc = np.arange(256); ac = 2 * np.pi * np.outer(tc, tc) / 256.0
    c["f_cc"] = bf(np.cos(ac) / 16.0); c["f_sc"] = bf(np.sin(ac) / 16.0)
    ii_ = np.arange(128)
    c["utri"] = bf((ii_[:, None] < ii_[None, :]).astype(np.float32))
    c["onesb"] = bf(np.ones((128, 128)))
    capv = np.zeros((128, 32), np.float32); capv[:, 0:16] = CAPC; capv[:, 16:32] = CAPX
    c["capv"] = capv
    c["jrev"] = np.eye(128, dtype=np.float32)[::-1].copy()
    ii = np.arange(128)
    c["trif"] = (ii[:, None] <= ii[None, :]).astype(np.float32)
    c["trib"] = (ii[:, None] >= ii[None, :]).astype(np.float32)
    c["rcos"] = np.cos(ang).astype(np.float32)
    c["rsin"] = np.sin(ang).astype(np.float32)
    return c


WNAMES = [("ada_w", [DEPTH, D, 6 * D]), ("ada_b", [DEPTH, 6 * D]), ("norm1_g", [DEPTH, D]),
          ("norm2_g", [DEPTH, D]), ("w_in", [DEPTH, D, INC]), ("m_conv_w", [DEPTH, 3, 512]),
          ("m_conv_b", [DEPTH, 512]), ("m_ib", [DEPTH, 2, 4]), ("m_fb", [DEPTH, 2, 4]),
          ("m_norm_g", [DEPTH, 256]), ("a_qnorm_g", [DEPTH, 384]), ("a_wq_up", [DEPTH, 384, 768]),
          ("a_kvnorm_g", [DEPTH, 256]), ("a_wkv_up", [DEPTH, 256, 1024]), ("w_out", [DEPTH, D, D]),
          ("router_w", [DEPTH, D, NE]), ("e_w_gate", [DEPTH, NE, D, D]), ("e_w_up", [DEPTH, NE, D, D]),
          ("e_w_down", [DEPTH, NE, D, D]), ("final_g", [D])]


def build(nlayers=DEPTH, dbg=False, tiles=None, stages=None, lite=False, heads=None, qgroups=None):
    nc = bass.Bass("TRN2", target_bir_lowering=False)
    tiles = list(range(NT)) if tiles is None else tiles
    inp = {}
    inp["x"] = nc.dram_tensor("x", [SEQ, D], F32, kind="ExternalInput").ap()
    inp["c"] = nc.dram_tensor("c", [D], F32, kind="ExternalInput").ap()
    inp["ctx"] = nc.dram_tensor("ctx", [CTX, D], F32, kind="ExternalInput").ap()
    inp["c_ctx"] = nc.dram_tensor("c_ctx", [D], F32, kind="ExternalInput").ap()
    for n, shp in WNAMES:
        shp = list(shp)
        if n != "final_g":
            shp[0] = nlayers
        if lite and n.startswith("e_w_"):
            shp[1] = 1
        inp[n] = nc.dram_tensor(n, shp, F32, kind="ExternalInput").ap()
    cn = _consts()
    for n, a in cn.items():
        inp[n] = nc.dram_tensor("k_" + n, list(a.shape), BF16 if a.dtype == ml_dtypes.bfloat16 else F32,
                                kind="ExternalInput").ap()
    out = nc.dram_tensor("out", [SEQ, D], F32, kind="ExternalOutput").ap()
    skind = "ExternalOutput" if dbg else "Internal"
    scr = {}

    def scratch(name, shape, dt):
        scr[name] = nc.dram_tensor(name, shape, dt, kind=skind).ap()
        return scr[name]

    XR = scratch("XR", [TOK, D], F32)
    MOD = scratch("MOD", [DEPTH, 2, 6 * D], F32)
    FAB = scratch("FAB", [TOK, 512], BF16)
    QKT = scratch("QKT", [512, TOK], F32)
    VOG = scratch("VOG", [TOK, 528], F32)
    QT = scratch("QT", [8, 96, TOK], BF16)
    KT = scratch("KT", [8, 96, TOK], BF16)
    VA = scratch("VA", [TOK, 8, 65], BF16)
    MIXT = scratch("MIXT", [D, TOK], BF16)
    HFB = scratch("HFB", [2, TOK, 256], F32)
    FY = scratch("FY", [128, 64, 512], BF16)
    XN2 = scratch("XN2", [TOK, D], BF16)
    XEx = [scratch("XEx%d" % e, [CAPX, D], BF16) for e in range(NE)]
    XEc = [scratch("XEc%d" % e, [CAPC, D], BF16) for e in range(NE)]
    YEx = [scratch("YEx%d" % e, [CAPX, D], BF16) for e in range(NE)]
    YEc = [scratch("YEc%d" % e, [CAPC, D], BF16) for e in range(NE)]

    es = ExitStack()
    with es:
        P = Prog(nc, es)
        pe, act, dve, pool, sp = P.pe, P.act, P.dve, P.pool, P.sp
        bcreg = {}
        for cap_ in (CAPC, CAPX):
            bcreg[cap_] = nc.gpsimd.alloc_register("bc%d" % cap_)
            nc.gpsimd.reg_mov(bcreg[cap_], cap_ - 1)
        identb = P.sb(es, "identb", [128, 128], BF16)
        identf = P.sb(es, "identf", [128, 128], F32)
        P.dma(sp, identb[:], inp["identb"][:, :])
        P.dma(sp, identf[:], inp["identf"][:, :])
        banks = P.ring(es, "bank", [128, 512], F32, 5, psum=True)
        accb = P.ring(es, "accb", [128, 512], F32, 3, psum=True)
        epsc = P.sb(es, "epsc", [128, 1], F32)
        onec = P.sb(es, "onec", [128, 128], F32)
        P.I(dve, "memset", onec[:], 1.0, _writes=[onec.res])
        jrev = P.sb(es, "jrev", [128, 128], F32)
        trif = P.sb(es, "trif", [128, 128], F32)
        trib = P.sb(es, "trib", [128, 128], F32)
        P.dma(sp, jrev[:], inp["jrev"][:, :])
        P.dma(sp, trif[:], inp["trif"][:, :])
        P.dma(sp, trib[:], inp["trib"][:, :])
        P.I(dve, "memset", epsc[:], EPS, _writes=[epsc.res])

        def bank_bf(shape3):
            b = banks.next()
            v = b[:].map(lambda a: a.bitcast(BF16))
            if shape3 is not None:
                v = v.map(lambda a: a[:, 0:shape3[0] * shape3[1]].rearrange("p (a b) -> p a b", b=shape3[1]))
            return v

        P.dma(sp, XR[0:CTX, :], inp["ctx"][:, :])
        for q in range(4):
            P.dma(sp, XR[CTX + q * 2048:CTX + (q + 1) * 2048, :], inp["x"][q * 2048:(q + 1) * 2048, :])

        with ExitStack() as sa:
            cc = P.sb(sa, "cc", [128, 2, 8], F32)
            cs = P.sb(sa, "cs", [128, 2, 8], F32)
            P.dma(sp, cc[:, 0, :], inp["c"].rearrange("(p k) -> p k", k=8))
            P.dma(sp, cc[:, 1, :], inp["c_ctx"].rearrange("(p k) -> p k", k=8))
            P.I(act, "activation", out=cs[:], in_=cc[:], func=AF.Silu)
            adab = P.sb(sa, "adab", [1, 6 * D], F32)
            awr = P.ring(sa, "aw", [128, 8, 512], F32, 3)
            mrow = P.ring(sa, "mrow", [1, 512], F32, 4)
            for l in range(nlayers):
                P.dma(sp, adab[:], inp["ada_b"][l:l + 1, :])
                awv = inp["ada_w"][l].rearrange("(p k) n -> p k n", k=8)
                for j in range(12):
                    aw = awr.next()
                    P.dma(sp if j % 2 == 0 else pool, aw[:], awv[:, :, j * 512:(j + 1) * 512])
                    for v in range(2):
                        b = banks.next()
                        for k in range(8):
                            P.mm(b[0:1, :], cs[:, v, k:k + 1], aw[:, k, :], start=(k == 0), stop=(k == 7))
                        mr = mrow.next()
                        P.I(dve, "tensor_tensor", out=mr[:], in0=b[0:1, :], in1=adab[:, j * 512:(j + 1) * 512],
                            op=ALU.add)
                        P.dma(sp, MOD[l, v:v + 1, j * 512:(j + 1) * 512], mr[:])
            P.barrier()
        if stages == "A":
            P.barrier()
            return nc, cn, scr

        for l in range(nlayers):
            with ExitStack() as sl:
                colv = P.sb(sl, "colv", [128, 12, 8], F32)
                for v in range(2):
                    P.dma(sp, colv[:, v * 6:(v + 1) * 6, :],
                          MOD[l, v, :].rearrange("(s k p) -> p s k", p=128, k=8), allow_slow_non_contiguous=True)
                n1g = P.sb(sl, "n1g", [128, 8], F32)
                n2g = P.sb(sl, "n2g", [128, 8], F32)
                P.dma(sp, n1g[:], inp["norm1_g"][l].rearrange("(k p) -> p k", p=128), allow_slow_non_contiguous=True)
                P.dma(sp, n2g[:], inp["norm2_g"][l].rearrange("(k p) -> p k", p=128), allow_slow_non_contiguous=True)
                Amod = P.sb(sl, "Amod", [128, 4, 8], F32)
                for v in range(2):
                    P.I(dve, "scalar_tensor_tensor", out=Amod[:, v, :], in0=colv[:, v * 6 + 1, :], scalar=1.0,
                        in1=n1g[:], op0=ALU.add, op1=ALU.mult)
                    P.I(dve, "scalar_tensor_tensor", out=Amod[:, 2 + v, :], in0=colv[:, v * 6 + 4, :], scalar=1.0,
                        in1=n2g[:], op0=ALU.add, op1=ALU.mult)
                qg = P.sb(sl, "qg", [128, 3], F32)
                kvg = P.sb(sl, "kvg", [128, 2], F32)
                P.dma(sp, qg[:], inp["a_qnorm_g"][l].rearrange("(k p) -> p k", p=128), allow_slow_non_contiguous=True)
                P.dma(sp, kvg[:], inp["a_kvnorm_g"][l].rearrange("(k p) -> p k", p=128), allow_slow_non_contiguous=True)

                with ExitStack() as sB:
                    Win = P.sb(sB, "Win", [128, 8, INC], BF16)
                    for k in range(8):
                        P.dma(pool, Win[:, k, :], inp["w_in"][l, k * 128:(k + 1) * 128, :])
                    Wq = P.sb(sB, "Wq", [128, 3, 768], BF16)
                    P.dma(pool, Wq[:], inp["a_wq_up"][l].rearrange("(k p) n -> p k n", p=128))
                    Wkv = P.sb(sB, "Wkv", [128, 2, 1024], BF16)
                    P.dma(pool, Wkv[:], inp["a_wkv_up"][l].rearrange("(k p) n -> p k n", p=128))
                    bdc = P.sb(sB, "bdc", [128, 128], BF16)
                    bds = P.sb(sB, "bds", [128, 128], BF16)
                    P.dma(sp, bdc[:], inp["bdc"][:, :])
                    P.dma(sp, bds[:], inp["bds"][:, :])
                    xr_ = P.ring(sB, "xt", [128, D], F32, 2)
                    junk = P.sb(sB, "junk", [128, D], BF16)
                    st_ = P.ring(sB, "st", [128, 8], F32, 3)
                    xn_ = P.ring(sB, "xn", [128, D], BF16, 2)
                    hT_ = P.ring(sB, "hT", [128, 8, 128], BF16, 2)
                    ub_ = P.ring(sB, "ub", [128, 1040], F32, 2)
                    fb_ = P.ring(sB, "fb", [128, 256], BF16, 2)
                    fT_ = P.ring(sB, "fT", [128, 2, 128], BF16, 2)
                    ab_ = P.ring(sB, "ab", [128, 512], BF16, 2)
                    qkT_ = P.ring(sB, "qkT", [128, 4, 128], F32, 2)
                    cqn_ = P.ring(sB, "cqn", [128, 640], BF16, 2)
                    cT_ = P.ring(sB, "cT", [128, 5, 128], BF16, 2)
                    kr_ = P.ring(sB, "kr", [128, 32], F32, 2)
                    qs_ = P.ring(sB, "qs", [128, 8, 96], F32, 2)
                    rt_ = P.ring(sB, "rt", [128, 4, 8, 16], F32, 2)
                    cs_ = P.ring(sB, "cs", [128, 2, 16], F32, 2)
                    qb_ = P.ring(sB, "qb", [128, 8, 96], BF16, 2)
                    kb_ = P.ring(sB, "kb", [128, 8, 96], BF16, 2)
                    va_ = P.ring(sB, "va", [128, 8, 65], BF16, 2)
                    for t_ in va_.tiles:
                        P.I(dve, "memset", t_[:], 1.0, _writes=[t_.res])
                    qT_ = P.ring(sB, "qT", [96, 8, 128], BF16, 2)
                    kT_ = P.ring(sB, "kT", [96, 8, 128], BF16, 2)

                    for i in tiles:
                        isx = i >= 2
                        v = 0 if isx else 1
                        tok0 = i * 128
                        xt = xr_.next()
                        P.dma(sp, xt[:], XR[tok0:tok0 + 128, :])
                        st = st_.next()
                        P.I(act, "activation", out=junk[:], in_=xt[:], func=AF.Square, accum_out=st[:, 0:1])
                        P.I(act, "activation", out=st[:, 1:2], in_=st[:, 0:1], func=AF.Sqrt, scale=1.0 / D, bias=epsc[:])
                        P.I(dve, "reciprocal", out=st[:, 2:3], in_=st[:, 1:2])
                        xn = xn_.next()
                        P.I(dve, "tensor_scalar", out=xn[:], in0=xt[:], scalar1=st[:, 2:3], scalar2=None, op0=ALU.mult)
                        pT = bank_bf((8, 128))
                        for k in range(8):
                            P.tr(pT.map(lambda a: a[:, k, :]), xn[:, k * 128:(k + 1) * 128], identb[:])
                        hT = hT_.next()
                        bc = lambda vv: vv.map(lambda a: a.unsqueeze(2).to_broadcast([128, 8, 128]))
                        P.I(dve, "tensor_tensor", out=hT[:], in0=pT, in1=bc(Amod[:, v, :]), op=ALU.mult)
                        P.I(pool, "tensor_tensor", out=hT[:], in0=hT[:], in1=bc(colv[:, v * 6 + 0, :]), op=ALU.add)
                        ub = [banks.next() for _ in range(4)]
                        for n in range(4):
                            lo, hi = n * 512, min(INC, (n + 1) * 512)
                            for k in range(8):
                                P.mm(ub[n][:, 0:hi - lo], hT[:, k, :], Win[:, k, lo:hi], start=(k == 0), stop=(k == 7))
                        fb = fb_.next()
                        P.I(act, "activation", out=fb[:], in_=ub[0][:, 0:256], func=AF.Copy)
                        ubuf = ub_.next()
                        P.I(act, "activation", out=ubuf[:, 0:256], in_=ub[0][:, 256:512], func=AF.Copy)
                        P.I(dve, "tensor_copy", out=ubuf[:, 256:768], in_=ub[1][:, :])
                        P.I(act, "activation", out=ubuf[:, 768:1040], in_=ub[2][:, 0:272], func=AF.Copy)
                        P.I(act, "activation", out=junk[:, 0:240], in_=ub[2][:, 272:512], func=AF.Square,
                            accum_out=st[:, 3:4])
                        P.I(act, "activation", out=junk[:, 240:384], in_=ub[3][:, 0:144], func=AF.Square,
                            accum_out=st[:, 4:5])
                        P.I(act, "activation", out=junk[:, 384:640], in_=ub[3][:, 144:400], func=AF.Square,
                            accum_out=st[:, 5:6])
                        P.I(dve, "tensor_tensor", out=st[:, 3:4], in0=st[:, 3:4], in1=st[:, 4:5], op=ALU.add)
                        P.I(act, "activation", out=st[:, 4:5], in_=st[:, 3:4], func=AF.Sqrt, scale=1.0 / 384, bias=epsc[:])
                        P.I(dve, "reciprocal", out=st[:, 6:7], in_=st[:, 4:5])
                        P.I(act, "activation", out=st[:, 3:4], in_=st[:, 5:6], func=AF.Sqrt, scale=1.0 / 256, bias=epsc[:])
                        P.I(dve, "reciprocal", out=st[:, 7:8], in_=st[:, 3:4])
                        cqn = cqn_.next()
                        P.I(dve, "tensor_scalar", out=cqn[:, 0:240], in0=ub[2][:, 272:512], scalar1=st[:, 6:7],
                            scalar2=None, op0=ALU.mult)
                        P.I(dve, "tensor_scalar", out=cqn[:, 240:384], in0=ub[3][:, 0:144], scalar1=st[:, 6:7],
                            scalar2=None, op0=ALU.mult)
                        P.I(dve, "tensor_scalar", out=cqn[:, 384:640], in0=ub[3][:, 144:400], scalar1=st[:, 7:8],
                            scalar2=None, op0=ALU.mult)
                        kr = kr_.next()
                        P.I(act, "activation", out=kr[:], in_=ub[3][:, 400:432], func=AF.Copy)
                        P.dma(sp, VOG[tok0:tok0 + 128, :], ubuf[:, 512:1040])
                        pf = bank_bf((2, 128))
                        for cc in range(2):
                            P.tr(pf.map(lambda a: a[:, cc, :]), fb[:, cc * 128:(cc + 1) * 128], identb[:])
                        fT = fT_.next()
                        P.I(act, "activation", out=fT[:], in_=pf, func=AF.Copy)
                        abp = banks.next()
                        for cc in range(2):
                            P.mm(abp[:, cc * 128:(cc + 1) * 128], fT[:, cc, :], bdc[:])
                            P.mm(abp[:, 256 + cc * 128:256 + (cc + 1) * 128], fT[:, cc, :], bds[:])
                        ab = ab_.next()
                        P.I(act, "activation", out=ab[:], in_=abp[:], func=AF.Copy)
                        P.dma(sp, FAB[tok0:tok0 + 128, :], ab[:])
                        pq = banks.next()
                        for cc in range(4):
                            P.tr(pq[:, cc * 128:(cc + 1) * 128], ubuf[:, cc * 128:(cc + 1) * 128], identf[:])
                        qkT = qkT_.next()
                        P.I(dve, "tensor_copy", out=qkT[:], in_=pq[:].map(lambda a: a.rearrange("p (c t) -> p c t", t=128)))
                        P.dma(sp, QKT.rearrange("(c p) t -> p c t", p=128)[:, :, tok0:tok0 + 128], qkT[:])
                        pc = bank_bf((5, 128))
                        for cc in range(5):
                            P.tr(pc.map(lambda a: a[:, cc, :]), cqn[:, cc * 128:(cc + 1) * 128], identb[:])
                        cT = cT_.next()
                        P.I(dve, "tensor_tensor", out=cT[:, 0:3, :], in0=pc.map(lambda a: a[:, 0:3, :]),
                            in1=qg[:].map(lambda a: a.unsqueeze(2).to_broadcast([128, 3, 128])), op=ALU.mult)
                        P.I(dve, "tensor_tensor", out=cT[:, 3:5, :], in0=pc.map(lambda a: a[:, 3:5, :]),
                            in1=kvg[:].map(lambda a: a.unsqueeze(2).to_broadcast([128, 2, 128])), op=ALU.mult)
                        qp = [banks.next(), banks.next()]
                        for n, (lo, hi) in enumerate([(0, 512), (512, 768)]):
                            for kk in range(3):
                                P.mm(qp[n][:, 0:hi - lo], cT[:, kk, :], Wq[:, kk, lo:hi], start=(kk == 0), stop=(kk == 2))
                        kvp = [banks.next(), banks.next()]
                        for n in range(2):
                            for kk in range(2):
                                P.mm(kvp[n][:, :], cT[:, 3 + kk, :], Wkv[:, kk, n * 512:(n + 1) * 512],
                                     start=(kk == 0), stop=(kk == 1))
                        qs = qs_.next()
                        sc = 96.0 ** -0.5
                        qsf = qs[:].map(lambda a: a.rearrange("p h e -> p (h e)"))
                        P.I(act, "activation", out=qsf.map(lambda a: a[:, 0:512]), in_=qp[0][:, :], func=AF.Copy, scale=sc)
                        P.I(act, "activation", out=qsf.map(lambda a: a[:, 512:768]), in_=qp[1][:, 0:256], func=AF.Copy, scale=sc)
                        qb = qb_.next()
                        kb = kb_.next()
                        va = va_.next()
                        P.I(act, "activation", out=qb[:, :, 0:64], in_=qs[:, :, 0:64], func=AF.Copy)
                        if isx:
                            cst = cs_.next()
                            P.dma(sp, cst[:, 0, :], inp["rcos"][tok0 - CTX:tok0 - CTX + 128, :])
                            P.dma(sp, cst[:, 1, :], inp["rsin"][tok0 - CTX:tok0 - CTX + 128, :])
                            rt = rt_.next()
                            cb = cst[:, 0, :].map(lambda a: a.unsqueeze(1).to_broadcast([128, 8, 16]))
                            sbn = cst[:, 1, :].map(lambda a: a.unsqueeze(1).to_broadcast([128, 8, 16]))
                            P.I(dve, "tensor_tensor", out=rt[:, 0], in0=qs[:, :, 64:80], in1=cb, op=ALU.mult)
                            P.I(dve, "tensor_tensor", out=rt[:, 1], in0=qs[:, :, 80:96], in1=sbn, op=ALU.mult)
                            P.I(pool, "tensor_tensor", out=rt[:, 2], in0=qs[:, :, 64:80], in1=sbn, op=ALU.mult)
                            P.I(pool, "tensor_tensor", out=rt[:, 3], in0=qs[:, :, 80:96], in1=cb, op=ALU.mult)
                            P.I(dve, "tensor_tensor", out=qb[:, :, 64:80], in0=rt[:, 0], in1=rt[:, 1], op=ALU.subtract)
                            P.I(dve, "tensor_tensor", out=qb[:, :, 80:96], in0=rt[:, 2], in1=rt[:, 3], op=ALU.add)
                            P.I(dve, "tensor_tensor", out=rt[:, 0, 0, :], in0=kr[:, 0:16], in1=cst[:, 0, :], op=ALU.mult)
                            P.I(dve, "tensor_tensor", out=rt[:, 1, 0, :], in0=kr[:, 16:32], in1=cst[:, 1, :], op=ALU.mult)
                            P.I(dve, "tensor_tensor", out=rt[:, 2, 0, :], in0=kr[:, 0:16], in1=cst[:, 1, :], op=ALU.mult)
                            P.I(dve, "tensor_tensor", out=rt[:, 3, 0, :], in0=kr[:, 16:32], in1=cst[:, 0, :], op=ALU.mult)
                            P.I(dve, "tensor_tensor", out=kr[:, 0:16], in0=rt[:, 0, 0, :], in1=rt[:, 1, 0, :], op=ALU.subtract)
                            P.I(dve, "tensor_tensor", out=kr[:, 16:32], in0=rt[:, 2, 0, :], in1=rt[:, 3, 0, :], op=ALU.add)
                        else:
                            P.I(act, "activation", out=qb[:, :, 64:96], in_=qs[:, :, 64:96], func=AF.Copy)
                        kv3 = lambda n: kvp[n][:, :].map(lambda a: a.rearrange("p (h e) -> p h e", e=128))
                        for n in range(2):
                            P.I(act, "activation", out=kb[:, n * 4:(n + 1) * 4, 0:64], in_=kv3(n).map(lambda a: a[:, :, 0:64]),
                                func=AF.Copy)
                            P.I(dve, "tensor_copy", out=va[:, n * 4:(n + 1) * 4, 0:64], in_=kv3(n).map(lambda a: a[:, :, 64:128]))
                        P.I(pool, "tensor_copy", out=kb[:, :, 64:96],
                            in_=kr[:].map(lambda a: a.unsqueeze(1).to_broadcast([128, 8, 32])))
                        P.dma(sp, VA[tok0:tok0 + 128, :, :], va[:])
                        for (src, ring_, dst) in ((qb, qT_, QT), (kb, kT_, KT)):
                            pt = banks.next()
                            ptv = pt[0:96, :].map(lambda a: a.bitcast(BF16)[:, 0:1024].rearrange("p (h t) -> p h t", t=128))
                            for h in range(8):
                                P.tr(ptv.map(lambda a: a[:, h, :]), src[:, h, :], identb[:])
                            tt = ring_.next()
                            P.I(act, "activation", out=tt[:], in_=ptv, func=AF.Copy)
                            P.dma(sp, dst.rearrange("h r t -> r h t")[:, :, tok0:tok0 + 128], tt[:])
                    P.barrier()
                if stages == "B":
                    return nc, cn, scr

                with_ctx = l < DEPTH - 1
                with ExitStack() as sC:
                    KTh_ = P.ring(sC, "KTh", [96, TOK], BF16, 2)
                    VAh_ = P.ring(sC, "VAh", [128, NT, 65], BF16, 2)
                    QTg_ = P.ring(sC, "QTg", [96, 512], BF16, 4)
                    PT_ = P.ring(sC, "PT", [128, 512], BF16, 6)
                    Usb_ = P.ring(sC, "Usb", [65, 512], F32, 2)
                    rec_ = P.ring(sC, "rec", [64, 512], F32, 2)
                    yc_ = P.ring(sC, "yc", [64, 512], BF16, 2)
                    esel = P.sb(sC, "esel", [65, 64], F32)
                    P.I(dve, "memset", esel[:], 0.0, _writes=[esel.res])
                    P.I(dve, "memset", esel[64:65, :], 1.0, _writes=[esel.res])
                    hlist = list(heads if heads is not None else range(8))
                    groups = [(CTX + g * 512, 512, list(range(NT))) for g in range(16)]
                    if qgroups is not None:
                        groups = [groups[g] for g in qgroups]
                    if with_ctx:
                        groups.append((0, 256, [0, 1]))
                    units = []
                    for h in hlist:
                        for gi, (q0, nq, ktiles) in enumerate(groups):
                            for jj, j in enumerate(ktiles):
                                units.append((h, gi, q0, nq, jj, j, len(ktiles)))
                    hd = {}
                    qt = {}
                    accs = {}
                    sbk = {}

                    def ensure_head(h):
                        if h in hd or h not in hlist:
                            return
                        KTh = KTh_.next()
                        VAh = VAh_.next()
                        P.dma(sp, KTh[:], KT[h, :, :])
                        for j0 in range(0, NT, 11):
                            P.dma(sp, VAh[:, j0:j0 + 11, :],
                                  VA[j0 * 128:(j0 + 11) * 128, h, :].rearrange("(j p) e -> p j e", p=128))
                        hd[h] = (KTh, VAh)

                    def ensure_q(h, gi):
                        if (h, gi) in qt or h not in hlist or gi >= len(groups):
                            return
                        q0, nq, _ = groups[gi]
                        QTg = QTg_.next()
                        P.dma(sp, QTg[:, 0:nq], QT[h, :, q0:q0 + nq])
                        qt[(h, gi)] = QTg

                    def finalize(h, gi):
                        q0, nq, _ = groups[gi]
                        acc = accs.pop((h, gi))
                        Usb = Usb_.next()
                        P.I(dve, "tensor_copy", out=Usb[:, 0:nq], in_=acc[0:65, 0:nq])
                        rp = banks.next()
                        P.mm(rp[0:64, 0:nq], esel[:], Usb[:, 0:nq])
                        rec = rec_.next()
                        P.I(dve, "reciprocal", out=rec[:, 0:nq], in_=rp[0:64, 0:nq])
                        yc = yc_.next()
                        P.I(dve, "tensor_tensor", out=yc[:, 0:nq], in0=Usb[0:64, 0:nq], in1=rec[:, 0:nq], op=ALU.mult)
                        P.dma(sp, MIXT[512 + h * 64:512 + (h + 1) * 64, q0:q0 + nq], yc[:, 0:nq])

                    LOOK = 2
                    pending = []
                    for idx in range(len(units) + LOOK + 4):
                        if idx < len(units):
                            h, gi, q0, nq, jj, j, nk = units[idx]
                            if jj == 0:
                                ensure_head(h)
                                ensure_q(h, gi)
                                if gi == 0:
                                    nh = hlist.index(h) + 1
                                    if nh < len(hlist):
                                        ensure_head(hlist[nh])
                                if gi + 1 < len(groups):
                                    ensure_q(h, gi + 1)
                                else:
                                    nh = hlist.index(h) + 1
                                    if nh < len(hlist):
                                        ensure_q(hlist[nh], 0)
                            sp_ = banks.next()
                            P.mm(sp_[:, 0:nq], hd[h][0][:, j * 128:(j + 1) * 128], qt[(h, gi)][:, 0:nq])
                            sbk[idx] = sp_
                        while pending and pending[0][0] <= idx:
                            _, h_, gi_ = pending.pop(0)
                            finalize(h_, gi_)
                        k = idx - LOOK
                        if 0 <= k < len(units):
                            h, gi, q0, nq, jj, j, nk = units[k]
                            sp_ = sbk.pop(k)
                            if jj == 0:
                                accs[(h, gi)] = accb.next()
                            acc = accs[(h, gi)]
                            PT = PT_.next()
                            P.I(act, "activation", out=PT[:, 0:nq], in_=sp_[:, 0:nq], func=AF.Exp)
                            P.mm(acc[0:65, 0:nq], hd[h][1][:, j, :], PT[:, 0:nq], start=(jj == 0), stop=(jj == nk - 1))
                            if jj == nk - 1:
                                pending.append((idx + 2, h, gi))
                                qt.pop((h, gi), None)
                        while pending and pending[0][0] <= idx:
                            _, h_, gi_ = pending.pop(0)
                            finalize(h_, gi_)
                    assert not pending and not accs
                    P.barrier()
                if stages == "C":
                    return nc, cn, scr

                def rtile(i):
                    return 1 - i if i < 2 else 67 - i

                def qkcol(i):
                    return i * 128 + (2 if i < 2 else 4)

                if os.environ.get("M2CUT") == "-1":
                    P.barrier()
                    return nc, cn, scr

                with ExitStack() as sM:
                    TKS = P.sb(sM, "TKS", [128, NT, 16], F32)
                    ECB = P.sb(sM, "ECB", [128, 8, NT], F32)
                    with ExitStack() as s2:
                        GI = P.sb(s2, "GI", [8, TOK], F32)
                        GF = P.sb(s2, "GF", [8, TOK], F32)
                        MM = P.sb(s2, "MM", [8, TOK], F32)
                        MST = P.sb(s2, "MST", [16, TOK], F32)
                        EF = P.sb(s2, "EF", [40, TOK], F32)
                        gall = P.sb(s2, "gall", [128, NT, 16], F32)
                        gb = P.sb(s2, "gb", [8, 4], F32)
                        ecs = P.sb(s2, "ecs", [8, NT], F32)
                        Dg = P.sb(s2, "Dg", [8, 8, NT], F32)
                        tk_ = P.ring(s2, "tk", [128, 40], F32, 2)
                        P.I(pool, "memset", EF[:], 0.0, _writes=[EF.res])

                        if os.environ.get("M2CUT") == "-0.5":
                            P.barrier()
                            return nc, cn, scr
                        P.dma(sp, gall[:], VOG[:, 512:528].rearrange("(j p) g -> p j g", p=128))

                        if os.environ.get("M2CUT") == "-0.3":
                            P.barrier()
                            return nc, cn, scr
                        grow = P.sb(s2, "grow", [1, 16], F32)
                        P.dma(sp, grow[:, 0:8], inp["m_ib"][l:l + 1].rearrange("o d h -> o (d h)"))
                        P.dma(sp, grow[:, 8:16], inp["m_fb"][l:l + 1].rearrange("o d h -> o (d h)"))
                        pgb = banks.next()
                        P.mm(pgb[0:8, 0:1], grow[:, 0:8], onec[0:1, 0:1])
                        P.mm(pgb[0:8, 1:2], grow[:, 8:16], onec[0:1, 0:1])
                        P.I(dve, "tensor_copy", out=gb[:, 0:2], in_=pgb[0:8, 0:2])
                        P.I(dve, "tensor_scalar", out=gb[:, 2:3], in0=gb[:, 1:2], scalar1=-1.0, scalar2=None, op0=ALU.mult)

                        if os.environ.get("M2CUT") == "0":
                            P.barrier()
                            return nc, cn, scr
                        for i in range(NT):
                            pg = banks.next()
                            sk = os.environ.get("M2SKIP", "")
                            if "a" not in sk:
                                P.mm(pg[0:16, 0:128], gall[:, i, :], identf[:])
                            if "b" not in sk:
                                P.mm(pg[0:16, 128:256], gall[:, i, :], jrev[:])
                            if "c" not in sk:
                                P.I(act, "activation", out=MST[0:16, i * 128:(i + 1) * 128], in_=pg[0:16, 0:128], func=AF.Copy)
                            r_ = rtile(i)
                            if "d" not in sk:
                                P.I(dve, "tensor_copy", out=EF[0:16, r_ * 128:(r_ + 1) * 128], in_=pg[0:16, 128:256])

                        if os.environ.get("M2CUT") == "0b":
                            P.barrier()
                            return nc, cn, scr
                        P.dma(sp, GI[0:4, :], MST[0:4, :])
                        P.dma(sp, GF[0:4, :], MST[4:8, :])
                        P.dma(sp, GI[4:8, :], EF[8:12, :])
                        P.dma(sp, GF[4:8, :], EF[12:16, :])

                        if os.environ.get("M2CUT") == "1":
                            P.barrier()
                            return nc, cn, scr
                        P.I(dve, "tensor_scalar", out=GI[:], in0=GI[:], scalar1=gb[:, 0:1], scalar2=None, op0=ALU.add)
                        P.I(act, "activation", out=GF[:], in_=GF[:], func=AF.Exp, scale=-1.0, bias=gb[:, 2:3])
                        P.I(act, "activation", out=GF[:], in_=GF[:], func=AF.Ln, bias=onec[0:8, 0:1])
                        onesb = onec[0:8, 0:1].map(lambda a: a.to_broadcast([8, TOK]))
                        P.I(dve, "tensor_tensor_scan", out=GF[:], data0=onesb, data1=GF[:], initial=0.0, op0=ALU.mult, op1=ALU.add)
                        P.I(dve, "tensor_tensor", out=GI[:], in0=GI[:], in1=GF[:], op=ALU.add)
                        P.I(dve, "tensor_tensor_scan", out=MM[:], data0=onesb, data1=GI[:], initial=0.0, op0=ALU.mult, op1=ALU.max)

                        if os.environ.get("M2CUT") == "2":
                            P.barrier()
                            return nc, cn, scr
                        v3 = lambda vv: vv.map(lambda a: a.rearrange("p (c t) -> p c t", t=128))
                        P.I(pool, "memset", MST[0:8, 0:128], 0.0, _writes=[MST.res])
                        P.I(dve, "tensor_copy", out=v3(MST[0:8, :]).map(lambda a: a[:, 1:NT, :]),
                            in_=v3(MM[:]).map(lambda a: a[:, 0:NT - 1, 127:128].to_broadcast([8, NT - 1, 128])))
                        P.I(dve, "tensor_tensor", out=ecs[:], in0=v3(MST[0:8, :]).map(lambda a: a[:, :, 0]),
                            in1=v3(MM[:]).map(lambda a: a[:, :, 127]), op=ALU.subtract)
                        P.I(act, "activation", out=ecs[:], in_=ecs[:], func=AF.Exp)
                        P.I(dve, "tensor_tensor", out=GI[:], in0=GI[:], in1=MST[0:8, :], op=ALU.subtract)
                        P.I(dve, "tensor_tensor", out=GF[:], in0=GF[:], in1=MST[0:8, :], op=ALU.subtract)
                        P.I(act, "activation", out=EF[0:8, :], in_=GI[:], func=AF.Exp)
                        P.I(act, "activation", out=EF[32:40, :], in_=GF[:], func=AF.Exp)

                        if os.environ.get("M2CUT") == "3":
                            P.barrier()
                            return nc, cn, scr
                        P.I(dve, "tensor_tensor", out=Dg[:],
                            in0=ecs[:].map(lambda a: a.unsqueeze(1).to_broadcast([8, 8, NT])),
                            in1=identf[0:8, 0:8].map(lambda a: a.unsqueeze(2).to_broadcast([8, 8, NT])), op=ALU.mult)
                        pe_ = [banks.next(), banks.next()]
                        Dgf = Dg[:].map(lambda a: a.rearrange("p a c -> p (a c)"))
                        hN = 4 * NT
                        for n in range(2):
                            P.mm(pe_[n][:, 0:hN], onec[0:8, :], Dgf.map(lambda a: a[:, n * hN:(n + 1) * hN]))
                            P.I(act, "activation", out=ECB[:, n * 4:(n + 1) * 4, :].map(lambda a: a.rearrange("p a c -> p (a c)")),
                                in_=pe_[n][:, 0:hN], func=AF.Copy)
                        for i in range(NT):
                            pt = banks.next()
                            P.tr(pt[:, 0:40], EF[0:40, i * 128:(i + 1) * 128], identf[0:40, 0:40])
                            r_ = rtile(i)
                            P.tr(pt[:, 64:104], EF[0:40, r_ * 128:(r_ + 1) * 128], identf[0:40, 0:40])
                            tk = tk_.next()
                            P.I(act, "activation", out=tk[:], in_=pt[:, 64:104], func=AF.Copy)
                            P.mm(pt[:, 128:168], jrev[:], tk[:])
                            tv = TKS[:, i, :].map(lambda a: a.rearrange("p (q j) -> p q j", j=8))
                            pv = lambda c0: pt[:, c0:c0 + 64].map(lambda a: a.rearrange("p (q j) -> p q j", j=32))
                            P.I(dve, "tensor_copy", out=tv.map(lambda a: a[:, :, 0:4]), in_=pv(0).map(lambda a: a[:, :, 0:4]))
                            P.I(dve, "tensor_copy", out=tv.map(lambda a: a[:, :, 4:8]), in_=pv(128).map(lambda a: a[:, :, 4:8]))
                        P.barrier()
                    PADW = TOK + 6
                    QKb = P.sb(sM, "QKb", [128, 4, PADW], BF16)
                    ktm = P.sb(sM, "ktm", [128, NT, 256], BF16)
                    with ExitStack() as s1:
                        cw = P.sb(s1, "cw", [128, 4, 3], F32)
                        cbi = P.sb(s1, "cbi", [128, 4], F32)
                        for kk in range(3):
                            P.dma(sp, cw[:, :, kk], inp["m_conv_w"][l, kk].rearrange("(c p) -> p c", p=128),
                                  allow_slow_non_contiguous=True)
                        P.dma(sp, cbi[:], inp["m_conv_b"][l].rearrange("(c p) -> p c", p=128), allow_slow_non_contiguous=True)
                        HP = 4230
                        stg_ = P.ring(s1, "stg", [128, HP], F32, 2)
                        yb_ = P.ring(s1, "ybuf", [128, HP], F32, 2)
                        for cc in range(4):
                            rows = QKT[cc * 128:(cc + 1) * 128, :]
                            for piece in range(2):
                                stg = stg_.next()
                                yb = yb_.next()
                                if piece == 0:
                                    n = 4230
                                    P.I(pool, "memset", stg[:, 0:2], 0.0, _writes=[stg.res])
                                    P.I(pool, "memset", stg[:, 258:260], 0.0, _writes=[stg.res])
                                    P.dma(sp, stg[:, 2:258], rows[:, 0:256])
                                    P.dma(sp, stg[:, 260:4230], rows[:, 256:256 + 3970])
                                    oc0 = 1
                                else:
                                    n = 4226
                                    P.dma(sp, stg[:, 0:4224], rows[:, 4224:8448])
                                    P.I(pool, "memset", stg[:, 4224:4226], 0.0, _writes=[stg.res])
                                    oc0 = 4229
                                m = n - 2
                                P.I(dve, "tensor_scalar", out=yb[:, 0:m], in0=stg[:, 1:1 + m], scalar1=cw[:, cc, 1:2],
                                    scalar2=cbi[:, cc:cc + 1], op0=ALU.mult, op1=ALU.add)
                                P.I(dve, "scalar_tensor_tensor", out=yb[:, 0:m], in0=stg[:, 0:m], scalar=cw[:, cc, 0:1],
                                    in1=yb[:, 0:m], op0=ALU.mult, op1=ALU.add)
                                P.I(dve, "scalar_tensor_tensor", out=yb[:, 0:m], in0=stg[:, 2:2 + m], scalar=cw[:, cc, 2:3],
                                    in1=yb[:, 0:m], op0=ALU.mult, op1=ALU.add)
                                if cc < 2:
                                    P.I(act, "activation", out=yb[:, 0:m], in_=yb[:, 0:m], func=AF.Silu)
                                    P.I(pool, "tensor_scalar", out=QKb[:, cc, oc0:oc0 + m], in0=yb[:, 0:m], scalar1=0.125,
                                        scalar2=None, op0=ALU.mult)
                                else:
                                    P.I(act, "activation", out=QKb[:, cc, oc0:oc0 + m], in_=yb[:, 0:m], func=AF.Silu)
                        for i in range(NT):
                            pk = bank_bf((2, 128))
                            c0 = qkcol(i)
                            for kc in range(2):
                                P.tr(pk.map(lambda a: a[:, kc, :]), QKb[:, 2 + kc, c0:c0 + 128], identb[:])
                            P.I(act, "activation", out=ktm[:, i, :].map(lambda a: a.rearrange("p (c t) -> p c t", t=128)),
                                in_=pk, func=AF.Copy)
                        P.barrier()
                    if stages == "M1":
                        return nc, cn, scr
                    with ExitStack() as s3:
                        V1 = P.sb(s3, "V1", [128, NT, 4, 65], BF16)
                        P.I(pool, "memset", V1[:], 1.0, _writes=[V1.res])
                        for j0 in range(0, NT, 11):
                            for h in range(4):
                                P.dma(pool, V1[:, j0:j0 + 11, h, 0:64],
                                      VOG[j0 * 128:(j0 + 11) * 128, h * 64:(h + 1) * 64].rearrange("(j p) e -> p j e", p=128))
                        CN = [P.sb(s3, "CN%d" % k, [128, 65], F32) for k in range(8)]
                        CNb = [P.sb(s3, "CNb%d" % k, [128, 65], BF16) for k in range(8)]
                        for k in range(8):
                            P.I(dve, "memset", CN[k][:], 0.0, _writes=[CN[k].res])
                            P.I(dve, "memset", CNb[k][:], 0.0, _writes=[CNb[k].res])
                        Sm_ = P.ring(s3, "Sm", [128, 128], BF16, 8)
                        Sr_ = P.ring(s3, "Sr", [128, 128], BF16, 8)
                        trifb = P.sb(s3, "trifb", [128, 128], BF16)
                        tribb = P.sb(s3, "tribb", [128, 128], BF16)
                        P.I(dve, "tensor_copy", out=trifb[:], in_=trif[:])
                        P.I(dve, "tensor_copy", out=tribb[:], in_=trib[:])
                        vpp_ = P.ring(s3, "vpp", [128, 65], BF16, 8)
                        dn_ = P.ring(s3, "dn", [128, 2], F32, 6)
                        tmp_ = P.ring(s3, "ctmp", [128, 65], F32, 6)
                        Hst_ = [P.ring(s3, "Hst%d" % d_, [128, 256], F32, 3) for d_ in range(2)]
                        munits = []
                        for c in range(NT):
                            for d_ in range(2):
                                i = c if d_ == 0 else (1 - c if c < 2 else 67 - c)
                                for h in range(4):
                                    munits.append((c, d_, i, h))
                        mstate = {}
                        hst_cur = {}

                        def m_phase_a(u):
                            c, d_, i, h = munits[u]
                            k = d_ * 4 + h
                            pb = (h % 2) * 64
                            c0 = qkcol(i)
                            qv = QKb[pb:pb + 64, h // 2, c0:c0 + 128]
                            kv_ = QKb[pb:pb + 64, 2 + h // 2, c0:c0 + 128]
                            sps = banks.next()
                            P.mm(sps[:, 0:128], kv_, qv)
                            Sr = Sr_.next()
                            P.I(act, "activation", out=Sr[:], in_=sps[:, 0:128], func=AF.Copy)
                            Sm = Sm_.next()
                            P.I(pool, "tensor_tensor", out=Sm[:], in0=Sr[:], in1=(trifb if d_ == 0 else tribb)[:], op=ALU.mult)
                            vpp = vpp_.next()
                            P.I(act, "activation", out=vpp[:], in_=V1[:, i, h, :], func=AF.Copy, scale=TKS[:, i, k:k + 1])
                            mstate[u] = (Sm, vpp, qv)

                        def m_phase_b(u):
                            c, d_, i, h = munits[u]
                            k = d_ * 4 + h
                            pb = (h % 2) * 64
                            Sm, vpp, qv = mstate.pop(u)
                            if h == 0:
                                hst_cur[d_] = Hst_[d_].next()
                            Hst = hst_cur[d_]
                            nd = banks.next()
                            P.mm(nd[:, 0:65], Sm[:], vpp[:], start=True, stop=False)
                            P.mm(nd[:, 0:65], qv, CNb[k][pb:pb + 64, :], start=False, stop=True)
                            P.mm(nd[0:64, 128:193], ktm[:, i, h * 64:(h + 1) * 64], vpp[:])
                            tmp = tmp_.next()
                            P.I(dve, "tensor_tensor", out=tmp[0:64, :], in0=CN[k][0:64, :],
                                in1=nd[0:64, 128:193], op=ALU.add)
                            P.I(act, "activation", out=CNb[k][pb:pb + 64, :], in_=tmp[0:64, :], func=AF.Copy,
                                scale=ECB[0:64, k, c:c + 1])
                            P.I(dve, "tensor_scalar", out=CN[k][0:64, :], in0=tmp[0:64, :],
                                scalar1=ECB[0:64, k, c:c + 1], scalar2=None, op0=ALU.mult)
                            dn = dn_.next()
                            P.I(dve, "tensor_scalar", out=dn[:, 1:2], in0=nd[:, 64:65], scalar1=-1.0,
                                scalar2=None, op0=ALU.mult)
                            P.I(dve, "scalar_tensor_tensor", out=dn[:, 0:1], in0=dn[:, 1:2], scalar=TKS[:, i, 8 + k:9 + k],
                                in1=nd[:, 64:65], op0=ALU.max, op1=ALU.max)
                            P.I(dve, "reciprocal", out=dn[:, 1:2], in_=dn[:, 0:1])
                            P.I(act, "activation", out=Hst[:, h * 64:(h + 1) * 64], in_=nd[:, 0:64], func=AF.Copy,
                                scale=dn[:, 1:2])
                            if h == 3:
                                P.dma(sp, HFB[d_, i * 128:(i + 1) * 128, :], Hst[:])

                        MLOOK = 3
                        for u in range(len(munits) + MLOOK):
                            if u < len(munits):
                                m_phase_a(u)
                            if u - MLOOK >= 0:
                                m_phase_b(u - MLOOK)
                        P.barrier()
                    if stages == "M3":
                        return nc, cn, scr
                    with ExitStack() as s4:
                        mng = P.sb(s4, "mng", [128, 256], F32)
                        P.dma(sp, mng[:], inp["m_norm_g"][l:l + 1, :].partition_broadcast(128))
                        hf_ = P.ring(s4, "hf", [128, 2, 256], F32, 2)
                        og_ = P.ring(s4, "og", [128, 256], F32, 2)
                        hs_ = P.ring(s4, "hs", [128, 256], F32, 2)
                        sq_ = P.ring(s4, "sq", [128, 256], F32, 2)
                        ms_ = P.ring(s4, "ms", [128, 8], F32, 2)
                        ybf_ = P.ring(s4, "ybf", [128, 256], BF16, 2)
                        ybT_ = P.ring(s4, "ybT", [128, 2, 128], BF16, 2)
                        for i in range(NT):
                            if i < 2 and not with_ctx:
                                continue
                            hf = hf_.next()
                            P.dma(sp, hf[:], HFB[:, i * 128:(i + 1) * 128, :].rearrange("d p e -> p d e"))
                            og = og_.next()
                            P.dma(sp, og[:], VOG[i * 128:(i + 1) * 128, 256:512])
                            hs = hs_.next()
                            P.I(dve, "tensor_tensor", out=hs[:], in0=hf[:, 0, :], in1=hf[:, 1, :], op=ALU.add)
                            sq = sq_.next()
                            P.I(pool, "tensor_tensor", out=sq[:], in0=hs[:], in1=hs[:], op=ALU.mult)
                            ms = ms_.next()
                            P.I(dve, "tensor_reduce", out=ms[:, 0:4], in_=sq[:].map(lambda a: a.rearrange("p (h e) -> p h e", e=64)),
                                axis=AX.X, op=ALU.add)
                            P.I(act, "activation", out=ms[:, 0:4], in_=ms[:, 0:4], func=AF.Sqrt, scale=1.0 / 64, bias=epsc[:])
                            P.I(dve, "reciprocal", out=ms[:, 4:8], in_=ms[:, 0:4])
                            P.I(act, "activation", out=og[:], in_=og[:], func=AF.Sigmoid)
                            h3 = lambda vv: vv.map(lambda a: a.rearrange("p (h e) -> p h e", e=64))
                            P.I(dve, "tensor_tensor", out=h3(hs[:]), in0=h3(hs[:]),
                                in1=ms[:, 4:8].map(lambda a: a.unsqueeze(2).to_broadcast([128, 4, 64])), op=ALU.mult)
                            P.I(pool, "tensor_tensor", out=hs[:], in0=hs[:], in1=mng[:], op=ALU.mult)
                            ybf = ybf_.next()
                            P.I(dve, "tensor_tensor", out=ybf[:], in0=hs[:], in1=og[:], op=ALU.mult)
                            py = bank_bf((2, 128))
                            for cc in range(2):
                                P.tr(py.map(lambda a: a[:, cc, :]), ybf[:, cc * 128:(cc + 1) * 128], identb[:])
                            ybT = ybT_.next()
                            P.I(act, "activation", out=ybT[:], in_=py, func=AF.Copy)
                            P.dma(sp, MIXT[256:512, i * 128:(i + 1) * 128].rearrange("(c p) t -> p c t", p=128), ybT[:])
                        P.barrier()
                if stages == "M":
                    return nc, cn, scr

                with ExitStack() as sF:
                    ld = lambda nm, shp, dt: P.sb(sF, nm, shp, dt)
                    c1 = ld("f_c1", [128, 128], BF16); s1t = ld("f_s1", [128, 128], BF16); ns1 = ld("f_ns1", [128, 128], BF16)
                    twc = ld("f_twc", [128, 64], F32); tws = ld("f_tws", [128, 64], F32)
                    c2 = ld("f_c2", [64, 64], BF16); s2t = ld("f_s2", [64, 64], BF16)
                    ccs = ld("f_ccs", [128, 2, 2, 256], BF16)
                    for tl, nm in ((c1, "f_c1"), (s1t, "f_s1"), (ns1, "f_ns1"), (twc, "f_twc"), (tws, "f_tws"), (c2, "f_c2"), (s2t, "f_s2")):
                        P.dma(sp, tl[:], inp[nm][:, :])
                    P.dma(sp, ccs[:, 0, :, :], inp["f_cc"].rearrange("(a p) k -> p a k", p=128))
                    P.dma(sp, ccs[:, 1, :, :], inp["f_sc"].rearrange("(a p) k -> p a k", p=128))
                    with ExitStack() as sF1:
                        X1 = P.sb(sF1, "X1", [128, 64, 512], BF16)
                        YT = P.sb(sF1, "YT", [128, 64, 512], BF16)
                        ftmp_ = P.ring(sF1, "ftmp", [128, 256], F32, 4)
                        for q in range(4):
                            P.dma(sp, X1[:, q * 16:(q + 1) * 16, :],
                                  FAB[CTX:, :].rearrange("(a b) c -> a b c", b=64)[:, q * 16:(q + 1) * 16, :])
                        for tp in range(32):
                            zr = X1[:, 2 * tp:2 * tp + 2, 0:256]
                            zi = X1[:, 2 * tp:2 * tp + 2, 256:512]
                            br = banks.next()
                            bi = banks.next()
                            P.mm(br[:, :], c1[:], zr, start=True, stop=False)
                            P.mm(br[:, :], s1t[:], zi, start=False, stop=True)
                            P.mm(bi[:, :], c1[:], zi, start=True, stop=False)
                            P.mm(bi[:, :], ns1[:], zr, start=False, stop=True)
                            for u in range(2):
                                t2_ = 2 * tp + u
                                yr = br[:, u * 256:(u + 1) * 256]
                                yi = bi[:, u * 256:(u + 1) * 256]
                                ta = ftmp_.next()
                                tb = ftmp_.next()
                                P.I(dve, "tensor_scalar", out=ta[:], in0=yi, scalar1=tws[:, t2_:t2_ + 1], scalar2=None, op0=ALU.mult)
                                P.I(dve, "scalar_tensor_tensor", out=YT[:, t2_, 0:256], in0=yr, scalar=twc[:, t2_:t2_ + 1],
                                    in1=ta[:], op0=ALU.mult, op1=ALU.add)
                                P.I(dve, "tensor_scalar", out=tb[:], in0=yr, scalar1=tws[:, t2_:t2_ + 1], scalar2=None, op0=ALU.mult)
                                P.I(dve, "scalar_tensor_tensor", out=YT[:, t2_, 256:512], in0=yi, scalar=twc[:, t2_:t2_ + 1],
                                    in1=tb[:], op0=ALU.mult, op1=ALU.subtract)
                        for q in range(4):
                            P.dma(sp, FY[:, q * 16:(q + 1) * 16, :], YT[:, q * 16:(q + 1) * 16, :])
                        P.barrier()
                    with ExitStack() as sF2:
                        y2_ = P.ring(sF2, "y2", [64, 8, 512], BF16, 3)
                        yaT = P.sb(sF2, "yaT", [128, 2, SEQ], BF16)
                        FYv = FY.rearrange("k t c -> t k c")
                        for kb in range(16):
                            y2 = y2_.next()
                            P.dma(sp, y2[:], FYv[:, kb * 8:(kb + 1) * 8, :])
                            for jc in range(2):
                                bk = banks.next()
                                for kl in range(8):
                                    P.mm(bk[:, kl * 64:(kl + 1) * 64], y2[:, kl, jc * 128:(jc + 1) * 128], c2[:], start=True, stop=False)
                                    P.mm(bk[:, kl * 64:(kl + 1) * 64], y2[:, kl, 256 + jc * 128:256 + (jc + 1) * 128], s2t[:],
                                         start=False, stop=True)
                                ov = yaT[:, jc, :].map(lambda a: a.rearrange("p (k2 k1) -> p k1 k2", k1=128)[:, kb * 8:(kb + 1) * 8, :])
                                iv = bk[:, :].map(lambda a: a.rearrange("p (kl k2) -> p kl k2", k2=64))
                                P.I(act if jc == 0 else dve, "activation" if jc == 0 else "tensor_copy", out=ov, in_=iv,
                                    **({"func": AF.Copy} if jc == 0 else {}))
                        P.dma(sp, MIXT[0:256, CTX:].rearrange("(c p) t -> p c t", p=128), yaT[:])
                        if with_ctx:
                            zc = P.sb(sF2, "zc", [128, 2, 512], BF16)
                            yc2 = P.sb(sF2, "yc2", [128, 2, 256], BF16)
                            P.dma(sp, zc[:], FAB[0:CTX, :].rearrange("(a p) c -> p a c", p=128))
                            for jc in range(2):
                                bk = banks.next()
                                for a_ in range(2):
                                    P.mm(bk[:, 0:256], zc[:, a_, jc * 128:(jc + 1) * 128], ccs[:, 0, a_, :], start=(a_ == 0), stop=False)
                                    P.mm(bk[:, 0:256], zc[:, a_, 256 + jc * 128:256 + (jc + 1) * 128], ccs[:, 1, a_, :],
                                         start=False, stop=(a_ == 1))
                                P.I(act, "activation", out=yc2[:, jc, :], in_=bk[:, 0:256], func=AF.Copy)
                            P.dma(sp, MIXT[0:256, 0:CTX].rearrange("(c p) t -> p c t", p=128), yc2[:])
                        P.barrier()
                if stages == "F":
                    return nc, cn, scr

                last = (l == nlayers - 1)
                tiles_e = [i for i in range(NT) if (i >= 2 or with_ctx)]
                sets = ([(1, [0, 1], CAPC, XEc, YEc, 0)] if with_ctx else []) + [(0, list(range(2, NT)), CAPX, XEx, YEx, 16)]
                with ExitStack() as sE:
                    AFF = P.sb(sE, "AFF", [128, NT, NE], F32)
                    GM = P.sb(sE, "GM", [128, NT, NE], F32)
                    SLOT = P.sb(sE, "SLOT", [128, NT, NE], I32)
                    Gb = P.sb(sE, "Gb", [128, 4, D], F32)
                    for v in range(2):
                        P.dma(sp, Gb[:, v, :], MOD[l, v:v + 1, 2 * D:3 * D].partition_broadcast(128))
                        P.dma(sp, Gb[:, 2 + v, :], MOD[l, v:v + 1, 5 * D:6 * D].partition_broadcast(128))
                    if not with_ctx:
                        P.I(pool, "memset", AFF[:, 0:2, :], 0.0, _writes=[AFF.res])
                    with ExitStack() as sE1:
                        Wout = P.sb(sE1, "Wout", [128, 8, D], BF16)
                        for k in range(8):
                            P.dma(pool, Wout[:, k, :], inp["w_out"][l, k * 128:(k + 1) * 128, :])
                        RW = P.sb(sE1, "RW", [128, 8, NE], BF16)
                        P.dma(pool, RW[:], inp["router_w"][l].rearrange("(k p) e -> p k e", p=128))
                        mT_ = P.ring(sE1, "mT", [128, 8, 128], BF16, 2)
                        xe_ = P.ring(sE1, "xe", [128, D], F32, 2)
                        tm_ = P.ring(sE1, "tm", [128, D], F32, 2)
                        st2_ = P.ring(sE1, "st2", [128, 8], F32, 3)
                        xn2_ = P.ring(sE1, "xn2", [128, D], BF16, 2)
                        h2_ = P.ring(sE1, "h2", [128, 8, 128], BF16, 2)
                        lg_ = P.ring(sE1, "lg", [128, NE], F32, 2)
                        jk2 = P.sb(sE1, "jk2", [128, D], BF16)
                        for i in tiles_e:
                            v = 0 if i >= 2 else 1
                            tok0 = i * 128
                            mT = mT_.next()
                            P.dma(sp, mT[:], MIXT[:, tok0:tok0 + 128].rearrange("(k p) t -> p k t", p=128))
                            xt = xe_.next()
                            P.dma(sp, xt[:], XR[tok0:tok0 + 128, :])
                            ob = [banks.next(), banks.next()]
                            for n in range(2):
                                for k in range(8):
                                    P.mm(ob[n][:, :], mT[:, k, :], Wout[:, k, n * 512:(n + 1) * 512], start=(k == 0), stop=(k == 7))
                            tm = tm_.next()
                            for n in range(2):
                                P.I(dve, "tensor_tensor", out=tm[:, n * 512:(n + 1) * 512], in0=ob[n][:, :],
                                    in1=Gb[:, v, n * 512:(n + 1) * 512], op=ALU.mult)
                            P.I(pool, "tensor_tensor", out=xt[:], in0=xt[:], in1=tm[:], op=ALU.add)
                            P.dma(sp, XR[tok0:tok0 + 128, :], xt[:])
                            st = st2_.next()
                            P.I(act, "activation", out=jk2[:], in_=xt[:], func=AF.Square, accum_out=st[:, 0:1])
                            P.I(act, "activation", out=st[:, 1:2], in_=st[:, 0:1], func=AF.Sqrt, scale=1.0 / D, bias=epsc[:])
                            P.I(dve, "reciprocal", out=st[:, 2:3], in_=st[:, 1:2])
                            xn2 = xn2_.next()
                            P.I(dve, "tensor_scalar", out=xn2[:], in0=xt[:], scalar1=st[:, 2:3], scalar2=None, op0=ALU.mult)
                            P.dma(sp, XN2[tok0:tok0 + 128, :], xn2[:])
                            pT = bank_bf((8, 128))
                            for k in range(8):
                                P.tr(pT.map(lambda a: a[:, k, :]), xn2[:, k * 128:(k + 1) * 128], identb[:])
                            h2 = h2_.next()
                            bc = lambda vv: vv.map(lambda a: a.unsqueeze(2).to_broadcast([128, 8, 128]))
                            P.I(dve, "tensor_tensor", out=h2[:], in0=pT, in1=bc(Amod[:, 2 + v, :]), op=ALU.mult)
                            P.I(pool, "tensor_tensor", out=h2[:], in0=h2[:], in1=bc(colv[:, v * 6 + 3, :]), op=ALU.add)
                            lb = banks.next()
                            for k in range(8):
                                P.mm(lb[:, 0:NE], h2[:, k, :], RW[:, k, :], start=(k == 0), stop=(k == 7))
                            lg = lg_.next()
                            P.I(dve, "tensor_reduce", out=st[:, 3:4], in_=lb[:, 0:NE], axis=AX.X, op=ALU.max)
                            P.I(dve, "tensor_scalar", out=st[:, 4:5], in0=st[:, 3:4], scalar1=-1.0, scalar2=None, op0=ALU.mult)
                            P.I(act, "activation", out=lg[:], in_=lb[:, 0:NE], func=AF.Exp, bias=st[:, 4:5], accum_out=st[:, 5:6])
                            P.I(dve, "reciprocal", out=st[:, 6:7], in_=st[:, 5:6])
                            P.I(dve, "tensor_scalar", out=AFF[:, i, :], in0=lg[:], scalar1=st[:, 6:7], scalar2=None, op0=ALU.mult)
                        P.barrier()
                    if stages == "E":
                        dbg_a = nc.dram_tensor("dbg_AFF", [128, NT, NE], F32, kind="ExternalOutput").ap()
                        P.dma(sp, dbg_a[:, :, :], AFF[:])
                        P.barrier()
                        return nc, cn, scr

                    with ExitStack() as sD2:
                        lo = P.sb(sD2, "lo", [128, 32], F32)
                        hi = P.sb(sD2, "hi", [128, 32], F32)
                        mid = P.sb(sD2, "mid", [128, 32], F32)
                        capv = P.sb(sD2, "capv", [128, 32], F32)
                        onesb = P.sb(sD2, "onesb", [128, 128], BF16)
                        utri = P.sb(sD2, "utri", [128, 128], BF16)
                        P.dma(sp, capv[:], inp["capv"][:, :])
                        P.dma(sp, onesb[:], inp["onesb"][:, :])
                        P.dma(sp, utri[:], inp["utri"][:, :])
                        P.I(dve, "memset", lo[:], 0.0, _writes=[lo.res])
                        P.I(dve, "memset", hi[:], 1.0, _writes=[hi.res])
                        cmp_ = P.sb(sD2, "cmp", [128, NT, NE], BF16)
                        pc = P.sb(sD2, "pc", [128, 32], BF16)
                        pcf = P.sb(sD2, "pcf", [128, 32], F32)
                        P.I(dve, "memset", pcf[:], 0.0, _writes=[pcf.res])
                        mge = P.sb(sD2, "mge", [128, 32], U32)
                        mlt = P.sb(sD2, "mlt", [128, 32], U32)
                        P.I(dve, "memset", pc[:], 0.0, _writes=[pc.res])
                        for it in range(34):
                            P.I(dve, "tensor_tensor", out=mid[:], in0=lo[:], in1=hi[:], op=ALU.add)
                            P.I(dve, "tensor_scalar", out=mid[:], in0=mid[:], scalar1=0.5, scalar2=None, op0=ALU.mult)
                            for (v, tl, cap, XE, YE, co) in sets:
                                nt_ = len(tl)
                                t0_ = tl[0]
                                P.I(dve, "tensor_tensor", out=cmp_[:, t0_:t0_ + nt_, :], in0=AFF[:, t0_:t0_ + nt_, :],
                                    in1=mid[:, co:co + 16].map(lambda a: a.unsqueeze(1).to_broadcast([128, nt_, NE])), op=ALU.is_gt)
                                P.I(dve, "tensor_reduce", out=pcf[:, co:co + 16],
                                    in_=cmp_[:, t0_:t0_ + nt_, :].map(lambda a: a.rearrange("p j e -> p e j")), axis=AX.X, op=ALU.add)
                            P.I(dve, "tensor_copy", out=pc[:], in_=pcf[:])
                            tb = banks.next()
                            P.mm(tb[:, 0:32], onesb[:], pc[:])
                            P.I(dve, "tensor_tensor", out=mge[:], in0=tb[:, 0:32], in1=capv[:], op=ALU.is_ge)
                            P.I(dve, "tensor_tensor", out=mlt[:], in0=tb[:, 0:32], in1=capv[:], op=ALU.is_lt)
                            P.I(dve, "copy_predicated", out=lo[:], mask=mge[:], data=mid[:])
                            P.I(dve, "copy_predicated", out=hi[:], mask=mlt[:], data=mid[:])
                        offb = P.sb(sD2, "offb", [128, NE], F32)
                        mk_ = P.ring(sD2, "mk", [128, NE], F32, 2)
                        mkb_ = P.ring(sD2, "mkb", [128, NE], BF16, 2)
                        sl_ = P.ring(sD2, "sl", [128, NE], F32, 2)
                        BIG = 1.0e6
                        for (v, tl, cap, XE, YE, co) in sets:
                            P.I(dve, "memset", offb[:], 0.0, _writes=[offb.res])
                            for i in tl:
                                mk = mk_.next()
                                P.I(dve, "tensor_tensor", out=mk[:], in0=AFF[:, i, :], in1=lo[:, co:co + 16], op=ALU.is_gt)
                                mkb = mkb_.next()
                                P.I(dve, "tensor_copy", out=mkb[:], in_=mk[:])
                                P.I(dve, "tensor_tensor", out=GM[:, i, :], in0=AFF[:, i, :], in1=mk[:], op=ALU.mult)
                                rb = banks.next()
                                P.mm(rb[:, 0:NE], utri[:], mkb[:])
                                P.mm(rb[:, NE:2 * NE], onesb[:], mkb[:])
                                sl = sl_.next()
                                P.I(dve, "tensor_tensor", out=sl[:], in0=rb[:, 0:NE], in1=offb[:], op=ALU.add)
                                P.I(dve, "tensor_tensor", out=offb[:], in0=rb[:, NE:2 * NE], in1=offb[:], op=ALU.add)
                                P.I(dve, "tensor_scalar", out=sl[:], in0=sl[:], scalar1=-BIG, scalar2=None, op0=ALU.add)
                                P.I(dve, "tensor_tensor", out=sl[:], in0=sl[:], in1=mk[:], op=ALU.mult)
                                P.I(dve, "tensor_scalar", out=SLOT[:, i, :], in0=sl[:], scalar1=BIG, scalar2=None, op0=ALU.add)
                        P.barrier()
                    if stages == "D3":
                        dbg_s = nc.dram_tensor("dbg_SLOT", [128, NT, NE], I32, kind="ExternalOutput").ap()
                        dbg_g = nc.dram_tensor("dbg_GM", [128, NT, NE], F32, kind="ExternalOutput").ap()
                        P.dma(sp, dbg_s[:, :, :], SLOT[:])
                        P.dma(sp, dbg_g[:, :, :], GM[:])
                        P.barrier()
                        return nc, cn, scr

                    xeres = [Res("xe%d" % e, multi=True) for e in range(NE)]
                    with ExitStack() as sD4:
                        Wg_ = P.ring(sD4, "Wg", [128, 8, D], BF16, 2)
                        Wu_ = P.ring(sD4, "Wu", [128, 8, D], BF16, 2)
                        Wd_ = P.ring(sD4, "Wd", [128, 8, D], BF16, 2)
                        xer_ = P.ring(sD4, "xer", [128, 4, D], BF16, 2)
                        xeT_ = P.ring(sD4, "xeT", [128, 8, 512], BF16, 2)
                        hid_ = P.ring(sD4, "hid", [128, 8, 512], BF16, 2)
                        sg_ = P.ring(sD4, "sg", [128, 512], F32, 3)
                        ye_ = P.ring(sD4, "ye", [128, D], BF16, 3)
                        ne_w = 1 if lite else NE
                        xs_ = P.ring(sD4, "xs", [128, D], BF16, 3)
                        EG = 4
                        wts = {}

                        def load_w(e):
                            if e >= NE or e in wts:
                                return
                            ew = e % ne_w
                            Wg = Wg_.next(); Wu = Wu_.next(); Wd = Wd_.next()
                            for k in range(8):
                                P.dma(pool, Wg[:, k, :], inp["e_w_gate"][l, ew, k * 128:(k + 1) * 128, :])
                                P.dma(pool, Wu[:, k, :], inp["e_w_up"][l, ew, k * 128:(k + 1) * 128, :])
                            for k in range(8):
                                P.dma(pool, Wd[:, k, :], inp["e_w_down"][l, ew, k * 128:(k + 1) * 128, :])
                            wts[e] = (Wg, Wu, Wd)

                        stiles = [(st_, i) for st_ in sets for i in st_[1]]

                        def scatter_part(g, part, nparts):
                            if g * EG >= NE:
                                return
                            n_ = len(stiles)
                            lo_, hi_ = (part * n_) // nparts, ((part + 1) * n_) // nparts
                            for (st_, i) in stiles[lo_:hi_]:
                                (v, tl, cap, XE, YE, co) = st_
                                xs = xs_.next()
                                P.dma(sp, xs[:], XN2[i * 128:(i + 1) * 128, :])
                                for e2 in range(g * EG, (g + 1) * EG):
                                    P.dma(pool, XE[e2][:, :], xs[:], meth="indirect_dma_start",
                                          out_offset=bass.IndirectOffsetOnAxis(ap=SLOT[:, i, e2:e2 + 1].ap, axis=0),
                                          in_offset=None, bounds_check=bcreg[cap], oob_is_err=False,
                                          _reads=[SLOT.res], _writes=[xeres[e2]])

                        load_w(0)
                        scatter_part(0, 0, 1)
                        load_w(1)
                        for e in range(NE):
                            load_w(e)
                            Wg, Wu, Wd = wts.pop(e)
                            for (v, tl, cap, XE, YE, co) in sets:
                                for ch0 in range(0, cap, 512):
                                    ns = min(512, cap - ch0)
                                    nsub = (ns + 127) // 128
                                    pr = min(128, ns)
                                    xer = xer_.next()
                                    P.dma(sp, xer[0:pr, 0:nsub, :], XE[e][ch0:ch0 + ns, :].rearrange("(a p) d -> p a d", p=pr),
                                          _reads=[xeres[e]])
                                    xeT = xeT_.next()
                                    for a_ in range(nsub):
                                        pT = bank_bf((8, 128))
                                        for k in range(8):
                                            P.tr(pT.map(lambda a: a[:, k, 0:pr]), xer[0:pr, a_, k * 128:(k + 1) * 128], identb[0:pr, 0:pr])
                                        bcx = lambda vv: vv.map(lambda a: a.unsqueeze(2).to_broadcast([128, 8, pr]))
                                        P.I(dve, "tensor_tensor", out=xeT[:, :, a_ * 128:a_ * 128 + pr], in0=pT.map(lambda a: a[:, :, 0:pr]),
                                            in1=bcx(Amod[:, 2 + v, :]), op=ALU.mult)
                                        P.I(dve, "tensor_tensor", out=xeT[:, :, a_ * 128:a_ * 128 + pr], in0=xeT[:, :, a_ * 128:a_ * 128 + pr],
                                            in1=bcx(colv[:, v * 6 + 3, :]), op=ALU.add)
                                    hid = hid_.next()
                                    for f in range(8):
                                        bg = banks.next()
                                        bu = banks.next()
                                        for k in range(8):
                                            P.mm(bg[:, 0:ns], Wg[:, k, f * 128:(f + 1) * 128], xeT[:, k, 0:ns], start=(k == 0), stop=(k == 7))
                                        for k in range(8):
                                            P.mm(bu[:, 0:ns], Wu[:, k, f * 128:(f + 1) * 128], xeT[:, k, 0:ns], start=(k == 0), stop=(k == 7))
                                        sg = sg_.next()
                                        P.I(act, "activation", out=sg[:, 0:ns], in_=bg[:, 0:ns], func=AF.Silu)
                                        P.I(dve, "tensor_tensor", out=hid[:, f, 0:ns], in0=sg[:, 0:ns], in1=bu[:, 0:ns], op=ALU.mult)
                                    for a_ in range(nsub):
                                        ye = ye_.next()
                                        for n in range(2):
                                            bd = banks.next()
                                            for f in range(8):
                                                P.mm(bd[0:pr, :], hid[:, f, a_ * 128:a_ * 128 + pr], Wd[:, f, n * 512:(n + 1) * 512],
                                                     start=(f == 0), stop=(f == 7))
                                            if n == 0:
                                                P.I(act, "activation", out=ye[0:pr, 0:512], in_=bd[0:pr, :], func=AF.Copy)
                                            else:
                                                P.I(dve, "tensor_copy", out=ye[0:pr, 512:1024], in_=bd[0:pr, :])
                                        P.dma(sp, YE[e][ch0 + a_ * 128:ch0 + a_ * 128 + pr, :], ye[0:pr, :])
                            scatter_part(e // EG + 1, e % EG, EG)
                            load_w(e + 2)
                        P.barrier()
                    with ExitStack() as sD5:
                        gt_ = P.ring(sD5, "gt", [128, D], BF16, 8)
                        for t_ in gt_.tiles:
                            P.I(pool, "memset", t_[:], 0.0, _writes=[t_.res])
                        xf_ = P.ring(sD5, "xf", [128, D], F32, 2)
                        tm5_ = P.ring(sD5, "tm5", [128, D], F32, 2)
                        st5_ = P.ring(sD5, "st5", [128, 4], F32, 2)
                        gh_ = P.ring(sD5, "gh", [128, 3, NE], F32, 2)
                        ghb_ = P.ring(sD5, "ghb", [128, NE], BF16, 2)
                        Dg_ = P.ring(sD5, "Dgd", [128, 2, NE, 128], BF16, 2)
                        jk5 = P.sb(sD5, "jk5", [128, D], BF16)
                        fgb = P.sb(sD5, "fgb", [128, D], F32)
                        P.dma(sp, fgb[:], inp["final_g"].unsqueeze(0).partition_broadcast(128))
                        idb = identf[:].map(lambda a: a.unsqueeze(1).to_broadcast([128, NE, 128]))
                        for (v, tl, cap, XE, YE, co) in sets:
                            for i in tl:
                                gh = gh_.next()
                                ghb = ghb_.next()
                                P.I(dve, "tensor_copy", out=ghb[:], in_=GM[:, i, :])
                                P.I(dve, "tensor_copy", out=gh[:, 0, :], in_=ghb[:])
                                P.I(dve, "tensor_tensor", out=gh[:, 1, :], in0=GM[:, i, :], in1=gh[:, 0, :], op=ALU.subtract)
                                Dgd = Dg_.next()
                                for q_ in range(2):
                                    P.I(dve, "tensor_tensor", out=Dgd[:, q_, :, :], in0=idb,
                                        in1=gh[:, q_, :].map(lambda a: a.unsqueeze(2).to_broadcast([128, NE, 128])), op=ALU.mult)
                                ab = [banks.next(), banks.next()]
                                for e in range(NE):
                                    gt = gt_.next()
                                    P.dma(pool, gt[:], YE[e][:, :], meth="indirect_dma_start", out_offset=None,
                                          in_offset=bass.IndirectOffsetOnAxis(ap=SLOT[:, i, e:e + 1].ap, axis=0),
                                          bounds_check=bcreg[cap], oob_is_err=False, _reads=[SLOT.res])
                                    for q_ in range(2):
                                        for n in range(2):
                                            P.mm(ab[n][:, :], Dgd[:, q_, e, :], gt[:, n * 512:(n + 1) * 512],
                                                 start=(e == 0 and q_ == 0), stop=(e == NE - 1 and q_ == 1))
                                xf = xf_.next()
                                P.dma(sp, xf[:], XR[i * 128:(i + 1) * 128, :])
                                tm = tm5_.next()
                                for n in range(2):
                                    P.I(dve, "tensor_tensor", out=tm[:, n * 512:(n + 1) * 512], in0=ab[n][:, :],
                                        in1=Gb[:, 2 + v, n * 512:(n + 1) * 512], op=ALU.mult)
                                P.I(dve, "tensor_tensor", out=xf[:], in0=xf[:], in1=tm[:], op=ALU.add)
                                if not last:
                                    P.dma(sp, XR[i * 128:(i + 1) * 128, :], xf[:])
                                elif i >= 2:
                                    st = st5_.next()
                                    P.I(act, "activation", out=jk5[:], in_=xf[:], func=AF.Square, accum_out=st[:, 0:1])
                                    P.I(act, "activation", out=st[:, 1:2], in_=st[:, 0:1], func=AF.Sqrt, scale=1.0 / D, bias=epsc[:])
                                    P.I(dve, "reciprocal", out=st[:, 2:3], in_=st[:, 1:2])
                                    P.I(dve, "scalar_tensor_tensor", out=xf[:], in0=xf[:], scalar=st[:, 2:3], in1=fgb[:],
                                        op0=ALU.mult, op1=ALU.mult)
                                    P.dma(sp, out[(i - 2) * 128:(i - 1) * 128, :], xf[:])
                        P.barrier()
    return nc, cn, scr


_CACHE = {}


def kernel(**inputs):
    nb = inputs["x"].shape[0]
    if "nc" not in _CACHE:
        _CACHE["nc"] = build(nlayers=DEPTH, dbg=False)
    nc, cn, _ = _CACHE["nc"]
    shared = {n: np.ascontiguousarray(inputs[n], dtype=np.float32) for n, _ in WNAMES}
    shared["c_ctx"] = np.ascontiguousarray(inputs["c_ctx"], dtype=np.float32)
    for n, a in cn.items():
        shared["k_" + n] = a
    in_maps = []
    for b in range(nb):
        m = dict(shared)
        m["x"] = np.ascontiguousarray(inputs["x"][b], dtype=np.float32)
        m["c"] = np.ascontiguousarray(inputs["c"][b], dtype=np.float32)
        m["ctx"] = np.ascontiguousarray(inputs["ctx"][b], dtype=np.float32)
        in_maps.append(m)
    res = run_bass_kernel_spmd(nc, in_maps, core_ids=list(range(nb)))
    return np.stack([np.asarray(r["out"], dtype=np.float32) for r in res.results], axis=0)
```

```python
import os
import numpy as np
import ml_dtypes
from contextlib import ExitStack
import concourse.bass as bass
import concourse.mybir as mybir
from concourse.bass_utils import run_bass_kernel_spmd

F32 = mybir.dt.float32
BF16 = mybir.dt.bfloat16
I32 = mybir.dt.int32
U32 = mybir.dt.uint32
AF = mybir.ActivationFunctionType
ALU = mybir.AluOpType
AX = mybir.AxisListType

D = 1024
SEQ = 8192
CTX = 256
DEPTH = 4
NT = (SEQ + CTX) // 128
TOK = SEQ + CTX
INC = 1968
NE = 16
CAPX = 1024
CAPC = 32
EPS = 1e-6


class Res:
    __slots__ = ("name", "w", "r", "excl", "multi", "wl")

    def __init__(self, name, excl=False, multi=False):
        self.name = name
        self.w = None
        self.r = []
        self.excl = excl
        self.multi = multi
        self.wl = []


class V:
    __slots__ = ("ap", "res")

    def __init__(self, ap, res):
        self.ap = ap
        self.res = res

    def map(self, fn):
        return V(fn(self.ap), self.res)


class Tile:
    def __init__(self, t, name):
        self.t = t
        self.res = Res(name)

    def __getitem__(self, idx):
        return V(self.t[idx], self.res)


class Eng:
    def __init__(self, name, h, sem):
        self.name = name
        self.h = h
        self.sem = sem
        self.count = 0
        self.seen = {}


def _ap(x):
    return x.ap if isinstance(x, V) else x


class Prog:
    WRITE_KEYS = ("out", "accum_out", "out_max", "out_indices")

    def __init__(self, nc, es, n_dma_sems=56):
        self.nc = nc
        self.es = es
        mk = lambda n: es.enter_context(nc.semaphore(n))
        self.pe = Eng("pe", nc.tensor, mk("s_pe"))
        self.act = Eng("act", nc.scalar, mk("s_act"))
        self.dve = Eng("dve", nc.vector, mk("s_dve"))
        self.pool = Eng("pool", nc.gpsimd, mk("s_pool"))
        self.sp = Eng("sp", nc.sync, mk("s_sp"))
        self.engs = [self.pe, self.act, self.dve, self.pool, self.sp]
        self.dsems = [[mk("s_d%d" % i), 0] for i in range(n_dma_sems)]
        self.dnext = 0
        self.ninst = 0

    def sb(self, es, name, shape, dt):
        self.nalloc = getattr(self, "nalloc", 0) + 1
        return Tile(es.enter_context(self.nc.sbuf_tensor("t%d_%s" % (self.nalloc, name), list(shape), dt)), name)

    def ps(self, es, name, shape, dt=F32):
        self.nalloc = getattr(self, "nalloc", 0) + 1
        t = Tile(es.enter_context(self.nc.psum_tensor("p%d_%s" % (self.nalloc, name), list(shape), dt)), name)
        t.res.excl = True
        return t

    def ring(self, es, name, shape, dt, n, psum=False):
        f = self.ps if psum else self.sb
        return Ring([f(es, "%s_%d" % (name, i), shape, dt) for i in range(n)])

    def _wait(self, E, tok):
        kind, key, val = tok
        if kind == "eng":
            sem = key.sem
            k = ("e", key.name)
        else:
            sem = self.dsems[key][0]
            k = ("d", key)
        if E.seen.get(k, 0) >= val:
            return
        E.h.wait_ge(sem, val)
        E.seen[k] = val
        self.ninst += 1

    def _deps(self, E, reads, writes):
        toks = []
        for r in reads:
            if r is not None and r.w is not None:
                toks.append(r.w)
            if r is not None and r.excl:
                toks.extend(r.r)
            if r is not None and r.multi:
                toks.extend(r.wl)
        for w in writes:
            if w is None:
                continue
            if w.w is not None and not w.multi:
                toks.append(w.w)
            toks.extend(w.r)
        for t in toks:
            if t[0] == "eng" and t[1] is E:
                continue
            self._wait(E, t)
        if E is not self.pe:
            for r in reads:
                if r is not None and r.w is not None and r.w[0] == "eng" and r.w[1] is E:
                    self._wait(E, r.w)

    def _commit(self, tok, reads, writes):
        for r in reads:
            if r is not None:
                r.r.append(tok)
        for w in writes:
            if w is not None:
                if w.multi:
                    w.wl.append(tok)
                    continue
                w.w = tok
                w.r = []

    def I(self, E, meth, *args, **kw):
        reads, writes = [], []
        for k, v in kw.items():
            if isinstance(v, V):
                (writes if k in self.WRITE_KEYS else reads).append(v.res)
        for v in args:
            if isinstance(v, V):
                reads.append(v.res)
        extra_r = kw.pop("_reads", ())
        extra_w = kw.pop("_writes", ())
        reads.extend(extra_r)
        writes.extend(extra_w)
        self._deps(E, reads, writes)
        ins = getattr(E.h, meth)(*[_ap(a) for a in args], **{k: _ap(v) for k, v in kw.items()})
        E.count += 1
        ins.then_inc(E.sem, 1)
        self.ninst += 1
        self._commit(("eng", E, E.count), reads, writes)
        return ins

    def dma(self, Q, out, in_, meth="dma_start", **kw):
        reads = [in_.res] if isinstance(in_, V) else []
        writes = [out.res] if isinstance(out, V) else []
        for k, v in kw.items():
            if isinstance(v, V):
                reads.append(v.res)
        reads.extend(kw.pop("_reads", ()))
        writes.extend(kw.pop("_writes", ()))
        self._deps(Q, reads, writes)
        i = self.dnext
        self.dnext = (self.dnext + 1) % len(self.dsems)
        sem, val = self.dsems[i]
        if val > 0:
            self._wait(Q, ("dma", i, val))
        val += 16
        self.dsems[i][1] = val
        ins = getattr(Q.h, meth)(out=_ap(out), in_=_ap(in_), **{k: _ap(v) for k, v in kw.items()})
        ins.then_inc(sem, 16)
        self.ninst += 1
        self._commit(("dma", i, val), reads, writes)
        return ins

    def barrier(self):
        toks = [("eng", e, e.count) for e in self.engs if e.count > 0]
        toks += [("dma", i, v) for i, (s, v) in enumerate(self.dsems) if v > 0]
        for e in self.engs:
            for t in toks:
                if t[0] == "eng" and t[1] is e:
                    continue
                self._wait(e, t)

    def mm(self, out, lhsT, rhs, start=True, stop=True):
        return self.I(self.pe, "matmul", out=out, lhsT=lhsT, rhs=rhs, start=start, stop=stop)

    def tr(self, out, in_, ident):
        return self.I(self.pe, "transpose", out=out, in_=in_, identity=ident)


class Ring:
    def __init__(self, tiles):
        self.tiles = tiles
        self.i = 0

    def next(self):
        t = self.tiles[self.i]
        self.i = (self.i + 1) % len(self.tiles)
        return t


def _consts():
    c = {}
    c["identb"] = np.eye(128, dtype=np.float32).astype(ml_dtypes.bfloat16)
    c["identf"] = np.eye(128, dtype=np.float32)
    k = np.arange(64)
    ang = 2 * np.pi * np.outer(k, k) / 64.0
    C64 = np.cos(ang) / 8.0
    S64 = -np.sin(ang) / 8.0
    bdc = np.zeros((128, 128)); bds = np.zeros((128, 128))
    for g in range(2):
        bdc[g * 64:(g + 1) * 64, g * 64:(g + 1) * 64] = C64
        bds[g * 64:(g + 1) * 64, g * 64:(g + 1) * 64] = S64
    c["bdc"] = bdc.astype(np.float32).astype(ml_dtypes.bfloat16)
    c["bds"] = bds.astype(np.float32).astype(ml_dtypes.bfloat16)
    t = np.arange(SEQ)
    row = (t // 64).astype(np.float64); col = (t % 64).astype(np.float64)
    inv = 10000.0 ** (-np.arange(8) / 8.0)
    ang = np.concatenate([row[:, None] * inv, col[:, None] * inv], -1)
    bf = lambda a: a.astype(np.float32).astype(ml_dtypes.bfloat16)
    k1 = np.arange(128); a1 = 2 * np.pi * np.outer(k1, k1) / 128.0
    c["f_c1"] = bf(np.cos(a1) / np.sqrt(128.0)); c["f_s1"] = bf(np.sin(a1) / np.sqrt(128.0))
    c["f_ns1"] = bf(-np.sin(a1) / np.sqrt(128.0))
    t2 = np.arange(64); atw = 2 * np.pi * np.outer(k1, t2) / 8192.0
    c["f_twc"] = np.cos(atw).astype(np.float32); c["f_tws"] = np.sin(atw).astype(np.float32)
    a2 = 2 * np.pi * np.outer(t2, t2) / 64.0
    c["f_c2"] = bf(np.cos(a2) / 8.0); c["f_s2"] = bf(np.sin(a2) / 8.0)
    tc = np.arange(256); ac = 2 * np.pi * np.outer(tc, tc) / 256.0
    c["f_cc"] = bf(np.cos(ac) / 16.0); c["f_sc"] = bf(np.sin(ac) / 16.0)
    ii_ = np.arange(128)
    c["utri"] = bf((ii_[:, None] < ii_[None, :]).astype(np.float32))
    c["onesb"] = bf(np.ones((128, 128)))
    capv = np.zeros((128, 32), np.float32); capv[:, 0:16] = CAPC; capv[:, 16:32] = CAPX
    c["capv"] = capv
    c["jrev"] = np.eye(128, dtype=np.float32)[::-1].copy()
    ii = np.arange(128)
    c["trif"] = (ii[:, None] <= ii[None, :]).astype(np.float32)
    c["trib"] = (ii[:, None] >= ii[None, :]).astype(np.float32)
    c["rcos"] = np.cos(ang).astype(np.float32)
    c["rsin"] = np.sin(ang).astype(np.float32)
    return c


WNAMES = [("ada_w", [DEPTH, D, 6 * D]), ("ada_b", [DEPTH, 6 * D]), ("norm1_g", [DEPTH, D]),
          ("norm2_g", [DEPTH, D]), ("w_in", [DEPTH, D, INC]), ("m_conv_w", [DEPTH, 3, 512]),
          ("m_conv_b", [DEPTH, 512]), ("m_ib", [DEPTH, 2, 4]), ("m_fb", [DEPTH, 2, 4]),
          ("m_norm_g", [DEPTH, 256]), ("a_qnorm_g", [DEPTH, 384]), ("a_wq_up", [DEPTH, 384, 768]),
          ("a_kvnorm_g", [DEPTH, 256]), ("a_wkv_up", [DEPTH, 256, 1024]), ("w_out", [DEPTH, D, D]),
          ("router_w", [DEPTH, D, NE]), ("e_w_gate", [DEPTH, NE, D, D]), ("e_w_up", [DEPTH, NE, D, D]),
          ("e_w_down", [DEPTH, NE, D, D]), ("final_g", [D])]


def build(nlayers=DEPTH, dbg=False, tiles=None, stages=None, lite=False, heads=None, qgroups=None):
    nc = bass.Bass("TRN2", target_bir_lowering=False)
    tiles = list(range(NT)) if tiles is None else tiles
    inp = {}
    inp["x"] = nc.dram_tensor("x", [SEQ, D], F32, kind="ExternalInput").ap()
    inp["c"] = nc.dram_tensor("c", [D], F32, kind="ExternalInput").ap()
    inp["ctx"] = nc.dram_tensor("ctx", [CTX, D], F32, kind="ExternalInput").ap()
    inp["c_ctx"] = nc.dram_tensor("c_ctx", [D], F32, kind="ExternalInput").ap()
    for n, shp in WNAMES:
        shp = list(shp)
        if n != "final_g":
            shp[0] = nlayers
        if lite and n.startswith("e_w_"):
            shp[1] = 1
        inp[n] = nc.dram_tensor(n, shp, F32, kind="ExternalInput").ap()
    cn = _consts()
    for n, a in cn.items():
        inp[n] = nc.dram_tensor("k_" + n, list(a.shape), BF16 if a.dtype == ml_dtypes.bfloat16 else F32,
                                kind="ExternalInput").ap()
    out = nc.dram_tensor("out", [SEQ, D], F32, kind="ExternalOutput").ap()
    skind = "ExternalOutput" if dbg else "Internal"
    scr = {}

    def scratch(name, shape, dt):
        scr[name] = nc.dram_tensor(name, shape, dt, kind=skind).ap()
        return scr[name]

    XR = scratch("XR", [TOK, D], F32)
    MOD = scratch("MOD", [DEPTH, 2, 6 * D], F32)
    FAB = scratch("FAB", [TOK, 512], BF16)
    QKT = scratch("QKT", [512, TOK], F32)
    VOG = scratch("VOG", [TOK, 528], F32)
    QT = scratch("QT", [8, 96, TOK], BF16)
    KT = scratch("KT", [8, 96, TOK], BF16)
    VA = scratch("VA", [TOK, 8, 65], BF16)
    MIXT = scratch("MIXT", [D, TOK], BF16)
    HFB = scratch("HFB", [2, TOK, 256], F32)
    FY = scratch("FY", [128, 64, 512], BF16)
    XN2 = scratch("XN2", [TOK, D], BF16)
    XEx = [scratch("XEx%d" % e, [CAPX, D], BF16) for e in range(NE)]
    XEc = [scratch("XEc%d" % e, [CAPC, D], BF16) for e in range(NE)]
    YEx = [scratch("YEx%d" % e, [CAPX, D], BF16) for e in range(NE)]
    YEc = [scratch("YEc%d" % e, [CAPC, D], BF16) for e in range(NE)]

    es = ExitStack()
    with es:
        P = Prog(nc, es)
        pe, act, dve, pool, sp = P.pe, P.act, P.dve, P.pool, P.sp
        bcreg = {}
        for cap_ in (CAPC, CAPX):
            bcreg[cap_] = nc.gpsimd.alloc_register("bc%d" % cap_)
            nc.gpsimd.reg_mov(bcreg[cap_], cap_ - 1)
        identb = P.sb(es, "identb", [128, 128], BF16)
        identf = P.sb(es, "identf", [128, 128], F32)
        P.dma(sp, identb[:], inp["identb"][:, :])
        P.dma(sp, identf[:], inp["identf"][:, :])
        pall = es.enter_context(nc.psum_tensor("pall", [128, 8, 512], F32))

        class BankView:
            def __init__(self, b):
                self.b = b
                self.res = Res("bank%d" % b, excl=True)

            def __getitem__(self, idx):
                if not isinstance(idx, tuple):
                    idx = (idx, slice(None))
                return V(pall[idx[0], self.b, idx[1]], self.res)

        bviews = [BankView(b) for b in range(8)]
        banks = Ring(bviews[0:5])
        accb = Ring(bviews[5:8])
        epsc = P.sb(es, "epsc", [128, 1], F32)
        onec = P.sb(es, "onec", [128, 128], F32)
        P.I(dve, "memset", onec[:], 1.0, _writes=[onec.res])
        jrev = P.sb(es, "jrev", [128, 128], F32)
        trif = P.sb(es, "trif", [128, 128], F32)
        trib = P.sb(es, "trib", [128, 128], F32)
        P.dma(sp, jrev[:], inp["jrev"][:, :])
        P.dma(sp, trif[:], inp["trif"][:, :])
        P.dma(sp, trib[:], inp["trib"][:, :])
        P.I(dve, "memset", epsc[:], EPS, _writes=[epsc.res])

        def bank_bf(shape3):
            b = banks.next()
            v = b[:].map(lambda a: a.bitcast(BF16))
            if shape3 is not None:
                v = v.map(lambda a: a[:, 0:shape3[0] * shape3[1]].rearrange("p (a b) -> p a b", b=shape3[1]))
            return v

        P.dma(sp, XR[0:CTX, :], inp["ctx"][:, :])
        for q in range(4):
            P.dma(sp, XR[CTX + q * 2048:CTX + (q + 1) * 2048, :], inp["x"][q * 2048:(q + 1) * 2048, :])

        with ExitStack() as sa:
            cc = P.sb(sa, "cc", [128, 2, 8], F32)
            cs = P.sb(sa, "cs", [128, 2, 8], F32)
            P.dma(sp, cc[:, 0, :], inp["c"].rearrange("(p k) -> p k", k=8))
            P.dma(sp, cc[:, 1, :], inp["c_ctx"].rearrange("(p k) -> p k", k=8))
            P.I(act, "activation", out=cs[:], in_=cc[:], func=AF.Silu)
            adab = P.sb(sa, "adab", [1, 6 * D], F32)
            awr = P.ring(sa, "aw", [128, 8, 512], F32, 3)
            mrow = P.ring(sa, "mrow", [1, 512], F32, 4)
            for l in range(nlayers):
                P.dma(sp, adab[:], inp["ada_b"][l:l + 1, :])
                awv = inp["ada_w"][l].rearrange("(p k) n -> p k n", k=8)
                for j in range(12):
                    aw = awr.next()
                    P.dma(sp if j % 2 == 0 else pool, aw[:], awv[:, :, j * 512:(j + 1) * 512])
                    for v in range(2):
                        b = banks.next()
                        for k in range(8):
                            P.mm(b[0:1, :], cs[:, v, k:k + 1], aw[:, k, :], start=(k == 0), stop=(k == 7))
                        mr = mrow.next()
                        P.I(dve, "tensor_tensor", out=mr[:], in0=b[0:1, :], in1=adab[:, j * 512:(j + 1) * 512],
                            op=ALU.add)
                        P.dma(sp, MOD[l, v:v + 1, j * 512:(j + 1) * 512], mr[:])
            P.barrier()
        if stages == "A":
            P.barrier()
            return nc, cn, scr

        for l in range(nlayers):
            with ExitStack() as sl:
                colv = P.sb(sl, "colv", [128, 12, 8], F32)
                for v in range(2):
                    P.dma(sp, colv[:, v * 6:(v + 1) * 6, :],
                          MOD[l, v, :].rearrange("(s k p) -> p s k", p=128, k=8), allow_slow_non_contiguous=True)
                n1g = P.sb(sl, "n1g", [128, 8], F32)
                n2g = P.sb(sl, "n2g", [128, 8], F32)
                P.dma(sp, n1g[:], inp["norm1_g"][l].rearrange("(k p) -> p k", p=128), allow_slow_non_contiguous=True)
                P.dma(sp, n2g[:], inp["norm2_g"][l].rearrange("(k p) -> p k", p=128), allow_slow_non_contiguous=True)
                Amod = P.sb(sl, "Amod", [128, 4, 8], F32)
                for v in range(2):
                    P.I(dve, "scalar_tensor_tensor", out=Amod[:, v, :], in0=colv[:, v * 6 + 1, :], scalar=1.0,
                        in1=n1g[:], op0=ALU.add, op1=ALU.mult)
                    P.I(dve, "scalar_tensor_tensor", out=Amod[:, 2 + v, :], in0=colv[:, v * 6 + 4, :], scalar=1.0,
                        in1=n2g[:], op0=ALU.add, op1=ALU.mult)
                qg = P.sb(sl, "qg", [128, 3], F32)
                kvg = P.sb(sl, "kvg", [128, 2], F32)
                P.dma(sp, qg[:], inp["a_qnorm_g"][l].rearrange("(k p) -> p k", p=128), allow_slow_non_contiguous=True)
                P.dma(sp, kvg[:], inp["a_kvnorm_g"][l].rearrange("(k p) -> p k", p=128), allow_slow_non_contiguous=True)

                with ExitStack() as sB:
                    Win = P.sb(sB, "Win", [128, 8, INC], BF16)
                    for k in range(8):
                        P.dma(pool, Win[:, k, :], inp["w_in"][l, k * 128:(k + 1) * 128, :])
                    Wq = P.sb(sB, "Wq", [128, 3, 768], BF16)
                    P.dma(pool, Wq[:], inp["a_wq_up"][l].rearrange("(k p) n -> p k n", p=128))
                    Wkv = P.sb(sB, "Wkv", [128, 2, 1024], BF16)
                    P.dma(pool, Wkv[:], inp["a_wkv_up"][l].rearrange("(k p) n -> p k n", p=128))
                    bdc = P.sb(sB, "bdc", [128, 128], BF16)
                    bds = P.sb(sB, "bds", [128, 128], BF16)
                    P.dma(sp, bdc[:], inp["bdc"][:, :])
                    P.dma(sp, bds[:], inp["bds"][:, :])
                    xr_ = P.ring(sB, "xt", [128, D], F32, 2)
                    junk = P.sb(sB, "junk", [128, D], BF16)
                    st_ = P.ring(sB, "st", [128, 8], F32, 3)
                    xn_ = P.ring(sB, "xn", [128, D], BF16, 2)
                    hT_ = P.ring(sB, "hT", [128, 8, 128], BF16, 2)
                    ub_ = P.ring(sB, "ub", [128, 1040], F32, 2)
                    fb_ = P.ring(sB, "fb", [128, 256], BF16, 2)
                    fT_ = P.ring(sB, "fT", [128, 2, 128], BF16, 2)
                    ab_ = P.ring(sB, "ab", [128, 512], BF16, 2)
                    qkT_ = P.ring(sB, "qkT", [128, 4, 128], F32, 2)
                    cqn_ = P.ring(sB, "cqn", [128, 640], BF16, 2)
                    cT_ = P.ring(sB, "cT", [128, 5, 128], BF16, 2)
                    kr_ = P.ring(sB, "kr", [128, 32], F32, 2)
                    qs_ = P.ring(sB, "qs", [128, 8, 96], F32, 2)
                    rt_ = P.ring(sB, "rt", [128, 4, 8, 16], F32, 2)
                    cs_ = P.ring(sB, "cs", [128, 2, 16], F32, 2)
                    qb_ = P.ring(sB, "qb", [128, 8, 96], BF16, 2)
                    kb_ = P.ring(sB, "kb", [128, 8, 96], BF16, 2)
                    va_ = P.ring(sB, "va", [128, 8, 65], BF16, 2)
                    for t_ in va_.tiles:
                        P.I(dve, "memset", t_[:], 1.0, _writes=[t_.res])
                    qT_ = P.ring(sB, "qT", [96, 8, 128], BF16, 2)
                    kT_ = P.ring(sB, "kT", [96, 8, 128], BF16, 2)

                    for i in tiles:
                        isx = i >= 2
                        v = 0 if isx else 1
                        tok0 = i * 128
                        xt = xr_.next()
                        P.dma(sp, xt[:], XR[tok0:tok0 + 128, :])
                        st = st_.next()
                        P.I(act, "activation", out=junk[:], in_=xt[:], func=AF.Square, accum_out=st[:, 0:1])
                        P.I(act, "activation", out=st[:, 1:2], in_=st[:, 0:1], func=AF.Sqrt, scale=1.0 / D, bias=epsc[:])
                        P.I(dve, "reciprocal", out=st[:, 2:3], in_=st[:, 1:2])
                        xn = xn_.next()
                        P.I(dve, "tensor_scalar", out=xn[:], in0=xt[:], scalar1=st[:, 2:3], scalar2=None, op0=ALU.mult)
                        pT = bank_bf((8, 128))
                        for k in range(8):
                            P.tr(pT.map(lambda a: a[:, k, :]), xn[:, k * 128:(k + 1) * 128], identb[:])
                        hT = hT_.next()
                        bc = lambda vv: vv.map(lambda a: a.unsqueeze(2).to_broadcast([128, 8, 128]))
                        P.I(dve, "tensor_tensor", out=hT[:], in0=pT, in1=bc(Amod[:, v, :]), op=ALU.mult)
                        P.I(pool, "tensor_tensor", out=hT[:], in0=hT[:], in1=bc(colv[:, v * 6 + 0, :]), op=ALU.add)
                        ub = [banks.next() for _ in range(4)]
                        for n in range(4):
                            lo, hi = n * 512, min(INC, (n + 1) * 512)
                            for k in range(8):
                                P.mm(ub[n][:, 0:hi - lo], hT[:, k, :], Win[:, k, lo:hi], start=(k == 0), stop=(k == 7))
                        fb = fb_.next()
                        P.I(act, "activation", out=fb[:], in_=ub[0][:, 0:256], func=AF.Copy)
                        ubuf = ub_.next()
                        P.I(act, "activation", out=ubuf[:, 0:256], in_=ub[0][:, 256:512], func=AF.Copy)
                        P.I(dve, "tensor_copy", out=ubuf[:, 256:768], in_=ub[1][:, :])
                        P.I(act, "activation", out=ubuf[:, 768:1040], in_=ub[2][:, 0:272], func=AF.Copy)
                        P.I(act, "activation", out=junk[:, 0:240], in_=ub[2][:, 272:512], func=AF.Square,
                            accum_out=st[:, 3:4])
                        P.I(act, "activation", out=junk[:, 240:384], in_=ub[3][:, 0:144], func=AF.Square,
                            accum_out=st[:, 4:5])
                        P.I(act, "activation", out=junk[:, 384:640], in_=ub[3][:, 144:400], func=AF.Square,
                            accum_out=st[:, 5:6])
                        P.I(dve, "tensor_tensor", out=st[:, 3:4], in0=st[:, 3:4], in1=st[:, 4:5], op=ALU.add)
                        P.I(act, "activation", out=st[:, 4:5], in_=st[:, 3:4], func=AF.Sqrt, scale=1.0 / 384, bias=epsc[:])
                        P.I(dve, "reciprocal", out=st[:, 6:7], in_=st[:, 4:5])
                        P.I(act, "activation", out=st[:, 3:4], in_=st[:, 5:6], func=AF.Sqrt, scale=1.0 / 256, bias=epsc[:])
                        P.I(dve, "reciprocal", out=st[:, 7:8], in_=st[:, 3:4])
                        cqn = cqn_.next()
                        P.I(dve, "tensor_scalar", out=cqn[:, 0:240], in0=ub[2][:, 272:512], scalar1=st[:, 6:7],
                            scalar2=None, op0=ALU.mult)
                        P.I(dve, "tensor_scalar", out=cqn[:, 240:384], in0=ub[3][:, 0:144], scalar1=st[:, 6:7],
                            scalar2=None, op0=ALU.mult)
                        P.I(dve, "tensor_scalar", out=cqn[:, 384:640], in0=ub[3][:, 144:400], scalar1=st[:, 7:8],
                            scalar2=None, op0=ALU.mult)
                        kr = kr_.next()
                        P.I(act, "activation", out=kr[:], in_=ub[3][:, 400:432], func=AF.Copy)
                        P.dma(sp, VOG[tok0:tok0 + 128, :], ubuf[:, 512:1040])
                        pf = bank_bf((2, 128))
                        for cc in range(2):
                            P.tr(pf.map(lambda a: a[:, cc, :]), fb[:, cc * 128:(cc + 1) * 128], identb[:])
                        fT = fT_.next()
                        P.I(act, "activation", out=fT[:], in_=pf, func=AF.Copy)
                        abp = banks.next()
                        for cc in range(2):
                            P.mm(abp[:, cc * 128:(cc + 1) * 128], fT[:, cc, :], bdc[:])
                            P.mm(abp[:, 256 + cc * 128:256 + (cc + 1) * 128], fT[:, cc, :], bds[:])
                        ab = ab_.next()
                        P.I(act, "activation", out=ab[:], in_=abp[:], func=AF.Copy)
                        P.dma(sp, FAB[tok0:tok0 + 128, :], ab[:])
                        pq = banks.next()
                        for cc in range(4):
                            P.tr(pq[:, cc * 128:(cc + 1) * 128], ubuf[:, cc * 128:(cc + 1) * 128], identf[:])
                        qkT = qkT_.next()
                        P.I(dve, "tensor_copy", out=qkT[:], in_=pq[:].map(lambda a: a.rearrange("p (c t) -> p c t", t=128)))
                        P.dma(sp, QKT.rearrange("(c p) t -> p c t", p=128)[:, :, tok0:tok0 + 128], qkT[:])
                        pc = bank_bf((5, 128))
                        for cc in range(5):
                            P.tr(pc.map(lambda a: a[:, cc, :]), cqn[:, cc * 128:(cc + 1) * 128], identb[:])
                        cT = cT_.next()
                        P.I(dve, "tensor_tensor", out=cT[:, 0:3, :], in0=pc.map(lambda a: a[:, 0:3, :]),
                            in1=qg[:].map(lambda a: a.unsqueeze(2).to_broadcast([128, 3, 128])), op=ALU.mult)
                        P.I(dve, "tensor_tensor", out=cT[:, 3:5, :], in0=pc.map(lambda a: a[:, 3:5, :]),
                            in1=kvg[:].map(lambda a: a.unsqueeze(2).to_broadcast([128, 2, 128])), op=ALU.mult)
                        qp = [banks.next(), banks.next()]
                        for n, (lo, hi) in enumerate([(0, 512), (512, 768)]):
                            for kk in range(3):
                                P.mm(qp[n][:, 0:hi - lo], cT[:, kk, :], Wq[:, kk, lo:hi], start=(kk == 0), stop=(kk == 2))
                        kvp = [banks.next(), banks.next()]
                        for n in range(2):
                            for kk in range(2):
                                P.mm(kvp[n][:, :], cT[:, 3 + kk, :], Wkv[:, kk, n * 512:(n + 1) * 512],
                                     start=(kk == 0), stop=(kk == 1))
                        qs = qs_.next()
                        sc = 96.0 ** -0.5
                        qsf = qs[:].map(lambda a: a.rearrange("p h e -> p (h e)"))
                        P.I(act, "activation", out=qsf.map(lambda a: a[:, 0:512]), in_=qp[0][:, :], func=AF.Copy, scale=sc)
                        P.I(act, "activation", out=qsf.map(lambda a: a[:, 512:768]), in_=qp[1][:, 0:256], func=AF.Copy, scale=sc)
                        qb = qb_.next()
                        kb = kb_.next()
                        va = va_.next()
                        P.I(act, "activation", out=qb[:, :, 0:64], in_=qs[:, :, 0:64], func=AF.Copy)
                        if isx:
                            cst = cs_.next()
                            P.dma(sp, cst[:, 0, :], inp["rcos"][tok0 - CTX:tok0 - CTX + 128, :])
                            P.dma(sp, cst[:, 1, :], inp["rsin"][tok0 - CTX:tok0 - CTX + 128, :])
                            rt = rt_.next()
                            cb = cst[:, 0, :].map(lambda a: a.unsqueeze(1).to_broadcast([128, 8, 16]))
                            sbn = cst[:, 1, :].map(lambda a: a.unsqueeze(1).to_broadcast([128, 8, 16]))
                            P.I(dve, "tensor_tensor", out=rt[:, 0], in0=qs[:, :, 64:80], in1=cb, op=ALU.mult)
                            P.I(dve, "tensor_tensor", out=rt[:, 1], in0=qs[:, :, 80:96], in1=sbn, op=ALU.mult)
                            P.I(pool, "tensor_tensor", out=rt[:, 2], in0=qs[:, :, 64:80], in1=sbn, op=ALU.mult)
                            P.I(pool, "tensor_tensor", out=rt[:, 3], in0=qs[:, :, 80:96], in1=cb, op=ALU.mult)
                            P.I(dve, "tensor_tensor", out=qb[:, :, 64:80], in0=rt[:, 0], in1=rt[:, 1], op=ALU.subtract)
                            P.I(dve, "tensor_tensor", out=qb[:, :, 80:96], in0=rt[:, 2], in1=rt[:, 3], op=ALU.add)
                            P.I(dve, "tensor_tensor", out=rt[:, 0, 0, :], in0=kr[:, 0:16], in1=cst[:, 0, :], op=ALU.mult)
                            P.I(dve, "tensor_tensor", out=rt[:, 1, 0, :], in0=kr[:, 16:32], in1=cst[:, 1, :], op=ALU.mult)
                            P.I(dve, "tensor_tensor", out=rt[:, 2, 0, :], in0=kr[:, 0:16], in1=cst[:, 1, :], op=ALU.mult)
                            P.I(dve, "tensor_tensor", out=rt[:, 3, 0, :], in0=kr[:, 16:32], in1=cst[:, 0, :], op=ALU.mult)
                            P.I(dve, "tensor_tensor", out=kr[:, 0:16], in0=rt[:, 0, 0, :], in1=rt[:, 1, 0, :], op=ALU.subtract)
                            P.I(dve, "tensor_tensor", out=kr[:, 16:32], in0=rt[:, 2, 0, :], in1=rt[:, 3, 0, :], op=ALU.add)
                        else:
                            P.I(act, "activation", out=qb[:, :, 64:96], in_=qs[:, :, 64:96], func=AF.Copy)
                        kv3 = lambda n: kvp[n][:, :].map(lambda a: a.rearrange("p (h e) -> p h e", e=128))
                        for n in range(2):
                            P.I(act, "activation", out=kb[:, n * 4:(n + 1) * 4, 0:64], in_=kv3(n).map(lambda a: a[:, :, 0:64]),
                                func=AF.Copy)
                            P.I(dve, "tensor_copy", out=va[:, n * 4:(n + 1) * 4, 0:64], in_=kv3(n).map(lambda a: a[:, :, 64:128]))
                        P.I(pool, "tensor_copy", out=kb[:, :, 64:96],
                            in_=kr[:].map(lambda a: a.unsqueeze(1).to_broadcast([128, 8, 32])))
                        P.dma(sp, VA[tok0:tok0 + 128, :, :], va[:])
                        for (src, ring_, dst) in ((qb, qT_, QT), (kb, kT_, KT)):
                            pt = banks.next()
                            ptv = pt[0:96, :].map(lambda a: a.bitcast(BF16)[:, 0:1024].rearrange("p (h t) -> p h t", t=128))
                            for h in range(8):
                                P.tr(ptv.map(lambda a: a[:, h, :]), src[:, h, :], identb[:])
                            tt = ring_.next()
                            P.I(act, "activation", out=tt[:], in_=ptv, func=AF.Copy)
                            P.dma(sp, dst.rearrange("h r t -> r h t")[:, :, tok0:tok0 + 128], tt[:])
                    P.barrier()
                if stages == "B":
                    return nc, cn, scr

                with_ctx = l < DEPTH - 1
                with ExitStack() as sC:
                    KTh_ = P.ring(sC, "KTh", [96, TOK], BF16, 2)
                    VAh_ = P.ring(sC, "VAh", [128, NT, 65], BF16, 2)
                    QTg_ = P.ring(sC, "QTg", [96, 512], BF16, 4)
                    PT_ = P.ring(sC, "PT", [128, 512], BF16, 6)
                    Usb_ = P.ring(sC, "Usb", [65, 512], F32, 2)
                    rec_ = P.ring(sC, "rec", [64, 512], F32, 2)
                    yc_ = P.ring(sC, "yc", [64, 512], BF16, 2)
                    esel = P.sb(sC, "esel", [65, 64], F32)
                    P.I(dve, "memset", esel[:], 0.0, _writes=[esel.res])
                    P.I(dve, "memset", esel[64:65, :], 1.0, _writes=[esel.res])
                    hlist = list(heads if heads is not None else range(8))
                    groups = [(CTX + g * 512, 512, list(range(NT))) for g in range(16)]
                    if qgroups is not None:
                        groups = [groups[g] for g in qgroups]
                    if with_ctx:
                        groups.append((0, 256, [0, 1]))
                    units = []
                    for h in hlist:
                        for gi, (q0, nq, ktiles) in enumerate(groups):
                            for jj, j in enumerate(ktiles):
                                units.append((h, gi, q0, nq, jj, j, len(ktiles)))
                    hd = {}
                    qt = {}
                    accs = {}
                    sbk = {}

                    def ensure_head(h):
                        if h in hd or h not in hlist:
                            return
                        KTh = KTh_.next()
                        VAh = VAh_.next()
                        P.dma(sp, KTh[:], KT[h, :, :])
                        for j0 in range(0, NT, 11):
                            P.dma(sp, VAh[:, j0:j0 + 11, :],
                                  VA[j0 * 128:(j0 + 11) * 128, h, :].rearrange("(j p) e -> p j e", p=128))
                        hd[h] = (KTh, VAh)

                    def ensure_q(h, gi):
                        if (h, gi) in qt or h not in hlist or gi >= len(groups):
                            return
                        q0, nq, _ = groups[gi]
                        QTg = QTg_.next()
                        P.dma(sp, QTg[:, 0:nq], QT[h, :, q0:q0 + nq])
                        qt[(h, gi)] = QTg

                    def finalize(h, gi):
                        q0, nq, _ = groups[gi]
                        acc = accs.pop((h, gi))
                        Usb = Usb_.next()
                        P.I(dve, "tensor_copy", out=Usb[:, 0:nq], in_=acc[0:65, 0:nq])
                        rp = acc
                        P.mm(rp[0:64, 0:nq], esel[:], Usb[:, 0:nq])
                        rec = rec_.next()
                        P.I(dve, "reciprocal", out=rec[:, 0:nq], in_=rp[0:64, 0:nq])
                        yc = yc_.next()
                        P.I(dve, "tensor_tensor", out=yc[:, 0:nq], in0=Usb[0:64, 0:nq], in1=rec[:, 0:nq], op=ALU.mult)
                        P.dma(sp, MIXT[512 + h * 64:512 + (h + 1) * 64, q0:q0 + nq], yc[:, 0:nq])

                    pairs = [units[2 * p_:2 * p_ + 2] for p_ in range(len(units) // 2)]
                    assert len(units) % 2 == 0
                    PT2_ = P.ring(sC, "PT2", [128, 2, 512], BF16, 5)
                    pend = []
                    sbk2 = {}
                    PLOOK = 2
                    acc2 = Ring(bviews[6:8])
                    for pi in range(len(pairs) + PLOOK + 3):
                        if pi < len(pairs):
                            pb2 = (pi % 3) * 2
                            for u_, (h, gi, q0, nq, jj, j, nk) in enumerate(pairs[pi]):
                                if jj == 0:
                                    ensure_head(h)
                                    ensure_q(h, gi)
                                    if gi == 0:
                                        nh = hlist.index(h) + 1
                                        if nh < len(hlist):
                                            ensure_head(hlist[nh])
                                    if gi + 1 < len(groups):
                                        ensure_q(h, gi + 1)
                                    else:
                                        nh = hlist.index(h) + 1
                                        if nh < len(hlist):
                                            ensure_q(hlist[nh], 0)
                                bv = bviews[pb2 + u_]
                                P.mm(bv[:, 0:nq], hd[h][0][:, j * 128:(j + 1) * 128], qt[(h, gi)][:, 0:nq])
                            sbk2[pi] = pb2
                        while pend and pend[0][0] <= pi:
                            _, h_, gi_ = pend.pop(0)
                            finalize(h_, gi_)
                        k = pi - PLOOK
                        if 0 <= k < len(pairs):
                            pb2 = sbk2.pop(k)
                            (h, gi, q0, nq, jj, j, nk) = pairs[k][0]
                            assert pairs[k][1][0] == h and pairs[k][1][1] == gi
                            if jj == 0:
                                accs[(h, gi)] = acc2.next()
                            acc = accs[(h, gi)]
                            PT2 = PT2_.next()
                            r0, r1 = bviews[pb2].res, bviews[pb2 + 1].res
                            P.I(act, "activation", out=PT2[:, :, 0:nq], in_=V(pall[:, pb2:pb2 + 2, 0:nq], r0), func=AF.Exp,
                                _reads=[r1])
                            for u_, (h, gi, q0, nq, jj, j, nk) in enumerate(pairs[k]):
                                P.mm(acc[0:65, 0:nq], hd[h][1][:, j, :], PT2[:, u_, 0:nq], start=(jj == 0), stop=(jj == nk - 1))
                                if jj == nk - 1:
                                    pend.append((pi + 1, h, gi))
                                    qt.pop((h, gi), None)
                    pending = pend
                    assert not pending and not accs
                    P.barrier()
                if stages == "C":
                    return nc, cn, scr

                def rtile(i):
                    return 1 - i if i < 2 else 67 - i

                def qkcol(i):
                    return i * 128 + (2 if i < 2 else 4)

                if os.environ.get("M2CUT") == "-1":
                    P.barrier()
                    return nc, cn, scr

                with ExitStack() as sM:
                    TKS = P.sb(sM, "TKS", [128, NT, 16], F32)
                    ECB = P.sb(sM, "ECB", [128, 8, NT], F32)
                    with ExitStack() as s2:
                        GI = P.sb(s2, "GI", [8, TOK], F32)
                        GF = P.sb(s2, "GF", [8, TOK], F32)
                        MM = P.sb(s2, "MM", [8, TOK], F32)
                        MST = P.sb(s2, "MST", [16, TOK], F32)
                        EF = P.sb(s2, "EF", [40, TOK], F32)
                        gall = P.sb(s2, "gall", [128, NT, 16], F32)
                        gb = P.sb(s2, "gb", [8, 4], F32)
                        ecs = P.sb(s2, "ecs", [8, NT], F32)
                        Dg = P.sb(s2, "Dg", [8, 8, NT], F32)
                        tk_ = P.ring(s2, "tk", [128, 40], F32, 2)
                        P.I(pool, "memset", EF[:], 0.0, _writes=[EF.res])

                        if os.environ.get("M2CUT") == "-0.5":
                            P.barrier()
                            return nc, cn, scr
                        P.dma(sp, gall[:], VOG[:, 512:528].rearrange("(j p) g -> p j g", p=128))

                        if os.environ.get("M2CUT") == "-0.3":
                            P.barrier()
                            return nc, cn, scr
                        grow = P.sb(s2, "grow", [1, 16], F32)
                        P.dma(sp, grow[:, 0:8], inp["m_ib"][l:l + 1].rearrange("o d h -> o (d h)"))
                        P.dma(sp, grow[:, 8:16], inp["m_fb"][l:l + 1].rearrange("o d h -> o (d h)"))
                        pgb = banks.next()
                        P.mm(pgb[0:8, 0:1], grow[:, 0:8], onec[0:1, 0:1])
                        P.mm(pgb[0:8, 1:2], grow[:, 8:16], onec[0:1, 0:1])
                        P.I(dve, "tensor_copy", out=gb[:, 0:2], in_=pgb[0:8, 0:2])
                        P.I(dve, "tensor_scalar", out=gb[:, 2:3], in0=gb[:, 1:2], scalar1=-1.0, scalar2=None, op0=ALU.mult)

                        if os.environ.get("M2CUT") == "0":
                            P.barrier()
                            return nc, cn, scr
                        for i in range(NT):
                            pg = banks.next()
                            sk = os.environ.get("M2SKIP", "")
                            if "a" not in sk:
                                P.mm(pg[0:16, 0:128], gall[:, i, :], identf[:])
                            if "b" not in sk:
                                P.mm(pg[0:16, 128:256], gall[:, i, :], jrev[:])
                            if "c" not in sk:
                                P.I(act, "activation", out=MST[0:16, i * 128:(i + 1) * 128], in_=pg[0:16, 0:128], func=AF.Copy)
                            r_ = rtile(i)
                            if "d" not in sk:
                                P.I(dve, "tensor_copy", out=EF[0:16, r_ * 128:(r_ + 1) * 128], in_=pg[0:16, 128:256])

                        if os.environ.get("M2CUT") == "0b":
                            P.barrier()
                            return nc, cn, scr
                        P.dma(sp, GI[0:4, :], MST[0:4, :])
                        P.dma(sp, GF[0:4, :], MST[4:8, :])
                        P.dma(sp, GI[4:8, :], EF[8:12, :])
                        P.dma(sp, GF[4:8, :], EF[12:16, :])

                        if os.environ.get("M2CUT") == "1":
                            P.barrier()
                            return nc, cn, scr
                        P.I(dve, "tensor_scalar", out=GI[:], in0=GI[:], scalar1=gb[:, 0:1], scalar2=None, op0=ALU.add)
                        P.I(act, "activation", out=GF[:], in_=GF[:], func=AF.Exp, scale=-1.0, bias=gb[:, 2:3])
                        P.I(act, "activation", out=GF[:], in_=GF[:], func=AF.Ln, bias=onec[0:8, 0:1])
                        onesb = onec[0:8, 0:1].map(lambda a: a.to_broadcast([8, TOK]))
                        P.I(dve, "tensor_tensor_scan", out=GF[:], data0=onesb, data1=GF[:], initial=0.0, op0=ALU.mult, op1=ALU.add)
                        P.I(dve, "tensor_tensor", out=GI[:], in0=GI[:], in1=GF[:], op=ALU.add)
                        P.I(dve, "tensor_tensor_scan", out=MM[:], data0=onesb, data1=GI[:], initial=0.0, op0=ALU.mult, op1=ALU.max)

                        if os.environ.get("M2CUT") == "2":
                            P.barrier()
                            return nc, cn, scr
                        v3 = lambda vv: vv.map(lambda a: a.rearrange("p (c t) -> p c t", t=128))
                        P.I(pool, "memset", MST[0:8, 0:128], 0.0, _writes=[MST.res])
                        P.I(dve, "tensor_copy", out=v3(MST[0:8, :]).map(lambda a: a[:, 1:NT, :]),
                            in_=v3(MM[:]).map(lambda a: a[:, 0:NT - 1, 127:128].to_broadcast([8, NT - 1, 128])))
                        P.I(dve, "tensor_tensor", out=ecs[:], in0=v3(MST[0:8, :]).map(lambda a: a[:, :, 0]),
                            in1=v3(MM[:]).map(lambda a: a[:, :, 127]), op=ALU.subtract)
                        P.I(act, "activation", out=ecs[:], in_=ecs[:], func=AF.Exp)
                        P.I(dve, "tensor_tensor", out=GI[:], in0=GI[:], in1=MST[0:8, :], op=ALU.subtract)
                        P.I(dve, "tensor_tensor", out=GF[:], in0=GF[:], in1=MST[0:8, :], op=ALU.subtract)
                        P.I(act, "activation", out=EF[0:8, :], in_=GI[:], func=AF.Exp)
                        P.I(act, "activation", out=EF[32:40, :], in_=GF[:], func=AF.Exp)

                        if os.environ.get("M2CUT") == "3":
                            P.barrier()
                            return nc, cn, scr
                        P.I(dve, "tensor_tensor", out=Dg[:],
                            in0=ecs[:].map(lambda a: a.unsqueeze(1).to_broadcast([8, 8, NT])),
                            in1=identf[0:8, 0:8].map(lambda a: a.unsqueeze(2).to_broadcast([8, 8, NT])), op=ALU.mult)
                        pe_ = [banks.next(), banks.next()]
                        Dgf = Dg[:].map(lambda a: a.rearrange("p a c -> p (a c)"))
                        hN = 4 * NT
                        for n in range(2):
                            P.mm(pe_[n][:, 0:hN], onec[0:8, :], Dgf.map(lambda a: a[:, n * hN:(n + 1) * hN]))
                            P.I(act, "activation", out=ECB[:, n * 4:(n + 1) * 4, :].map(lambda a: a.rearrange("p a c -> p (a c)")),
                                in_=pe_[n][:, 0:hN], func=AF.Copy)
                        for i in range(NT):
                            pt = banks.next()
                            P.tr(pt[:, 0:40], EF[0:40, i * 128:(i + 1) * 128], identf[0:40, 0:40])
                            r_ = rtile(i)
                            P.tr(pt[:, 64:104], EF[0:40, r_ * 128:(r_ + 1) * 128], identf[0:40, 0:40])
                            tk = tk_.next()
                            P.I(act, "activation", out=tk[:], in_=pt[:, 64:104], func=AF.Copy)
                            P.mm(pt[:, 128:168], jrev[:], tk[:])
                            tv = TKS[:, i, :].map(lambda a: a.rearrange("p (q j) -> p q j", j=8))
                            pv = lambda c0: pt[:, c0:c0 + 64].map(lambda a: a.rearrange("p (q j) -> p q j", j=32))
                            P.I(dve, "tensor_copy", out=tv.map(lambda a: a[:, :, 0:4]), in_=pv(0).map(lambda a: a[:, :, 0:4]))
                            P.I(dve, "tensor_copy", out=tv.map(lambda a: a[:, :, 4:8]), in_=pv(128).map(lambda a: a[:, :, 4:8]))
                        P.barrier()
                    PADW = TOK + 6
                    QKb = P.sb(sM, "QKb", [128, 4, PADW], BF16)
                    ktm = P.sb(sM, "ktm", [128, NT, 256], BF16)
                    with ExitStack() as s1:
                        cw = P.sb(s1, "cw", [128, 4, 3], F32)
                        cbi = P.sb(s1, "cbi", [128, 4], F32)
                        for kk in range(3):
                            P.dma(sp, cw[:, :, kk], inp["m_conv_w"][l, kk].rearrange("(c p) -> p c", p=128),
                                  allow_slow_non_contiguous=True)
                        P.dma(sp, cbi[:], inp["m_conv_b"][l].rearrange("(c p) -> p c", p=128), allow_slow_non_contiguous=True)
                        HP = 4230
                        stg_ = P.ring(s1, "stg", [128, HP], F32, 2)
                        yb_ = P.ring(s1, "ybuf", [128, HP], F32, 2)
                        for cc in range(4):
                            rows = QKT[cc * 128:(cc + 1) * 128, :]
                            for piece in range(2):
                                stg = stg_.next()
                                yb = yb_.next()
                                if piece == 0:
                                    n = 4230
                                    P.I(pool, "memset", stg[:, 0:2], 0.0, _writes=[stg.res])
                                    P.I(pool, "memset", stg[:, 258:260], 0.0, _writes=[stg.res])
                                    P.dma(sp, stg[:, 2:258], rows[:, 0:256])
                                    P.dma(sp, stg[:, 260:4230], rows[:, 256:256 + 3970])
                                    oc0 = 1
                                else:
                                    n = 4226
                                    P.dma(sp, stg[:, 0:4224], rows[:, 4224:8448])
                                    P.I(pool, "memset", stg[:, 4224:4226], 0.0, _writes=[stg.res])
                                    oc0 = 4229
                                m = n - 2
                                P.I(dve, "tensor_scalar", out=yb[:, 0:m], in0=stg[:, 1:1 + m], scalar1=cw[:, cc, 1:2],
                                    scalar2=cbi[:, cc:cc + 1], op0=ALU.mult, op1=ALU.add)
                                P.I(dve, "scalar_tensor_tensor", out=yb[:, 0:m], in0=stg[:, 0:m], scalar=cw[:, cc, 0:1],
                                    in1=yb[:, 0:m], op0=ALU.mult, op1=ALU.add)
                                P.I(dve, "scalar_tensor_tensor", out=yb[:, 0:m], in0=stg[:, 2:2 + m], scalar=cw[:, cc, 2:3],
                                    in1=yb[:, 0:m], op0=ALU.mult, op1=ALU.add)
                                if cc < 2:
                                    P.I(act, "activation", out=yb[:, 0:m], in_=yb[:, 0:m], func=AF.Silu)
                                    P.I(pool, "tensor_scalar", out=QKb[:, cc, oc0:oc0 + m], in0=yb[:, 0:m], scalar1=0.125,
                                        scalar2=None, op0=ALU.mult)
                                else:
                                    P.I(act, "activation", out=QKb[:, cc, oc0:oc0 + m], in_=yb[:, 0:m], func=AF.Silu)
                        for i in range(NT):
                            pk = bank_bf((2, 128))
                            c0 = qkcol(i)
                            for kc in range(2):
                                P.tr(pk.map(lambda a: a[:, kc, :]), QKb[:, 2 + kc, c0:c0 + 128], identb[:])
                            P.I(act, "activation", out=ktm[:, i, :].map(lambda a: a.rearrange("p (c t) -> p c t", t=128)),
                                in_=pk, func=AF.Copy)
                        P.barrier()
                    if stages == "M1":
                        return nc, cn, scr
                    with ExitStack() as s3:
                        V1 = P.sb(s3, "V1", [128, NT, 4, 65], BF16)
                        P.I(pool, "memset", V1[:], 1.0, _writes=[V1.res])
                        for j0 in range(0, NT, 11):
                            for h in range(4):
                                P.dma(pool, V1[:, j0:j0 + 11, h, 0:64],
                                      VOG[j0 * 128:(j0 + 11) * 128, h * 64:(h + 1) * 64].rearrange("(j p) e -> p j e", p=128))
                        CN = [P.sb(s3, "CN%d" % k, [128, 65], F32) for k in range(8)]
                        CNb = [P.sb(s3, "CNb%d" % k, [128, 65], BF16) for k in range(8)]
                        for k in range(8):
                            P.I(dve, "memset", CN[k][:], 0.0, _writes=[CN[k].res])
                            P.I(dve, "memset", CNb[k][:], 0.0, _writes=[CNb[k].res])
                        Sm_ = P.ring(s3, "Sm", [128, 128], BF16, 8)
                        Sr_ = P.ring(s3, "Sr", [128, 128], BF16, 8)
                        trifb = P.sb(s3, "trifb", [128, 128], BF16)
                        tribb = P.sb(s3, "tribb", [128, 128], BF16)
                        P.I(dve, "tensor_copy", out=trifb[:], in_=trif[:])
                        P.I(dve, "tensor_copy", out=tribb[:], in_=trib[:])
                        vpp_ = P.ring(s3, "vpp", [128, 65], BF16, 8)
                        dn_ = P.ring(s3, "dn", [128, 2], F32, 6)
                        tmp_ = P.ring(s3, "ctmp", [128, 65], F32, 6)
                        Hst_ = [P.ring(s3, "Hst%d" % d_, [128, 256], F32, 3) for d_ in range(2)]
                        munits = []
                        for c in range(NT):
                            for d_ in range(2):
                                i = c if d_ == 0 else (1 - c if c < 2 else 67 - c)
                                for h in range(4):
                                    munits.append((c, d_, i, h))
                        mstate = {}
                        hst_cur = {}

                        def m_phase_a(u):
                            c, d_, i, h = munits[u]
                            k = d_ * 4 + h
                            pb = (h % 2) * 64
                            c0 = qkcol(i)
                            qv = QKb[pb:pb + 64, h // 2, c0:c0 + 128]
                            kv_ = QKb[pb:pb + 64, 2 + h // 2, c0:c0 + 128]
                            sps = banks.next()
                            P.mm(sps[:, 0:128], kv_, qv)
                            Sr = Sr_.next()
                            P.I(act, "activation", out=Sr[:], in_=sps[:, 0:128], func=AF.Copy)
                            Sm = Sm_.next()
                            P.I(pool, "tensor_tensor", out=Sm[:], in0=Sr[:], in1=(trifb if d_ == 0 else tribb)[:], op=ALU.mult)
                            vpp = vpp_.next()
                            P.I(act, "activation", out=vpp[:], in_=V1[:, i, h, :], func=AF.Copy, scale=TKS[:, i, k:k + 1])
                            mstate[u] = (Sm, vpp, qv)

                        def m_phase_b(u):
                            c, d_, i, h = munits[u]
                            k = d_ * 4 + h
                            pb = (h % 2) * 64
                            Sm, vpp, qv = mstate.pop(u)
                            if h == 0:
                                hst_cur[d_] = Hst_[d_].next()
                            Hst = hst_cur[d_]
                            nd = banks.next()
                            P.mm(nd[:, 0:65], Sm[:], vpp[:], start=True, stop=False)
                            P.mm(nd[:, 0:65], qv, CNb[k][pb:pb + 64, :], start=False, stop=True)
                            P.mm(nd[0:64, 128:193], ktm[:, i, h * 64:(h + 1) * 64], vpp[:])
                            tmp = tmp_.next()
                            P.I(dve, "tensor_tensor", out=tmp[0:64, :], in0=CN[k][0:64, :],
                                in1=nd[0:64, 128:193], op=ALU.add)
                            P.I(act, "activation", out=CNb[k][pb:pb + 64, :], in_=tmp[0:64, :], func=AF.Copy,
                                scale=ECB[0:64, k, c:c + 1])
                            P.I(dve, "tensor_scalar", out=CN[k][0:64, :], in0=tmp[0:64, :],
                                scalar1=ECB[0:64, k, c:c + 1], scalar2=None, op0=ALU.mult)
                            dn = dn_.next()
                            P.I(dve, "tensor_scalar", out=dn[:, 1:2], in0=nd[:, 64:65], scalar1=-1.0,
                                scalar2=None, op0=ALU.mult)
                            P.I(dve, "scalar_tensor_tensor", out=dn[:, 0:1], in0=dn[:, 1:2], scalar=TKS[:, i, 8 + k:9 + k],
                                in1=nd[:, 64:65], op0=ALU.max, op1=ALU.max)
                            P.I(dve, "reciprocal", out=dn[:, 1:2], in_=dn[:, 0:1])
                            P.I(act, "activation", out=Hst[:, h * 64:(h + 1) * 64], in_=nd[:, 0:64], func=AF.Copy,
                                scale=dn[:, 1:2])
                            if h == 3:
                                P.dma(sp, HFB[d_, i * 128:(i + 1) * 128, :], Hst[:])

                        MLOOK = 3
                        for u in range(len(munits) + MLOOK):
                            if u < len(munits):
                                m_phase_a(u)
                            if u - MLOOK >= 0:
                                m_phase_b(u - MLOOK)
                        P.barrier()
                    if stages == "M3":
                        return nc, cn, scr
                    with ExitStack() as s4:
                        mng = P.sb(s4, "mng", [128, 256], F32)
                        P.dma(sp, mng[:], inp["m_norm_g"][l:l + 1, :].partition_broadcast(128))
                        hf_ = P.ring(s4, "hf", [128, 2, 256], F32, 2)
                        og_ = P.ring(s4, "og", [128, 256], F32, 2)
                        hs_ = P.ring(s4, "hs", [128, 256], F32, 2)
                        sq_ = P.ring(s4, "sq", [128, 256], F32, 2)
                        ms_ = P.ring(s4, "ms", [128, 8], F32, 2)
                        ybf_ = P.ring(s4, "ybf", [128, 256], BF16, 2)
                        ybT_ = P.ring(s4, "ybT", [128, 2, 128], BF16, 2)
                        for i in range(NT):
                            if i < 2 and not with_ctx:
                                continue
                            hf = hf_.next()
                            P.dma(sp, hf[:], HFB[:, i * 128:(i + 1) * 128, :].rearrange("d p e -> p d e"))
                            og = og_.next()
                            P.dma(sp, og[:], VOG[i * 128:(i + 1) * 128, 256:512])
                            hs = hs_.next()
                            P.I(dve, "tensor_tensor", out=hs[:], in0=hf[:, 0, :], in1=hf[:, 1, :], op=ALU.add)
                            sq = sq_.next()
                            P.I(pool, "tensor_tensor", out=sq[:], in0=hs[:], in1=hs[:], op=ALU.mult)
                            ms = ms_.next()
                            P.I(dve, "tensor_reduce", out=ms[:, 0:4], in_=sq[:].map(lambda a: a.rearrange("p (h e) -> p h e", e=64)),
                                axis=AX.X, op=ALU.add)
                            P.I(act, "activation", out=ms[:, 0:4], in_=ms[:, 0:4], func=AF.Sqrt, scale=1.0 / 64, bias=epsc[:])
                            P.I(dve, "reciprocal", out=ms[:, 4:8], in_=ms[:, 0:4])
                            P.I(act, "activation", out=og[:], in_=og[:], func=AF.Sigmoid)
                            h3 = lambda vv: vv.map(lambda a: a.rearrange("p (h e) -> p h e", e=64))
                            P.I(dve, "tensor_tensor", out=h3(hs[:]), in0=h3(hs[:]),
                                in1=ms[:, 4:8].map(lambda a: a.unsqueeze(2).to_broadcast([128, 4, 64])), op=ALU.mult)
                            P.I(pool, "tensor_tensor", out=hs[:], in0=hs[:], in1=mng[:], op=ALU.mult)
                            ybf = ybf_.next()
                            P.I(dve, "tensor_tensor", out=ybf[:], in0=hs[:], in1=og[:], op=ALU.mult)
                            py = bank_bf((2, 128))
                            for cc in range(2):
                                P.tr(py.map(lambda a: a[:, cc, :]), ybf[:, cc * 128:(cc + 1) * 128], identb[:])
                            ybT = ybT_.next()
                            P.I(act, "activation", out=ybT[:], in_=py, func=AF.Copy)
                            P.dma(sp, MIXT[256:512, i * 128:(i + 1) * 128].rearrange("(c p) t -> p c t", p=128), ybT[:])
                        P.barrier()
                if stages == "M":
                    return nc, cn, scr

                with ExitStack() as sF:
                    ld = lambda nm, shp, dt: P.sb(sF, nm, shp, dt)
                    c1 = ld("f_c1", [128, 128], BF16); s1t = ld("f_s1", [128, 128], BF16); ns1 = ld("f_ns1", [128, 128], BF16)
                    twc = ld("f_twc", [128, 64], F32); tws = ld("f_tws", [128, 64], F32)
                    c2 = ld("f_c2", [64, 64], BF16); s2t = ld("f_s2", [64, 64], BF16)
                    ccs = ld("f_ccs", [128, 2, 2, 256], BF16)
                    for tl, nm in ((c1, "f_c1"), (s1t, "f_s1"), (ns1, "f_ns1"), (twc, "f_twc"), (tws, "f_tws"), (c2, "f_c2"), (s2t, "f_s2")):
                        P.dma(sp, tl[:], inp[nm][:, :])
                    P.dma(sp, ccs[:, 0, :, :], inp["f_cc"].rearrange("(a p) k -> p a k", p=128))
                    P.dma(sp, ccs[:, 1, :, :], inp["f_sc"].rearrange("(a p) k -> p a k", p=128))
                    with ExitStack() as sF1:
                        X1 = P.sb(sF1, "X1", [128, 64, 512], BF16)
                        YT = P.sb(sF1, "YT", [128, 64, 512], BF16)
                        ftmp_ = P.ring(sF1, "ftmp", [128, 256], F32, 4)
                        for q in range(4):
                            P.dma(sp, X1[:, q * 16:(q + 1) * 16, :],
                                  FAB[CTX:, :].rearrange("(a b) c -> a b c", b=64)[:, q * 16:(q + 1) * 16, :])
                        for tp in range(32):
                            zr = X1[:, 2 * tp:2 * tp + 2, 0:256]
                            zi = X1[:, 2 * tp:2 * tp + 2, 256:512]
                            br = banks.next()
                            bi = banks.next()
                            P.mm(br[:, :], c1[:], zr, start=True, stop=False)
                            P.mm(br[:, :], s1t[:], zi, start=False, stop=True)
                            P.mm(bi[:, :], c1[:], zi, start=True, stop=False)
                            P.mm(bi[:, :], ns1[:], zr, start=False, stop=True)
                            for u in range(2):
                                t2_ = 2 * tp + u
                                yr = br[:, u * 256:(u + 1) * 256]
                                yi = bi[:, u * 256:(u + 1) * 256]
                                ta = ftmp_.next()
                                tb = ftmp_.next()
                                P.I(dve, "tensor_scalar", out=ta[:], in0=yi, scalar1=tws[:, t2_:t2_ + 1], scalar2=None, op0=ALU.mult)
                                P.I(dve, "scalar_tensor_tensor", out=YT[:, t2_, 0:256], in0=yr, scalar=twc[:, t2_:t2_ + 1],
                                    in1=ta[:], op0=ALU.mult, op1=ALU.add)
                                P.I(dve, "tensor_scalar", out=tb[:], in0=yr, scalar1=tws[:, t2_:t2_ + 1], scalar2=None, op0=ALU.mult)
                                P.I(dve, "scalar_tensor_tensor", out=YT[:, t2_, 256:512], in0=yi, scalar=twc[:, t2_:t2_ + 1],
                                    in1=tb[:], op0=ALU.mult, op1=ALU.subtract)
                        for q in range(4):
                            P.dma(sp, FY[:, q * 16:(q + 1) * 16, :], YT[:, q * 16:(q + 1) * 16, :])
                        P.barrier()
                    with ExitStack() as sF2:
                        y2_ = P.ring(sF2, "y2", [64, 8, 512], BF16, 3)
                        yaT = P.sb(sF2, "yaT", [128, 2, SEQ], BF16)
                        FYv = FY.rearrange("k t c -> t k c")
                        for kb in range(16):
                            y2 = y2_.next()
                            P.dma(sp, y2[:], FYv[:, kb * 8:(kb + 1) * 8, :])
                            for jc in range(2):
                                bk = banks.next()
                                for kl in range(8):
                                    P.mm(bk[:, kl * 64:(kl + 1) * 64], y2[:, kl, jc * 128:(jc + 1) * 128], c2[:], start=True, stop=False)
                                    P.mm(bk[:, kl * 64:(kl + 1) * 64], y2[:, kl, 256 + jc * 128:256 + (jc + 1) * 128], s2t[:],
                                         start=False, stop=True)
                                ov = yaT[:, jc, :].map(lambda a: a.rearrange("p (k2 k1) -> p k1 k2", k1=128)[:, kb * 8:(kb + 1) * 8, :])
                                iv = bk[:, :].map(lambda a: a.rearrange("p (kl k2) -> p kl k2", k2=64))
                                P.I(act if jc == 0 else dve, "activation" if jc == 0 else "tensor_copy", out=ov, in_=iv,
                                    **({"func": AF.Copy} if jc == 0 else {}))
                        P.dma(sp, MIXT[0:256, CTX:].rearrange("(c p) t -> p c t", p=128), yaT[:])
                        if with_ctx:
                            zc = P.sb(sF2, "zc", [128, 2, 512], BF16)
                            yc2 = P.sb(sF2, "yc2", [128, 2, 256], BF16)
                            P.dma(sp, zc[:], FAB[0:CTX, :].rearrange("(a p) c -> p a c", p=128))
                            for jc in range(2):
                                bk = banks.next()
                                for a_ in range(2):
                                    P.mm(bk[:, 0:256], zc[:, a_, jc * 128:(jc + 1) * 128], ccs[:, 0, a_, :], start=(a_ == 0), stop=False)
                                    P.mm(bk[:, 0:256], zc[:, a_, 256 + jc * 128:256 + (jc + 1) * 128], ccs[:, 1, a_, :],
                                         start=False, stop=(a_ == 1))
                                P.I(act, "activation", out=yc2[:, jc, :], in_=bk[:, 0:256], func=AF.Copy)
                            P.dma(sp, MIXT[0:256, 0:CTX].rearrange("(c p) t -> p c t", p=128), yc2[:])
                        P.barrier()
                if stages == "F":
                    return nc, cn, scr

                last = (l == nlayers - 1)
                tiles_e = [i for i in range(NT) if (i >= 2 or with_ctx)]
                sets = ([(1, [0, 1], CAPC, XEc, YEc, 0)] if with_ctx else []) + [(0, list(range(2, NT)), CAPX, XEx, YEx, 16)]
                with ExitStack() as sE:
                    AFF = P.sb(sE, "AFF", [128, NT, NE], F32)
                    GM = P.sb(sE, "GM", [128, NT, NE], F32)
                    SLOT = P.sb(sE, "SLOT", [128, NT, NE], I32)
                    Gb = P.sb(sE, "Gb", [128, 4, D], F32)
                    for v in range(2):
                        P.dma(sp, Gb[:, v, :], MOD[l, v:v + 1, 2 * D:3 * D].partition_broadcast(128))
                        P.dma(sp, Gb[:, 2 + v, :], MOD[l, v:v + 1, 5 * D:6 * D].partition_broadcast(128))
                    if not with_ctx:
                        P.I(pool, "memset", AFF[:, 0:2, :], 0.0, _writes=[AFF.res])
                    with ExitStack() as sE1:
                        Wout = P.sb(sE1, "Wout", [128, 8, D], BF16)
                        for k in range(8):
                            P.dma(pool, Wout[:, k, :], inp["w_out"][l, k * 128:(k + 1) * 128, :])
                        RW = P.sb(sE1, "RW", [128, 8, NE], BF16)
                        P.dma(pool, RW[:], inp["router_w"][l].rearrange("(k p) e -> p k e", p=128))
                        mT_ = P.ring(sE1, "mT", [128, 8, 128], BF16, 2)
                        xe_ = P.ring(sE1, "xe", [128, D], F32, 2)
                        tm_ = P.ring(sE1, "tm", [128, D], F32, 2)
                        st2_ = P.ring(sE1, "st2", [128, 8], F32, 3)
                        xn2_ = P.ring(sE1, "xn2", [128, D], BF16, 2)
                        h2_ = P.ring(sE1, "h2", [128, 8, 128], BF16, 2)
                        lg_ = P.ring(sE1, "lg", [128, NE], F32, 2)
                        jk2 = P.sb(sE1, "jk2", [128, D], BF16)
                        for i in tiles_e:
                            v = 0 if i >= 2 else 1
                            tok0 = i * 128
                            mT = mT_.next()
                            P.dma(sp, mT[:], MIXT[:, tok0:tok0 + 128].rearrange("(k p) t -> p k t", p=128))
                            xt = xe_.next()
                            P.dma(sp, xt[:], XR[tok0:tok0 + 128, :])
                            ob = [banks.next(), banks.next()]
                            for n in range(2):
                                for k in range(8):
                                    P.mm(ob[n][:, :], mT[:, k, :], Wout[:, k, n * 512:(n + 1) * 512], start=(k == 0), stop=(k == 7))
                            tm = tm_.next()
                            for n in range(2):
                                P.I(dve, "tensor_tensor", out=tm[:, n * 512:(n + 1) * 512], in0=ob[n][:, :],
                                    in1=Gb[:, v, n * 512:(n + 1) * 512], op=ALU.mult)
                            P.I(pool, "tensor_tensor", out=xt[:], in0=xt[:], in1=tm[:], op=ALU.add)
                            P.dma(sp, XR[tok0:tok0 + 128, :], xt[:])
                            st = st2_.next()
                            P.I(act, "activation", out=jk2[:], in_=xt[:], func=AF.Square, accum_out=st[:, 0:1])
                            P.I(act, "activation", out=st[:, 1:2], in_=st[:, 0:1], func=AF.Sqrt, scale=1.0 / D, bias=epsc[:])
                            P.I(dve, "reciprocal", out=st[:, 2:3], in_=st[:, 1:2])
                            xn2 = xn2_.next()
                            P.I(dve, "tensor_scalar", out=xn2[:], in0=xt[:], scalar1=st[:, 2:3], scalar2=None, op0=ALU.mult)
                            P.dma(sp, XN2[tok0:tok0 + 128, :], xn2[:])
                            pT = bank_bf((8, 128))
                            for k in range(8):
                                P.tr(pT.map(lambda a: a[:, k, :]), xn2[:, k * 128:(k + 1) * 128], identb[:])
                            h2 = h2_.next()
                            bc = lambda vv: vv.map(lambda a: a.unsqueeze(2).to_broadcast([128, 8, 128]))
                            P.I(dve, "tensor_tensor", out=h2[:], in0=pT, in1=bc(Amod[:, 2 + v, :]), op=ALU.mult)
                            P.I(pool, "tensor_tensor", out=h2[:], in0=h2[:], in1=bc(colv[:, v * 6 + 3, :]), op=ALU.add)
                            lb = banks.next()
                            for k in range(8):
                                P.mm(lb[:, 0:NE], h2[:, k, :], RW[:, k, :], start=(k == 0), stop=(k == 7))
                            lg = lg_.next()
                            P.I(dve, "tensor_reduce", out=st[:, 3:4], in_=lb[:, 0:NE], axis=AX.X, op=ALU.max)
                            P.I(dve, "tensor_scalar", out=st[:, 4:5], in0=st[:, 3:4], scalar1=-1.0, scalar2=None, op0=ALU.mult)
                            P.I(act, "activation", out=lg[:], in_=lb[:, 0:NE], func=AF.Exp, bias=st[:, 4:5], accum_out=st[:, 5:6])
                            P.I(dve, "reciprocal", out=st[:, 6:7], in_=st[:, 5:6])
                            P.I(dve, "tensor_scalar", out=AFF[:, i, :], in0=lg[:], scalar1=st[:, 6:7], scalar2=None, op0=ALU.mult)
                        P.barrier()
                    if stages == "E":
                        dbg_a = nc.dram_tensor("dbg_AFF", [128, NT, NE], F32, kind="ExternalOutput").ap()
                        P.dma(sp, dbg_a[:, :, :], AFF[:])
                        P.barrier()
                        return nc, cn, scr

                    with ExitStack() as sD2:
                        lo = P.sb(sD2, "lo", [128, 32], F32)
                        hi = P.sb(sD2, "hi", [128, 32], F32)
                        mid = P.sb(sD2, "mid", [128, 32], F32)
                        capv = P.sb(sD2, "capv", [128, 32], F32)
                        onesb = P.sb(sD2, "onesb", [128, 128], BF16)
                        utri = P.sb(sD2, "utri", [128, 128], BF16)
                        P.dma(sp, capv[:], inp["capv"][:, :])
                        P.dma(sp, onesb[:], inp["onesb"][:, :])
                        P.dma(sp, utri[:], inp["utri"][:, :])
                        P.I(dve, "memset", lo[:], 0.0, _writes=[lo.res])
                        P.I(dve, "memset", hi[:], 1.0, _writes=[hi.res])
                        cmp_ = P.sb(sD2, "cmp", [128, NT, NE], BF16)
                        pc = P.sb(sD2, "pc", [128, 32], BF16)
                        pcf = P.sb(sD2, "pcf", [128, 32], F32)
                        P.I(dve, "memset", pcf[:], 0.0, _writes=[pcf.res])
                        mge = P.sb(sD2, "mge", [128, 32], U32)
                        mlt = P.sb(sD2, "mlt", [128, 32], U32)
                        P.I(dve, "memset", pc[:], 0.0, _writes=[pc.res])
                        for it in range(34):
                            P.I(dve, "tensor_tensor", out=mid[:], in0=lo[:], in1=hi[:], op=ALU.add)
                            P.I(dve, "tensor_scalar", out=mid[:], in0=mid[:], scalar1=0.5, scalar2=None, op0=ALU.mult)
                            for (v, tl, cap, XE, YE, co) in sets:
                                nt_ = len(tl)
                                t0_ = tl[0]
                                P.I(dve, "tensor_tensor", out=cmp_[:, t0_:t0_ + nt_, :], in0=AFF[:, t0_:t0_ + nt_, :],
                                    in1=mid[:, co:co + 16].map(lambda a: a.unsqueeze(1).to_broadcast([128, nt_, NE])), op=ALU.is_gt)
                                P.I(dve, "tensor_reduce", out=pcf[:, co:co + 16],
                                    in_=cmp_[:, t0_:t0_ + nt_, :].map(lambda a: a.rearrange("p j e -> p e j")), axis=AX.X, op=ALU.add)
                            P.I(dve, "tensor_copy", out=pc[:], in_=pcf[:])
                            tb = banks.next()
                            P.mm(tb[:, 0:32], onesb[:], pc[:])
                            P.I(dve, "tensor_tensor", out=mge[:], in0=tb[:, 0:32], in1=capv[:], op=ALU.is_ge)
                            P.I(dve, "tensor_tensor", out=mlt[:], in0=tb[:, 0:32], in1=capv[:], op=ALU.is_lt)
                            P.I(dve, "copy_predicated", out=lo[:], mask=mge[:], data=mid[:])
                            P.I(dve, "copy_predicated", out=hi[:], mask=mlt[:], data=mid[:])
                        offb = P.sb(sD2, "offb", [128, NE], F32)
                        mk_ = P.ring(sD2, "mk", [128, NE], F32, 2)
                        mkb_ = P.ring(sD2, "mkb", [128, NE], BF16, 2)
                        sl_ = P.ring(sD2, "sl", [128, NE], F32, 2)
                        BIG = 1.0e6
                        for (v, tl, cap, XE, YE, co) in sets:
                            P.I(dve, "memset", offb[:], 0.0, _writes=[offb.res])
                            for i in tl:
                                mk = mk_.next()
                                P.I(dve, "tensor_tensor", out=mk[:], in0=AFF[:, i, :], in1=lo[:, co:co + 16], op=ALU.is_gt)
                                mkb = mkb_.next()
                                P.I(dve, "tensor_copy", out=mkb[:], in_=mk[:])
                                P.I(dve, "tensor_tensor", out=GM[:, i, :], in0=AFF[:, i, :], in1=mk[:], op=ALU.mult)
                                rb = banks.next()
                                P.mm(rb[:, 0:NE], utri[:], mkb[:])
                                P.mm(rb[:, NE:2 * NE], onesb[:], mkb[:])
                                sl = sl_.next()
                                P.I(dve, "tensor_tensor", out=sl[:], in0=rb[:, 0:NE], in1=offb[:], op=ALU.add)
                                P.I(dve, "tensor_tensor", out=offb[:], in0=rb[:, NE:2 * NE], in1=offb[:], op=ALU.add)
                                P.I(dve, "tensor_scalar", out=sl[:], in0=sl[:], scalar1=-BIG, scalar2=None, op0=ALU.add)
                                P.I(dve, "tensor_tensor", out=sl[:], in0=sl[:], in1=mk[:], op=ALU.mult)
                                P.I(dve, "tensor_scalar", out=SLOT[:, i, :], in0=sl[:], scalar1=BIG, scalar2=None, op0=ALU.add)
                        P.barrier()
                    if stages == "D3":
                        dbg_s = nc.dram_tensor("dbg_SLOT", [128, NT, NE], I32, kind="ExternalOutput").ap()
                        dbg_g = nc.dram_tensor("dbg_GM", [128, NT, NE], F32, kind="ExternalOutput").ap()
                        P.dma(sp, dbg_s[:, :, :], SLOT[:])
                        P.dma(sp, dbg_g[:, :, :], GM[:])
                        P.barrier()
                        return nc, cn, scr

                    xeres = [Res("xe%d" % e, multi=True) for e in range(NE)]
                    with ExitStack() as sD4:
                        Wg_ = P.ring(sD4, "Wg", [128, 8, D], BF16, 2)
                        Wu_ = P.ring(sD4, "Wu", [128, 8, D], BF16, 2)
                        Wd_ = P.ring(sD4, "Wd", [128, 8, D], BF16, 2)
                        xer_ = P.ring(sD4, "xer", [128, 4, D], BF16, 2)
                        xeT_ = P.ring(sD4, "xeT", [128, 8, 512], BF16, 2)
                        hid_ = P.ring(sD4, "hid", [128, 8, 512], BF16, 2)
                        sg_ = P.ring(sD4, "sg", [128, 512], F32, 3)
                        ye_ = P.ring(sD4, "ye", [128, D], BF16, 3)
                        ne_w = 1 if lite else NE
                        xs_ = P.ring(sD4, "xs", [128, D], BF16, 3)
                        EG = 4
                        wts = {}

                        def load_w(e):
                            if e >= NE or e in wts:
                                return
                            ew = e % ne_w
                            Wg = Wg_.next(); Wu = Wu_.next(); Wd = Wd_.next()
                            for k in range(8):
                                P.dma(pool, Wg[:, k, :], inp["e_w_gate"][l, ew, k * 128:(k + 1) * 128, :])
                                P.dma(pool, Wu[:, k, :], inp["e_w_up"][l, ew, k * 128:(k + 1) * 128, :])
                            for k in range(8):
                                P.dma(pool, Wd[:, k, :], inp["e_w_down"][l, ew, k * 128:(k + 1) * 128, :])
                            wts[e] = (Wg, Wu, Wd)

                        stiles = [(st_, i) for st_ in sets for i in st_[1]]

                        def scatter_part(g, part, nparts):
                            if g * EG >= NE:
                                return
                            n_ = len(stiles)
                            lo_, hi_ = (part * n_) // nparts, ((part + 1) * n_) // nparts
                            for (st_, i) in stiles[lo_:hi_]:
                                (v, tl, cap, XE, YE, co) = st_
                                xs = xs_.next()
                                P.dma(sp, xs[:], XN2[i * 128:(i + 1) * 128, :])
                                for e2 in range(g * EG, (g + 1) * EG):
                                    P.dma(pool, XE[e2][:, :], xs[:], meth="indirect_dma_start",
                                          out_offset=bass.IndirectOffsetOnAxis(ap=SLOT[:, i, e2:e2 + 1].ap, axis=0),
                                          in_offset=None, bounds_check=bcreg[cap], oob_is_err=False,
                                          _reads=[SLOT.res], _writes=[xeres[e2]])

                        load_w(0)
                        scatter_part(0, 0, 1)
                        load_w(1)
                        for e in range(NE):
                            load_w(e)
                            Wg, Wu, Wd = wts.pop(e)
                            for (v, tl, cap, XE, YE, co) in sets:
                                for ch0 in range(0, cap, 512):
                                    ns = min(512, cap - ch0)
                                    nsub = (ns + 127) // 128
                                    pr = min(128, ns)
                                    xer = xer_.next()
                                    P.dma(sp, xer[0:pr, 0:nsub, :], XE[e][ch0:ch0 + ns, :].rearrange("(a p) d -> p a d", p=pr),
                                          _reads=[xeres[e]])
                                    xeT = xeT_.next()
                                    for a_ in range(nsub):
                                        pT = bank_bf((8, 128))
                                        for k in range(8):
                                            P.tr(pT.map(lambda a: a[:, k, 0:pr]), xer[0:pr, a_, k * 128:(k + 1) * 128], identb[0:pr, 0:pr])
                                        bcx = lambda vv: vv.map(lambda a: a.unsqueeze(2).to_broadcast([128, 8, pr]))
                                        P.I(dve, "tensor_tensor", out=xeT[:, :, a_ * 128:a_ * 128 + pr], in0=pT.map(lambda a: a[:, :, 0:pr]),
                                            in1=bcx(Amod[:, 2 + v, :]), op=ALU.mult)
                                        P.I(dve, "tensor_tensor", out=xeT[:, :, a_ * 128:a_ * 128 + pr], in0=xeT[:, :, a_ * 128:a_ * 128 + pr],
                                            in1=bcx(colv[:, v * 6 + 3, :]), op=ALU.add)
                                    hid = hid_.next()
                                    for f in range(8):
                                        bg = banks.next()
                                        bu = banks.next()
                                        for k in range(8):
                                            P.mm(bg[:, 0:ns], Wg[:, k, f * 128:(f + 1) * 128], xeT[:, k, 0:ns], start=(k == 0), stop=(k == 7))
                                        for k in range(8):
                                            P.mm(bu[:, 0:ns], Wu[:, k, f * 128:(f + 1) * 128], xeT[:, k, 0:ns], start=(k == 0), stop=(k == 7))
                                        sg = sg_.next()
                                        P.I(act, "activation", out=sg[:, 0:ns], in_=bg[:, 0:ns], func=AF.Silu)
                                        P.I(dve, "tensor_tensor", out=hid[:, f, 0:ns], in0=sg[:, 0:ns], in1=bu[:, 0:ns], op=ALU.mult)
                                    for a_ in range(nsub):
                                        ye = ye_.next()
                                        for n in range(2):
                                            bd = banks.next()
                                            for f in range(8):
                                                P.mm(bd[0:pr, :], hid[:, f, a_ * 128:a_ * 128 + pr], Wd[:, f, n * 512:(n + 1) * 512],
                                                     start=(f == 0), stop=(f == 7))
                                            if n == 0:
                                                P.I(act, "activation", out=ye[0:pr, 0:512], in_=bd[0:pr, :], func=AF.Copy)
                                            else:
                                                P.I(dve, "tensor_copy", out=ye[0:pr, 512:1024], in_=bd[0:pr, :])
                                        P.dma(sp, YE[e][ch0 + a_ * 128:ch0 + a_ * 128 + pr, :], ye[0:pr, :])
                            scatter_part(e // EG + 1, e % EG, EG)
                            load_w(e + 2)
                        P.barrier()
                    with ExitStack() as sD5:
                        gt_ = P.ring(sD5, "gt", [128, D], BF16, 8)
                        for t_ in gt_.tiles:
                            P.I(pool, "memset", t_[:], 0.0, _writes=[t_.res])
                        xf_ = P.ring(sD5, "xf", [128, D], F32, 2)
                        tm5_ = P.ring(sD5, "tm5", [128, D], F32, 2)
                        st5_ = P.ring(sD5, "st5", [128, 4], F32, 2)
                        gh_ = P.ring(sD5, "gh", [128, 3, NE], F32, 2)
                        ghb_ = P.ring(sD5, "ghb", [128, NE], BF16, 2)
                        Dg_ = P.ring(sD5, "Dgd", [128, 2, NE, 128], BF16, 2)
                        jk5 = P.sb(sD5, "jk5", [128, D], BF16)
                        fgb = P.sb(sD5, "fgb", [128, D], F32)
                        P.dma(sp, fgb[:], inp["final_g"].unsqueeze(0).partition_broadcast(128))
                        idb = identf[:].map(lambda a: a.unsqueeze(1).to_broadcast([128, NE, 128]))
                        for (v, tl, cap, XE, YE, co) in sets:
                            for i in tl:
                                gh = gh_.next()
                                ghb = ghb_.next()
                                P.I(dve, "tensor_copy", out=ghb[:], in_=GM[:, i, :])
                                P.I(dve, "tensor_copy", out=gh[:, 0, :], in_=ghb[:])
                                P.I(dve, "tensor_tensor", out=gh[:, 1, :], in0=GM[:, i, :], in1=gh[:, 0, :], op=ALU.subtract)
                                Dgd = Dg_.next()
                                for q_ in range(2):
                                    P.I(dve, "tensor_tensor", out=Dgd[:, q_, :, :], in0=idb,
                                        in1=gh[:, q_, :].map(lambda a: a.unsqueeze(2).to_broadcast([128, NE, 128])), op=ALU.mult)
                                ab = [banks.next(), banks.next()]
                                for e in range(NE):
                                    gt = gt_.next()
                                    P.dma(pool, gt[:], YE[e][:, :], meth="indirect_dma_start", out_offset=None,
                                          in_offset=bass.IndirectOffsetOnAxis(ap=SLOT[:, i, e:e + 1].ap, axis=0),
                                          bounds_check=bcreg[cap], oob_is_err=False, _reads=[SLOT.res])
                                    for q_ in range(2):
                                        for n in range(2):
                                            P.mm(ab[n][:, :], Dgd[:, q_, e, :], gt[:, n * 512:(n + 1) * 512],
                                                 start=(e == 0 and q_ == 0), stop=(e == NE - 1 and q_ == 1))
                                xf = xf_.next()
                                P.dma(sp, xf[:], XR[i * 128:(i + 1) * 128, :])
                                tm = tm5_.next()
                                for n in range(2):
                                    P.I(dve, "tensor_tensor", out=tm[:, n * 512:(n + 1) * 512], in0=ab[n][:, :],
                                        in1=Gb[:, 2 + v, n * 512:(n + 1) * 512], op=ALU.mult)
                                P.I(dve, "tensor_tensor", out=xf[:], in0=xf[:], in1=tm[:], op=ALU.add)
                                if not last:
                                    P.dma(sp, XR[i * 128:(i + 1) * 128, :], xf[:])
                                elif i >= 2:
                                    st = st5_.next()
                                    P.I(act, "activation", out=jk5[:], in_=xf[:], func=AF.Square, accum_out=st[:, 0:1])
                                    P.I(act, "activation", out=st[:, 1:2], in_=st[:, 0:1], func=AF.Sqrt, scale=1.0 / D, bias=epsc[:])
                                    P.I(dve, "reciprocal", out=st[:, 2:3], in_=st[:, 1:2])
                                    P.I(dve, "scalar_tensor_tensor", out=xf[:], in0=xf[:], scalar=st[:, 2:3], in1=fgb[:],
                                        op0=ALU.mult, op1=ALU.mult)
                                    P.dma(sp, out[(i - 2) * 128:(i - 1) * 128, :], xf[:])
                        P.barrier()
    return nc, cn, scr


_CACHE = {}


def kernel(**inputs):
    nb = inputs["x"].shape[0]
    if "nc" not in _CACHE:
        _CACHE["nc"] = build(nlayers=DEPTH, dbg=False)
    nc, cn, _ = _CACHE["nc"]
    shared = {n: np.ascontiguousarray(inputs[n], dtype=np.float32) for n, _ in WNAMES}
    shared["c_ctx"] = np.ascontiguousarray(inputs["c_ctx"], dtype=np.float32)
    for n, a in cn.items():
        shared["k_" + n] = a
    in_maps = []
    for b in range(nb):
        m = dict(shared)
        m["x"] = np.ascontiguousarray(inputs["x"][b], dtype=np.float32)
        m["c"] = np.ascontiguousarray(inputs["c"][b], dtype=np.float32)
        m["ctx"] = np.ascontiguousarray(inputs["ctx"][b], dtype=np.float32)
        in_maps.append(m)
    res = run_bass_kernel_spmd(nc, in_maps, core_ids=list(range(nb)))
    return np.stack([np.asarray(r["out"], dtype=np.float32) for r in res.results], axis=0)
```
